# Optimizing a Trainium2 kernel written in Bass

```python
import math
import jax
import jax.numpy as jnp
from jax import lax
import numpy as np

D_MODEL = 1024
BATCH = 2
SEQ = 16384
DEPTH = 2

GRID_W = 64
CTX_LEN = 256
EPS = 1e-6

CONV_DIM = 512
CONV_WIDTH = 31
CONV_PAD = CONV_WIDTH // 2
DIFF_HEADS = 4
DIFF_HD = 64
DIFF_VD = 2 * DIFF_HD
DIFF_QK = DIFF_HEADS * 2 * DIFF_HD
DIFF_DIM = DIFF_HEADS * DIFF_VD
DIFF_SCALE = DIFF_HD ** -0.5
ATTN_BLOCK = 128
ROPE_BASE = 10000.0
AB_Q0 = 2 * CONV_DIM
AB_K0 = AB_Q0 + DIFF_QK
AB_V0 = AB_K0 + DIFF_QK
AB_IN = AB_V0 + DIFF_DIM

GLA_HEADS = 4
GLA_DK = D_MODEL // (2 * GLA_HEADS)
GLA_DV = D_MODEL // GLA_HEADS
GLA_KDIM = GLA_HEADS * GLA_DK
GLA_VDIM = GLA_HEADS * GLA_DV
GLA_RANK = 16
GLA_TAU = 16.0
GLA_CHUNK = 64
GLA_K0 = GLA_KDIM
GLA_V0 = 2 * GLA_KDIM
GLA_G0 = GLA_V0 + GLA_VDIM
GLA_A0 = GLA_G0 + GLA_VDIM
GLA_IN = GLA_A0 + 2 * GLA_RANK

N_GROUPS = 4
EXPERTS_PER_GROUP = 8
N_EXPERTS = N_GROUPS * EXPERTS_PER_GROUP
TOP_K = 2
D_EXPERT = 512
MOE_BLOCK = 256

kernel_name = 'hybrid_conv_diffattn_gla_hmoe_dit'


def rmsnorm(x, g):
    xf = x.astype(jnp.float32)
    y = xf * lax.rsqrt(jnp.mean(xf * xf, axis=-1, keepdims=True) + EPS)
    return (y * g.astype(jnp.float32)).astype(x.dtype)


def layernorm(x, g, b):
    xf = x.astype(jnp.float32)
    mu = jnp.mean(xf, axis=-1, keepdims=True)
    var = jnp.mean(jnp.square(xf - mu), axis=-1, keepdims=True)
    y = (xf - mu) * lax.rsqrt(var + EPS)
    return (y * g.astype(jnp.float32) + b.astype(jnp.float32)).astype(x.dtype)


def axial_rope_tables(n_lat):
    rows = n_lat // GRID_W
    r, col = jnp.meshgrid(jnp.arange(rows, dtype=jnp.float32),
                          jnp.arange(GRID_W, dtype=jnp.float32), indexing='ij')
    half = DIFF_HD // 2
    inv = ROPE_BASE ** (-jnp.arange(0, half, 2, dtype=jnp.float32) / half)
    ar = r.reshape(-1, 1) * inv
    ac = col.reshape(-1, 1) * inv
    ang = jnp.concatenate([ar, ar, ac, ac], axis=-1)
    return jnp.cos(ang), jnp.sin(ang)


def apply_rope(t, cos, sin):
    tr = t.reshape(t.shape[:-1] + (2, 2, DIFF_HD // 4))
    rot = jnp.stack([-tr[..., 1, :], tr[..., 0, :]], axis=-2).reshape(t.shape)
    cs = cos[:, None, None, :].astype(t.dtype)
    sn = sin[:, None, None, :].astype(t.dtype)
    return t * cs + rot * sn


def conformer_conv(a, g, w, b, ln_g, ln_b):
    h = a * jax.nn.sigmoid(g)
    h = lax.conv_general_dilated(h, w[:, None, :].astype(h.dtype), (1,), ((CONV_PAD, CONV_PAD),),
                                 dimension_numbers=('NWC', 'WIO', 'NWC'),
                                 feature_group_count=CONV_DIM) + b
    return jax.nn.silu(layernorm(h, ln_g, ln_b))


def conv_diffattn_mixer(nl, nc, w_in, conv_w, conv_b, ln_g, ln_b, qn_g, kn_g, lq1, lk1, lq2, lk2,
                        subln_g, w_out, lambda_init, cos, sin, need_ctx):
    bsz, n_lat, _ = nl.shape
    ul = nl @ w_in
    uc = nc @ w_in

    def qkv(u):
        b_, L = u.shape[:2]
        q = rmsnorm(u[..., AB_Q0:AB_K0].reshape(b_, L, DIFF_HEADS, 2, DIFF_HD), qn_g)
        k = rmsnorm(u[..., AB_K0:AB_V0].reshape(b_, L, DIFF_HEADS, 2, DIFF_HD), kn_g)
        v = u[..., AB_V0:].reshape(b_, L, DIFF_HEADS, DIFF_VD)
        return q, k, v

    ql, kl, vl = qkv(ul)
    qc, kc, vc = qkv(uc)
    ql = apply_rope(ql, cos, sin)
    kl = apply_rope(kl, cos, sin)
    lam = (jnp.exp(jnp.sum(lq1 * lk1).astype(jnp.float32))
           - jnp.exp(jnp.sum(lq2 * lk2).astype(jnp.float32)) + lambda_init)

    tq = lambda t: t.transpose(0, 2, 3, 1, 4)
    tv = lambda t: t.transpose(0, 2, 1, 3)

    def diff_softmax(q, k, v):
        s = jnp.einsum('bhmqd,bhmkd->bhmqk', q, k).astype(jnp.float32) * DIFF_SCALE
        p = jax.nn.softmax(s, axis=-1)
        a = p[:, :, 0] - lam * p[:, :, 1]
        return jnp.einsum('bhqk,bhkd->bhqd', a.astype(v.dtype), v)

    k_all = jnp.concatenate([tq(kc), tq(kl)], axis=3)
    v_all = jnp.concatenate([tv(vc), tv(vl)], axis=2)
    nb = n_lat // ATTN_BLOCK
    qb = tq(ql).reshape(bsz, DIFF_HEADS, 2, nb, ATTN_BLOCK, DIFF_HD).transpose(3, 0, 1, 2, 4, 5)
    ob = lax.map(lambda q: diff_softmax(q, k_all, v_all), qb)
    o_l = ob.transpose(1, 0, 3, 2, 4).reshape(bsz, n_lat, DIFF_HEADS, DIFF_VD)

    def finish(o, u):
        b_, L = u.shape[:2]
        attn = (rmsnorm(o, subln_g) * (1.0 - lambda_init)).reshape(b_, L, DIFF_DIM)
        conv = conformer_conv(u[..., :CONV_DIM], u[..., CONV_DIM:AB_Q0], conv_w, conv_b, ln_g, ln_b)
        return jnp.concatenate([conv, attn], axis=-1) @ w_out

    y_l = finish(o_l, ul)
    y_c = None
    if need_ctx:
        o_c = diff_softmax(tq(qc), tq(kc), tv(vc)).transpose(0, 2, 1, 3)
        y_c = finish(o_c, uc)
    return y_l, y_c


def gla_chunked(q, k, v, log_a, s0):
    bsz, nh, L, dk = q.shape
    dv = v.shape[-1]
    n = L // GLA_CHUNK

    def chunks(t):
        return t.astype(jnp.float32).reshape(bsz, nh, n, GLA_CHUNK, t.shape[-1]).transpose(2, 0, 1, 3, 4)

    incl = jnp.tril(jnp.ones((GLA_CHUNK, GLA_CHUNK), dtype=bool))[:, :, None]

    def step(state, inp):
        qc, kc, vc, ac = inp
        b = jnp.cumsum(ac, axis=-2)
        b_end = b[..., -1:, :]
        o_inter = jnp.einsum('bhid,bhde->bhie', qc * jnp.exp(b), state)
        decay = jnp.exp(jnp.where(incl, b[..., :, None, :] - b[..., None, :, :], -jnp.inf))
        att = jnp.einsum('bhid,bhjd,bhijd->bhij', qc, kc, decay)
        o = o_inter + jnp.einsum('bhij,bhje->bhie', att, vc)
        state = (jnp.exp(b_end[..., 0, :])[..., None] * state
                 + jnp.einsum('bhjd,bhje->bhde', kc * jnp.exp(b_end - b), vc))
        return state, o

    s_end, o = lax.scan(step, s0, (chunks(q), chunks(k), chunks(v), chunks(log_a)))
    return o.transpose(1, 2, 0, 3, 4).reshape(bsz, nh, L, dv).astype(v.dtype), s_end


def bigla_mixer(nl, nc, w_in, w_a2, b_a2, norm_g, w_out, need_ctx):
    def project(h):
        bsz, L, _ = h.shape
        u = h @ w_in
        heads = lambda t, dh: t.reshape(bsz, L, GLA_HEADS, dh).transpose(0, 2, 1, 3)
        q = heads(u[..., :GLA_K0], GLA_DK) * (GLA_DK ** -0.5)
        k = heads(u[..., GLA_K0:GLA_V0], GLA_DK)
        v = heads(u[..., GLA_V0:GLA_G0], GLA_DV)
        g = u[..., GLA_G0:GLA_A0]
        a1 = u[..., GLA_A0:].reshape(bsz, L, 2, GLA_RANK)
        z = jnp.einsum('bldr,drk->bldk', a1, w_a2) + b_a2
        log_a = jax.nn.log_sigmoid(z.astype(jnp.float32)) / GLA_TAU
        return q, k, v, g, heads(log_a[:, :, 0], GLA_DK), heads(log_a[:, :, 1], GLA_DK)

    flip = lambda t: jnp.flip(t, axis=2)
    qc, kc, vc, gc, afc, abc = project(nc)
    ql, kl, vl, gl, afl, abl = project(nl)
    s0 = jnp.zeros((nc.shape[0], GLA_HEADS, GLA_DK, GLA_DV), jnp.float32)
    o_cf, s_cf = gla_chunked(qc, kc, vc, afc, s0)
    o_cb_rev, s_cb = gla_chunked(flip(qc), flip(kc), flip(vc), flip(abc), s0)
    o_lf, _ = gla_chunked(ql, kl, vl, afl, s_cf)
    o_lb_rev, _ = gla_chunked(flip(ql), flip(kl), flip(vl), flip(abl), s_cb)

    def out(o_f, o_b_rev, g):
        bsz, _, L, _ = o_f.shape
        o = (o_f + flip(o_b_rev)).transpose(0, 2, 1, 3)
        o = rmsnorm(o, norm_g).reshape(bsz, L, GLA_VDIM)
        return (o * jax.nn.silu(g)) @ w_out

    y_l = out(o_lf, o_lb_rev, gl)
    y_c = out(o_cf, o_cb_rev, gc) if need_ctx else None
    return y_l, y_c


def hier_moe(xt, rg_w, rg_b, re_w, re_b, w1, w3, w2):
    n_tok, d = xt.shape
    lg = (xt @ rg_w).astype(jnp.float32) + rg_b
    pg = jax.nn.softmax(lg, axis=-1)
    _, gi = lax.top_k(lg, 1)
    pg_sel = jnp.take_along_axis(pg, gi, axis=-1)
    le = ((xt @ re_w).astype(jnp.float32) + re_b).reshape(n_tok, N_GROUPS, EXPERTS_PER_GROUP)
    le_sel = jnp.take_along_axis(
        le, jnp.broadcast_to(gi[:, :, None], (n_tok, 1, EXPERTS_PER_GROUP)), axis=1)[:, 0]
    top_v, top_i = lax.top_k(le_sel, TOP_K)
    gate = pg_sel * jax.nn.softmax(top_v, axis=-1)
    expert = gi * EXPERTS_PER_GROUP + top_i

    n_slot = n_tok * TOP_K
    e_flat = expert.reshape(-1)
    g_flat = gate.reshape(-1)
    tok = jnp.arange(n_slot, dtype=jnp.int32) // TOP_K
    order = jnp.argsort(e_flat)
    se = e_flat[order]
    counts = jnp.bincount(e_flat, length=N_EXPERTS)
    offs = jnp.cumsum(counts) - counts
    pcounts = (counts + MOE_BLOCK - 1) // MOE_BLOCK * MOE_BLOCK
    pends = jnp.cumsum(pcounts)
    poffs = pends - pcounts
    dest = poffs[se] + jnp.arange(n_slot, dtype=jnp.int32) - offs[se]
    n_blk = (n_slot + N_EXPERTS * (MOE_BLOCK - 1) + MOE_BLOCK - 1) // MOE_BLOCK
    n_row = n_blk * MOE_BLOCK
    row_tok = jnp.full((n_row,), n_tok, jnp.int32).at[dest].set(tok[order])
    row_gate = jnp.zeros((n_row,), jnp.float32).at[dest].set(g_flat[order])
    blk_exp = jnp.minimum(jnp.searchsorted(pends, jnp.arange(n_blk, dtype=jnp.int32) * MOE_BLOCK,
                                           side='right'), N_EXPERTS - 1)
    x_rows = jnp.concatenate([xt, jnp.zeros((1, d), xt.dtype)])[row_tok].reshape(n_blk, MOE_BLOCK, d)

    def expert_block(args):
        xb, e = args
        h = jax.nn.silu(xb @ w1[e]) * (xb @ w3[e])
        return h @ w2[e]

    y_rows = lax.map(expert_block, (x_rows, blk_exp)).reshape(n_row, d)
    y = jax.ops.segment_sum(y_rows * row_gate[:, None].astype(y_rows.dtype), row_tok,
                            num_segments=n_tok + 1)
    return y[:n_tok]


def setup_inputs(seed: int = 0) -> dict:
    key = jax.random.key(seed)
    ks = iter(jax.random.split(key, 40))

    def nrm(shape, scale):
        return jax.random.normal(next(ks), shape, jnp.float32) * scale

    def gain(shape):
        return 1.0 + nrm(shape, 0.02)

    ne, no = (DEPTH + 1) // 2, DEPTH // 2
    d = D_MODEL
    return {
        'x': nrm((BATCH, SEQ, d), 1.0),
        'c': nrm((BATCH, d), 1.0),
        'ctx': nrm((BATCH, CTX_LEN, d), 1.0),
        'c_ctx': nrm((d,), 1.0),
        'ada_w': nrm((DEPTH, d, 6 * d), 0.5 * d ** -0.5),
        'ada_b': nrm((DEPTH, 6 * d), 0.01),
        'norm1_g': gain((DEPTH, d)),
        'norm2_g': gain((DEPTH, d)),
        'ab_w_in': nrm((ne, d, AB_IN), d ** -0.5),
        'conv_w': nrm((ne, CONV_WIDTH, CONV_DIM), CONV_WIDTH ** -0.5),
        'conv_b': nrm((ne, CONV_DIM), 0.01),
        'conv_ln_g': gain((ne, CONV_DIM)),
        'conv_ln_b': nrm((ne, CONV_DIM), 0.01),
        'diff_qnorm_g': gain((ne, DIFF_HD)),
        'diff_knorm_g': gain((ne, DIFF_HD)),
        'diff_lq1': nrm((ne, DIFF_HD), 0.1),
        'diff_lk1': nrm((ne, DIFF_HD), 0.1),
        'diff_lq2': nrm((ne, DIFF_HD), 0.1),
        'diff_lk2': nrm((ne, DIFF_HD), 0.1),
        'diff_subln_g': gain((ne, DIFF_VD)),
        'ab_w_out': nrm((ne, CONV_DIM + DIFF_DIM, d), (CONV_DIM + DIFF_DIM) ** -0.5),
        'gla_w_in': nrm((no, d, GLA_IN), d ** -0.5),
        'gla_w_a2': nrm((no, 2, GLA_RANK, GLA_KDIM), GLA_RANK ** -0.5),
        'gla_b_a2': nrm((no, 2, GLA_KDIM), 0.1),
        'gla_norm_g': gain((no, GLA_DV)),
        'gla_w_out': nrm((no, GLA_VDIM, d), GLA_VDIM ** -0.5),
        'rg_w': nrm((DEPTH, d, N_GROUPS), d ** -0.5),
        'rg_b': nrm((DEPTH, N_GROUPS), 0.01),
        're_w': nrm((DEPTH, d, N_EXPERTS), d ** -0.5),
        're_b': nrm((DEPTH, N_EXPERTS), 0.01),
        'moe_w1': nrm((DEPTH, N_EXPERTS, d, D_EXPERT), d ** -0.5),
        'moe_w3': nrm((DEPTH, N_EXPERTS, d, D_EXPERT), d ** -0.5),
        'moe_w2': nrm((DEPTH, N_EXPERTS, D_EXPERT, d), D_EXPERT ** -0.5),
    }


def reference(x, c, ctx, c_ctx, ada_w, ada_b, norm1_g, norm2_g, ab_w_in, conv_w, conv_b, conv_ln_g,
              conv_ln_b, diff_qnorm_g, diff_knorm_g, diff_lq1, diff_lk1, diff_lq2, diff_lk2,
              diff_subln_g, ab_w_out, gla_w_in, gla_w_a2, gla_b_a2, gla_norm_g, gla_w_out,
              rg_w, rg_b, re_w, re_b, moe_w1, moe_w3, moe_w2):
    bsz, n_lat, d = x.shape
    cos, sin = axial_rope_tables(n_lat)
    s_lat = jax.nn.silu(c)
    s_ctx = jax.nn.silu(c_ctx)
    hl, hc = x, ctx
    for l in range(DEPTH):
        last = l == DEPTH - 1
        i = l // 2
        ml = jnp.split((s_lat @ ada_w[l] + ada_b[l])[:, None, :], 6, axis=-1)
        mc = jnp.split(s_ctx @ ada_w[l] + ada_b[l], 6, axis=-1)
        nl = rmsnorm(hl, norm1_g[l]) * (1 + ml[1]) + ml[0]
        nc = rmsnorm(hc, norm1_g[l]) * (1 + mc[1]) + mc[0]
        if l % 2 == 0:
            lambda_init = 0.8 - 0.6 * math.exp(-0.3 * l)
            yl, yc = conv_diffattn_mixer(nl, nc, ab_w_in[i], conv_w[i], conv_b[i], conv_ln_g[i],
                                         conv_ln_b[i], diff_qnorm_g[i], diff_knorm_g[i],
                                         diff_lq1[i], diff_lk1[i], diff_lq2[i], diff_lk2[i],
                                         diff_subln_g[i], ab_w_out[i], lambda_init, cos, sin,
                                         not last)
        else:
            yl, yc = bigla_mixer(nl, nc, gla_w_in[i], gla_w_a2[i], gla_b_a2[i], gla_norm_g[i],
                                 gla_w_out[i], not last)
        hl = hl + ml[2] * yl
        nl = rmsnorm(hl, norm2_g[l]) * (1 + ml[4]) + ml[3]
        if last:
            y = hier_moe(nl.reshape(-1, d), rg_w[l], rg_b[l], re_w[l], re_b[l],
                         moe_w1[l], moe_w3[l], moe_w2[l])
            hl = hl + ml[5] * y.reshape(hl.shape)
        else:
            hc = hc + mc[2] * yc
            nc = rmsnorm(hc, norm2_g[l]) * (1 + mc[4]) + mc[3]
            n_l = bsz * n_lat
            y = hier_moe(jnp.concatenate([nl.reshape(-1, d), nc.reshape(-1, d)], axis=0),
                         rg_w[l], rg_b[l], re_w[l], re_b[l], moe_w1[l], moe_w3[l], moe_w2[l])
            hl = hl + ml[5] * y[:n_l].reshape(hl.shape)
            hc = hc + mc[5] * y[n_l:].reshape(hc.shape)
    return hl
```

```python
import numpy as np
from contextlib import ExitStack
import concourse.bass as bass
import concourse.mybir as mybir
from concourse.bass_utils import run_bass_kernel_spmd
import ml_dtypes

F32 = mybir.dt.float32
BF16 = mybir.dt.bfloat16
I32 = mybir.dt.int32
AF = mybir.ActivationFunctionType
ALU = mybir.AluOpType
AX = mybir.AxisListType
NPBF16 = ml_dtypes.bfloat16


def interleave(gens, width):
    active = []
    it = iter(gens)
    while True:
        while len(active) < width:
            g = next(it, None)
            if g is None:
                break
            active.append(g)
        if not active:
            break
        for g in list(active):
            try:
                next(g)
            except StopIteration:
                active.remove(g)


def ag_row(i, rank, chunk_rows, total_rows, world=4):
    r0 = (i // chunk_rows) * chunk_rows
    n = min(chunk_rows, total_rows - r0)
    return world * r0 + rank * n + (i - r0)


class T:
    def __init__(self, h, name, kind):
        self.h = h
        self.name = name
        self.kind = kind
        self.w = None
        self.r = {}
        self.dkey = None

    def __getitem__(self, idx):
        return self.h[idx]


class KB:
    def __init__(self):
        self.nc = bass.Bass("TRN2", target_bir_lowering=False)
        nc = self.nc
        self.es = ExitStack()
        self.eng = {"pe": nc.tensor, "act": nc.scalar, "dve": nc.vector, "pool": nc.gpsimd, "sp": nc.sync}
        self.sems = {}
        self.cnt = {}
        self.seen = {e: {} for e in self.eng}
        for e in self.eng:
            self.sems[e] = self.es.enter_context(nc.semaphore("e_" + e))
            self.cnt[e] = 0
        self.issued = {}
        self.n_ins = 0
        self.outs = []
        self._uid = 0
        self.cur = self.es
        self.prefix = ""
        self.tiles = []
        self.free_dsems = []
        self.stage_tiles0 = 0

    def sb(self, name, shape, dt, es=None):
        h = (es or self.cur).enter_context(self.nc.sbuf_tensor(self.prefix + name, list(shape), dt))
        t = T(h, name, "sb")
        self.tiles.append(t)
        return t

    def ps(self, name, shape=(128, 512), dt=F32):
        h = self.es.enter_context(self.nc.psum_tensor(name, list(shape), dt))
        t = T(h, name, "ps")
        self.tiles.append(t)
        return t

    def dram(self, name, shape, dt, kind="Internal"):
        h = self.nc.dram_tensor(self.prefix + name, list(shape), dt, kind=kind)
        t = T(h.ap(), name, "dram")
        self.tiles.append(t)
        if kind == "ExternalOutput":
            self.outs.append(t)
        return t

    def _dsem(self, t):
        if t.dkey is None:
            self._uid += 1
            t.dkey = "d%d_%s" % (self._uid, t.name)
            if self.free_dsems:
                h, v = self.free_dsems.pop()
                self.sems[t.dkey] = h
                self.issued[t.dkey] = v
            else:
                self.sems[t.dkey] = self.es.enter_context(self.nc.semaphore(t.dkey))
                self.issued[t.dkey] = 0
        return t.dkey

    def begin_stage(self, prefix):
        self.prefix = prefix
        self.cur = ExitStack()
        self.stage_tiles0 = len(self.tiles)

    def end_stage(self):
        self.barrier()
        self.cur.close()
        self.cur = self.es
        for t in self.tiles[self.stage_tiles0:]:
            if t.kind == "sb" and t.dkey is not None:
                self.free_dsems.append((self.sems[t.dkey], self.issued[t.dkey]))
                del self.issued[t.dkey]
                del self.sems[t.dkey]
                t.dkey = None
        for t in self.tiles:
            t.w = None
            t.r = {}
        for e in self.eng:
            self._uid += 1
            self.sems[e] = self.es.enter_context(self.nc.semaphore("e%d_%s" % (self._uid, e)))
            self.cnt[e] = 0
        self.seen = {e: {} for e in self.eng}
        self.prefix = ""

    def all_gather(self, src, dst, groups, chunk_rows):
        self.barrier()
        self._uid += 1
        sem = self.es.enter_context(self.nc.semaphore("cc%d" % self._uid))
        R = src.h.shape[0]
        k = 0
        for r0 in range(0, R, chunk_rows):
            n = min(chunk_rows, R - r0)
            self.nc.gpsimd.collective_compute("AllGather", ALU.bypass, replica_groups=groups, ins=[src.h[r0:r0 + n, :]],
                                              outs=[dst.h[4 * r0:4 * r0 + 4 * n, :]]).then_inc(sem, 1)
            k += 1
        self.nc.gpsimd.wait_ge(sem, k)
        if not hasattr(self, "_fence"):
            self._fence = T(self.es.enter_context(self.nc.sbuf_tensor("cc_fence", [128, 8], F32)), "cc_fence", "sb")
            self.tiles.append(self._fence)
        f = self._fence
        self.op("pool", lambda e: e.memset(f[:], 0.0), writes=[f])
        for en in self.eng:
            if en != "pool":
                self._waits(en, {"pool": self.cnt["pool"]})
        self.n_ins += k + 1

    def _deps(self, en, reads, writes, is_dma=False):
        deps = {}

        def add(key, val, kind):
            if key == en:
                if en == "pe" or kind == "war":
                    return
            if is_dma and kind == "waw" and key in self.issued:
                return
            deps[key] = max(deps.get(key, 0), val)

        for t in reads:
            if t.w is not None:
                add(t.w[0], t.w[1], "raw")
            if t.kind == "ps":
                for k, v in t.r.items():
                    if k != en:
                        add(k, v, "rar")
        for t in writes:
            if t.w is not None:
                add(t.w[0], t.w[1], "waw")
            for k, v in t.r.items():
                add(k, v, "war")
        return deps

    def _waits(self, en, deps):
        e = self.eng[en]
        for key, val in deps.items():
            if key in self.issued:
                val = self.issued[key]
            if self.seen[en].get(key, 0) >= val:
                continue
            e.wait_ge(self.sems[key], val)
            self.seen[en][key] = val
            self.n_ins += 1

    def op(self, en, fn, reads=(), writes=()):
        self._waits(en, self._deps(en, reads, writes))
        ins = fn(self.eng[en])
        self.cnt[en] += 1
        self.n_ins += 1
        ins.then_inc(self.sems[en], 1)
        c = self.cnt[en]
        for t in reads:
            t.r[en] = c
        for t in writes:
            t.w = (en, c)
            t.r = {}
        return ins

    def dma(self, q, fn, reads=(), writes=()):
        self._waits(q, self._deps(q, reads, writes, is_dma=True))
        cand = [t for t in writes if t.kind != "dram"] or [t for t in reads if t.kind != "dram"] or list(writes) or list(reads)
        key = self._dsem(cand[0])
        ins = fn(self.eng[q])
        self.issued[key] += 16
        self.n_ins += 1
        ins.then_inc(self.sems[key], 16)
        v = self.issued[key]
        for t in reads:
            t.r[key] = v
        for t in writes:
            t.w = (key, v)
            t.r = {}
        return ins

    def load(self, q, dst_t, dst_ap, src_ap, src_t=None, **kw):
        return self.dma(q, lambda e: e.dma_start(out=dst_ap, in_=src_ap, **kw),
                        reads=[src_t] if src_t is not None else [], writes=[dst_t])

    def store(self, q, dst_t, dst_ap, src_t, src_ap, **kw):
        return self.dma(q, lambda e: e.dma_start(out=dst_ap, in_=src_ap, **kw), reads=[src_t], writes=[dst_t])

    def finish(self):
        deps = {}
        for t in self.outs:
            if t.w is not None:
                deps[t.w[0]] = max(deps.get(t.w[0], 0), t.w[1])
        self._waits("sp", deps)
        self.es.close()
        return self.nc

    def barrier(self):
        for en in self.eng:
            deps = {}
            for k in self.eng:
                if k != en and self.cnt[k] > 0:
                    deps[k] = self.cnt[k]
            for k, v in self.issued.items():
                if v > 0:
                    deps[k] = v
            self._waits(en, deps)

    def identity(self, name, dt):
        f = self.sb(name + "_f", [128, 128], F32)
        self.op("pool", lambda e: e.memset(f[:], 0.0), writes=[f])
        self.op("pool", lambda e: e.affine_select(out=f[:], in_=f[:], pattern=[[-1, 128]], compare_op=ALU.not_equal,
                                                  fill=1.0, base=0, channel_multiplier=1), reads=[f], writes=[f])
        if dt == F32:
            return f
        b = self.sb(name, [128, 128], dt)
        self.op("pool", lambda e: e.tensor_copy(out=b[:], in_=f[:]), reads=[f], writes=[b])
        return b

EPS = 1e-6
S = 16384
LC = 256

NKT = (S + LC) // 128
ATT_NSPLIT = 512


def build_l0a(kb, banks, x1in, n_groups=32, debug=False):
    kb.begin_stage("a0_")
    x = kb.dram("x", [S, 1024], F32, "ExternalInput")
    ctx = kb.dram("ctx", [LC, 1024], F32, "ExternalInput")
    svec = kb.dram("svec", [128, 16], F32, "ExternalInput")
    adaw = kb.dram("adaw", [1024, 2048], F32, "ExternalInput")
    adab = kb.dram("adab", [128, 16], F32, "ExternalInput")
    g1 = kb.dram("g1", [128, 8], F32, "ExternalInput")
    w = kb.dram("w", [1024, 384], F32, "ExternalInput")
    small = kb.dram("small", [640], F32, "ExternalInput")
    cos4 = kb.dram("cos4", [S, 256], F32, "ExternalInput")
    sin4 = kb.dram("sin4", [S, 256], F32, "ExternalInput")
    zpad = kb.sb("zpad", [128, 4, 64], BF16)
    kb.op("pool", lambda e: e.memset(zpad[:], 0.0), writes=[zpad])
    kb.store("sp", x1in, x1in.h[:, 4160:4224].rearrange("(q p) n -> p q n", p=128), zpad, zpad[:])

    def bfv(t):
        return t[:].bitcast(BF16)

    identb = kb.identity("identb", BF16)
    smallb = kb.sb("smallb", [128, 640], F32)
    kb.load("sp", smallb, smallb[:], small.h.partition_broadcast(128), small)

    tmp64 = kb.sb("tmp64", [128, 2, 64], F32)
    dots = kb.sb("dots", [128, 4], F32)
    kb.op("dve", lambda e: e.tensor_tensor(out=tmp64[:, 0, :], in0=smallb[:, 256:320], in1=smallb[:, 320:384], op=ALU.mult), reads=[smallb], writes=[tmp64])
    kb.op("dve", lambda e: e.tensor_tensor(out=tmp64[:, 1, :], in0=smallb[:, 384:448], in1=smallb[:, 448:512], op=ALU.mult), reads=[smallb], writes=[tmp64])
    kb.op("dve", lambda e: e.tensor_reduce(out=dots[:, 0:2], in_=tmp64[:], axis=AX.X, op=ALU.add), reads=[tmp64], writes=[dots])
    kb.op("act", lambda e: e.activation(out=dots[:, 2:4], in_=dots[:, 0:2], func=AF.Exp), reads=[dots], writes=[dots])
    neglam = kb.sb("neglam", [128, 1], F32)
    kb.op("dve", lambda e: e.scalar_tensor_tensor(out=neglam[:], in0=dots[:, 3:4], scalar=-0.2, in1=dots[:, 2:3], op0=ALU.add, op1=ALU.subtract), reads=[dots], writes=[neglam])
    subg_s = kb.sb("subg_s", [128, 128], F32)
    kb.op("dve", lambda e: e.tensor_scalar(out=subg_s[:], in0=smallb[:, 512:640], scalar1=0.8, scalar2=None, op0=ALU.mult), reads=[smallb], writes=[subg_s])

    s_sb = kb.sb("s_sb", [128, 16], F32)
    kb.load("sp", s_sb, s_sb[:], svec.h, svec)
    kb.op("act", lambda e: e.activation(out=s_sb[:], in_=s_sb[:], func=AF.Silu), reads=[s_sb], writes=[s_sb])
    adab_sb = kb.sb("adab_sb", [128, 16], F32)
    kb.load("sp", adab_sb, adab_sb[:], adab.h, adab)
    g1_sb = kb.sb("g1_sb", [128, 8], F32)
    kb.load("sp", g1_sb, g1_sb[:], g1.h, g1)
    mod = kb.sb("mod", [128, 16, 2], F32)
    gs = kb.sb("gs", [128, 8, 2], F32)
    zer = kb.sb("zer", [128, 128], F32)
    wq = [kb.sb("wq%d" % j, [128, 8, 384], BF16) for j in range(2)]
    bias = [kb.sb("bias%d" % j, [128, 384], F32) for j in range(2)]
    p0 = ExitStack()
    adaw_sb = kb.sb("adaw_sb", [128, 8, 512], F32, p0)
    pm = banks[0]
    for v in range(4):
        kb.load("sp", adaw_sb, adaw_sb[:], adaw.h[:, v * 512:(v + 1) * 512].rearrange("(kc p) n -> p kc n", p=128), adaw)
        for oc in range(4):
            g = v * 4 + oc
            for kc in range(8):
                kb.op("pe", lambda e: e.matmul(pm[:, g * 2:g * 2 + 2], lhsT=adaw_sb[:, kc, oc * 128:(oc + 1) * 128],
                                              rhs=s_sb[:, kc * 2:kc * 2 + 2], start=(kc == 0), stop=(kc == 7)),
                      reads=[adaw_sb, s_sb], writes=[pm])
    pm3 = pm[:, 0:32].rearrange("p (g j) -> p g j", j=2)
    for j in range(2):
        kb.op("dve", lambda e: e.tensor_tensor(out=mod[:, :, j], in0=pm3[:, :, j], in1=adab_sb[:], op=ALU.add), reads=[pm, adab_sb], writes=[mod])
        kb.op("dve", lambda e: e.scalar_tensor_tensor(out=gs[:, :, j], in0=mod[:, 8:16, j], scalar=1.0, in1=g1_sb[:], op0=ALU.add, op1=ALU.mult),
              reads=[mod, g1_sb], writes=[gs])

    w_sb = kb.sb("w_sb", [128, 8, 384], F32, p0)
    kb.load("sp", w_sb, w_sb[:], w.h.rearrange("(kc p) n -> p kc n", p=128), w)
    kb.op("pool", lambda e: e.memset(zer[:], 0.0), writes=[zer])
    shiftbc = kb.sb("shiftbc", [128, 8, 128], F32, p0)
    for j in range(2):
        for kc in range(8):
            kb.op("dve", lambda e: e.tensor_scalar(out=wq[j][:, kc, :], in0=w_sb[:, kc, :], scalar1=gs[:, kc, j:j + 1], scalar2=None, op0=ALU.mult),
                  reads=[w_sb, gs], writes=[wq[j]])
            kb.op("dve", lambda e: e.tensor_scalar(out=shiftbc[:, kc, :], in0=zer[:], scalar1=mod[:, kc, j:j + 1], scalar2=None, op0=ALU.add),
                  reads=[zer, mod], writes=[shiftbc])
        pb = banks[1]
        for kc in range(8):
            kb.op("pe", lambda e: e.matmul(pb[:, 0:384], lhsT=shiftbc[:, kc, :], rhs=w_sb[:, kc, :], start=(kc == 0), stop=(kc == 7)),
                  reads=[shiftbc, w_sb], writes=[pb])
        kb.op("dve", lambda e: e.tensor_copy(out=bias[j][:], in_=pb[:, 0:384]), reads=[pb], writes=[bias[j]])

    kb.barrier()
    p0.close()
    QT = kb.sb("QT", [128, S + LC], BF16)
    KTm = [kb.sb("KT%d" % m, [128, S + LC], BF16) for m in range(2)]
    kb.op("pool", lambda e: e.memset(KTm[0][64:128, :], 0.0), writes=[KTm[0]])
    kb.op("pool", lambda e: e.memset(KTm[1][0:64, :], 0.0), writes=[KTm[1]])
    Vx = kb.sb("Vx", [128, NKT, 129], BF16)
    kb.op("pool", lambda e: e.memset(Vx[:, :, 128:129], 1.0), writes=[Vx])

    def dbl(name, shape, dt, n=2, es=None):
        return [kb.sb("%s%d" % (name, i), shape, dt, es) for i in range(n)]

    p2 = ExitStack()

    xt = dbl("xt", [128, 1024], F32, 2, p2)
    junk = kb.sb("junk", [128, 1024], BF16, p2)
    st1 = dbl("st1", [128, 4], F32, 2, p2)
    xn = dbl("xn", [128, 1024], BF16, 2, p2)
    xnT = dbl("xnT", [128, 1024], BF16, 2, p2)
    qkv = dbl("qkv", [128, 384], F32, 2, p2)
    cs = dbl("cs", [128, 256], F32, 2, p2)
    sn = dbl("sn", [128, 256], F32, 2, p2)
    sq = dbl("sq", [128, 256], F32, 2, p2)
    st2 = dbl("st2", [128, 12], F32, 2, p2)
    qkn = dbl("qkn", [128, 256], F32, 2, p2)
    sw = dbl("sw", [128, 256], F32, 2, p2)
    t1 = dbl("t1", [128, 256], F32, 2, p2)
    rr = dbl("rr", [128, 256], BF16, 2, p2)

    def rstd_chain(stt, c_in, c_tmp, c_out, n, inv_n, srcs):
        kb.op("dve", lambda e: e.tensor_scalar(out=stt[:, c_tmp:c_tmp + n], in0=stt[:, c_in:c_in + n], scalar1=inv_n, scalar2=EPS, op0=ALU.mult, op1=ALU.add),
              reads=[stt], writes=[stt])
        kb.op("act", lambda e: e.activation(out=stt[:, c_tmp:c_tmp + n], in_=stt[:, c_tmp:c_tmp + n], func=AF.Sqrt), reads=[stt], writes=[stt])
        kb.op("dve", lambda e: e.reciprocal(out=stt[:, c_out:c_out + n], in_=stt[:, c_tmp:c_tmp + n]), reads=[stt], writes=[stt])

    def proj_tile(i, src, row0, is_ctx, qcol, kcol, kt):
        p = i % 2
        j = 1 if is_ctx else 0
        kb.load("sp", xt[p], xt[p][:], src.h[row0:row0 + 128, :], src)
        yield
        if not is_ctx:
            kb.load("pool", cs[p], cs[p][:], cos4.h[row0:row0 + 128, :], cos4)
            yield
            kb.load("pool", sn[p], sn[p][:], sin4.h[row0:row0 + 128, :], sin4)
            yield
        kb.op("act", lambda e: e.activation(out=junk[:], in_=xt[p][:], func=AF.Square, accum_out=st1[p][:, 0:1]), reads=[xt[p]], writes=[junk, st1[p]])
        yield
        rstd_chain(st1[p], 0, 1, 2, 1, 1.0 / 1024, None)
        kb.op("act", lambda e: e.activation(out=xn[p][:], in_=xt[p][:], func=AF.Copy, scale=st1[p][:, 2:3]), reads=[xt[p], st1[p]], writes=[xn[p]])
        yield
        psT = banks[p]
        for kc in range(8):
            kb.op("pe", lambda e: e.transpose(out=bfv(psT)[:, kc * 128:(kc + 1) * 128], in_=xn[p][:, kc * 128:(kc + 1) * 128], identity=identb[:]),
                  reads=[xn[p], identb], writes=[psT])
            yield
        kb.op("dve", lambda e: e.tensor_copy(out=xnT[p][:], in_=bfv(psT)[:, 0:1024]), reads=[psT], writes=[xnT[p]])
        yield
        pp = banks[2 + p]
        for kc in range(8):
            kb.op("pe", lambda e: e.matmul(pp[:, 0:384], lhsT=xnT[p][:, kc * 128:(kc + 1) * 128], rhs=wq[j][:, kc, :], start=(kc == 0), stop=(kc == 7)),
                  reads=[xnT[p], wq[j]], writes=[pp])
            yield
        kb.op("dve", lambda e: e.tensor_tensor(out=qkv[p][:], in0=pp[:, 0:384], in1=bias[j][:], op=ALU.add), reads=[pp, bias[j]], writes=[qkv[p]])
        yield
        kb.op("pool", lambda e: e.tensor_copy(out=Vx[:, kt, 0:128], in_=qkv[p][:, 256:384]), reads=[qkv[p]], writes=[Vx])
        yield
        kb.op("act", lambda e: e.activation(out=sq[p][:], in_=qkv[p][:, 0:256], func=AF.Square), reads=[qkv[p]], writes=[sq[p]])
        yield
        kb.op("dve", lambda e: e.tensor_reduce(out=st2[p][:, 0:4], in_=sq[p][:].rearrange("p (g d) -> p g d", g=4), axis=AX.X, op=ALU.add),
              reads=[sq[p]], writes=[st2[p]])
        yield
        rstd_chain(st2[p], 0, 4, 8, 4, 1.0 / 64, None)
        for g in range(4):
            kb.op("dve", lambda e: e.scalar_tensor_tensor(out=qkn[p][:, g * 64:(g + 1) * 64], in0=qkv[p][:, g * 64:(g + 1) * 64], scalar=st2[p][:, 8 + g:9 + g],
                                                          in1=smallb[:, g * 64:(g + 1) * 64], op0=ALU.mult, op1=ALU.mult),
                  reads=[qkv[p], st2[p], smallb], writes=[qkn[p]])
            yield
        if is_ctx:
            kb.op("pool", lambda e: e.tensor_copy(out=rr[p][:], in_=qkn[p][:]), reads=[qkn[p]], writes=[rr[p]])
            yield
        else:
            q5 = qkn[p][:].rearrange("p (a h d) -> p a h d", h=2, d=16)
            s5 = sw[p][:].rearrange("p (a h d) -> p a h d", h=2, d=16)
            kb.op("pool", lambda e: e.tensor_copy(out=s5[:, :, 0, :], in_=q5[:, :, 1, :]), reads=[qkn[p]], writes=[sw[p]])
            yield
            kb.op("pool", lambda e: e.tensor_copy(out=s5[:, :, 1, :], in_=q5[:, :, 0, :]), reads=[qkn[p]], writes=[sw[p]])
            yield
            kb.op("pool", lambda e: e.tensor_tensor(out=sw[p][:], in0=sw[p][:], in1=sn[p][:], op=ALU.mult), reads=[sw[p], sn[p]], writes=[sw[p]])
            yield
            kb.op("dve", lambda e: e.tensor_tensor(out=t1[p][:], in0=qkn[p][:], in1=cs[p][:], op=ALU.mult), reads=[qkn[p], cs[p]], writes=[t1[p]])
            yield
            kb.op("dve", lambda e: e.tensor_tensor(out=rr[p][:], in0=t1[p][:], in1=sw[p][:], op=ALU.add), reads=[t1[p], sw[p]], writes=[rr[p]])
            yield
        pq = banks[4 + p]
        for hh in range(2):
            kb.op("pe", lambda e: e.transpose(out=bfv(pq)[:, hh * 128:(hh + 1) * 128], in_=rr[p][:, hh * 128:(hh + 1) * 128], identity=identb[:]),
                  reads=[rr[p], identb], writes=[pq])
            yield
        kb.op("act", lambda e: e.copy(out=QT[:, qcol:qcol + 128], in_=bfv(pq)[:, 0:128]), reads=[pq], writes=[QT])
        yield
        kb.op("act", lambda e: e.copy(out=KTm[0][0:64, kcol:kcol + 128], in_=bfv(pq)[0:64, 128:256]), reads=[pq], writes=[KTm[0]])
        kb.op("act", lambda e: e.copy(out=KTm[1][64:128, kcol:kcol + 128], in_=bfv(pq)[64:128, 128:256]), reads=[pq], writes=[KTm[1]])
        yield

    gens = []
    i = 0
    for c in range(LC // 128):
        gens.append(proj_tile(i, ctx, c * 128, True, S + c * 128, c * 128, c))
        i += 1
    for t in range(S // 128):
        gens.append(proj_tile(i, x, t * 128, False, t * 128, LC + t * 128, LC // 128 + t))
        i += 1
    interleave(gens, 2)
    kb.barrier()
    p2.close()

    ST = banks[0:3]
    OT = [banks[4], banks[5]]
    PL = [banks[6], banks[7]]
    PS_ = banks[3]
    PT = dbl("pt", [128, 512], BF16, 4)
    Pacc = [kb.sb("pacc%d" % m, [128, 512], F32) for m in range(2)]
    ones_bb = kb.sb("ones_bb", [128, 128], BF16)
    kb.op("pool", lambda e: e.memset(ones_bb[:], 1.0), writes=[ones_bb])
    ones_ff = kb.sb("ones_ff", [128, 128], F32)
    kb.op("pool", lambda e: e.memset(ones_ff[:], 1.0), writes=[ones_ff])
    subg_col = kb.sb("subg_col", [128, 1], F32)
    kb.load("sp", subg_col, subg_col[:], small.h[512:640].rearrange("(p o) -> p o", o=1), small)
    kb.op("dve", lambda e: e.tensor_scalar(out=subg_col[:], in0=subg_col[:], scalar1=0.8, scalar2=None, op0=ALU.mult), reads=[subg_col], writes=[subg_col])
    rlb = dbl("rlb", [128, 512], F32)
    eo = dbl("eo", [128, 512], F32)
    esq = kb.sb("esq", [128, 512], F32)
    outT = dbl("outT", [128, 512], BF16)
    gcount = [0]
    NSPLIT = ATT_NSPLIT

    def attend(qc0, nq, kts):
        steps = [(m, idx, kt) for m in range(2) for idx, kt in enumerate(kts)]
        nk = len(kts)
        gi = gcount[0]
        gcount[0] += 1

        def score(s):
            m, idx, kt = steps[s]
            st = ST[s % 3]
            for c0 in range(0, nq, NSPLIT):
                kb.op("pe", lambda e: e.matmul(st[:, c0:min(nq, c0 + NSPLIT)], lhsT=KTm[m][:, kt * 128:(kt + 1) * 128], rhs=QT[:, qc0 + c0:qc0 + min(nq, c0 + NSPLIT)],
                                              start=True, stop=True), reads=[KTm[m], QT], writes=[st])

        used = {}

        def rest(s):
            m, idx, kt = steps[s]
            st = ST[s % 3]
            pt = PT[s % 4]
            kb.op("act", lambda e: e.activation(out=pt[:, 0:nq], in_=st[:, 0:nq], func=AF.Exp, scale=0.125), reads=[st], writes=[pt])
            for c0 in range(0, nq, NSPLIT):
                kb.op("pe", lambda e: e.matmul(OT[m][:, c0:min(nq, c0 + NSPLIT)], lhsT=Vx[:, kt, 0:128], rhs=pt[:, c0:min(nq, c0 + NSPLIT)], start=(idx == 0 and c0 == 0), stop=(idx == nk - 1),
                                              skip_group_check=True), reads=[pt, Vx], writes=[OT[m]])
            if s + 3 < len(steps):
                score(s + 3)
            if idx % 3 == 2:
                kb.op("pe", lambda e: e.matmul(PL[m][:, 0:nq], lhsT=ones_bb[:], rhs=pt[:, 0:nq], start=((m, "pe") not in used), stop=False), reads=[ones_bb, pt], writes=[PL[m]])
                used[(m, "pe")] = True
            elif (m, "dve") not in used:
                used[(m, "dve")] = True
                kb.op("dve", lambda e: e.tensor_copy(out=Pacc[m][:, 0:nq], in_=pt[:, 0:nq]), reads=[pt], writes=[Pacc[m]])
            else:
                kb.op("dve", lambda e: e.tensor_tensor(out=Pacc[m][:, 0:nq], in0=pt[:, 0:nq], in1=Pacc[m][:, 0:nq], op=ALU.add), reads=[pt, Pacc[m]], writes=[Pacc[m]])

        for s0 in range(min(3, len(steps))):
            score(s0)
        for s in range(len(steps)):
            rest(s)
        for m in range(2):
            kb.op("pe", lambda e: e.matmul(PL[m][:, 0:nq], lhsT=ones_ff[:], rhs=Pacc[m][:, 0:nq], start=((m, "pe") not in used), stop=True), reads=[ones_ff, Pacc[m]], writes=[PL[m]])
            kb.op("dve", lambda e: e.reciprocal(out=rlb[m][:, 0:nq], in_=PL[m][:, 0:nq]), reads=[PL[m]], writes=[rlb[m]])
            kb.op("dve", lambda e: e.tensor_tensor(out=eo[m][:, 0:nq], in0=OT[m][:, 0:nq], in1=rlb[m][:, 0:nq], op=ALU.mult), reads=[OT[m], rlb[m]], writes=[eo[m]])
        kb.op("dve", lambda e: e.scalar_tensor_tensor(out=eo[0][:, 0:nq], in0=eo[1][:, 0:nq], scalar=neglam[:, 0:1], in1=eo[0][:, 0:nq], op0=ALU.mult, op1=ALU.add),
              reads=[eo[1], neglam, eo[0]], writes=[eo[0]])
        kb.op("act", lambda e: e.activation(out=esq[:, 0:nq], in_=eo[0][:, 0:nq], func=AF.Square), reads=[eo[0]], writes=[esq])
        kb.op("pe", lambda e: e.matmul(PS_[:, 0:nq], lhsT=ones_ff[:], rhs=esq[:, 0:nq], start=True, stop=True), reads=[ones_ff, esq], writes=[PS_])
        kb.op("dve", lambda e: e.tensor_scalar(out=rlb[0][:, 0:nq], in0=PS_[:, 0:nq], scalar1=1.0 / 128, scalar2=EPS, op0=ALU.mult, op1=ALU.add), reads=[PS_], writes=[rlb[0]])
        kb.op("act", lambda e: e.activation(out=rlb[0][:, 0:nq], in_=rlb[0][:, 0:nq], func=AF.Sqrt), reads=[rlb[0]], writes=[rlb[0]])
        kb.op("dve", lambda e: e.reciprocal(out=rlb[1][:, 0:nq], in_=rlb[0][:, 0:nq]), reads=[rlb[0]], writes=[rlb[1]])
        ot = outT[gi % 2]
        kb.op("dve", lambda e: e.scalar_tensor_tensor(out=ot[:, 0:nq], in0=eo[0][:, 0:nq], scalar=subg_col[:, 0:1], in1=rlb[1][:, 0:nq], op0=ALU.mult, op1=ALU.mult),
              reads=[eo[0], subg_col, rlb[1]], writes=[ot])
        if qc0 >= S:
            for q in range(4):
                kb.store("sp", x1in, x1in.h[q * 128:(q + 1) * 128, 4096:4160], ot, ot[:, q * 64:(q + 1) * 64])
        else:
            q, col = qc0 // 4096, qc0 % 4096
            kb.store("sp", x1in, x1in.h[q * 128:(q + 1) * 128, col:col + nq], ot, ot[:, 0:nq])

    attend(S, LC, list(range(LC // 128)))
    for g in range(n_groups):
        attend(g * 512, 512, list(range(NKT)))
    print("l0a instructions:", kb.n_ins)
    kb.end_stage()


def rope_tables():
    half = 32
    inv = (10000.0 ** (-np.arange(0, half, 2, dtype=np.float32) / half)).astype(np.float32)
    t = np.arange(S)
    r = (t // 64).astype(np.float32)[:, None] * inv[None, :]
    c = (t % 64).astype(np.float32)[:, None] * inv[None, :]
    ang = np.concatenate([r, r, c, c], axis=-1).astype(np.float32)
    cos = np.cos(ang).astype(np.float32)
    sin = np.sin(ang).astype(np.float32)
    sgn = np.concatenate([-np.ones(16), np.ones(16), -np.ones(16), np.ones(16)]).astype(np.float32)
    sin = sin * sgn[None, :]
    return np.ascontiguousarray(np.tile(cos, (1, 4))), np.ascontiguousarray(np.tile(sin, (1, 4)))


def fop(v, n):
    return np.ascontiguousarray(np.asarray(v, np.float32).reshape(n, 128).T)


def host_l0a(inp):
    cos4, sin4 = rope_tables()
    maps = []
    wi = inp["ab_w_in"][0]
    for b in range(2):
        for h in range(4):
            sv = np.stack([inp["c"][b], inp["c_ctx"]], -1).reshape(8, 128, 2).transpose(1, 0, 2).reshape(128, 16)
            w = np.concatenate([wi[:, 1024 + h * 128:1024 + (h + 1) * 128], wi[:, 1536 + h * 128:1536 + (h + 1) * 128],
                                wi[:, 2048 + h * 128:2048 + (h + 1) * 128]], axis=1)
            qg, kg = inp["diff_qnorm_g"][0], inp["diff_knorm_g"][0]
            small = np.concatenate([qg, qg, kg, kg, inp["diff_lq1"][0], inp["diff_lk1"][0], inp["diff_lq2"][0], inp["diff_lk2"][0],
                                    inp["diff_subln_g"][0]]).astype(np.float32)
            maps.append({
                "x": np.ascontiguousarray(inp["x"][b]), "ctx": np.ascontiguousarray(inp["ctx"][b]),
                "svec": np.ascontiguousarray(sv.astype(np.float32)),
                "adaw": np.ascontiguousarray(inp["ada_w"][0][:, 0:2048]), "adab": fop(inp["ada_b"][0][0:2048], 16),
                "g1": fop(inp["norm1_g"][0], 8), "w": np.ascontiguousarray(w), "small": small, "cos4": cos4, "sin4": sin4,
            })
    return maps


BIG = 1.0e30


def build_b(layer, kb, banks, mixsrc, hin_t, hout_t, debug=False):
    L0 = (layer == 0)
    NTL = 32
    NT = NTL + (1 if L0 else 0)
    NTOK = NT * 128
    NTOKV = 4096 + (64 if L0 else 0)
    NB = (2 * NTOKV + 32 * 255 + 255) // 256
    NROWS = NB * 256
    NMIX = 4 if L0 else 8

    kb.begin_stage("b%d_" % layer)
    hin = hin_t if hin_t is not None else kb.dram("hin", [NTOK, 1024], F32, "ExternalInput")
    svec = kb.dram("svec", [128, 16], F32, "ExternalInput")
    adaw = kb.dram("adaw", [1024, 6144], F32, "ExternalInput")
    adabf = kb.dram("adabf", [128, 48], F32, "ExternalInput")
    adabr = kb.dram("adabr", [2048], F32, "ExternalInput")
    gfop = kb.dram("gfop", [128, 16], F32, "ExternalInput")
    mixidx = kb.dram("mixidx", [128, NMIX], I32, "ExternalInput")
    wout = kb.dram("wout", [1024, 1024], F32, "ExternalInput")
    rw = kb.dram("rw", [1024, 36], F32, "ExternalInput")
    rb = kb.dram("rb", [36], F32, "ExternalInput")
    w1t = kb.dram("w1t", [4096, 4096], F32, "ExternalInput")
    w3t = kb.dram("w3t", [4096, 4096], F32, "ExternalInput")
    w2t = kb.dram("w2t", [4096, 4096], F32, "ExternalInput")
    valid = kb.dram("valid", [128, 1], F32, "ExternalInput")
    if L0:
        xhalo = kb.dram("xhalo", [128, 1024], F32, "ExternalInput")
        cxh = kb.dram("cxh", [128, 1024], F32, "ExternalInput")
        edge = kb.dram("edge", [2], F32, "ExternalInput")
        win = kb.dram("win", [1024, 1024], F32, "ExternalInput")
        cw = kb.dram("cw", [128, 124], F32, "ExternalInput")
        cvec = kb.dram("cvec", [128, 12], F32, "ExternalInput")
    hout = hout_t if hout_t is not None else kb.dram("hout", [NTOK, 1024], F32, "ExternalOutput")
    hlm = kb.dram("hlm", [NTOK, 1024], F32)
    nl2d = kb.dram("nl2d", [NTOK, 1024], BF16)
    xs = kb.dram("xs", [NROWS + 128, 1024], BF16)
    ys = kb.dram("ys", [NROWS + 128, 1024], F32)

    def bfv(t):
        return t[:].bitcast(BF16)

    def dbl(name, shape, dt, n=2, es=None):
        return [kb.sb("%s%d" % (name, i), shape, dt, es) for i in range(n)]

    identb = kb.identity("identb", BF16)
    zer = kb.sb("zer", [128, 128], F32)
    kb.op("pool", lambda e: e.memset(zer[:], 0.0), writes=[zer])
    zerb = kb.sb("zerb", [128, 2048], BF16)
    kb.op("pool", lambda e: e.memset(zerb[:], 0.0), writes=[zerb])
    for a in range(0, NROWS // 128, 2):
        kb.store("pool", xs, xs.h[a * 128:(a + 2) * 128, :].rearrange("(a p) n -> p a n", p=128), zerb, zerb[:].rearrange("p (a n) -> p a n", a=2))

    OH = kb.sb("OH", [128, NT, 2, 32], F32)
    GT = kb.sb("GT", [128, NT, 2], F32)
    RK = kb.sb("RK", [128, NT, 2], F32)
    Rbc = kb.sb("Rbc", [128, 32], F32)
    DESTI = kb.sb("DESTI", [128, NT * 2], I32)
    WIDX = kb.sb("WIDX", [128, NB], I32)
    validt = kb.sb("validt", [128, 1], F32)
    gate_bc = [[kb.sb("gate_bc%d%d" % (j, w), [128, 1024], F32) for w in range(2)] for j in range(2)]
    mod = kb.sb("mod", [128, 48, 2], F32)
    gs1 = kb.sb("gs1", [128, 8, 2], F32)
    gs2 = kb.sb("gs2", [128, 8, 2], F32)
    s_sb = kb.sb("s_sb", [128, 16], F32)
    adabf_sb = kb.sb("adabf_sb", [128, 48], F32)
    gfop_sb = kb.sb("gfop_sb", [128, 16], F32)
    iop = kb.sb("iop", [128, 1], F32)
    blkst = kb.sb("blkst", [128, NB], F32)
    ltri_b = kb.sb("ltri_b", [128, 128], BF16)
    ones_b = kb.sb("ones_b", [128, 128], BF16)
    pesA = ExitStack()

    def rstd_chain(stt, c_in, c_tmp, c_out, n, inv_n):
        kb.op("dve", lambda e: e.tensor_scalar(out=stt[:, c_tmp:c_tmp + n], in0=stt[:, c_in:c_in + n], scalar1=inv_n, scalar2=EPS, op0=ALU.mult, op1=ALU.add),
              reads=[stt], writes=[stt])
        kb.op("act", lambda e: e.activation(out=stt[:, c_tmp:c_tmp + n], in_=stt[:, c_tmp:c_tmp + n], func=AF.Sqrt), reads=[stt], writes=[stt])
        kb.op("dve", lambda e: e.reciprocal(out=stt[:, c_out:c_out + n], in_=stt[:, c_tmp:c_tmp + n]), reads=[stt], writes=[stt])

    kb.load("sp", s_sb, s_sb[:], svec.h, svec)
    kb.op("act", lambda e: e.activation(out=s_sb[:], in_=s_sb[:], func=AF.Silu), reads=[s_sb], writes=[s_sb])
    kb.load("sp", adabf_sb, adabf_sb[:], adabf.h, adabf)
    kb.load("sp", gfop_sb, gfop_sb[:], gfop.h, gfop)
    kb.load("sp", validt, validt[:], valid.h, valid)
    pm = banks[0]
    with ExitStack() as pes:
        adabr_sb = kb.sb("adabr_sb", [128, 2048], F32, pes)
        kb.load("sp", adabr_sb, adabr_sb[:], adabr.h.partition_broadcast(128), adabr)
        s_bc = [kb.sb("s_bc%d" % j, [128, 8, 128], F32, pes) for j in range(2)]
        for j in range(2):
            for kc in range(8):
                kb.op("dve", lambda e: e.tensor_scalar(out=s_bc[j][:, kc, :], in0=zer[:, 0:128], scalar1=s_sb[:, kc * 2 + j:kc * 2 + j + 1], scalar2=None, op0=ALU.add),
                      reads=[zer, s_sb], writes=[s_bc[j]])
        adaw_sb = dbl("adaw_sb", [128, 8, 512], F32, 2, pes)
        for v in range(12):
            aw = adaw_sb[v % 2]
            kb.load("sp", aw, aw[:], adaw.h[:, v * 512:(v + 1) * 512].rearrange("(kc p) n -> p kc n", p=128), adaw)
            for oc in range(4):
                g = v * 4 + oc
                for kc in range(8):
                    kb.op("pe", lambda e: e.matmul(pm[:, g * 2:g * 2 + 2], lhsT=aw[:, kc, oc * 128:(oc + 1) * 128], rhs=s_sb[:, kc * 2:kc * 2 + 2],
                                                  start=(kc == 0), stop=(kc == 7)), reads=[aw, s_sb], writes=[pm])
            if v in (4, 5, 10, 11):
                which = 0 if v < 6 else 1
                half = v % 2
                for j in range(2):
                    pr = banks[1 + j]
                    for kc in range(8):
                        kb.op("pe", lambda e: e.matmul(pr[:, :], lhsT=s_bc[j][:, kc, :], rhs=aw[:, kc, :], start=(kc == 0), stop=(kc == 7)),
                              reads=[s_bc[j], aw], writes=[pr])
                    kb.op("dve", lambda e: e.tensor_tensor(out=gate_bc[j][which][:, half * 512:(half + 1) * 512], in0=pr[:, :],
                                                           in1=adabr_sb[:, which * 1024 + half * 512: which * 1024 + (half + 1) * 512], op=ALU.add),
                          reads=[pr, adabr_sb], writes=[gate_bc[j][which]])
        pm3 = pm[:, 0:96].rearrange("p (g j) -> p g j", j=2)
        for j in range(2):
            kb.op("dve", lambda e: e.tensor_tensor(out=mod[:, :, j], in0=pm3[:, :, j], in1=adabf_sb[:], op=ALU.add), reads=[pm, adabf_sb], writes=[mod])
        kb.barrier()
    for j in range(2):
        kb.op("dve", lambda e: e.scalar_tensor_tensor(out=gs1[:, :, j], in0=mod[:, 8:16, j], scalar=1.0, in1=gfop_sb[:, 0:8], op0=ALU.add, op1=ALU.mult),
              reads=[mod, gfop_sb], writes=[gs1])
        kb.op("dve", lambda e: e.scalar_tensor_tensor(out=gs2[:, :, j], in0=mod[:, 32:40, j], scalar=1.0, in1=gfop_sb[:, 8:16], op0=ALU.add, op1=ALU.mult),
              reads=[mod, gfop_sb], writes=[gs2])
    SH1, SH2 = 0, 24

    xn_b = dbl("xn_b", [128, 1024], BF16, 2, pesA)
    junk = kb.sb("junk", [128, 1024], BF16, pesA)
    stn = dbl("stn", [128, 4], F32, 2, pesA)

    def norm_T(i, xt_tile, gs, shoff, j, dstT, dcol, psT):
        p = i % 2
        kb.op("act", lambda e: e.activation(out=junk[:], in_=xt_tile[:], func=AF.Square, accum_out=stn[p][:, 0:1]), reads=[xt_tile], writes=[junk, stn[p]])
        rstd_chain(stn[p], 0, 1, 2, 1, 1.0 / 1024)
        kb.op("act", lambda e: e.activation(out=xn_b[p][:], in_=xt_tile[:], func=AF.Copy, scale=stn[p][:, 2:3]), reads=[xt_tile, stn[p]], writes=[xn_b[p]])
        for kc in range(8):
            kb.op("pe", lambda e: e.transpose(out=bfv(psT)[:, kc * 128:(kc + 1) * 128], in_=xn_b[p][:, kc * 128:(kc + 1) * 128], identity=identb[:]),
                  reads=[xn_b[p], identb], writes=[psT])
        for kc in range(8):
            kb.op("act", lambda e: e.activation(out=dstT[:, kc, dcol:dcol + 128], in_=bfv(psT)[:, kc * 128:(kc + 1) * 128], func=AF.Identity,
                                                scale=gs[:, kc, j:j + 1], bias=mod[:, shoff + kc, j:j + 1]), reads=[psT, gs, mod], writes=[dstT])

    xt = dbl("xt", [128, 1024], F32, 2, pesA)
    convT = kb.sb("convT", [128, 4, NTOK], BF16, pesA) if L0 else None

    if L0:
        with ExitStack() as pes:
            HW = 15 + 4096 + 15
            hT = kb.sb("hT", [128, 4, HW], F32, pes)
            hTc = kb.sb("hTc", [128, 4, 128], F32, pes)
            win_b = kb.sb("win_b", [128, 8, 1024], BF16, pes)
            stg = xt
            for kc in range(8):
                kb.load("sp", stg[kc % 2], stg[kc % 2][:], win.h[kc * 128:(kc + 1) * 128, :], win)
                kb.op("pool", lambda e: e.tensor_copy(out=win_b[:, kc, :], in_=stg[kc % 2][:]), reads=[stg[kc % 2]], writes=[win_b])
            cw_sb = kb.sb("cw_sb", [128, 4, 31], F32, pes)
            kb.load("sp", cw_sb, cw_sb[:], cw.h.rearrange("p (c t) -> p c t", c=4), cw)
            cvec_sb = kb.sb("cvec_sb", [128, 12], F32, pes)
            kb.load("sp", cvec_sb, cvec_sb[:], cvec.h, cvec)
            edge_sb = kb.sb("edge_sb", [128, 2], F32, pes)
            kb.load("sp", edge_sb, edge_sb[:], edge.h.partition_broadcast(128), edge)
            ones_s = kb.sb("ones_s", [128, 128], F32, pes)
            kb.op("pool", lambda e: e.memset(ones_s[:], 1.0 / 512), writes=[ones_s])
            nlT = dbl("nlT", [128, 8, 512], BF16, 1, pes) * 2
            sig = dbl("sig", [128, 512], F32, 2, pes)
            htmp = kb.sb("htmp", [128, 4, 128], F32, pes)

            def u_group(gi, nl, ncols, dst_fn):
                for cc in range(4):
                    pa, pg = banks[2], banks[3]
                    for kc in range(8):
                        kb.op("pe", lambda e: e.matmul(pa[:, 0:ncols], lhsT=win_b[:, kc, cc * 128:(cc + 1) * 128], rhs=nl[:, kc, 0:ncols], start=(kc == 0), stop=(kc == 7)),
                              reads=[win_b, nl], writes=[pa])
                    for kc in range(8):
                        kb.op("pe", lambda e: e.matmul(pg[:, 0:ncols], lhsT=win_b[:, kc, 512 + cc * 128:512 + (cc + 1) * 128], rhs=nl[:, kc, 0:ncols], start=(kc == 0), stop=(kc == 7)),
                              reads=[win_b, nl], writes=[pg])
                    sg = sig[cc % 2]
                    kb.op("act", lambda e: e.activation(out=sg[:, 0:ncols], in_=pg[:, 0:ncols], func=AF.Sigmoid), reads=[pg], writes=[sg])
                    dt_, dap = dst_fn(cc)
                    kb.op("dve", lambda e: e.tensor_tensor(out=dap, in0=pa[:, 0:ncols], in1=sg[:, 0:ncols], op=ALU.mult), reads=[pa, sg], writes=[dt_])

            ti = 0
            for g in range(8):
                nl = nlT[g % 2]
                for tt in range(4):
                    t = g * 4 + tt
                    kb.load("sp", xt[ti % 2], xt[ti % 2][:], hin.h[t * 128:(t + 1) * 128, :], hin)
                    norm_T(ti, xt[ti % 2], gs1, SH1, 0, nl, tt * 128, banks[ti % 2])
                    ti += 1
                u_group(g, nl, 512, lambda cc: (hT, hT[:, cc, 15 + g * 512:15 + (g + 1) * 512]))
            nl = nlT[0]
            kb.load("sp", xt[ti % 2], xt[ti % 2][:], xhalo.h, xhalo)
            norm_T(ti, xt[ti % 2], gs1, SH1, 0, nl, 0, banks[ti % 2])
            ti += 1
            u_group(8, nl, 128, lambda cc: (htmp, htmp[:, cc, :]))
            for cc in range(4):
                kb.op("dve", lambda e: e.tensor_scalar(out=hT[:, cc, 0:15], in0=htmp[:, cc, 0:15], scalar1=edge_sb[:, 0:1], scalar2=None, op0=ALU.mult),
                      reads=[htmp, edge_sb], writes=[hT])
                kb.op("dve", lambda e: e.tensor_scalar(out=hT[:, cc, 15 + 4096:HW], in0=htmp[:, cc, 15:30], scalar1=edge_sb[:, 1:2], scalar2=None, op0=ALU.mult),
                      reads=[htmp, edge_sb], writes=[hT])
            nl = nlT[1]
            kb.load("sp", xt[ti % 2], xt[ti % 2][:], cxh.h, cxh)
            norm_T(ti, xt[ti % 2], gs1, SH1, 1, nl, 0, banks[ti % 2])
            ti += 1
            u_group(9, nl, 128, lambda cc: (hTc, hTc[:, cc, :]))
            for cc in range(4):
                kb.op("dve", lambda e: e.tensor_scalar(out=hTc[:, cc, 0:15], in0=hTc[:, cc, 0:15], scalar1=edge_sb[:, 0:1], scalar2=None, op0=ALU.mult),
                      reads=[hTc, edge_sb], writes=[hTc])
                kb.op("dve", lambda e: e.tensor_scalar(out=hTc[:, cc, 79:94], in0=hTc[:, cc, 79:94], scalar1=edge_sb[:, 1:2], scalar2=None, op0=ALU.mult),
                      reads=[hTc, edge_sb], writes=[hTc])

            acc = [kb.sb("acc%d" % c, [128, 512], F32, pes) for c in range(4)]
            sqt = dbl("sqt", [128, 512], F32, 1, pes) * 2
            mean_sb = kb.sb("mean_sb", [128, 512], F32, pes)
            m2 = kb.sb("m2", [128, 512], F32, pes)
            rstd_bc = kb.sb("rstd_bc", [128, 512], F32, pes)
            tt_ = dbl("tt_", [128, 512], F32, 1, pes) * 2

            def conv_block(src, c0, n, out_c0):
                for tau in range(31):
                    for cc in range(4):
                        en = "dve"
                        if tau == 0:
                            kb.op(en, lambda e: e.tensor_scalar(out=acc[cc][:, 0:n], in0=src[:, cc, c0:c0 + n], scalar1=cw_sb[:, cc, 0:1], scalar2=cvec_sb[:, cc:cc + 1],
                                                                op0=ALU.mult, op1=ALU.add), reads=[src, cw_sb, cvec_sb], writes=[acc[cc]])
                        else:
                            kb.op(en, lambda e: e.scalar_tensor_tensor(out=acc[cc][:, 0:n], in0=src[:, cc, c0 + tau:c0 + tau + n], scalar=cw_sb[:, cc, tau:tau + 1],
                                                                       in1=acc[cc][:, 0:n], op0=ALU.mult, op1=ALU.add), reads=[src, cw_sb, acc[cc]], writes=[acc[cc]])
                pmean, pex2 = banks[4], banks[5]
                for cc in range(4):
                    kb.op("pe", lambda e: e.matmul(pmean[:, 0:n], lhsT=ones_s[:], rhs=acc[cc][:, 0:n], start=(cc == 0), stop=(cc == 3)), reads=[ones_s, acc[cc]], writes=[pmean])
                for cc in range(4):
                    sq_ = sqt[cc % 2]
                    kb.op("act", lambda e: e.activation(out=sq_[:, 0:n], in_=acc[cc][:, 0:n], func=AF.Square), reads=[acc[cc]], writes=[sq_])
                    kb.op("pe", lambda e: e.matmul(pex2[:, 0:n], lhsT=ones_s[:], rhs=sq_[:, 0:n], start=(cc == 0), stop=(cc == 3)), reads=[ones_s, sq_], writes=[pex2])
                kb.op("act", lambda e: e.copy(out=mean_sb[:, 0:n], in_=pmean[:, 0:n]), reads=[pmean], writes=[mean_sb])
                kb.op("pool", lambda e: e.tensor_tensor(out=m2[:, 0:n], in0=mean_sb[:, 0:n], in1=mean_sb[:, 0:n], op=ALU.mult), reads=[mean_sb], writes=[m2])
                kb.op("dve", lambda e: e.tensor_tensor(out=m2[:, 0:n], in0=pex2[:, 0:n], in1=m2[:, 0:n], op=ALU.subtract), reads=[pex2, m2], writes=[m2])
                kb.op("dve", lambda e: e.tensor_scalar(out=m2[:, 0:n], in0=m2[:, 0:n], scalar1=EPS, scalar2=None, op0=ALU.add), reads=[m2], writes=[m2])
                kb.op("act", lambda e: e.activation(out=m2[:, 0:n], in_=m2[:, 0:n], func=AF.Sqrt), reads=[m2], writes=[m2])
                kb.op("dve", lambda e: e.reciprocal(out=rstd_bc[:, 0:n], in_=m2[:, 0:n]), reads=[m2], writes=[rstd_bc])
                for cc in range(4):
                    t_ = tt_[cc % 2]
                    kb.op("dve", lambda e: e.tensor_tensor(out=t_[:, 0:n], in0=acc[cc][:, 0:n], in1=mean_sb[:, 0:n], op=ALU.subtract), reads=[acc[cc], mean_sb], writes=[t_])
                    kb.op("pool", lambda e: e.tensor_tensor(out=t_[:, 0:n], in0=t_[:, 0:n], in1=rstd_bc[:, 0:n], op=ALU.mult), reads=[t_, rstd_bc], writes=[t_])
                    kb.op("act", lambda e: e.activation(out=convT[:, cc, out_c0:out_c0 + n], in_=t_[:, 0:n], func=AF.Silu, scale=cvec_sb[:, 4 + cc:5 + cc],
                                                        bias=cvec_sb[:, 8 + cc:9 + cc]), reads=[t_, cvec_sb], writes=[convT])

            for tb in range(8):
                conv_block(hT, tb * 512, 512, tb * 512)
            conv_block(hTc, 0, 64, 4096)
            kb.op("pool", lambda e: e.memset(convT[:, :, 4096 + 64:4096 + 128], 0.0), writes=[convT])
            kb.barrier()

    mix_sb = kb.sb("mix_sb", [128, NMIX, NTOK], BF16, pesA)
    mixidx_sb = kb.sb("mixidx_sb", [128, NMIX], I32, pesA)
    kb.load("sp", mixidx_sb, mixidx_sb[:], mixidx.h, mixidx)
    for hh in range(NMIX):
        kb.dma("pool", lambda e: e.indirect_dma_start(out=mix_sb[:, hh, :], out_offset=None, in_=mixsrc.h[:, :],
                                                      in_offset=bass.IndirectOffsetOnAxis(ap=mixidx_sb[:, hh:hh + 1], axis=0)), reads=[mixidx_sb, mixsrc], writes=[mix_sb])
    wout_b = kb.sb("wout_b", [128, 8, 1024], BF16, pesA)
    rw_b = kb.sb("rw_b", [128, 8, 36], BF16, pesA)
    rb_bc = kb.sb("rb_bc", [128, 36], F32, pesA)
    kb.load("sp", rb_bc, rb_bc[:], rb.h.partition_broadcast(128), rb)
    kb.op("pool", lambda e: e.memset(Rbc[:], 0.0), writes=[Rbc])
    ltri = kb.sb("ltri", [128, 128], F32, pesA)
    kb.op("pool", lambda e: e.memset(ltri[:], 1.0), writes=[ltri])
    kb.op("pool", lambda e: e.affine_select(out=ltri[:], in_=ltri[:], pattern=[[1, 128]], compare_op=ALU.is_gt, fill=0.0, base=0, channel_multiplier=-1),
          reads=[ltri], writes=[ltri])
    kb.op("pool", lambda e: e.tensor_copy(out=ltri_b[:], in_=ltri[:]), reads=[ltri], writes=[ltri_b])
    kb.op("pool", lambda e: e.memset(ones_b[:], 1.0), writes=[ones_b])
    kb.op("pool", lambda e: e.iota(iop[:], pattern=[[0, 1]], base=0, channel_multiplier=1, allow_small_or_imprecise_dtypes=True), writes=[iop])
    kb.op("pool", lambda e: e.iota(blkst[:], pattern=[[256, NB]], base=0, channel_multiplier=0, allow_small_or_imprecise_dtypes=True), writes=[blkst])

    with ExitStack() as pes:
        stg = dbl("stg2", [128, 1024], F32, 2, pes)
        for kc in range(8):
            kb.load("sp", stg[kc % 2], stg[kc % 2][:], wout.h[kc * 128:(kc + 1) * 128, :], wout)
            kb.op("pool", lambda e: e.tensor_copy(out=wout_b[:, kc, :], in_=stg[kc % 2][:]), reads=[stg[kc % 2]], writes=[wout_b])
        rw_f = kb.sb("rw_f", [128, 8, 36], F32, pes)
        kb.load("sp", rw_f, rw_f[:], rw.h.rearrange("(kc p) n -> p kc n", p=128), rw)
        kb.op("pool", lambda e: e.tensor_copy(out=rw_b[:], in_=rw_f[:]), reads=[rw_f], writes=[rw_b])

        ytmp = dbl("ytmp", [128, 1024], F32, 2, pes)
        hl = dbl("hl", [128, 1024], F32, 2, pes)
        nl2T = dbl("nl2T", [128, 8, 128], BF16, 2, pes)
        nl2 = dbl("nl2", [128, 1024], BF16, 2, pes)
        lg = dbl("lg", [128, 36], F32, 2, pes)
        rt = dbl("rt", [128, 16], F32, 2, pes)
        lem = dbl("lem", [128, 32], F32, 2, pes)
        lem2 = dbl("lem2", [128, 32], F32, 2, pes)
        cb_ = dbl("cb_", [128, 32], BF16, 2, pes)
        rbase = dbl("rbase", [128, 32], F32, 2, pes)
        tmp32 = dbl("tmp32", [128, 2, 32], F32, 2, pes)
        ejunk = kb.sb("ejunk", [128, 4], F32, pes)

        for t in range(NT):
            p = t % 2
            j = 1 if (L0 and t == NT - 1) else 0
            kb.load("sp", xt[p], xt[p][:], hin.h[t * 128:(t + 1) * 128, :], hin)
            chunks = []
            if L0:
                for cc in range(4):
                    chunks.append((convT, convT[:, cc, t * 128:(t + 1) * 128]))
            for hh in range(NMIX):
                chunks.append((mix_sb, mix_sb[:, hh, t * 128:(t + 1) * 128]))
            for half in range(2):
                py = banks[half]
                for ci, (ct, cap) in enumerate(chunks):
                    kb.op("pe", lambda e: e.matmul(py[:, :], lhsT=cap, rhs=wout_b[:, ci, half * 512:(half + 1) * 512], start=(ci == 0), stop=(ci == 7)),
                          reads=[ct, wout_b], writes=[py])
                kb.op("dve", lambda e: e.tensor_tensor(out=ytmp[p][:, half * 512:(half + 1) * 512], in0=py[:, :], in1=gate_bc[j][0][:, half * 512:(half + 1) * 512], op=ALU.mult),
                      reads=[py, gate_bc[j][0]], writes=[ytmp[p]])
            kb.op("pool", lambda e: e.tensor_tensor(out=hl[p][:], in0=ytmp[p][:], in1=xt[p][:], op=ALU.add), reads=[ytmp[p], xt[p]], writes=[hl[p]])
            kb.store("sp", hlm, hlm.h[t * 128:(t + 1) * 128, :], hl[p], hl[p][:])
            norm_T(t, hl[p], gs2, SH2, j, nl2T[p], 0, banks[2])
            pl = banks[3]
            for kc in range(8):
                kb.op("pe", lambda e: e.matmul(pl[:, 0:36], lhsT=nl2T[p][:, kc, :], rhs=rw_b[:, kc, :], start=(kc == 0), stop=(kc == 7)), reads=[nl2T[p], rw_b], writes=[pl])
            kb.op("dve", lambda e: e.tensor_tensor(out=lg[p][:], in0=pl[:, 0:36], in1=rb_bc[:], op=ALU.add), reads=[pl, rb_bc], writes=[lg[p]])
            pbk = banks[4]
            for kc in range(8):
                kb.op("pe", lambda e: e.transpose(out=bfv(pbk)[:, kc * 128:(kc + 1) * 128], in_=nl2T[p][:, kc, :], identity=identb[:]), reads=[nl2T[p], identb], writes=[pbk])
            kb.op("act", lambda e: e.copy(out=nl2[p][:], in_=bfv(pbk)[:, 0:1024]), reads=[pbk], writes=[nl2[p]])
            kb.store("sp", nl2d, nl2d.h[t * 128:(t + 1) * 128, :], nl2[p], nl2[p][:])
            r_ = rt[p]
            kb.op("dve", lambda e: e.tensor_reduce(out=r_[:, 0:1], in_=lg[p][:, 0:4], axis=AX.X, op=ALU.max), reads=[lg[p]], writes=[r_])
            kb.op("dve", lambda e: e.tensor_scalar(out=r_[:, 1:5], in0=lg[p][:, 0:4], scalar1=r_[:, 0:1], scalar2=None, op0=ALU.is_equal), reads=[lg[p], r_], writes=[r_])
            kb.op("dve", lambda e: e.tensor_scalar(out=r_[:, 5:6], in0=r_[:, 0:1], scalar1=-1.0, scalar2=None, op0=ALU.mult), reads=[r_], writes=[r_])
            kb.op("act", lambda e: e.activation(out=ejunk[:], in_=lg[p][:, 0:4], func=AF.Exp, bias=r_[:, 5:6], accum_out=r_[:, 6:7]), reads=[lg[p], r_], writes=[ejunk, r_])
            kb.op("dve", lambda e: e.reciprocal(out=r_[:, 7:8], in_=r_[:, 6:7]), reads=[r_], writes=[r_])
            kb.op("dve", lambda e: e.tensor_scalar(out=r_[:, 8:12], in0=r_[:, 1:5], scalar1=-1.0, scalar2=BIG, op0=ALU.add, op1=ALU.mult), reads=[r_], writes=[r_])
            for g in range(4):
                kb.op("dve", lambda e: e.tensor_scalar(out=lem[p][:, g * 8:(g + 1) * 8], in0=lg[p][:, 4 + g * 8:4 + (g + 1) * 8], scalar1=r_[:, 8 + g:9 + g], scalar2=None, op0=ALU.add),
                      reads=[lg[p], r_], writes=[lem[p]])
            oh1 = OH[:, t, 0, :]
            oh2 = OH[:, t, 1, :]
            kb.op("dve", lambda e: e.tensor_reduce(out=r_[:, 12:13], in_=lem[p][:], axis=AX.X, op=ALU.max), reads=[lem[p]], writes=[r_])
            kb.op("dve", lambda e: e.tensor_scalar(out=oh1, in0=lem[p][:], scalar1=r_[:, 12:13], scalar2=None, op0=ALU.is_equal), reads=[lem[p], r_], writes=[OH])
            kb.op("dve", lambda e: e.scalar_tensor_tensor(out=lem2[p][:], in0=oh1, scalar=-BIG, in1=lem[p][:], op0=ALU.mult, op1=ALU.add), reads=[OH, lem[p]], writes=[lem2[p]])
            kb.op("dve", lambda e: e.tensor_reduce(out=r_[:, 13:14], in_=lem2[p][:], axis=AX.X, op=ALU.max), reads=[lem2[p]], writes=[r_])
            kb.op("dve", lambda e: e.tensor_scalar(out=oh2, in0=lem2[p][:], scalar1=r_[:, 13:14], scalar2=None, op0=ALU.is_equal), reads=[lem2[p], r_], writes=[OH])
            kb.op("dve", lambda e: e.tensor_tensor(out=r_[:, 14:15], in0=r_[:, 12:13], in1=r_[:, 13:14], op=ALU.subtract), reads=[r_], writes=[r_])
            kb.op("act", lambda e: e.activation(out=r_[:, 15:16], in_=r_[:, 14:15], func=AF.Sigmoid), reads=[r_], writes=[r_])
            kb.op("dve", lambda e: e.tensor_tensor(out=GT[:, t, 0:1], in0=r_[:, 15:16], in1=r_[:, 7:8], op=ALU.mult), reads=[r_], writes=[GT])
            kb.op("dve", lambda e: e.tensor_tensor(out=GT[:, t, 1:2], in0=r_[:, 7:8], in1=GT[:, t, 0:1], op=ALU.subtract), reads=[r_, GT], writes=[GT])
            if j == 1:
                kb.op("dve", lambda e: e.tensor_scalar(out=OH[:, t, :, :], in0=OH[:, t, :, :], scalar1=validt[:, 0:1], scalar2=None, op0=ALU.mult), reads=[OH, validt], writes=[OH])
            kb.op("dve", lambda e: e.tensor_tensor(out=cb_[p][:], in0=OH[:, t, 0, :], in1=OH[:, t, 1, :], op=ALU.add), reads=[OH], writes=[cb_[p]])
            pc = banks[5]
            kb.op("pe", lambda e: e.matmul(pc[:, 0:32], lhsT=ltri_b[:], rhs=cb_[p][:], start=True, stop=True), reads=[ltri_b, cb_[p]], writes=[pc])
            kb.op("dve", lambda e: e.tensor_tensor(out=rbase[p][:], in0=pc[:, 0:32], in1=Rbc[:], op=ALU.add), reads=[pc, Rbc], writes=[rbase[p]])
            pt_ = banks[6]
            kb.op("pe", lambda e: e.matmul(pt_[:, 0:32], lhsT=ones_b[:], rhs=cb_[p][:], start=True, stop=True), reads=[ones_b, cb_[p]], writes=[pt_])
            kb.op("dve", lambda e: e.tensor_tensor(out=Rbc[:], in0=pt_[:, 0:32], in1=Rbc[:], op=ALU.add), reads=[pt_, Rbc], writes=[Rbc])
            for k in range(2):
                kb.op("dve", lambda e: e.tensor_tensor(out=tmp32[p][:, k, :], in0=OH[:, t, k, :], in1=rbase[p][:], op=ALU.mult), reads=[OH, rbase[p]], writes=[tmp32[p]])
            kb.op("dve", lambda e: e.tensor_reduce(out=RK[:, t, :], in_=tmp32[p][:], axis=AX.X, op=ALU.add), reads=[tmp32[p]], writes=[RK])
        kb.barrier()
    pesA.close()
    pesD = ExitStack()

    cnt_i = kb.sb("cnt_i", [128, 32], I32, pesD)
    pcnt = kb.sb("pcnt", [128, 32], F32, pesD)
    pend = [kb.sb("pend%d" % i, [128, 32], F32, pesD) for i in range(2)]
    kb.op("dve", lambda e: e.tensor_scalar(out=pcnt[:], in0=Rbc[:], scalar1=255.0, scalar2=None, op0=ALU.add), reads=[Rbc], writes=[pcnt])
    kb.op("dve", lambda e: e.tensor_copy(out=cnt_i[:], in_=pcnt[:]), reads=[pcnt], writes=[cnt_i])
    kb.op("dve", lambda e: e.tensor_scalar(out=cnt_i[:], in0=cnt_i[:], scalar1=8, scalar2=8, op0=ALU.arith_shift_right, op1=ALU.logical_shift_left), reads=[cnt_i], writes=[cnt_i])
    kb.op("dve", lambda e: e.tensor_copy(out=pcnt[:], in_=cnt_i[:]), reads=[cnt_i], writes=[pcnt])
    kb.op("dve", lambda e: e.tensor_copy(out=pend[0][:], in_=pcnt[:]), reads=[pcnt], writes=[pend[0]])
    cur = 0
    for sft in (1, 2, 4, 8, 16):
        a, b = pend[cur], pend[1 - cur]
        kb.op("dve", lambda e: e.tensor_copy(out=b[:, 0:sft], in_=a[:, 0:sft]), reads=[a], writes=[b])
        kb.op("dve", lambda e: e.tensor_tensor(out=b[:, sft:32], in0=a[:, sft:32], in1=a[:, 0:32 - sft], op=ALU.add), reads=[a], writes=[b])
        cur = 1 - cur
    pendf = pend[cur]
    poff = kb.sb("poff", [128, 32], F32, pesD)
    kb.op("dve", lambda e: e.tensor_tensor(out=poff[:], in0=pendf[:], in1=pcnt[:], op=ALU.subtract), reads=[pendf, pcnt], writes=[poff])
    DEST = kb.sb("DEST", [128, NT, 2], F32, pesD)
    tmpd = kb.sb("tmpd", [128, NT * 2, 32], F32, pesD)
    for t in range(NT):
        for k in range(2):
            kb.op("dve", lambda e: e.tensor_tensor(out=tmpd[:, t * 2 + k, :], in0=OH[:, t, k, :], in1=poff[:], op=ALU.mult), reads=[OH, poff], writes=[tmpd])
    kb.op("dve", lambda e: e.tensor_reduce(out=DEST[:].rearrange("p t k -> p (t k)"), in_=tmpd[:], axis=AX.X, op=ALU.add), reads=[tmpd], writes=[DEST])
    kb.op("dve", lambda e: e.tensor_tensor(out=DEST[:], in0=DEST[:], in1=RK[:], op=ALU.add), reads=[DEST, RK], writes=[DEST])
    if L0:
        inval = kb.sb("inval", [128, 2], F32, pesD)
        kb.op("dve", lambda e: e.tensor_scalar(out=inval[:, 0:1], in0=validt[:], scalar1=-1.0, scalar2=-1.0, op0=ALU.add, op1=ALU.mult), reads=[validt], writes=[inval])
        kb.op("dve", lambda e: e.scalar_tensor_tensor(out=inval[:, 1:2], in0=iop[:], scalar=float(NROWS), in1=inval[:, 0:1], op0=ALU.add, op1=ALU.mult), reads=[iop, inval], writes=[inval])
        kb.op("dve", lambda e: e.tensor_scalar(out=DEST[:, NT - 1, :], in0=DEST[:, NT - 1, :], scalar1=validt[:, 0:1], scalar2=inval[:, 1:2], op0=ALU.mult, op1=ALU.add),
              reads=[DEST, validt, inval], writes=[DEST])
    kb.op("dve", lambda e: e.tensor_copy(out=DESTI[:], in_=DEST[:].rearrange("p t k -> p (t k)")), reads=[DEST], writes=[DESTI])
    eb = kb.sb("eb", [128, NB], F32, pesD)
    kb.op("pool", lambda e: e.memset(eb[:], 0.0), writes=[eb])
    for ee in range(32):
        kb.op("dve", lambda e: e.scalar_tensor_tensor(out=eb[:], in0=blkst[:], scalar=pendf[:, ee:ee + 1], in1=eb[:], op0=ALU.is_ge, op1=ALU.add), reads=[blkst, pendf, eb], writes=[eb])
    kb.op("dve", lambda e: e.tensor_scalar(out=eb[:], in0=eb[:], scalar1=31.0, scalar2=128.0, op0=ALU.min, op1=ALU.mult), reads=[eb], writes=[eb])
    kb.op("dve", lambda e: e.tensor_scalar(out=eb[:], in0=eb[:], scalar1=iop[:, 0:1], scalar2=None, op0=ALU.add), reads=[eb, iop], writes=[eb])
    kb.op("dve", lambda e: e.tensor_copy(out=WIDX[:], in_=eb[:]), reads=[eb], writes=[WIDX])

    srow = dbl("srow", [128, 1024], BF16, 3, pesD)
    for t in range(NT):
        sr = srow[t % 3]
        kb.load("sp", sr, sr[:], nl2d.h[t * 128:(t + 1) * 128, :], nl2d)
        for k in range(2):
            kb.dma("pool", lambda e: e.indirect_dma_start(out=xs.h[:, :], out_offset=bass.IndirectOffsetOnAxis(ap=DESTI[:, t * 2 + k:t * 2 + k + 1], axis=0),
                                                          in_=sr[:], in_offset=None), reads=[DESTI, sr], writes=[xs])
    kb.barrier()
    pesD.close()
    pesE = ExitStack()

    w1f = dbl("w1f", [128, 4096], F32, 2, pesE)
    w3f = dbl("w3f", [128, 4096], F32, 2, pesE)
    w2f = dbl("w2f", [128, 4096], F32, 2, pesE)
    w1b = dbl("w1b", [128, 8, 512], BF16, 1, pesE) * 2
    w3b = dbl("w3b", [128, 8, 512], BF16, 1, pesE) * 2
    w2b = dbl("w2b", [128, 4, 1024], BF16, 1, pesE) * 2
    xr = dbl("xr", [128, 2, 1024], BF16, 2, pesE)
    xsT = dbl("xsT", [128, 8, 256], BF16, 2, pesE)
    sl = dbl("sl", [128, 256], F32, 2, pesE)
    hhT = dbl("hhT", [128, 4, 256], BF16, 2, pesE)
    yo = dbl("yo", [128, 1024], F32, 2, pesE)
    for b in range(NB):
        p = b % 2
        for (tab, wf) in ((w1t, w1f[p]), (w3t, w3f[p]), (w2t, w2f[p])):
            kb.dma("pool", lambda e: e.indirect_dma_start(out=wf[:], out_offset=None, in_=tab.h[:, :], in_offset=bass.IndirectOffsetOnAxis(ap=WIDX[:, b:b + 1], axis=0)),
                   reads=[WIDX, tab], writes=[wf])
        kb.op("act", lambda e: e.copy(out=w1b[p][:].rearrange("p a b -> p (a b)"), in_=w1f[p][:]), reads=[w1f[p]], writes=[w1b[p]])
        kb.op("dve", lambda e: e.tensor_copy(out=w3b[p][:].rearrange("p a b -> p (a b)"), in_=w3f[p][:]), reads=[w3f[p]], writes=[w3b[p]])
        kb.op("pool", lambda e: e.tensor_copy(out=w2b[p][:].rearrange("p a b -> p (a b)"), in_=w2f[p][:]), reads=[w2f[p]], writes=[w2b[p]])
        kb.load("sp", xr[p], xr[p][:], xs.h[b * 256:(b + 1) * 256, :].rearrange("(a p) n -> p a n", p=128), xs)
        for sub in range(2):
            pT = banks[6]
            for kc in range(8):
                kb.op("pe", lambda e: e.transpose(out=bfv(pT)[:, kc * 128:(kc + 1) * 128], in_=xr[p][:, sub, kc * 128:(kc + 1) * 128], identity=identb[:]),
                      reads=[xr[p], identb], writes=[pT])
            kb.op("act", lambda e: e.copy(out=xsT[p][:, :, sub * 128:(sub + 1) * 128], in_=bfv(pT)[:, 0:1024].rearrange("p (a b) -> p a b", a=8)), reads=[pT], writes=[xsT[p]])
        for fc in range(4):
            ph1 = banks[0 + fc // 2]
            ph3 = banks[2 + fc // 2]
            c0 = (fc % 2) * 256
            for kc in range(8):
                kb.op("pe", lambda e: e.matmul(ph1[:, c0:c0 + 256], lhsT=w1b[p][:, kc, fc * 128:(fc + 1) * 128], rhs=xsT[p][:, kc, :], start=(kc == 0), stop=(kc == 7)),
                      reads=[w1b[p], xsT[p]], writes=[ph1])
            for kc in range(8):
                kb.op("pe", lambda e: e.matmul(ph3[:, c0:c0 + 256], lhsT=w3b[p][:, kc, fc * 128:(fc + 1) * 128], rhs=xsT[p][:, kc, :], start=(kc == 0), stop=(kc == 7)),
                      reads=[w3b[p], xsT[p]], writes=[ph3])
            s_ = sl[fc % 2]
            kb.op("act", lambda e: e.activation(out=s_[:], in_=ph1[:, c0:c0 + 256], func=AF.Silu), reads=[ph1], writes=[s_])
            kb.op("dve", lambda e: e.tensor_tensor(out=hhT[p][:, fc, :], in0=ph3[:, c0:c0 + 256], in1=s_[:], op=ALU.mult), reads=[ph3, s_], writes=[hhT[p]])
        for sub in range(2):
            y_ = yo[sub]
            for half in range(2):
                py = banks[4 + half]
                for fc in range(4):
                    kb.op("pe", lambda e: e.matmul(py[:, :], lhsT=hhT[p][:, fc, sub * 128:(sub + 1) * 128], rhs=w2b[p][:, fc, half * 512:(half + 1) * 512], start=(fc == 0), stop=(fc == 3)),
                          reads=[hhT[p], w2b[p]], writes=[py])
                if half == 0:
                    kb.op("act", lambda e: e.copy(out=y_[:, 0:512], in_=py[:, :]), reads=[py], writes=[y_])
                else:
                    kb.op("dve", lambda e: e.tensor_copy(out=y_[:, 512:1024], in_=py[:, :]), reads=[py], writes=[y_])
            kb.store("sp", ys, ys.h[b * 256 + sub * 128:b * 256 + (sub + 1) * 128, :], y_, y_[:])
    kb.barrier()
    pesE.close()
    pesF = ExitStack()

    y1 = dbl("y1", [128, 1024], F32, 2, pesF)
    y2 = dbl("y2", [128, 1024], F32, 2, pesF)
    hm = dbl("hm", [128, 1024], F32, 2, pesF)
    for t in range(NT):
        p = t % 2
        j = 1 if (L0 and t == NT - 1) else 0
        if j == 1:
            kb.op("pool", lambda e: e.memset(y1[p][:], 0.0), writes=[y1[p]])
            kb.op("pool", lambda e: e.memset(y2[p][:], 0.0), writes=[y2[p]])
        for k, yk in ((0, y1[p]), (1, y2[p])):
            kb.dma("pool", lambda e: e.indirect_dma_start(out=yk[:], out_offset=None, in_=ys.h[:, :], in_offset=bass.IndirectOffsetOnAxis(ap=DESTI[:, t * 2 + k:t * 2 + k + 1], axis=0)), reads=[DESTI, ys], writes=[yk])
        kb.load("sp", hm[p], hm[p][:], hlm.h[t * 128:(t + 1) * 128, :], hlm)
        kb.op("dve", lambda e: e.tensor_scalar(out=y1[p][:], in0=y1[p][:], scalar1=GT[:, t, 0:1], scalar2=None, op0=ALU.mult), reads=[y1[p], GT], writes=[y1[p]])
        kb.op("dve", lambda e: e.scalar_tensor_tensor(out=y1[p][:], in0=y2[p][:], scalar=GT[:, t, 1:2], in1=y1[p][:], op0=ALU.mult, op1=ALU.add), reads=[y2[p], GT, y1[p]], writes=[y1[p]])
        kb.op("pool", lambda e: e.tensor_tensor(out=y1[p][:], in0=y1[p][:], in1=gate_bc[j][1][:], op=ALU.mult), reads=[y1[p], gate_bc[j][1]], writes=[y1[p]])
        kb.op("dve", lambda e: e.tensor_tensor(out=hm[p][:], in0=hm[p][:], in1=y1[p][:], op=ALU.add), reads=[hm[p], y1[p]], writes=[hm[p]])
        kb.store("sp", hout, hout.h[t * 128:(t + 1) * 128, :], hm[p], hm[p][:])
    print("lb%d instructions:" % layer, kb.n_ins, "sems:", len(kb.sems))
    pesF.close()
    kb.end_stage()


def fop(v, n):
    return np.ascontiguousarray(np.asarray(v, np.float32).reshape(n, 128).T)


def moe_tables(inp, l):
    w1 = np.ascontiguousarray(inp["moe_w1"][l].reshape(32, 8, 128, 512).transpose(0, 2, 1, 3).reshape(4096, 4096))
    w3 = np.ascontiguousarray(inp["moe_w3"][l].reshape(32, 8, 128, 512).transpose(0, 2, 1, 3).reshape(4096, 4096))
    w2 = np.ascontiguousarray(inp["moe_w2"][l].reshape(32, 4, 128, 1024).transpose(0, 2, 1, 3).reshape(4096, 4096))
    return w1, w3, w2


def host_b(layer, inp):
    L0 = layer == 0
    l = layer
    w1, w3, w2 = moe_tables(inp, l)
    rw = np.ascontiguousarray(np.concatenate([inp["rg_w"][l], inp["re_w"][l]], axis=1).astype(np.float32))
    rb = np.concatenate([inp["rg_b"][l], inp["re_b"][l]]).astype(np.float32)
    adabr = np.concatenate([inp["ada_b"][l][2048:3072], inp["ada_b"][l][5120:6144]]).astype(np.float32)
    gfop = np.ascontiguousarray(np.concatenate([fop(inp["norm1_g"][l], 8), fop(inp["norm2_g"][l], 8)], axis=1))
    wout = np.ascontiguousarray(inp["ab_w_out"][0] if L0 else inp["gla_w_out"][0])
    hin_lat, hin_ctx = inp["x"], inp["ctx"]
    maps = []
    pp = np.arange(128, dtype=np.int32)
    for b in range(2):
        sv = np.stack([inp["c"][b], inp["c_ctx"]], -1).reshape(8, 128, 2).transpose(1, 0, 2).reshape(128, 16).astype(np.float32)
        for jq in range(4):
            r0, r1 = jq * 4096, (jq + 1) * 4096
            m = {"svec": np.ascontiguousarray(sv), "adaw": np.ascontiguousarray(inp["ada_w"][l]), "adabf": fop(inp["ada_b"][l], 48), "adabr": adabr, "gfop": gfop,
                 "wout": wout, "rw": rw, "rb": rb, "w1t": w1, "w3t": w3, "w2t": w2}
            if L0:
                cpad = np.zeros((128, 1024), np.float32)
                cpad[:64] = hin_ctx[b, 64 * jq:64 * jq + 64]
                m["hin"] = np.ascontiguousarray(np.concatenate([hin_lat[b, r0:r1], cpad], 0))
                m["mixidx"] = np.ascontiguousarray(np.stack([np.array([ag_row(jq * 128 + int(p_), h, 64, 512) for p_ in pp]) for h in range(4)], axis=1).astype(np.int32))
                xh = np.zeros((128, 1024), np.float32)
                if jq > 0:
                    xh[0:15] = hin_lat[b, r0 - 15:r0]
                if jq < 3:
                    xh[15:30] = hin_lat[b, r1:r1 + 15]
                m["xhalo"] = xh
                ch = np.zeros((128, 1024), np.float32)
                for r in range(94):
                    pos = 64 * jq - 15 + r
                    if 0 <= pos < 256:
                        ch[r] = hin_ctx[b, pos]
                m["cxh"] = ch
                m["edge"] = np.array([1.0 if jq > 0 else 0.0, 1.0 if jq < 3 else 0.0], np.float32)
                v = np.zeros((128, 1), np.float32)
                v[:64] = 1
                m["valid"] = v
                m["win"] = np.ascontiguousarray(inp["ab_w_in"][0][:, 0:1024])
                m["cw"] = np.ascontiguousarray(inp["conv_w"][0].T.reshape(4, 128, 31).transpose(1, 0, 2).reshape(128, 124))
                m["cvec"] = np.ascontiguousarray(np.concatenate([fop(inp["conv_b"][0], 4), fop(inp["conv_ln_g"][0], 4), fop(inp["conv_ln_b"][0], 4)], axis=1))
            else:
                m["mixidx"] = np.ascontiguousarray(np.stack([np.array([ag_row(jq * 256 + c2 * 128 + int(p_), h, 128, 1024) for p_ in pp]) for h in range(4) for c2 in range(2)], axis=1).astype(np.int32))
                m["valid"] = np.ones((128, 1), np.float32)
            maps.append(m)
    return maps


def gather_b(layer, results):
    L0 = layer == 0
    hl = np.zeros((2, 16384, 1024), np.float32)
    hc = np.zeros((2, 256, 1024), np.float32) if L0 else None
    for b in range(2):
        for jq in range(4):
            o = results[b * 4 + jq]["hout"]
            hl[b, jq * 4096:(jq + 1) * 4096] = o[:4096]
            if L0:
                hc[b, 64 * jq:64 * jq + 64] = o[4096:4160]
    return hl, hc


def build_l1a(kb, banks, x2out, x3in, n_lat_tiles=128, do_scan=True):
    kb.begin_stage("a1_")
    svec = kb.dram("svec", [128, 16], F32, "ExternalInput")
    adaw = kb.dram("adaw", [1024, 2048], F32, "ExternalInput")
    adab = kb.dram("adab", [128, 16], F32, "ExternalInput")
    g1 = kb.dram("g1", [128, 8], F32, "ExternalInput")
    w = kb.dram("w", [1024, 768], F32, "ExternalInput")
    waT = kb.dram("waT", [2, 16, 1024], F32, "ExternalInput")
    wa2 = kb.dram("wa2", [2, 16, 128], F32, "ExternalInput")
    small = kb.dram("small", [512], F32, "ExternalInput")
    proj = kb.dram("proj", [S + LC, 1024], F32)
    of_d = kb.dram("of_d", [S, 256], F32)

    def bfv(t):
        return t[:].bitcast(BF16)

    def dbl(name, shape, dt, n=2, es=None):
        return [kb.sb("%s%d" % (name, i), shape, dt, es) for i in range(n)]

    identb = kb.identity("identb", BF16)
    smallb = kb.sb("smallb", [128, 512], F32)
    kb.load("sp", smallb, smallb[:], small.h.partition_broadcast(128), small)
    zer = kb.sb("zer", [128, 128], F32)
    kb.op("pool", lambda e: e.memset(zer[:], 0.0), writes=[zer])

    def rstd_chain(stt, c_in, c_tmp, c_out, n, inv_n):
        kb.op("dve", lambda e: e.tensor_scalar(out=stt[:, c_tmp:c_tmp + n], in0=stt[:, c_in:c_in + n], scalar1=inv_n, scalar2=EPS, op0=ALU.mult, op1=ALU.add),
              reads=[stt], writes=[stt])
        kb.op("act", lambda e: e.activation(out=stt[:, c_tmp:c_tmp + n], in_=stt[:, c_tmp:c_tmp + n], func=AF.Sqrt), reads=[stt], writes=[stt])
        kb.op("dve", lambda e: e.reciprocal(out=stt[:, c_out:c_out + n], in_=stt[:, c_tmp:c_tmp + n]), reads=[stt], writes=[stt])

    wq = [kb.sb("wq%d" % j, [128, 8, 1024], BF16) for j in range(2)]
    bias = [kb.sb("bias%d" % j, [128, 1024], F32) for j in range(2)]
    pesA = ExitStack()
    s_sb = kb.sb("s_sb", [128, 16], F32, pesA)
    kb.load("sp", s_sb, s_sb[:], svec.h, svec)
    kb.op("act", lambda e: e.activation(out=s_sb[:], in_=s_sb[:], func=AF.Silu), reads=[s_sb], writes=[s_sb])
    adab_sb = kb.sb("adab_sb", [128, 16], F32, pesA)
    kb.load("sp", adab_sb, adab_sb[:], adab.h, adab)
    g1_sb = kb.sb("g1_sb", [128, 8], F32, pesA)
    kb.load("sp", g1_sb, g1_sb[:], g1.h, g1)
    mod = kb.sb("mod", [128, 16, 2], F32, pesA)
    gs = kb.sb("gs", [128, 8, 2], F32, pesA)
    w_sb = kb.sb("w_sb", [128, 8, 1024], F32, pesA)
    shiftbc = kb.sb("shiftbc", [128, 8, 128], F32, pesA)
    pm = banks[0]
    with ExitStack() as pes:
        adaw_sb = kb.sb("adaw_sb", [128, 8, 512], F32, pes)
        for v in range(4):
            kb.load("sp", adaw_sb, adaw_sb[:], adaw.h[:, v * 512:(v + 1) * 512].rearrange("(kc p) n -> p kc n", p=128), adaw)
            for oc in range(4):
                g = v * 4 + oc
                for kc in range(8):
                    kb.op("pe", lambda e: e.matmul(pm[:, g * 2:g * 2 + 2], lhsT=adaw_sb[:, kc, oc * 128:(oc + 1) * 128], rhs=s_sb[:, kc * 2:kc * 2 + 2],
                                                  start=(kc == 0), stop=(kc == 7)), reads=[adaw_sb, s_sb], writes=[pm])
        pm3 = pm[:, 0:32].rearrange("p (g j) -> p g j", j=2)
        for j in range(2):
            kb.op("dve", lambda e: e.tensor_tensor(out=mod[:, :, j], in0=pm3[:, :, j], in1=adab_sb[:], op=ALU.add), reads=[pm, adab_sb], writes=[mod])
            kb.op("dve", lambda e: e.scalar_tensor_tensor(out=gs[:, :, j], in0=mod[:, 8:16, j], scalar=1.0, in1=g1_sb[:], op0=ALU.add, op1=ALU.mult),
                  reads=[mod, g1_sb], writes=[gs])
        kb.load("sp", w_sb, w_sb[:, :, 0:768], w.h.rearrange("(kc p) n -> p kc n", p=128), w)
        waT_sb = [kb.sb("waT_sb%d" % d, [32, 1024], F32, pes) for d in range(2)]
        wa2_sb = [kb.sb("wa2_sb%d" % d, [32, 128], F32, pes) for d in range(2)]
        for d in range(2):
            kb.op("pool", lambda e: e.memset(waT_sb[d][:], 0.0), writes=[waT_sb[d]])
            kb.op("pool", lambda e: e.memset(wa2_sb[d][:], 0.0), writes=[wa2_sb[d]])
            kb.load("sp", waT_sb[d], waT_sb[d][0:16, :], waT.h[d], waT)
            kb.load("sp", wa2_sb[d], wa2_sb[d][0:16, :], wa2.h[d], wa2)
            for kc in range(8):
                pz = banks[1]
                kb.op("pe", lambda e: e.matmul(pz[:, 0:128], lhsT=waT_sb[d][:, kc * 128:(kc + 1) * 128], rhs=wa2_sb[d][:], start=True, stop=True),
                      reads=[waT_sb[d], wa2_sb[d]], writes=[pz])
                kb.op("dve", lambda e: e.tensor_copy(out=w_sb[:, kc, 768 + d * 128:768 + (d + 1) * 128], in_=pz[:, 0:128]), reads=[pz], writes=[w_sb])
        for j in range(2):
            for kc in range(8):
                kb.op("dve", lambda e: e.tensor_scalar(out=wq[j][:, kc, :], in0=w_sb[:, kc, :], scalar1=gs[:, kc, j:j + 1], scalar2=None, op0=ALU.mult),
                      reads=[w_sb, gs], writes=[wq[j]])
                kb.op("dve", lambda e: e.tensor_scalar(out=shiftbc[:, kc, :], in0=zer[:], scalar1=mod[:, kc, j:j + 1], scalar2=None, op0=ALU.add),
                      reads=[zer, mod], writes=[shiftbc])
            for half in range(2):
                pb = banks[2 + half]
                for kc in range(8):
                    kb.op("pe", lambda e: e.matmul(pb[:, :], lhsT=shiftbc[:, kc, :], rhs=w_sb[:, kc, half * 512:(half + 1) * 512], start=(kc == 0), stop=(kc == 7)),
                          reads=[shiftbc, w_sb], writes=[pb])
                kb.op("dve", lambda e: e.tensor_copy(out=bias[j][:, half * 512:(half + 1) * 512], in_=pb[:, :]), reads=[pb], writes=[bias[j]])
            kb.op("dve", lambda e: e.tensor_tensor(out=bias[j][:, 768:1024], in0=bias[j][:, 768:1024], in1=smallb[:, 0:256], op=ALU.add), reads=[bias[j], smallb], writes=[bias[j]])
        kb.barrier()
    kb.barrier()
    pesA.close()

    pesB = ExitStack()
    xt = dbl("xt", [128, 1024], F32, 2, pesB)
    junk = kb.sb("junk", [128, 1024], BF16, pesB)
    st1 = dbl("st1", [128, 4], F32, 2, pesB)
    xn = dbl("xn", [128, 1024], BF16, 2, pesB)
    xnT = dbl("xnT", [128, 1024], BF16, 2, pesB)
    pj = dbl("pj", [128, 1024], F32, 2, pesB)
    ez = dbl("ez", [128, 256], F32, 2, pesB)

    def proj_tile(i, src, row0, is_ctx, drow):
        p = i % 2
        j = 1 if is_ctx else 0
        if is_ctx:
            c = row0 // 128
            for hf in range(2):
                r = ag_row(4096, 2 * c + hf, 256, 4224)
                kb.load("sp", xt[p], xt[p][hf * 64:(hf + 1) * 64, :], x2out.h[r:r + 64, :], x2out)
                yield
        else:
            t = row0 // 128
            r = ag_row((t % 32) * 128, t // 32, 256, 4224)
            kb.load("sp", xt[p], xt[p][:], x2out.h[r:r + 128, :], x2out)
            yield
        kb.op("act", lambda e: e.activation(out=junk[:], in_=xt[p][:], func=AF.Square, accum_out=st1[p][:, 0:1]), reads=[xt[p]], writes=[junk, st1[p]])
        yield
        rstd_chain(st1[p], 0, 1, 2, 1, 1.0 / 1024)
        kb.op("act", lambda e: e.activation(out=xn[p][:], in_=xt[p][:], func=AF.Copy, scale=st1[p][:, 2:3]), reads=[xt[p], st1[p]], writes=[xn[p]])
        yield
        psT = banks[p]
        for kc in range(8):
            kb.op("pe", lambda e: e.transpose(out=bfv(psT)[:, kc * 128:(kc + 1) * 128], in_=xn[p][:, kc * 128:(kc + 1) * 128], identity=identb[:]),
                  reads=[xn[p], identb], writes=[psT])
            yield
        kb.op("dve", lambda e: e.tensor_copy(out=xnT[p][:], in_=bfv(psT)[:, 0:1024]), reads=[psT], writes=[xnT[p]])
        yield
        for half in range(2):
            pp = banks[2 + 2 * p + half]
            for kc in range(8):
                kb.op("pe", lambda e: e.matmul(pp[:, :], lhsT=xnT[p][:, kc * 128:(kc + 1) * 128], rhs=wq[j][:, kc, half * 512:(half + 1) * 512], start=(kc == 0), stop=(kc == 7)),
                      reads=[xnT[p], wq[j]], writes=[pp])
                yield
            kb.op("dve", lambda e: e.tensor_tensor(out=pj[p][:, half * 512:(half + 1) * 512], in0=pp[:, :], in1=bias[j][:, half * 512:(half + 1) * 512], op=ALU.add),
                  reads=[pp, bias[j]], writes=[pj[p]])
            yield
        kb.op("act", lambda e: e.activation(out=ez[p][:], in_=pj[p][:, 768:1024], func=AF.Exp, scale=-1.0), reads=[pj[p]], writes=[ez[p]])
        yield
        kb.op("pool", lambda e: e.tensor_scalar(out=ez[p][:], in0=ez[p][:], scalar1=1.0, scalar2=None, op0=ALU.add), reads=[ez[p]], writes=[ez[p]])
        yield
        kb.op("act", lambda e: e.activation(out=ez[p][:], in_=ez[p][:], func=AF.Ln), reads=[ez[p]], writes=[ez[p]])
        yield
        kb.op("pool", lambda e: e.tensor_scalar(out=pj[p][:, 768:1024], in0=ez[p][:], scalar1=-1.0 / 16, scalar2=None, op0=ALU.mult), reads=[ez[p]], writes=[pj[p]])
        yield
        kb.store("sp", proj, proj.h[drow:drow + 128, :], pj[p], pj[p][:])
        yield

    gens = []
    i = 0
    for c in range(2):
        gens.append(proj_tile(i, None, c * 128, True, S + c * 128))
        i += 1
    for t in range(n_lat_tiles):
        gens.append(proj_tile(i, None, t * 128, False, t * 128))
        i += 1
    interleave(gens, 2)
    kb.barrier()
    pesB.close()

    mask = []
    for d in range(2):
        mf = kb.sb("mask%d" % d, [128, 128], F32)
        kb.op("pool", lambda e: e.memset(mf[:], 1.0), writes=[mf])
        if d == 0:
            kb.op("pool", lambda e: e.affine_select(out=mf[:], in_=mf[:], pattern=[[1, 128]], compare_op=ALU.is_ge, fill=0.0, base=0, channel_multiplier=-1), reads=[mf], writes=[mf])
        else:
            kb.op("pool", lambda e: e.affine_select(out=mf[:], in_=mf[:], pattern=[[-1, 128]], compare_op=ALU.is_ge, fill=0.0, base=0, channel_multiplier=1), reads=[mf], writes=[mf])
        mask.append(mf)
    ones_f = kb.sb("ones_f", [128, 128], F32)
    kb.op("pool", lambda e: e.memset(ones_f[:], 1.0), writes=[ones_f])
    Sst = kb.sb("Sst", [128, 256], F32)
    Sb = dbl("Sb", [128, 256], BF16)
    pt = dbl("pt", [128, 1024], F32, 3)
    bc = dbl("bc", [128, 128], F32)
    eb = dbl("eb", [128, 128], F32)
    enb = dbl("enb", [128, 128], F32)
    dlt = dbl("dlt", [128, 128], F32)
    dec = dbl("dec", [128, 1], F32)
    qt = dbl("qt", [128, 128], BF16)
    ktl = dbl("ktl", [128, 128], BF16)
    kh = dbl("kh", [128, 128], BF16)
    vb = dbl("vb", [128, 256], BF16)
    qkT = dbl("qkT", [128, 256], BF16)
    attm = dbl("attm", [128, 128], BF16)
    ofs = dbl("ofs", [128, 256], F32)
    osum = dbl("osum", [128, 256], F32)
    fst = dbl("fst", [128, 4], F32)
    sg = dbl("sg", [128, 256], F32)
    ogb = dbl("ogb", [128, 256], BF16)
    ogT_sb = dbl("ogT_sb", [128, 2, 128], BF16)
    QS = 128.0 ** -0.5
    step = [0]

    def gla_prep(c, row, d):
        p = c % 2
        B = banks[4 * p:4 * p + 4]
        t_ = pt[c % 3]
        kb.load("sp", t_, t_[:], proj.h[row:row + 128, :], proj)
        yield
        la = t_[:, 768 + d * 128:768 + (d + 1) * 128]
        kb.op("pe", lambda e: e.matmul(B[0][:, 0:128], lhsT=mask[d][:], rhs=la, start=True, stop=True), reads=[mask[d], t_], writes=[B[0]])
        yield
        kb.op("pe", lambda e: e.matmul(B[0][:, 128:256], lhsT=ones_f[:], rhs=la, start=True, stop=True), reads=[ones_f, t_], writes=[B[0]])
        yield
        kb.op("pe", lambda e: e.matmul(B[0][:, 256:384], lhsT=la, rhs=ones_f[:], start=True, stop=True), reads=[ones_f, t_], writes=[B[0]])
        yield
        kb.op("act", lambda e: e.copy(out=bc[p][:], in_=B[0][:, 0:128]), reads=[B[0]], writes=[bc[p]])
        yield
        kb.op("act", lambda e: e.activation(out=eb[p][:], in_=B[0][:, 0:128], func=AF.Exp), reads=[B[0]], writes=[eb[p]])
        yield
        kb.op("act", lambda e: e.activation(out=enb[p][:], in_=B[0][:, 0:128], func=AF.Exp, scale=-1.0), reads=[B[0]], writes=[enb[p]])
        yield
        kb.op("dve", lambda e: e.tensor_tensor(out=dlt[p][:], in0=B[0][:, 128:256], in1=bc[p][:], op=ALU.subtract), reads=[B[0], bc[p]], writes=[dlt[p]])
        yield
        kb.op("act", lambda e: e.activation(out=dlt[p][:], in_=dlt[p][:], func=AF.Exp), reads=[dlt[p]], writes=[dlt[p]])
        yield
        kb.op("act", lambda e: e.activation(out=dec[p][:], in_=B[0][:, 256:257], func=AF.Exp), reads=[B[0]], writes=[dec[p]])
        yield
        kb.op("dve", lambda e: e.scalar_tensor_tensor(out=qt[p][:], in0=t_[:, 0:128], scalar=QS, in1=eb[p][:], op0=ALU.mult, op1=ALU.mult), reads=[t_, eb[p]], writes=[qt[p]])
        yield
        kb.op("dve", lambda e: e.tensor_tensor(out=ktl[p][:], in0=t_[:, 128:256], in1=enb[p][:], op=ALU.mult), reads=[t_, enb[p]], writes=[ktl[p]])
        yield
        kb.op("pool", lambda e: e.tensor_tensor(out=kh[p][:], in0=t_[:, 128:256], in1=dlt[p][:], op=ALU.mult), reads=[t_, dlt[p]], writes=[kh[p]])
        yield
        kb.op("pool", lambda e: e.tensor_copy(out=vb[p][:], in_=t_[:, 256:512]), reads=[t_], writes=[vb[p]])
        yield
        kb.op("pe", lambda e: e.transpose(out=bfv(B[1])[:, 0:128], in_=qt[p][:], identity=identb[:]), reads=[qt[p], identb], writes=[B[1]])
        yield
        kb.op("pe", lambda e: e.transpose(out=bfv(B[1])[:, 128:256], in_=ktl[p][:], identity=identb[:]), reads=[ktl[p], identb], writes=[B[1]])
        yield
        kb.op("act", lambda e: e.copy(out=qkT[p][:], in_=bfv(B[1])[:, 0:256]), reads=[B[1]], writes=[qkT[p]])
        yield
        kb.op("pe", lambda e: e.matmul(B[2][:, 0:128], lhsT=qkT[p][:, 128:256], rhs=qkT[p][:, 0:128], start=True, stop=True), reads=[qkT[p]], writes=[B[2]])
        yield
        kb.op("dve", lambda e: e.tensor_tensor(out=attm[p][:], in0=B[2][:, 0:128], in1=mask[d][:], op=ALU.mult), reads=[B[2], mask[d]], writes=[attm[p]])
        yield

    def gla_fin(c, d, out_mode, out_row):
        p = c % 2
        B = banks[4 * p:4 * p + 4]
        t_ = pt[c % 3]
        sb_cur = Sb[c % 2]
        sb_next = Sb[(c + 1) % 2]
        if out_mode is not None:
            kb.op("pe", lambda e: e.matmul(B[3][:, 0:256], lhsT=qkT[p][:, 0:128], rhs=sb_cur[:], start=True, stop=False), reads=[qkT[p], sb_cur], writes=[B[3]])
            yield
            kb.op("pe", lambda e: e.matmul(B[3][:, 0:256], lhsT=attm[p][:], rhs=vb[p][:], start=False, stop=True), reads=[attm[p], vb[p]], writes=[B[3]])
            yield
        kb.op("pe", lambda e: e.matmul(B[2][:, 128:384], lhsT=kh[p][:], rhs=vb[p][:], start=True, stop=True), reads=[kh[p], vb[p]], writes=[B[2]])
        yield
        kb.op("dve", lambda e: e.scalar_tensor_tensor(out=Sst[:], in0=Sst[:], scalar=dec[p][:, 0:1], in1=B[2][:, 128:384], op0=ALU.mult, op1=ALU.add),
              reads=[Sst, dec[p], B[2]], writes=[Sst])
        yield
        kb.op("act", lambda e: e.copy(out=sb_next[:], in_=Sst[:]), reads=[Sst], writes=[sb_next])
        yield
        if out_mode == "store":
            kb.op("act", lambda e: e.copy(out=ofs[p][:], in_=B[3][:, 0:256]), reads=[B[3]], writes=[ofs[p]])
            yield
            kb.store("sp", of_d, of_d.h[out_row:out_row + 128, :], ofs[p], ofs[p][:])
            yield
        elif out_mode == "final":
            kb.load("sp", ofs[p], ofs[p][:], of_d.h[out_row:out_row + 128, :], of_d)
            yield
            kb.op("dve", lambda e: e.tensor_tensor(out=osum[p][:], in0=B[3][:, 0:256], in1=ofs[p][:], op=ALU.add), reads=[B[3], ofs[p]], writes=[osum[p]])
            yield
            kb.op("act", lambda e: e.activation(out=sg[p][:], in_=osum[p][:], func=AF.Square, accum_out=fst[p][:, 0:1]), reads=[osum[p]], writes=[sg[p], fst[p]])
            yield
            rstd_chain(fst[p], 0, 1, 2, 1, 1.0 / 256)
            kb.op("dve", lambda e: e.scalar_tensor_tensor(out=osum[p][:], in0=osum[p][:], scalar=fst[p][:, 2:3], in1=smallb[:, 256:512], op0=ALU.mult, op1=ALU.mult),
                  reads=[osum[p], fst[p], smallb], writes=[osum[p]])
            yield
            kb.op("act", lambda e: e.activation(out=sg[p][:], in_=t_[:, 512:768], func=AF.Silu), reads=[t_], writes=[sg[p]])
            yield
            kb.op("dve", lambda e: e.tensor_tensor(out=ogb[p][:], in0=osum[p][:], in1=sg[p][:], op=ALU.mult), reads=[osum[p], sg[p]], writes=[ogb[p]])
            yield
            for hh in range(2):
                kb.op("pe", lambda e: e.transpose(out=bfv(B[1])[:, 256 + hh * 128:256 + (hh + 1) * 128], in_=ogb[p][:, hh * 128:(hh + 1) * 128], identity=identb[:]),
                      reads=[ogb[p], identb], writes=[B[1]])
                yield
            kb.op("act", lambda e: e.copy(out=ogT_sb[p][:].rearrange("p a b -> p (a b)"), in_=bfv(B[1])[:, 256:512]), reads=[B[1]], writes=[ogT_sb[p]])
            yield
            tq_, tc_ = (out_row // 128) // 32, ((out_row // 128) % 32) * 128
            kb.store("sp", x3in, x3in.h[tq_ * 256:(tq_ + 1) * 256, tc_:tc_ + 128].rearrange("(a p) n -> p a n", p=128), ogT_sb[p], ogT_sb[p][:])
            yield

    def reset_state(c):
        kb.op("pool", lambda e: e.memset(Sst[:], 0.0), writes=[Sst])
        kb.op("pool", lambda e: e.memset(Sb[c % 2][:], 0.0), writes=[Sb[c % 2]])

    def run_scan(chunks, c0):
        n = len(chunks)
        for _ in gla_prep(c0, chunks[0][0], chunks[0][1]):
            pass
        for k in range(n):
            row, d, om, orow = chunks[k]
            gens = [gla_fin(c0 + k, d, om, orow)]
            if k + 1 < n:
                gens.append(gla_prep(c0 + k + 1, chunks[k + 1][0], chunks[k + 1][1]))
            interleave(gens, 2)
        return c0 + n

    fwd = [(S + c * 128, 0, None, None) for c in range(2)] + [(t * 128, 0, "store", t * 128) for t in range(n_lat_tiles)]
    bwd = [(S + c * 128, 1, None, None) for c in (1, 0)] + [(t * 128, 1, "final", t * 128) for t in range(n_lat_tiles - 1, -1, -1)]
    reset_state(0)
    cn = run_scan(fwd, 0)
    kb.barrier()
    reset_state(cn)
    run_scan(bwd, cn)
    print("l1a instructions:", kb.n_ins, "sems:", len(kb.sems))
    kb.end_stage()


def fop(v, n):
    return np.ascontiguousarray(np.asarray(v, np.float32).reshape(n, 128).T)


def host_l1a(inp):
    maps = []
    wi = inp["gla_w_in"][0]
    for b in range(2):
        sv = np.stack([inp["c"][b], inp["c_ctx"]], -1).reshape(8, 128, 2).transpose(1, 0, 2).reshape(128, 16).astype(np.float32)
        for h in range(4):
            w = np.concatenate([wi[:, h * 128:(h + 1) * 128], wi[:, 512 + h * 128:512 + (h + 1) * 128], wi[:, 1024 + h * 256:1024 + (h + 1) * 256],
                                wi[:, 2048 + h * 256:2048 + (h + 1) * 256]], axis=1)
            waT = np.ascontiguousarray(wi[:, 3072:3104].T.reshape(2, 16, 1024))
            wa2 = np.ascontiguousarray(inp["gla_w_a2"][0][:, :, h * 128:(h + 1) * 128])
            small = np.concatenate([inp["gla_b_a2"][0][0, h * 128:(h + 1) * 128], inp["gla_b_a2"][0][1, h * 128:(h + 1) * 128], inp["gla_norm_g"][0]]).astype(np.float32)
            maps.append({"svec": np.ascontiguousarray(sv),
                         "adaw": np.ascontiguousarray(inp["ada_w"][1][:, 0:2048]), "adab": fop(inp["ada_b"][1][0:2048], 16), "g1": fop(inp["norm1_g"][1], 8),
                         "w": np.ascontiguousarray(w), "waT": waT, "wa2": wa2, "small": small})
    return maps


RG = [[0, 1, 2, 3], [4, 5, 6, 7]]


def build_all():
    kb = KB()
    banks = [kb.ps("bank%d" % i) for i in range(8)]
    x1in = kb.dram("x1in", [512, 4224], BF16)
    x1out = kb.dram("x1out", [2048, 4224], BF16)
    x2in = kb.dram("x2in", [4224, 1024], F32)
    x2out = kb.dram("x2out", [4 * 4224, 1024], F32)
    x3in = kb.dram("x3in", [1024, 4096], BF16)
    x3out = kb.dram("x3out", [4096, 4096], BF16)
    build_l0a(kb, banks, x1in)
    kb.all_gather(x1in, x1out, RG, 64)
    build_b(0, kb, banks, x1out, None, x2in)
    kb.all_gather(x2in, x2out, RG, 256)
    build_l1a(kb, banks, x2out, x3in)
    kb.all_gather(x3in, x3out, RG, 128)
    build_b(1, kb, banks, x3out, x2in, None)
    print("total instructions:", kb.n_ins, "sems:", len(kb.sems))
    return kb.finish()


def kernel(**inputs):
    inp = {k: np.asarray(v) for k, v in inputs.items()}
    parts = [("a0_", host_l0a(inp)), ("b0_", host_b(0, inp)), ("a1_", host_l1a(inp)), ("b1_", host_b(1, inp))]
    maps = []
    for c in range(8):
        m = {}
        for pre, ms in parts:
            for k, v in ms[c].items():
                m[pre + k] = v
        maps.append(m)
    nc = build_all()
    res = run_bass_kernel_spmd(nc, maps, core_ids=list(range(8)))
    out = np.zeros((2, 16384, 1024), np.float32)
    for b in range(2):
        for jq in range(4):
            out[b, jq * 4096:(jq + 1) * 4096] = np.asarray(res.results[b * 4 + jq]["b1_hout"])[:4096]
    return out
```

```python
import numpy as np
from contextlib import ExitStack
import concourse.bass as bass
import concourse.mybir as mybir
from concourse.bass_utils import run_bass_kernel_spmd
import ml_dtypes

F32 = mybir.dt.float32
BF16 = mybir.dt.bfloat16
I32 = mybir.dt.int32
AF = mybir.ActivationFunctionType
ALU = mybir.AluOpType
AX = mybir.AxisListType
NPBF16 = ml_dtypes.bfloat16


def interleave(gens, width):
    active = []
    it = iter(gens)
    while True:
        while len(active) < width:
            g = next(it, None)
            if g is None:
                break
            active.append(g)
        if not active:
            break
        for g in list(active):
            try:
                next(g)
            except StopIteration:
                active.remove(g)


def ag_row(i, rank, chunk_rows, total_rows, world=4):
    r0 = (i // chunk_rows) * chunk_rows
    n = min(chunk_rows, total_rows - r0)
    return world * r0 + rank * n + (i - r0)


class T:
    def __init__(self, h, name, kind):
        self.h = h
        self.name = name
        self.kind = kind
        self.w = None
        self.r = {}
        self.dkey = None

    def __getitem__(self, idx):
        return self.h[idx]


class KB:
    def __init__(self):
        self.nc = bass.Bass("TRN2", target_bir_lowering=False)
        nc = self.nc
        self.es = ExitStack()
        self.eng = {"pe": nc.tensor, "act": nc.scalar, "dve": nc.vector, "pool": nc.gpsimd, "sp": nc.sync}
        self.sems = {}
        self.cnt = {}
        self.seen = {e: {} for e in self.eng}
        for e in self.eng:
            self.sems[e] = self.es.enter_context(nc.semaphore("e_" + e))
            self.cnt[e] = 0
        self.issued = {}
        self.n_ins = 0
        self.outs = []
        self._uid = 0
        self.cur = self.es
        self.prefix = ""
        self.tiles = []
        self.free_dsems = []
        self.stage_tiles0 = 0

    def sb(self, name, shape, dt, es=None):
        h = (es or self.cur).enter_context(self.nc.sbuf_tensor(self.prefix + name, list(shape), dt))
        t = T(h, name, "sb")
        self.tiles.append(t)
        return t

    def ps(self, name, shape=(128, 512), dt=F32):
        h = self.es.enter_context(self.nc.psum_tensor(name, list(shape), dt))
        t = T(h, name, "ps")
        self.tiles.append(t)
        return t

    def dram(self, name, shape, dt, kind="Internal"):
        h = self.nc.dram_tensor(self.prefix + name, list(shape), dt, kind=kind)
        t = T(h.ap(), name, "dram")
        self.tiles.append(t)
        if kind == "ExternalOutput":
            self.outs.append(t)
        return t

    def _dsem(self, t):
        if t.dkey is None:
            self._uid += 1
            t.dkey = "d%d_%s" % (self._uid, t.name)
            if self.free_dsems:
                h, v = self.free_dsems.pop()
                self.sems[t.dkey] = h
                self.issued[t.dkey] = v
            else:
                self.sems[t.dkey] = self.es.enter_context(self.nc.semaphore(t.dkey))
                self.issued[t.dkey] = 0
        return t.dkey

    def begin_stage(self, prefix):
        self.prefix = prefix
        self.cur = ExitStack()
        self.stage_tiles0 = len(self.tiles)

    def end_stage(self):
        self.barrier()
        self.cur.close()
        self.cur = self.es
        for t in self.tiles[self.stage_tiles0:]:
            if t.kind == "sb" and t.dkey is not None:
                self.free_dsems.append((self.sems[t.dkey], self.issued[t.dkey]))
                del self.issued[t.dkey]
                del self.sems[t.dkey]
                t.dkey = None
        for t in self.tiles:
            t.w = None
            t.r = {}
        for e in self.eng:
            self._uid += 1
            self.sems[e] = self.es.enter_context(self.nc.semaphore("e%d_%s" % (self._uid, e)))
            self.cnt[e] = 0
        self.seen = {e: {} for e in self.eng}
        self.prefix = ""

    def all_gather(self, src, dst, groups, chunk_rows):
        self.barrier()
        self._uid += 1
        sem = self.es.enter_context(self.nc.semaphore("cc%d" % self._uid))
        R = src.h.shape[0]
        k = 0
        for r0 in range(0, R, chunk_rows):
            n = min(chunk_rows, R - r0)
            self.nc.gpsimd.collective_compute("AllGather", ALU.bypass, replica_groups=groups, ins=[src.h[r0:r0 + n, :]],
                                              outs=[dst.h[4 * r0:4 * r0 + 4 * n, :]]).then_inc(sem, 1)
            k += 1
        self.nc.gpsimd.wait_ge(sem, k)
        if not hasattr(self, "_fence"):
            self._fence = T(self.es.enter_context(self.nc.sbuf_tensor("cc_fence", [128, 8], F32)), "cc_fence", "sb")
            self.tiles.append(self._fence)
        f = self._fence
        self.op("pool", lambda e: e.memset(f[:], 0.0), writes=[f])
        for en in self.eng:
            if en != "pool":
                self._waits(en, {"pool": self.cnt["pool"]})
        self.n_ins += k + 1

    def _deps(self, en, reads, writes, is_dma=False):
        deps = {}

        def add(key, val, kind):
            if key == en:
                if en == "pe" or kind == "war":
                    return
            if is_dma and kind == "waw" and key in self.issued:
                return
            deps[key] = max(deps.get(key, 0), val)

        for t in reads:
            if t.w is not None:
                add(t.w[0], t.w[1], "raw")
            if t.kind == "ps":
                for k, v in t.r.items():
                    if k != en:
                        add(k, v, "rar")
        for t in writes:
            if t.w is not None:
                add(t.w[0], t.w[1], "waw")
            for k, v in t.r.items():
                add(k, v, "war")
        return deps

    def _waits(self, en, deps):
        e = self.eng[en]
        for key, val in deps.items():
            if key in self.issued:
                val = self.issued[key]
            if self.seen[en].get(key, 0) >= val:
                continue
            e.wait_ge(self.sems[key], val)
            self.seen[en][key] = val
            self.n_ins += 1

    def op(self, en, fn, reads=(), writes=()):
        self._waits(en, self._deps(en, reads, writes))
        ins = fn(self.eng[en])
        self.cnt[en] += 1
        self.n_ins += 1
        ins.then_inc(self.sems[en], 1)
        c = self.cnt[en]
        for t in reads:
            t.r[en] = c
        for t in writes:
            t.w = (en, c)
            t.r = {}
        return ins

    def dma(self, q, fn, reads=(), writes=()):
        self._waits(q, self._deps(q, reads, writes, is_dma=True))
        cand = [t for t in writes if t.kind != "dram"] or [t for t in reads if t.kind != "dram"] or list(writes) or list(reads)
        key = self._dsem(cand[0])
        ins = fn(self.eng[q])
        self.issued[key] += 16
        self.n_ins += 1
        ins.then_inc(self.sems[key], 16)
        v = self.issued[key]
        for t in reads:
            t.r[key] = v
        for t in writes:
            t.w = (key, v)
            t.r = {}
        return ins

    def load(self, q, dst_t, dst_ap, src_ap, src_t=None, **kw):
        return self.dma(q, lambda e: e.dma_start(out=dst_ap, in_=src_ap, **kw),
                        reads=[src_t] if src_t is not None else [], writes=[dst_t])

    def store(self, q, dst_t, dst_ap, src_t, src_ap, **kw):
        return self.dma(q, lambda e: e.dma_start(out=dst_ap, in_=src_ap, **kw), reads=[src_t], writes=[dst_t])

    def finish(self):
        deps = {}
        for t in self.outs:
            if t.w is not None:
                deps[t.w[0]] = max(deps.get(t.w[0], 0), t.w[1])
        self._waits("sp", deps)
        self.es.close()
        return self.nc

    def barrier(self):
        for en in self.eng:
            deps = {}
            for k in self.eng:
                if k != en and self.cnt[k] > 0:
                    deps[k] = self.cnt[k]
            for k, v in self.issued.items():
                if v > 0:
                    deps[k] = v
            self._waits(en, deps)

    def identity(self, name, dt):
        f = self.sb(name + "_f", [128, 128], F32)
        self.op("pool", lambda e: e.memset(f[:], 0.0), writes=[f])
        self.op("pool", lambda e: e.affine_select(out=f[:], in_=f[:], pattern=[[-1, 128]], compare_op=ALU.not_equal,
                                                  fill=1.0, base=0, channel_multiplier=1), reads=[f], writes=[f])
        if dt == F32:
            return f
        b = self.sb(name, [128, 128], dt)
        self.op("pool", lambda e: e.tensor_copy(out=b[:], in_=f[:]), reads=[f], writes=[b])
        return b

EPS = 1e-6
S = 16384
LC = 256

NKT = (S + LC) // 128
ATT_NSPLIT = 512


def build_l0a(kb, banks, x1in, n_groups=32, debug=False):
    kb.begin_stage("a0_")
    x = kb.dram("x", [S, 1024], F32, "ExternalInput")
    ctx = kb.dram("ctx", [LC, 1024], F32, "ExternalInput")
    svec = kb.dram("svec", [128, 16], F32, "ExternalInput")
    adaw = kb.dram("adaw", [1024, 2048], F32, "ExternalInput")
    adab = kb.dram("adab", [128, 16], F32, "ExternalInput")
    g1 = kb.dram("g1", [128, 8], F32, "ExternalInput")
    w = kb.dram("w", [1024, 384], F32, "ExternalInput")
    small = kb.dram("small", [640], F32, "ExternalInput")
    cos4 = kb.dram("cos4", [S, 256], F32, "ExternalInput")
    sin4 = kb.dram("sin4", [S, 256], F32, "ExternalInput")
    zpad = kb.sb("zpad", [128, 4, 64], BF16)
    kb.op("pool", lambda e: e.memset(zpad[:], 0.0), writes=[zpad])
    kb.store("sp", x1in, x1in.h[:, 4160:4224].rearrange("(q p) n -> p q n", p=128), zpad, zpad[:])

    def bfv(t):
        return t[:].bitcast(BF16)

    identb = kb.identity("identb", BF16)
    smallb = kb.sb("smallb", [128, 640], F32)
    kb.load("sp", smallb, smallb[:], small.h.partition_broadcast(128), small)

    tmp64 = kb.sb("tmp64", [128, 2, 64], F32)
    dots = kb.sb("dots", [128, 4], F32)
    kb.op("dve", lambda e: e.tensor_tensor(out=tmp64[:, 0, :], in0=smallb[:, 256:320], in1=smallb[:, 320:384], op=ALU.mult), reads=[smallb], writes=[tmp64])
    kb.op("dve", lambda e: e.tensor_tensor(out=tmp64[:, 1, :], in0=smallb[:, 384:448], in1=smallb[:, 448:512], op=ALU.mult), reads=[smallb], writes=[tmp64])
    kb.op("dve", lambda e: e.tensor_reduce(out=dots[:, 0:2], in_=tmp64[:], axis=AX.X, op=ALU.add), reads=[tmp64], writes=[dots])
    kb.op("act", lambda e: e.activation(out=dots[:, 2:4], in_=dots[:, 0:2], func=AF.Exp), reads=[dots], writes=[dots])
    neglam = kb.sb("neglam", [128, 1], F32)
    kb.op("dve", lambda e: e.scalar_tensor_tensor(out=neglam[:], in0=dots[:, 3:4], scalar=-0.2, in1=dots[:, 2:3], op0=ALU.add, op1=ALU.subtract), reads=[dots], writes=[neglam])
    subg_s = kb.sb("subg_s", [128, 128], F32)
    kb.op("dve", lambda e: e.tensor_scalar(out=subg_s[:], in0=smallb[:, 512:640], scalar1=0.8, scalar2=None, op0=ALU.mult), reads=[smallb], writes=[subg_s])

    s_sb = kb.sb("s_sb", [128, 16], F32)
    kb.load("sp", s_sb, s_sb[:], svec.h, svec)
    kb.op("act", lambda e: e.activation(out=s_sb[:], in_=s_sb[:], func=AF.Silu), reads=[s_sb], writes=[s_sb])
    adab_sb = kb.sb("adab_sb", [128, 16], F32)
    kb.load("sp", adab_sb, adab_sb[:], adab.h, adab)
    g1_sb = kb.sb("g1_sb", [128, 8], F32)
    kb.load("sp", g1_sb, g1_sb[:], g1.h, g1)
    mod = kb.sb("mod", [128, 16, 2], F32)
    gs = kb.sb("gs", [128, 8, 2], F32)
    zer = kb.sb("zer", [128, 128], F32)
    wq = [kb.sb("wq%d" % j, [128, 8, 384], BF16) for j in range(2)]
    bias = [kb.sb("bias%d" % j, [128, 384], F32) for j in range(2)]
    p0 = ExitStack()
    adaw_sb = kb.sb("adaw_sb", [128, 8, 512], F32, p0)
    pm = banks[0]
    for v in range(4):
        kb.load("sp", adaw_sb, adaw_sb[:], adaw.h[:, v * 512:(v + 1) * 512].rearrange("(kc p) n -> p kc n", p=128), adaw)
        for oc in range(4):
            g = v * 4 + oc
            for kc in range(8):
                kb.op("pe", lambda e: e.matmul(pm[:, g * 2:g * 2 + 2], lhsT=adaw_sb[:, kc, oc * 128:(oc + 1) * 128],
                                              rhs=s_sb[:, kc * 2:kc * 2 + 2], start=(kc == 0), stop=(kc == 7)),
                      reads=[adaw_sb, s_sb], writes=[pm])
    pm3 = pm[:, 0:32].rearrange("p (g j) -> p g j", j=2)
    for j in range(2):
        kb.op("dve", lambda e: e.tensor_tensor(out=mod[:, :, j], in0=pm3[:, :, j], in1=adab_sb[:], op=ALU.add), reads=[pm, adab_sb], writes=[mod])
        kb.op("dve", lambda e: e.scalar_tensor_tensor(out=gs[:, :, j], in0=mod[:, 8:16, j], scalar=1.0, in1=g1_sb[:], op0=ALU.add, op1=ALU.mult),
              reads=[mod, g1_sb], writes=[gs])

    w_sb = kb.sb("w_sb", [128, 8, 384], F32, p0)
    kb.load("sp", w_sb, w_sb[:], w.h.rearrange("(kc p) n -> p kc n", p=128), w)
    kb.op("pool", lambda e: e.memset(zer[:], 0.0), writes=[zer])
    shiftbc = kb.sb("shiftbc", [128, 8, 128], F32, p0)
    for j in range(2):
        for kc in range(8):
            kb.op("dve", lambda e: e.tensor_scalar(out=wq[j][:, kc, :], in0=w_sb[:, kc, :], scalar1=gs[:, kc, j:j + 1], scalar2=None, op0=ALU.mult),
                  reads=[w_sb, gs], writes=[wq[j]])
            kb.op("dve", lambda e: e.tensor_scalar(out=shiftbc[:, kc, :], in0=zer[:], scalar1=mod[:, kc, j:j + 1], scalar2=None, op0=ALU.add),
                  reads=[zer, mod], writes=[shiftbc])
        pb = banks[1]
        for kc in range(8):
            kb.op("pe", lambda e: e.matmul(pb[:, 0:384], lhsT=shiftbc[:, kc, :], rhs=w_sb[:, kc, :], start=(kc == 0), stop=(kc == 7)),
                  reads=[shiftbc, w_sb], writes=[pb])
        kb.op("dve", lambda e: e.tensor_copy(out=bias[j][:], in_=pb[:, 0:384]), reads=[pb], writes=[bias[j]])

    kb.barrier()
    p0.close()
    QT = kb.sb("QT", [128, S + LC], BF16)
    KTm = [kb.sb("KT%d" % m, [128, S + LC], BF16) for m in range(2)]
    kb.op("pool", lambda e: e.memset(KTm[0][64:128, :], 0.0), writes=[KTm[0]])
    kb.op("pool", lambda e: e.memset(KTm[1][0:64, :], 0.0), writes=[KTm[1]])
    Vx = kb.sb("Vx", [128, NKT, 129], BF16)
    kb.op("pool", lambda e: e.memset(Vx[:, :, 128:129], 1.0), writes=[Vx])

    def dbl(name, shape, dt, n=2, es=None):
        return [kb.sb("%s%d" % (name, i), shape, dt, es) for i in range(n)]

    p2 = ExitStack()

    xt = dbl("xt", [128, 1024], F32, 2, p2)
    junk = kb.sb("junk", [128, 1024], BF16, p2)
    st1 = dbl("st1", [128, 4], F32, 2, p2)
    xn = dbl("xn", [128, 1024], BF16, 2, p2)
    xnT = dbl("xnT", [128, 1024], BF16, 2, p2)
    qkv = dbl("qkv", [128, 384], F32, 2, p2)
    cs = dbl("cs", [128, 256], F32, 2, p2)
    sn = dbl("sn", [128, 256], F32, 2, p2)
    sq = dbl("sq", [128, 256], F32, 2, p2)
    st2 = dbl("st2", [128, 12], F32, 2, p2)
    qkn = dbl("qkn", [128, 256], F32, 2, p2)
    sw = dbl("sw", [128, 256], F32, 2, p2)
    t1 = dbl("t1", [128, 256], F32, 2, p2)
    rr = dbl("rr", [128, 256], BF16, 2, p2)

    def rstd_chain(stt, c_in, c_tmp, c_out, n, inv_n, srcs):
        kb.op("dve", lambda e: e.tensor_scalar(out=stt[:, c_tmp:c_tmp + n], in0=stt[:, c_in:c_in + n], scalar1=inv_n, scalar2=EPS, op0=ALU.mult, op1=ALU.add),
              reads=[stt], writes=[stt])
        kb.op("act", lambda e: e.activation(out=stt[:, c_tmp:c_tmp + n], in_=stt[:, c_tmp:c_tmp + n], func=AF.Sqrt), reads=[stt], writes=[stt])
        kb.op("dve", lambda e: e.reciprocal(out=stt[:, c_out:c_out + n], in_=stt[:, c_tmp:c_tmp + n]), reads=[stt], writes=[stt])

    def proj_tile(i, src, row0, is_ctx, qcol, kcol, kt):
        p = i % 2
        j = 1 if is_ctx else 0
        kb.load("sp", xt[p], xt[p][:], src.h[row0:row0 + 128, :], src)
        yield
        if not is_ctx:
            kb.load("pool", cs[p], cs[p][:], cos4.h[row0:row0 + 128, :], cos4)
            yield
            kb.load("pool", sn[p], sn[p][:], sin4.h[row0:row0 + 128, :], sin4)
            yield
        kb.op("act", lambda e: e.activation(out=junk[:], in_=xt[p][:], func=AF.Square, accum_out=st1[p][:, 0:1]), reads=[xt[p]], writes=[junk, st1[p]])
        yield
        rstd_chain(st1[p], 0, 1, 2, 1, 1.0 / 1024, None)
        kb.op("act", lambda e: e.activation(out=xn[p][:], in_=xt[p][:], func=AF.Copy, scale=st1[p][:, 2:3]), reads=[xt[p], st1[p]], writes=[xn[p]])
        yield
        psT = banks[p]
        for kc in range(8):
            kb.op("pe", lambda e: e.transpose(out=bfv(psT)[:, kc * 128:(kc + 1) * 128], in_=xn[p][:, kc * 128:(kc + 1) * 128], identity=identb[:]),
                  reads=[xn[p], identb], writes=[psT])
            yield
        kb.op("dve", lambda e: e.tensor_copy(out=xnT[p][:], in_=bfv(psT)[:, 0:1024]), reads=[psT], writes=[xnT[p]])
        yield
        pp = banks[2 + p]
        for kc in range(8):
            kb.op("pe", lambda e: e.matmul(pp[:, 0:384], lhsT=xnT[p][:, kc * 128:(kc + 1) * 128], rhs=wq[j][:, kc, :], start=(kc == 0), stop=(kc == 7)),
                  reads=[xnT[p], wq[j]], writes=[pp])
            yield
        kb.op("dve", lambda e: e.tensor_tensor(out=qkv[p][:], in0=pp[:, 0:384], in1=bias[j][:], op=ALU.add), reads=[pp, bias[j]], writes=[qkv[p]])
        yield
        kb.op("pool", lambda e: e.tensor_copy(out=Vx[:, kt, 0:128], in_=qkv[p][:, 256:384]), reads=[qkv[p]], writes=[Vx])
        yield
        kb.op("act", lambda e: e.activation(out=sq[p][:], in_=qkv[p][:, 0:256], func=AF.Square), reads=[qkv[p]], writes=[sq[p]])
        yield
        kb.op("dve", lambda e: e.tensor_reduce(out=st2[p][:, 0:4], in_=sq[p][:].rearrange("p (g d) -> p g d", g=4), axis=AX.X, op=ALU.add),
              reads=[sq[p]], writes=[st2[p]])
        yield
        rstd_chain(st2[p], 0, 4, 8, 4, 1.0 / 64, None)
        for g in range(4):
            kb.op("dve", lambda e: e.scalar_tensor_tensor(out=qkn[p][:, g * 64:(g + 1) * 64], in0=qkv[p][:, g * 64:(g + 1) * 64], scalar=st2[p][:, 8 + g:9 + g],
                                                          in1=smallb[:, g * 64:(g + 1) * 64], op0=ALU.mult, op1=ALU.mult),
                  reads=[qkv[p], st2[p], smallb], writes=[qkn[p]])
            yield
        if is_ctx:
            kb.op("pool", lambda e: e.tensor_copy(out=rr[p][:], in_=qkn[p][:]), reads=[qkn[p]], writes=[rr[p]])
            yield
        else:
            q5 = qkn[p][:].rearrange("p (a h d) -> p a h d", h=2, d=16)
            s5 = sw[p][:].rearrange("p (a h d) -> p a h d", h=2, d=16)
            kb.op("pool", lambda e: e.tensor_copy(out=s5[:, :, 0, :], in_=q5[:, :, 1, :]), reads=[qkn[p]], writes=[sw[p]])
            yield
            kb.op("pool", lambda e: e.tensor_copy(out=s5[:, :, 1, :], in_=q5[:, :, 0, :]), reads=[qkn[p]], writes=[sw[p]])
            yield
            kb.op("pool", lambda e: e.tensor_tensor(out=sw[p][:], in0=sw[p][:], in1=sn[p][:], op=ALU.mult), reads=[sw[p], sn[p]], writes=[sw[p]])
            yield
            kb.op("dve", lambda e: e.tensor_tensor(out=t1[p][:], in0=qkn[p][:], in1=cs[p][:], op=ALU.mult), reads=[qkn[p], cs[p]], writes=[t1[p]])
            yield
            kb.op("dve", lambda e: e.tensor_tensor(out=rr[p][:], in0=t1[p][:], in1=sw[p][:], op=ALU.add), reads=[t1[p], sw[p]], writes=[rr[p]])
            yield
        pq = banks[4 + p]
        for hh in range(2):
            kb.op("pe", lambda e: e.transpose(out=bfv(pq)[:, hh * 128:(hh + 1) * 128], in_=rr[p][:, hh * 128:(hh + 1) * 128], identity=identb[:]),
                  reads=[rr[p], identb], writes=[pq])
            yield
        kb.op("act", lambda e: e.copy(out=QT[:, qcol:qcol + 128], in_=bfv(pq)[:, 0:128]), reads=[pq], writes=[QT])
        yield
        kb.op("act", lambda e: e.copy(out=KTm[0][0:64, kcol:kcol + 128], in_=bfv(pq)[0:64, 128:256]), reads=[pq], writes=[KTm[0]])
        kb.op("act", lambda e: e.copy(out=KTm[1][64:128, kcol:kcol + 128], in_=bfv(pq)[64:128, 128:256]), reads=[pq], writes=[KTm[1]])
        yield

    gens = []
    i = 0
    for c in range(LC // 128):
        gens.append(proj_tile(i, ctx, c * 128, True, S + c * 128, c * 128, c))
        i += 1
    for t in range(S // 128):
        gens.append(proj_tile(i, x, t * 128, False, t * 128, LC + t * 128, LC // 128 + t))
        i += 1
    interleave(gens, 2)
    kb.barrier()
    p2.close()

    ST = banks[0:3]
    OT = [banks[4], banks[5]]
    PL = [banks[6], banks[7]]
    PS_ = banks[3]
    PT = dbl("pt", [128, 512], BF16, 4)
    Pacc = [kb.sb("pacc%d" % m, [128, 512], F32) for m in range(2)]
    ones_bb = kb.sb("ones_bb", [128, 128], BF16)
    kb.op("pool", lambda e: e.memset(ones_bb[:], 1.0), writes=[ones_bb])
    ones_ff = kb.sb("ones_ff", [128, 128], F32)
    kb.op("pool", lambda e: e.memset(ones_ff[:], 1.0), writes=[ones_ff])
    subg_col = kb.sb("subg_col", [128, 1], F32)
    kb.load("sp", subg_col, subg_col[:], small.h[512:640].rearrange("(p o) -> p o", o=1), small)
    kb.op("dve", lambda e: e.tensor_scalar(out=subg_col[:], in0=subg_col[:], scalar1=0.8, scalar2=None, op0=ALU.mult), reads=[subg_col], writes=[subg_col])
    rlb = dbl("rlb", [128, 512], F32)
    eo = dbl("eo", [128, 512], F32)
    esq = kb.sb("esq", [128, 512], F32)
    outT = dbl("outT", [128, 512], BF16)
    gcount = [0]
    NSPLIT = ATT_NSPLIT

    def attend(qc0, nq, kts):
        steps = [(m, idx, kt) for m in range(2) for idx, kt in enumerate(kts)]
        nk = len(kts)
        gi = gcount[0]
        gcount[0] += 1

        def score(s):
            m, idx, kt = steps[s]
            st = ST[s % 3]
            for c0 in range(0, nq, NSPLIT):
                kb.op("pe", lambda e: e.matmul(st[:, c0:min(nq, c0 + NSPLIT)], lhsT=KTm[m][:, kt * 128:(kt + 1) * 128], rhs=QT[:, qc0 + c0:qc0 + min(nq, c0 + NSPLIT)],
                                              start=True, stop=True), reads=[KTm[m], QT], writes=[st])

        used = {}

        def rest(s):
            m, idx, kt = steps[s]
            st = ST[s % 3]
            pt = PT[s % 4]
            kb.op("act", lambda e: e.activation(out=pt[:, 0:nq], in_=st[:, 0:nq], func=AF.Exp, scale=0.125), reads=[st], writes=[pt])
            for c0 in range(0, nq, NSPLIT):
                kb.op("pe", lambda e: e.matmul(OT[m][:, c0:min(nq, c0 + NSPLIT)], lhsT=Vx[:, kt, 0:128], rhs=pt[:, c0:min(nq, c0 + NSPLIT)], start=(idx == 0 and c0 == 0), stop=(idx == nk - 1),
                                              skip_group_check=True), reads=[pt, Vx], writes=[OT[m]])
            if s + 3 < len(steps):
                score(s + 3)
            if idx % 3 == 2:
                kb.op("pe", lambda e: e.matmul(PL[m][:, 0:nq], lhsT=ones_bb[:], rhs=pt[:, 0:nq], start=((m, "pe") not in used), stop=False), reads=[ones_bb, pt], writes=[PL[m]])
                used[(m, "pe")] = True
            elif (m, "dve") not in used:
                used[(m, "dve")] = True
                kb.op("dve", lambda e: e.tensor_copy(out=Pacc[m][:, 0:nq], in_=pt[:, 0:nq]), reads=[pt], writes=[Pacc[m]])
            else:
                kb.op("dve", lambda e: e.tensor_tensor(out=Pacc[m][:, 0:nq], in0=pt[:, 0:nq], in1=Pacc[m][:, 0:nq], op=ALU.add), reads=[pt, Pacc[m]], writes=[Pacc[m]])

        for s0 in range(min(3, len(steps))):
            score(s0)
        for s in range(len(steps)):
            rest(s)
        for m in range(2):
            kb.op("pe", lambda e: e.matmul(PL[m][:, 0:nq], lhsT=ones_ff[:], rhs=Pacc[m][:, 0:nq], start=((m, "pe") not in used), stop=True), reads=[ones_ff, Pacc[m]], writes=[PL[m]])
            kb.op("dve", lambda e: e.reciprocal(out=rlb[m][:, 0:nq], in_=PL[m][:, 0:nq]), reads=[PL[m]], writes=[rlb[m]])
            kb.op("dve", lambda e: e.tensor_tensor(out=eo[m][:, 0:nq], in0=OT[m][:, 0:nq], in1=rlb[m][:, 0:nq], op=ALU.mult), reads=[OT[m], rlb[m]], writes=[eo[m]])
        kb.op("dve", lambda e: e.scalar_tensor_tensor(out=eo[0][:, 0:nq], in0=eo[1][:, 0:nq], scalar=neglam[:, 0:1], in1=eo[0][:, 0:nq], op0=ALU.mult, op1=ALU.add),
              reads=[eo[1], neglam, eo[0]], writes=[eo[0]])
        kb.op("act", lambda e: e.activation(out=esq[:, 0:nq], in_=eo[0][:, 0:nq], func=AF.Square), reads=[eo[0]], writes=[esq])
        kb.op("pe", lambda e: e.matmul(PS_[:, 0:nq], lhsT=ones_ff[:], rhs=esq[:, 0:nq], start=True, stop=True), reads=[ones_ff, esq], writes=[PS_])
        kb.op("dve", lambda e: e.tensor_scalar(out=rlb[0][:, 0:nq], in0=PS_[:, 0:nq], scalar1=1.0 / 128, scalar2=EPS, op0=ALU.mult, op1=ALU.add), reads=[PS_], writes=[rlb[0]])
        kb.op("act", lambda e: e.activation(out=rlb[0][:, 0:nq], in_=rlb[0][:, 0:nq], func=AF.Sqrt), reads=[rlb[0]], writes=[rlb[0]])
        kb.op("dve", lambda e: e.reciprocal(out=rlb[1][:, 0:nq], in_=rlb[0][:, 0:nq]), reads=[rlb[0]], writes=[rlb[1]])
        ot = outT[gi % 2]
        kb.op("dve", lambda e: e.scalar_tensor_tensor(out=ot[:, 0:nq], in0=eo[0][:, 0:nq], scalar=subg_col[:, 0:1], in1=rlb[1][:, 0:nq], op0=ALU.mult, op1=ALU.mult),
              reads=[eo[0], subg_col, rlb[1]], writes=[ot])
        if qc0 >= S:
            for q in range(4):
                kb.store("sp", x1in, x1in.h[q * 128:(q + 1) * 128, 4096:4160], ot, ot[:, q * 64:(q + 1) * 64])
        else:
            q, col = qc0 // 4096, qc0 % 4096
            kb.store("sp", x1in, x1in.h[q * 128:(q + 1) * 128, col:col + nq], ot, ot[:, 0:nq])

    attend(S, LC, list(range(LC // 128)))
    for g in range(n_groups):
        attend(g * 512, 512, list(range(NKT)))
    print("l0a instructions:", kb.n_ins)
    kb.end_stage()


def rope_tables():
    half = 32
    inv = (10000.0 ** (-np.arange(0, half, 2, dtype=np.float32) / half)).astype(np.float32)
    t = np.arange(S)
    r = (t // 64).astype(np.float32)[:, None] * inv[None, :]
    c = (t % 64).astype(np.float32)[:, None] * inv[None, :]
    ang = np.concatenate([r, r, c, c], axis=-1).astype(np.float32)
    cos = np.cos(ang).astype(np.float32)
    sin = np.sin(ang).astype(np.float32)
    sgn = np.concatenate([-np.ones(16), np.ones(16), -np.ones(16), np.ones(16)]).astype(np.float32)
    sin = sin * sgn[None, :]
    return np.ascontiguousarray(np.tile(cos, (1, 4))), np.ascontiguousarray(np.tile(sin, (1, 4)))


def fop(v, n):
    return np.ascontiguousarray(np.asarray(v, np.float32).reshape(n, 128).T)


def host_l0a(inp):
    cos4, sin4 = rope_tables()
    maps = []
    wi = inp["ab_w_in"][0]
    for b in range(2):
        for h in range(4):
            sv = np.stack([inp["c"][b], inp["c_ctx"]], -1).reshape(8, 128, 2).transpose(1, 0, 2).reshape(128, 16)
            w = np.concatenate([wi[:, 1024 + h * 128:1024 + (h + 1) * 128], wi[:, 1536 + h * 128:1536 + (h + 1) * 128],
                                wi[:, 2048 + h * 128:2048 + (h + 1) * 128]], axis=1)
            qg, kg = inp["diff_qnorm_g"][0], inp["diff_knorm_g"][0]
            small = np.concatenate([qg, qg, kg, kg, inp["diff_lq1"][0], inp["diff_lk1"][0], inp["diff_lq2"][0], inp["diff_lk2"][0],
                                    inp["diff_subln_g"][0]]).astype(np.float32)
            maps.append({
                "x": np.ascontiguousarray(inp["x"][b]), "ctx": np.ascontiguousarray(inp["ctx"][b]),
                "svec": np.ascontiguousarray(sv.astype(np.float32)),
                "adaw": np.ascontiguousarray(inp["ada_w"][0][:, 0:2048]), "adab": fop(inp["ada_b"][0][0:2048], 16),
                "g1": fop(inp["norm1_g"][0], 8), "w": np.ascontiguousarray(w), "small": small, "cos4": cos4, "sin4": sin4,
            })
    return maps


BIG = 1.0e30


def build_b(layer, kb, banks, mixsrc, hin_t, hout_t, debug=False):
    L0 = (layer == 0)
    NTL = 32
    NT = NTL + (1 if L0 else 0)
    NTOK = NT * 128
    NTOKV = 4096 + (64 if L0 else 0)
    NB = (2 * NTOKV + 32 * 255 + 255) // 256
    NROWS = NB * 256
    NMIX = 4 if L0 else 8

    kb.begin_stage("b%d_" % layer)
    hin = hin_t if hin_t is not None else kb.dram("hin", [NTOK, 1024], F32, "ExternalInput")
    svec = kb.dram("svec", [128, 16], F32, "ExternalInput")
    adaw = kb.dram("adaw", [1024, 6144], F32, "ExternalInput")
    adabf = kb.dram("adabf", [128, 48], F32, "ExternalInput")
    adabr = kb.dram("adabr", [2048], F32, "ExternalInput")
    gfop = kb.dram("gfop", [128, 16], F32, "ExternalInput")
    mixidx = kb.dram("mixidx", [128, NMIX], I32, "ExternalInput")
    wout = kb.dram("wout", [1024, 1024], F32, "ExternalInput")
    rw = kb.dram("rw", [1024, 36], F32, "ExternalInput")
    rb = kb.dram("rb", [36], F32, "ExternalInput")
    w1t = kb.dram("w1t", [4096, 4096], F32, "ExternalInput")
    w3t = kb.dram("w3t", [4096, 4096], F32, "ExternalInput")
    w2t = kb.dram("w2t", [4096, 4096], F32, "ExternalInput")
    valid = kb.dram("valid", [128, 1], F32, "ExternalInput")
    if L0:
        xhalo = kb.dram("xhalo", [128, 1024], F32, "ExternalInput")
        cxh = kb.dram("cxh", [128, 1024], F32, "ExternalInput")
        edge = kb.dram("edge", [2], F32, "ExternalInput")
        win = kb.dram("win", [1024, 1024], F32, "ExternalInput")
        cw = kb.dram("cw", [128, 124], F32, "ExternalInput")
        cvec = kb.dram("cvec", [128, 12], F32, "ExternalInput")
    hout = hout_t if hout_t is not None else kb.dram("hout", [NTOK, 1024], F32, "ExternalOutput")
    hlm = kb.dram("hlm", [NTOK, 1024], F32)
    nl2d = kb.dram("nl2d", [NTOK, 1024], BF16)
    xs = kb.dram("xs", [NROWS + 128, 1024], BF16)
    ys = kb.dram("ys", [NROWS + 128, 1024], F32)

    def bfv(t):
        return t[:].bitcast(BF16)

    def dbl(name, shape, dt, n=2, es=None):
        return [kb.sb("%s%d" % (name, i), shape, dt, es) for i in range(n)]

    identb = kb.identity("identb", BF16)
    zer = kb.sb("zer", [128, 128], F32)
    kb.op("pool", lambda e: e.memset(zer[:], 0.0), writes=[zer])
    zerb = kb.sb("zerb", [128, 2048], BF16)
    kb.op("pool", lambda e: e.memset(zerb[:], 0.0), writes=[zerb])
    for a in range(0, NROWS // 128, 2):
        kb.store("pool", xs, xs.h[a * 128:(a + 2) * 128, :].rearrange("(a p) n -> p a n", p=128), zerb, zerb[:].rearrange("p (a n) -> p a n", a=2))

    OH = kb.sb("OH", [128, NT, 2, 32], F32)
    GT = kb.sb("GT", [128, NT, 2], F32)
    RK = kb.sb("RK", [128, NT, 2], F32)
    Rbc = kb.sb("Rbc", [128, 32], F32)
    DESTI = kb.sb("DESTI", [128, NT * 2], I32)
    WIDX = kb.sb("WIDX", [128, NB], I32)
    validt = kb.sb("validt", [128, 1], F32)
    gate_bc = [[kb.sb("gate_bc%d%d" % (j, w), [128, 1024], F32) for w in range(2)] for j in range(2)]
    mod = kb.sb("mod", [128, 48, 2], F32)
    gs1 = kb.sb("gs1", [128, 8, 2], F32)
    gs2 = kb.sb("gs2", [128, 8, 2], F32)
    s_sb = kb.sb("s_sb", [128, 16], F32)
    adabf_sb = kb.sb("adabf_sb", [128, 48], F32)
    gfop_sb = kb.sb("gfop_sb", [128, 16], F32)
    iop = kb.sb("iop", [128, 1], F32)
    blkst = kb.sb("blkst", [128, NB], F32)
    ltri_b = kb.sb("ltri_b", [128, 128], BF16)
    ones_b = kb.sb("ones_b", [128, 128], BF16)
    pesA = ExitStack()

    def rstd_chain(stt, c_in, c_tmp, c_out, n, inv_n):
        kb.op("dve", lambda e: e.tensor_scalar(out=stt[:, c_tmp:c_tmp + n], in0=stt[:, c_in:c_in + n], scalar1=inv_n, scalar2=EPS, op0=ALU.mult, op1=ALU.add),
              reads=[stt], writes=[stt])
        kb.op("act", lambda e: e.activation(out=stt[:, c_tmp:c_tmp + n], in_=stt[:, c_tmp:c_tmp + n], func=AF.Sqrt), reads=[stt], writes=[stt])
        kb.op("dve", lambda e: e.reciprocal(out=stt[:, c_out:c_out + n], in_=stt[:, c_tmp:c_tmp + n]), reads=[stt], writes=[stt])

    kb.load("sp", s_sb, s_sb[:], svec.h, svec)
    kb.op("act", lambda e: e.activation(out=s_sb[:], in_=s_sb[:], func=AF.Silu), reads=[s_sb], writes=[s_sb])
    kb.load("sp", adabf_sb, adabf_sb[:], adabf.h, adabf)
    kb.load("sp", gfop_sb, gfop_sb[:], gfop.h, gfop)
    kb.load("sp", validt, validt[:], valid.h, valid)
    pm = banks[0]
    with ExitStack() as pes:
        adabr_sb = kb.sb("adabr_sb", [128, 2048], F32, pes)
        kb.load("sp", adabr_sb, adabr_sb[:], adabr.h.partition_broadcast(128), adabr)
        s_bc = [kb.sb("s_bc%d" % j, [128, 8, 128], F32, pes) for j in range(2)]
        for j in range(2):
            for kc in range(8):
                kb.op("dve", lambda e: e.tensor_scalar(out=s_bc[j][:, kc, :], in0=zer[:, 0:128], scalar1=s_sb[:, kc * 2 + j:kc * 2 + j + 1], scalar2=None, op0=ALU.add),
                      reads=[zer, s_sb], writes=[s_bc[j]])
        adaw_sb = dbl("adaw_sb", [128, 8, 512], F32, 2, pes)
        for v in range(12):
            aw = adaw_sb[v % 2]
            kb.load("sp", aw, aw[:], adaw.h[:, v * 512:(v + 1) * 512].rearrange("(kc p) n -> p kc n", p=128), adaw)
            for oc in range(4):
                g = v * 4 + oc
                for kc in range(8):
                    kb.op("pe", lambda e: e.matmul(pm[:, g * 2:g * 2 + 2], lhsT=aw[:, kc, oc * 128:(oc + 1) * 128], rhs=s_sb[:, kc * 2:kc * 2 + 2],
                                                  start=(kc == 0), stop=(kc == 7)), reads=[aw, s_sb], writes=[pm])
            if v in (4, 5, 10, 11):
                which = 0 if v < 6 else 1
                half = v % 2
                for j in range(2):
                    pr = banks[1 + j]
                    for kc in range(8):
                        kb.op("pe", lambda e: e.matmul(pr[:, :], lhsT=s_bc[j][:, kc, :], rhs=aw[:, kc, :], start=(kc == 0), stop=(kc == 7)),
                              reads=[s_bc[j], aw], writes=[pr])
                    kb.op("dve", lambda e: e.tensor_tensor(out=gate_bc[j][which][:, half * 512:(half + 1) * 512], in0=pr[:, :],
                                                           in1=adabr_sb[:, which * 1024 + half * 512: which * 1024 + (half + 1) * 512], op=ALU.add),
                          reads=[pr, adabr_sb], writes=[gate_bc[j][which]])
        pm3 = pm[:, 0:96].rearrange("p (g j) -> p g j", j=2)
        for j in range(2):
            kb.op("dve", lambda e: e.tensor_tensor(out=mod[:, :, j], in0=pm3[:, :, j], in1=adabf_sb[:], op=ALU.add), reads=[pm, adabf_sb], writes=[mod])
        kb.barrier()
    for j in range(2):
        kb.op("dve", lambda e: e.scalar_tensor_tensor(out=gs1[:, :, j], in0=mod[:, 8:16, j], scalar=1.0, in1=gfop_sb[:, 0:8], op0=ALU.add, op1=ALU.mult),
              reads=[mod, gfop_sb], writes=[gs1])
        kb.op("dve", lambda e: e.scalar_tensor_tensor(out=gs2[:, :, j], in0=mod[:, 32:40, j], scalar=1.0, in1=gfop_sb[:, 8:16], op0=ALU.add, op1=ALU.mult),
              reads=[mod, gfop_sb], writes=[gs2])
    SH1, SH2 = 0, 24

    xn_b = dbl("xn_b", [128, 1024], BF16, 2, pesA)
    junk = kb.sb("junk", [128, 1024], BF16, pesA)
    stn = dbl("stn", [128, 4], F32, 2, pesA)

    def norm_T(i, xt_tile, gs, shoff, j, dstT, dcol, psT):
        p = i % 2
        kb.op("act", lambda e: e.activation(out=junk[:], in_=xt_tile[:], func=AF.Square, accum_out=stn[p][:, 0:1]), reads=[xt_tile], writes=[junk, stn[p]])
        rstd_chain(stn[p], 0, 1, 2, 1, 1.0 / 1024)
        kb.op("act", lambda e: e.activation(out=xn_b[p][:], in_=xt_tile[:], func=AF.Copy, scale=stn[p][:, 2:3]), reads=[xt_tile, stn[p]], writes=[xn_b[p]])
        for kc in range(8):
            kb.op("pe", lambda e: e.transpose(out=bfv(psT)[:, kc * 128:(kc + 1) * 128], in_=xn_b[p][:, kc * 128:(kc + 1) * 128], identity=identb[:]),
                  reads=[xn_b[p], identb], writes=[psT])
        for kc in range(8):
            kb.op("act", lambda e: e.activation(out=dstT[:, kc, dcol:dcol + 128], in_=bfv(psT)[:, kc * 128:(kc + 1) * 128], func=AF.Identity,
                                                scale=gs[:, kc, j:j + 1], bias=mod[:, shoff + kc, j:j + 1]), reads=[psT, gs, mod], writes=[dstT])

    xt = dbl("xt", [128, 1024], F32, 2, pesA)
    convT = kb.sb("convT", [128, 4, NTOK], BF16, pesA) if L0 else None

    if L0:
        with ExitStack() as pes:
            HW = 15 + 4096 + 15
            hT = kb.sb("hT", [128, 4, HW], F32, pes)
            hTc = kb.sb("hTc", [128, 4, 128], F32, pes)
            win_b = kb.sb("win_b", [128, 8, 1024], BF16, pes)
            stg = xt
            for kc in range(8):
                kb.load("sp", stg[kc % 2], stg[kc % 2][:], win.h[kc * 128:(kc + 1) * 128, :], win)
                kb.op("pool", lambda e: e.tensor_copy(out=win_b[:, kc, :], in_=stg[kc % 2][:]), reads=[stg[kc % 2]], writes=[win_b])
            cw_sb = kb.sb("cw_sb", [128, 4, 31], F32, pes)
            kb.load("sp", cw_sb, cw_sb[:], cw.h.rearrange("p (c t) -> p c t", c=4), cw)
            cvec_sb = kb.sb("cvec_sb", [128, 12], F32, pes)
            kb.load("sp", cvec_sb, cvec_sb[:], cvec.h, cvec)
            edge_sb = kb.sb("edge_sb", [128, 2], F32, pes)
            kb.load("sp", edge_sb, edge_sb[:], edge.h.partition_broadcast(128), edge)
            ones_s = kb.sb("ones_s", [128, 128], F32, pes)
            kb.op("pool", lambda e: e.memset(ones_s[:], 1.0 / 512), writes=[ones_s])
            nlT = dbl("nlT", [128, 8, 512], BF16, 1, pes) * 2
            sig = dbl("sig", [128, 512], F32, 2, pes)
            htmp = kb.sb("htmp", [128, 4, 128], F32, pes)

            def u_group(gi, nl, ncols, dst_fn):
                for cc in range(4):
                    pa, pg = banks[2], banks[3]
                    for kc in range(8):
                        kb.op("pe", lambda e: e.matmul(pa[:, 0:ncols], lhsT=win_b[:, kc, cc * 128:(cc + 1) * 128], rhs=nl[:, kc, 0:ncols], start=(kc == 0), stop=(kc == 7)),
                              reads=[win_b, nl], writes=[pa])
                    for kc in range(8):
                        kb.op("pe", lambda e: e.matmul(pg[:, 0:ncols], lhsT=win_b[:, kc, 512 + cc * 128:512 + (cc + 1) * 128], rhs=nl[:, kc, 0:ncols], start=(kc == 0), stop=(kc == 7)),
                              reads=[win_b, nl], writes=[pg])
                    sg = sig[cc % 2]
                    kb.op("act", lambda e: e.activation(out=sg[:, 0:ncols], in_=pg[:, 0:ncols], func=AF.Sigmoid), reads=[pg], writes=[sg])
                    dt_, dap = dst_fn(cc)
                    kb.op("dve", lambda e: e.tensor_tensor(out=dap, in0=pa[:, 0:ncols], in1=sg[:, 0:ncols], op=ALU.mult), reads=[pa, sg], writes=[dt_])

            ti = 0
            for g in range(8):
                nl = nlT[g % 2]
                for tt in range(4):
                    t = g * 4 + tt
                    kb.load("sp", xt[ti % 2], xt[ti % 2][:], hin.h[t * 128:(t + 1) * 128, :], hin)
                    norm_T(ti, xt[ti % 2], gs1, SH1, 0, nl, tt * 128, banks[ti % 2])
                    ti += 1
                u_group(g, nl, 512, lambda cc: (hT, hT[:, cc, 15 + g * 512:15 + (g + 1) * 512]))
            nl = nlT[0]
            kb.load("sp", xt[ti % 2], xt[ti % 2][:], xhalo.h, xhalo)
            norm_T(ti, xt[ti % 2], gs1, SH1, 0, nl, 0, banks[ti % 2])
            ti += 1
            u_group(8, nl, 128, lambda cc: (htmp, htmp[:, cc, :]))
            for cc in range(4):
                kb.op("dve", lambda e: e.tensor_scalar(out=hT[:, cc, 0:15], in0=htmp[:, cc, 0:15], scalar1=edge_sb[:, 0:1], scalar2=None, op0=ALU.mult),
                      reads=[htmp, edge_sb], writes=[hT])
                kb.op("dve", lambda e: e.tensor_scalar(out=hT[:, cc, 15 + 4096:HW], in0=htmp[:, cc, 15:30], scalar1=edge_sb[:, 1:2], scalar2=None, op0=ALU.mult),
                      reads=[htmp, edge_sb], writes=[hT])
            nl = nlT[1]
            kb.load("sp", xt[ti % 2], xt[ti % 2][:], cxh.h, cxh)
            norm_T(ti, xt[ti % 2], gs1, SH1, 1, nl, 0, banks[ti % 2])
            ti += 1
            u_group(9, nl, 128, lambda cc: (hTc, hTc[:, cc, :]))
            for cc in range(4):
                kb.op("dve", lambda e: e.tensor_scalar(out=hTc[:, cc, 0:15], in0=hTc[:, cc, 0:15], scalar1=edge_sb[:, 0:1], scalar2=None, op0=ALU.mult),
                      reads=[hTc, edge_sb], writes=[hTc])
                kb.op("dve", lambda e: e.tensor_scalar(out=hTc[:, cc, 79:94], in0=hTc[:, cc, 79:94], scalar1=edge_sb[:, 1:2], scalar2=None, op0=ALU.mult),
                      reads=[hTc, edge_sb], writes=[hTc])

            acc = [kb.sb("acc%d" % c, [128, 512], F32, pes) for c in range(4)]
            sqt = dbl("sqt", [128, 512], F32, 1, pes) * 2
            mean_sb = kb.sb("mean_sb", [128, 512], F32, pes)
            m2 = kb.sb("m2", [128, 512], F32, pes)
            rstd_bc = kb.sb("rstd_bc", [128, 512], F32, pes)
            tt_ = dbl("tt_", [128, 512], F32, 1, pes) * 2

            def conv_block(src, c0, n, out_c0):
                for tau in range(31):
                    for cc in range(4):
                        en = "dve"
                        if tau == 0:
                            kb.op(en, lambda e: e.tensor_scalar(out=acc[cc][:, 0:n], in0=src[:, cc, c0:c0 + n], scalar1=cw_sb[:, cc, 0:1], scalar2=cvec_sb[:, cc:cc + 1],
                                                                op0=ALU.mult, op1=ALU.add), reads=[src, cw_sb, cvec_sb], writes=[acc[cc]])
                        else:
                            kb.op(en, lambda e: e.scalar_tensor_tensor(out=acc[cc][:, 0:n], in0=src[:, cc, c0 + tau:c0 + tau + n], scalar=cw_sb[:, cc, tau:tau + 1],
                                                                       in1=acc[cc][:, 0:n], op0=ALU.mult, op1=ALU.add), reads=[src, cw_sb, acc[cc]], writes=[acc[cc]])
                pmean, pex2 = banks[4], banks[5]
                for cc in range(4):
                    kb.op("pe", lambda e: e.matmul(pmean[:, 0:n], lhsT=ones_s[:], rhs=acc[cc][:, 0:n], start=(cc == 0), stop=(cc == 3)), reads=[ones_s, acc[cc]], writes=[pmean])
                for cc in range(4):
                    sq_ = sqt[cc % 2]
                    kb.op("act", lambda e: e.activation(out=sq_[:, 0:n], in_=acc[cc][:, 0:n], func=AF.Square), reads=[acc[cc]], writes=[sq_])
                    kb.op("pe", lambda e: e.matmul(pex2[:, 0:n], lhsT=ones_s[:], rhs=sq_[:, 0:n], start=(cc == 0), stop=(cc == 3)), reads=[ones_s, sq_], writes=[pex2])
                kb.op("act", lambda e: e.copy(out=mean_sb[:, 0:n], in_=pmean[:, 0:n]), reads=[pmean], writes=[mean_sb])
                kb.op("pool", lambda e: e.tensor_tensor(out=m2[:, 0:n], in0=mean_sb[:, 0:n], in1=mean_sb[:, 0:n], op=ALU.mult), reads=[mean_sb], writes=[m2])
                kb.op("dve", lambda e: e.tensor_tensor(out=m2[:, 0:n], in0=pex2[:, 0:n], in1=m2[:, 0:n], op=ALU.subtract), reads=[pex2, m2], writes=[m2])
                kb.op("dve", lambda e: e.tensor_scalar(out=m2[:, 0:n], in0=m2[:, 0:n], scalar1=EPS, scalar2=None, op0=ALU.add), reads=[m2], writes=[m2])
                kb.op("act", lambda e: e.activation(out=m2[:, 0:n], in_=m2[:, 0:n], func=AF.Sqrt), reads=[m2], writes=[m2])
                kb.op("dve", lambda e: e.reciprocal(out=rstd_bc[:, 0:n], in_=m2[:, 0:n]), reads=[m2], writes=[rstd_bc])
                for cc in range(4):
                    t_ = tt_[cc % 2]
                    kb.op("dve", lambda e: e.tensor_tensor(out=t_[:, 0:n], in0=acc[cc][:, 0:n], in1=mean_sb[:, 0:n], op=ALU.subtract), reads=[acc[cc], mean_sb], writes=[t_])
                    kb.op("pool", lambda e: e.tensor_tensor(out=t_[:, 0:n], in0=t_[:, 0:n], in1=rstd_bc[:, 0:n], op=ALU.mult), reads=[t_, rstd_bc], writes=[t_])
                    kb.op("act", lambda e: e.activation(out=convT[:, cc, out_c0:out_c0 + n], in_=t_[:, 0:n], func=AF.Silu, scale=cvec_sb[:, 4 + cc:5 + cc],
                                                        bias=cvec_sb[:, 8 + cc:9 + cc]), reads=[t_, cvec_sb], writes=[convT])

            for tb in range(8):
                conv_block(hT, tb * 512, 512, tb * 512)
            conv_block(hTc, 0, 64, 4096)
            kb.op("pool", lambda e: e.memset(convT[:, :, 4096 + 64:4096 + 128], 0.0), writes=[convT])
            kb.barrier()

    mix_sb = kb.sb("mix_sb", [128, NMIX, NTOK], BF16, pesA)
    mixidx_sb = kb.sb("mixidx_sb", [128, NMIX], I32, pesA)
    kb.load("sp", mixidx_sb, mixidx_sb[:], mixidx.h, mixidx)
    for hh in range(NMIX):
        kb.dma("pool", lambda e: e.indirect_dma_start(out=mix_sb[:, hh, :], out_offset=None, in_=mixsrc.h[:, :],
                                                      in_offset=bass.IndirectOffsetOnAxis(ap=mixidx_sb[:, hh:hh + 1], axis=0)), reads=[mixidx_sb, mixsrc], writes=[mix_sb])
    wout_b = kb.sb("wout_b", [128, 8, 1024], BF16, pesA)
    rw_b = kb.sb("rw_b", [128, 8, 36], BF16, pesA)
    rb_bc = kb.sb("rb_bc", [128, 36], F32, pesA)
    kb.load("sp", rb_bc, rb_bc[:], rb.h.partition_broadcast(128), rb)
    kb.op("pool", lambda e: e.memset(Rbc[:], 0.0), writes=[Rbc])
    ltri = kb.sb("ltri", [128, 128], F32, pesA)
    kb.op("pool", lambda e: e.memset(ltri[:], 1.0), writes=[ltri])
    kb.op("pool", lambda e: e.affine_select(out=ltri[:], in_=ltri[:], pattern=[[1, 128]], compare_op=ALU.is_gt, fill=0.0, base=0, channel_multiplier=-1),
          reads=[ltri], writes=[ltri])
    kb.op("pool", lambda e: e.tensor_copy(out=ltri_b[:], in_=ltri[:]), reads=[ltri], writes=[ltri_b])
    kb.op("pool", lambda e: e.memset(ones_b[:], 1.0), writes=[ones_b])
    kb.op("pool", lambda e: e.iota(iop[:], pattern=[[0, 1]], base=0, channel_multiplier=1, allow_small_or_imprecise_dtypes=True), writes=[iop])
    kb.op("pool", lambda e: e.iota(blkst[:], pattern=[[256, NB]], base=0, channel_multiplier=0, allow_small_or_imprecise_dtypes=True), writes=[blkst])

    with ExitStack() as pes:
        stg = dbl("stg2", [128, 1024], F32, 2, pes)
        for kc in range(8):
            kb.load("sp", stg[kc % 2], stg[kc % 2][:], wout.h[kc * 128:(kc + 1) * 128, :], wout)
            kb.op("pool", lambda e: e.tensor_copy(out=wout_b[:, kc, :], in_=stg[kc % 2][:]), reads=[stg[kc % 2]], writes=[wout_b])
        rw_f = kb.sb("rw_f", [128, 8, 36], F32, pes)
        kb.load("sp", rw_f, rw_f[:], rw.h.rearrange("(kc p) n -> p kc n", p=128), rw)
        kb.op("pool", lambda e: e.tensor_copy(out=rw_b[:], in_=rw_f[:]), reads=[rw_f], writes=[rw_b])

        ytmp = dbl("ytmp", [128, 1024], F32, 2, pes)
        hl = dbl("hl", [128, 1024], F32, 2, pes)
        nl2T = dbl("nl2T", [128, 8, 128], BF16, 2, pes)
        nl2 = dbl("nl2", [128, 1024], BF16, 2, pes)
        lg = dbl("lg", [128, 36], F32, 2, pes)
        rt = dbl("rt", [128, 16], F32, 2, pes)
        lem = dbl("lem", [128, 32], F32, 2, pes)
        lem2 = dbl("lem2", [128, 32], F32, 2, pes)
        cb_ = dbl("cb_", [128, 32], BF16, 2, pes)
        rbase = dbl("rbase", [128, 32], F32, 2, pes)
        tmp32 = dbl("tmp32", [128, 2, 32], F32, 2, pes)
        ejunk = kb.sb("ejunk", [128, 4], F32, pes)

        for t in range(NT):
            p = t % 2
            j = 1 if (L0 and t == NT - 1) else 0
            kb.load("sp", xt[p], xt[p][:], hin.h[t * 128:(t + 1) * 128, :], hin)
            chunks = []
            if L0:
                for cc in range(4):
                    chunks.append((convT, convT[:, cc, t * 128:(t + 1) * 128]))
            for hh in range(NMIX):
                chunks.append((mix_sb, mix_sb[:, hh, t * 128:(t + 1) * 128]))
            for half in range(2):
                py = banks[half]
                for ci, (ct, cap) in enumerate(chunks):
                    kb.op("pe", lambda e: e.matmul(py[:, :], lhsT=cap, rhs=wout_b[:, ci, half * 512:(half + 1) * 512], start=(ci == 0), stop=(ci == 7)),
                          reads=[ct, wout_b], writes=[py])
                kb.op("dve", lambda e: e.tensor_tensor(out=ytmp[p][:, half * 512:(half + 1) * 512], in0=py[:, :], in1=gate_bc[j][0][:, half * 512:(half + 1) * 512], op=ALU.mult),
                      reads=[py, gate_bc[j][0]], writes=[ytmp[p]])
            kb.op("pool", lambda e: e.tensor_tensor(out=hl[p][:], in0=ytmp[p][:], in1=xt[p][:], op=ALU.add), reads=[ytmp[p], xt[p]], writes=[hl[p]])
            kb.store("sp", hlm, hlm.h[t * 128:(t + 1) * 128, :], hl[p], hl[p][:])
            norm_T(t, hl[p], gs2, SH2, j, nl2T[p], 0, banks[2])
            pl = banks[3]
            for kc in range(8):
                kb.op("pe", lambda e: e.matmul(pl[:, 0:36], lhsT=nl2T[p][:, kc, :], rhs=rw_b[:, kc, :], start=(kc == 0), stop=(kc == 7)), reads=[nl2T[p], rw_b], writes=[pl])
            kb.op("dve", lambda e: e.tensor_tensor(out=lg[p][:], in0=pl[:, 0:36], in1=rb_bc[:], op=ALU.add), reads=[pl, rb_bc], writes=[lg[p]])
            pbk = banks[4]
            for kc in range(8):
                kb.op("pe", lambda e: e.transpose(out=bfv(pbk)[:, kc * 128:(kc + 1) * 128], in_=nl2T[p][:, kc, :], identity=identb[:]), reads=[nl2T[p], identb], writes=[pbk])
            kb.op("act", lambda e: e.copy(out=nl2[p][:], in_=bfv(pbk)[:, 0:1024]), reads=[pbk], writes=[nl2[p]])
            kb.store("sp", nl2d, nl2d.h[t * 128:(t + 1) * 128, :], nl2[p], nl2[p][:])
            r_ = rt[p]
            kb.op("dve", lambda e: e.tensor_reduce(out=r_[:, 0:1], in_=lg[p][:, 0:4], axis=AX.X, op=ALU.max), reads=[lg[p]], writes=[r_])
            kb.op("dve", lambda e: e.tensor_scalar(out=r_[:, 1:5], in0=lg[p][:, 0:4], scalar1=r_[:, 0:1], scalar2=None, op0=ALU.is_equal), reads=[lg[p], r_], writes=[r_])
            kb.op("dve", lambda e: e.tensor_scalar(out=r_[:, 5:6], in0=r_[:, 0:1], scalar1=-1.0, scalar2=None, op0=ALU.mult), reads=[r_], writes=[r_])
            kb.op("act", lambda e: e.activation(out=ejunk[:], in_=lg[p][:, 0:4], func=AF.Exp, bias=r_[:, 5:6], accum_out=r_[:, 6:7]), reads=[lg[p], r_], writes=[ejunk, r_])
            kb.op("dve", lambda e: e.reciprocal(out=r_[:, 7:8], in_=r_[:, 6:7]), reads=[r_], writes=[r_])
            kb.op("dve", lambda e: e.tensor_scalar(out=r_[:, 8:12], in0=r_[:, 1:5], scalar1=-1.0, scalar2=BIG, op0=ALU.add, op1=ALU.mult), reads=[r_], writes=[r_])
            for g in range(4):
                kb.op("dve", lambda e: e.tensor_scalar(out=lem[p][:, g * 8:(g + 1) * 8], in0=lg[p][:, 4 + g * 8:4 + (g + 1) * 8], scalar1=r_[:, 8 + g:9 + g], scalar2=None, op0=ALU.add),
                      reads=[lg[p], r_], writes=[lem[p]])
            oh1 = OH[:, t, 0, :]
            oh2 = OH[:, t, 1, :]
            kb.op("dve", lambda e: e.tensor_reduce(out=r_[:, 12:13], in_=lem[p][:], axis=AX.X, op=ALU.max), reads=[lem[p]], writes=[r_])
            kb.op("dve", lambda e: e.tensor_scalar(out=oh1, in0=lem[p][:], scalar1=r_[:, 12:13], scalar2=None, op0=ALU.is_equal), reads=[lem[p], r_], writes=[OH])
            kb.op("dve", lambda e: e.scalar_tensor_tensor(out=lem2[p][:], in0=oh1, scalar=-BIG, in1=lem[p][:], op0=ALU.mult, op1=ALU.add), reads=[OH, lem[p]], writes=[lem2[p]])
            kb.op("dve", lambda e: e.tensor_reduce(out=r_[:, 13:14], in_=lem2[p][:], axis=AX.X, op=ALU.max), reads=[lem2[p]], writes=[r_])
            kb.op("dve", lambda e: e.tensor_scalar(out=oh2, in0=lem2[p][:], scalar1=r_[:, 13:14], scalar2=None, op0=ALU.is_equal), reads=[lem2[p], r_], writes=[OH])
            kb.op("dve", lambda e: e.tensor_tensor(out=r_[:, 14:15], in0=r_[:, 12:13], in1=r_[:, 13:14], op=ALU.subtract), reads=[r_], writes=[r_])
            kb.op("act", lambda e: e.activation(out=r_[:, 15:16], in_=r_[:, 14:15], func=AF.Sigmoid), reads=[r_], writes=[r_])
            kb.op("dve", lambda e: e.tensor_tensor(out=GT[:, t, 0:1], in0=r_[:, 15:16], in1=r_[:, 7:8], op=ALU.mult), reads=[r_], writes=[GT])
            kb.op("dve", lambda e: e.tensor_tensor(out=GT[:, t, 1:2], in0=r_[:, 7:8], in1=GT[:, t, 0:1], op=ALU.subtract), reads=[r_, GT], writes=[GT])
            if j == 1:
                kb.op("dve", lambda e: e.tensor_scalar(out=OH[:, t, :, :], in0=OH[:, t, :, :], scalar1=validt[:, 0:1], scalar2=None, op0=ALU.mult), reads=[OH, validt], writes=[OH])
            kb.op("dve", lambda e: e.tensor_tensor(out=cb_[p][:], in0=OH[:, t, 0, :], in1=OH[:, t, 1, :], op=ALU.add), reads=[OH], writes=[cb_[p]])
            pc = banks[5]
            kb.op("pe", lambda e: e.matmul(pc[:, 0:32], lhsT=ltri_b[:], rhs=cb_[p][:], start=True, stop=True), reads=[ltri_b, cb_[p]], writes=[pc])
            kb.op("dve", lambda e: e.tensor_tensor(out=rbase[p][:], in0=pc[:, 0:32], in1=Rbc[:], op=ALU.add), reads=[pc, Rbc], writes=[rbase[p]])
            pt_ = banks[6]
            kb.op("pe", lambda e: e.matmul(pt_[:, 0:32], lhsT=ones_b[:], rhs=cb_[p][:], start=True, stop=True), reads=[ones_b, cb_[p]], writes=[pt_])
            kb.op("dve", lambda e: e.tensor_tensor(out=Rbc[:], in0=pt_[:, 0:32], in1=Rbc[:], op=ALU.add), reads=[pt_, Rbc], writes=[Rbc])
            for k in range(2):
                kb.op("dve", lambda e: e.tensor_tensor(out=tmp32[p][:, k, :], in0=OH[:, t, k, :], in1=rbase[p][:], op=ALU.mult), reads=[OH, rbase[p]], writes=[tmp32[p]])
            kb.op("dve", lambda e: e.tensor_reduce(out=RK[:, t, :], in_=tmp32[p][:], axis=AX.X, op=ALU.add), reads=[tmp32[p]], writes=[RK])
        kb.barrier()
    pesA.close()
    pesD = ExitStack()

    cnt_i = kb.sb("cnt_i", [128, 32], I32, pesD)
    pcnt = kb.sb("pcnt", [128, 32], F32, pesD)
    pend = [kb.sb("pend%d" % i, [128, 32], F32, pesD) for i in range(2)]
    kb.op("dve", lambda e: e.tensor_scalar(out=pcnt[:], in0=Rbc[:], scalar1=255.0, scalar2=None, op0=ALU.add), reads=[Rbc], writes=[pcnt])
    kb.op("dve", lambda e: e.tensor_copy(out=cnt_i[:], in_=pcnt[:]), reads=[pcnt], writes=[cnt_i])
    kb.op("dve", lambda e: e.tensor_scalar(out=cnt_i[:], in0=cnt_i[:], scalar1=8, scalar2=8, op0=ALU.arith_shift_right, op1=ALU.logical_shift_left), reads=[cnt_i], writes=[cnt_i])
    kb.op("dve", lambda e: e.tensor_copy(out=pcnt[:], in_=cnt_i[:]), reads=[cnt_i], writes=[pcnt])
    kb.op("dve", lambda e: e.tensor_copy(out=pend[0][:], in_=pcnt[:]), reads=[pcnt], writes=[pend[0]])
    cur = 0
    for sft in (1, 2, 4, 8, 16):
        a, b = pend[cur], pend[1 - cur]
        kb.op("dve", lambda e: e.tensor_copy(out=b[:, 0:sft], in_=a[:, 0:sft]), reads=[a], writes=[b])
        kb.op("dve", lambda e: e.tensor_tensor(out=b[:, sft:32], in0=a[:, sft:32], in1=a[:, 0:32 - sft], op=ALU.add), reads=[a], writes=[b])
        cur = 1 - cur
    pendf = pend[cur]
    poff = kb.sb("poff", [128, 32], F32, pesD)
    kb.op("dve", lambda e: e.tensor_tensor(out=poff[:], in0=pendf[:], in1=pcnt[:], op=ALU.subtract), reads=[pendf, pcnt], writes=[poff])
    DEST = kb.sb("DEST", [128, NT, 2], F32, pesD)
    tmpd = kb.sb("tmpd", [128, NT * 2, 32], F32, pesD)
    for t in range(NT):
        for k in range(2):
            kb.op("dve", lambda e: e.tensor_tensor(out=tmpd[:, t * 2 + k, :], in0=OH[:, t, k, :], in1=poff[:], op=ALU.mult), reads=[OH, poff], writes=[tmpd])
    kb.op("dve", lambda e: e.tensor_reduce(out=DEST[:].rearrange("p t k -> p (t k)"), in_=tmpd[:], axis=AX.X, op=ALU.add), reads=[tmpd], writes=[DEST])
    kb.op("dve", lambda e: e.tensor_tensor(out=DEST[:], in0=DEST[:], in1=RK[:], op=ALU.add), reads=[DEST, RK], writes=[DEST])
    if L0:
        inval = kb.sb("inval", [128, 2], F32, pesD)
        kb.op("dve", lambda e: e.tensor_scalar(out=inval[:, 0:1], in0=validt[:], scalar1=-1.0, scalar2=-1.0, op0=ALU.add, op1=ALU.mult), reads=[validt], writes=[inval])
        kb.op("dve", lambda e: e.scalar_tensor_tensor(out=inval[:, 1:2], in0=iop[:], scalar=float(NROWS), in1=inval[:, 0:1], op0=ALU.add, op1=ALU.mult), reads=[iop, inval], writes=[inval])
        kb.op("dve", lambda e: e.tensor_scalar(out=DEST[:, NT - 1, :], in0=DEST[:, NT - 1, :], scalar1=validt[:, 0:1], scalar2=inval[:, 1:2], op0=ALU.mult, op1=ALU.add),
              reads=[DEST, validt, inval], writes=[DEST])
    kb.op("dve", lambda e: e.tensor_copy(out=DESTI[:], in_=DEST[:].rearrange("p t k -> p (t k)")), reads=[DEST], writes=[DESTI])
    eb = kb.sb("eb", [128, NB], F32, pesD)
    kb.op("pool", lambda e: e.memset(eb[:], 0.0), writes=[eb])
    for ee in range(32):
        kb.op("dve", lambda e: e.scalar_tensor_tensor(out=eb[:], in0=blkst[:], scalar=pendf[:, ee:ee + 1], in1=eb[:], op0=ALU.is_ge, op1=ALU.add), reads=[blkst, pendf, eb], writes=[eb])
    kb.op("dve", lambda e: e.tensor_scalar(out=eb[:], in0=eb[:], scalar1=31.0, scalar2=128.0, op0=ALU.min, op1=ALU.mult), reads=[eb], writes=[eb])
    kb.op("dve", lambda e: e.tensor_scalar(out=eb[:], in0=eb[:], scalar1=iop[:, 0:1], scalar2=None, op0=ALU.add), reads=[eb, iop], writes=[eb])
    kb.op("dve", lambda e: e.tensor_copy(out=WIDX[:], in_=eb[:]), reads=[eb], writes=[WIDX])

    srow = dbl("srow", [128, 1024], BF16, 3, pesD)
    for t in range(NT):
        sr = srow[t % 3]
        kb.load("sp", sr, sr[:], nl2d.h[t * 128:(t + 1) * 128, :], nl2d)
        for k in range(2):
            kb.dma("pool", lambda e: e.indirect_dma_start(out=xs.h[:, :], out_offset=bass.IndirectOffsetOnAxis(ap=DESTI[:, t * 2 + k:t * 2 + k + 1], axis=0),
                                                          in_=sr[:], in_offset=None), reads=[DESTI, sr], writes=[xs])
    kb.barrier()
    pesD.close()
    pesE = ExitStack()

    w1f = dbl("w1f", [128, 4096], F32, 2, pesE)
    w3f = dbl("w3f", [128, 4096], F32, 2, pesE)
    w2f = dbl("w2f", [128, 4096], F32, 2, pesE)
    w1b = dbl("w1b", [128, 8, 512], BF16, 1, pesE) * 2
    w3b = dbl("w3b", [128, 8, 512], BF16, 1, pesE) * 2
    w2b = dbl("w2b", [128, 4, 1024], BF16, 1, pesE) * 2
    xr = dbl("xr", [128, 2, 1024], BF16, 2, pesE)
    xsT = dbl("xsT", [128, 8, 256], BF16, 2, pesE)
    sl = dbl("sl", [128, 256], F32, 2, pesE)
    hhT = dbl("hhT", [128, 4, 256], BF16, 2, pesE)
    yo = dbl("yo", [128, 1024], F32, 2, pesE)
    for b in range(NB):
        p = b % 2
        for (tab, wf) in ((w1t, w1f[p]), (w3t, w3f[p]), (w2t, w2f[p])):
            kb.dma("pool", lambda e: e.indirect_dma_start(out=wf[:], out_offset=None, in_=tab.h[:, :], in_offset=bass.IndirectOffsetOnAxis(ap=WIDX[:, b:b + 1], axis=0)),
                   reads=[WIDX, tab], writes=[wf])
        kb.op("act", lambda e: e.copy(out=w1b[p][:].rearrange("p a b -> p (a b)"), in_=w1f[p][:]), reads=[w1f[p]], writes=[w1b[p]])
        kb.op("dve", lambda e: e.tensor_copy(out=w3b[p][:].rearrange("p a b -> p (a b)"), in_=w3f[p][:]), reads=[w3f[p]], writes=[w3b[p]])
        kb.op("act", lambda e: e.copy(out=w2b[p][:].rearrange("p a b -> p (a b)")[:, 0:2048], in_=w2f[p][:, 0:2048]), reads=[w2f[p]], writes=[w2b[p]])
        kb.op("dve", lambda e: e.tensor_copy(out=w2b[p][:].rearrange("p a b -> p (a b)")[:, 2048:4096], in_=w2f[p][:, 2048:4096]), reads=[w2f[p]], writes=[w2b[p]])
        kb.load("sp", xr[p], xr[p][:], xs.h[b * 256:(b + 1) * 256, :].rearrange("(a p) n -> p a n", p=128), xs)
        for sub in range(2):
            pT = banks[6]
            for kc in range(8):
                kb.op("pe", lambda e: e.transpose(out=bfv(pT)[:, kc * 128:(kc + 1) * 128], in_=xr[p][:, sub, kc * 128:(kc + 1) * 128], identity=identb[:]),
                      reads=[xr[p], identb], writes=[pT])
            kb.op("act", lambda e: e.copy(out=xsT[p][:, :, sub * 128:(sub + 1) * 128], in_=bfv(pT)[:, 0:1024].rearrange("p (a b) -> p a b", a=8)), reads=[pT], writes=[xsT[p]])
        for fc in range(4):
            ph1 = banks[0 + fc // 2]
            ph3 = banks[2 + fc // 2]
            c0 = (fc % 2) * 256
            for kc in range(8):
                kb.op("pe", lambda e: e.matmul(ph1[:, c0:c0 + 256], lhsT=w1b[p][:, kc, fc * 128:(fc + 1) * 128], rhs=xsT[p][:, kc, :], start=(kc == 0), stop=(kc == 7)),
                      reads=[w1b[p], xsT[p]], writes=[ph1])
            for kc in range(8):
                kb.op("pe", lambda e: e.matmul(ph3[:, c0:c0 + 256], lhsT=w3b[p][:, kc, fc * 128:(fc + 1) * 128], rhs=xsT[p][:, kc, :], start=(kc == 0), stop=(kc == 7)),
                      reads=[w3b[p], xsT[p]], writes=[ph3])
            s_ = sl[fc % 2]
            kb.op("act", lambda e: e.activation(out=s_[:], in_=ph1[:, c0:c0 + 256], func=AF.Silu), reads=[ph1], writes=[s_])
            kb.op("dve", lambda e: e.tensor_tensor(out=hhT[p][:, fc, :], in0=ph3[:, c0:c0 + 256], in1=s_[:], op=ALU.mult), reads=[ph3, s_], writes=[hhT[p]])
        for sub in range(2):
            y_ = yo[sub]
            for half in range(2):
                py = banks[4 + half]
                for fc in range(4):
                    kb.op("pe", lambda e: e.matmul(py[:, :], lhsT=hhT[p][:, fc, sub * 128:(sub + 1) * 128], rhs=w2b[p][:, fc, half * 512:(half + 1) * 512], start=(fc == 0), stop=(fc == 3)),
                          reads=[hhT[p], w2b[p]], writes=[py])
                if half == 0:
                    kb.op("act", lambda e: e.copy(out=y_[:, 0:512], in_=py[:, :]), reads=[py], writes=[y_])
                else:
                    kb.op("dve", lambda e: e.tensor_copy(out=y_[:, 512:1024], in_=py[:, :]), reads=[py], writes=[y_])
            kb.store("sp", ys, ys.h[b * 256 + sub * 128:b * 256 + (sub + 1) * 128, :], y_, y_[:])
    kb.barrier()
    pesE.close()
    pesF = ExitStack()

    y1 = dbl("y1", [128, 1024], F32, 2, pesF)
    y2 = dbl("y2", [128, 1024], F32, 2, pesF)
    hm = dbl("hm", [128, 1024], F32, 2, pesF)
    for t in range(NT):
        p = t % 2
        j = 1 if (L0 and t == NT - 1) else 0
        if j == 1:
            kb.op("pool", lambda e: e.memset(y1[p][:], 0.0), writes=[y1[p]])
            kb.op("pool", lambda e: e.memset(y2[p][:], 0.0), writes=[y2[p]])
        for k, yk in ((0, y1[p]), (1, y2[p])):
            kb.dma("pool", lambda e: e.indirect_dma_start(out=yk[:], out_offset=None, in_=ys.h[:, :], in_offset=bass.IndirectOffsetOnAxis(ap=DESTI[:, t * 2 + k:t * 2 + k + 1], axis=0)), reads=[DESTI, ys], writes=[yk])
        kb.load("sp", hm[p], hm[p][:], hlm.h[t * 128:(t + 1) * 128, :], hlm)
        kb.op("dve", lambda e: e.tensor_scalar(out=y1[p][:], in0=y1[p][:], scalar1=GT[:, t, 0:1], scalar2=None, op0=ALU.mult), reads=[y1[p], GT], writes=[y1[p]])
        kb.op("dve", lambda e: e.scalar_tensor_tensor(out=y1[p][:], in0=y2[p][:], scalar=GT[:, t, 1:2], in1=y1[p][:], op0=ALU.mult, op1=ALU.add), reads=[y2[p], GT, y1[p]], writes=[y1[p]])
        kb.op("pool", lambda e: e.tensor_tensor(out=y1[p][:], in0=y1[p][:], in1=gate_bc[j][1][:], op=ALU.mult), reads=[y1[p], gate_bc[j][1]], writes=[y1[p]])
        kb.op("dve", lambda e: e.tensor_tensor(out=hm[p][:], in0=hm[p][:], in1=y1[p][:], op=ALU.add), reads=[hm[p], y1[p]], writes=[hm[p]])
        kb.store("sp", hout, hout.h[t * 128:(t + 1) * 128, :], hm[p], hm[p][:])
    print("lb%d instructions:" % layer, kb.n_ins, "sems:", len(kb.sems))
    pesF.close()
    kb.end_stage()


def fop(v, n):
    return np.ascontiguousarray(np.asarray(v, np.float32).reshape(n, 128).T)


def moe_tables(inp, l):
    w1 = np.ascontiguousarray(inp["moe_w1"][l].reshape(32, 8, 128, 512).transpose(0, 2, 1, 3).reshape(4096, 4096))
    w3 = np.ascontiguousarray(inp["moe_w3"][l].reshape(32, 8, 128, 512).transpose(0, 2, 1, 3).reshape(4096, 4096))
    w2 = np.ascontiguousarray(inp["moe_w2"][l].reshape(32, 4, 128, 1024).transpose(0, 2, 1, 3).reshape(4096, 4096))
    return w1, w3, w2


def host_b(layer, inp):
    L0 = layer == 0
    l = layer
    w1, w3, w2 = moe_tables(inp, l)
    rw = np.ascontiguousarray(np.concatenate([inp["rg_w"][l], inp["re_w"][l]], axis=1).astype(np.float32))
    rb = np.concatenate([inp["rg_b"][l], inp["re_b"][l]]).astype(np.float32)
    adabr = np.concatenate([inp["ada_b"][l][2048:3072], inp["ada_b"][l][5120:6144]]).astype(np.float32)
    gfop = np.ascontiguousarray(np.concatenate([fop(inp["norm1_g"][l], 8), fop(inp["norm2_g"][l], 8)], axis=1))
    wout = np.ascontiguousarray(inp["ab_w_out"][0] if L0 else inp["gla_w_out"][0])
    hin_lat, hin_ctx = inp["x"], inp["ctx"]
    maps = []
    pp = np.arange(128, dtype=np.int32)
    for b in range(2):
        sv = np.stack([inp["c"][b], inp["c_ctx"]], -1).reshape(8, 128, 2).transpose(1, 0, 2).reshape(128, 16).astype(np.float32)
        for jq in range(4):
            r0, r1 = jq * 4096, (jq + 1) * 4096
            m = {"svec": np.ascontiguousarray(sv), "adaw": np.ascontiguousarray(inp["ada_w"][l]), "adabf": fop(inp["ada_b"][l], 48), "adabr": adabr, "gfop": gfop,
                 "wout": wout, "rw": rw, "rb": rb, "w1t": w1, "w3t": w3, "w2t": w2}
            if L0:
                cpad = np.zeros((128, 1024), np.float32)
                cpad[:64] = hin_ctx[b, 64 * jq:64 * jq + 64]
                m["hin"] = np.ascontiguousarray(np.concatenate([hin_lat[b, r0:r1], cpad], 0))
                m["mixidx"] = np.ascontiguousarray(np.stack([np.array([ag_row(jq * 128 + int(p_), h, 64, 512) for p_ in pp]) for h in range(4)], axis=1).astype(np.int32))
                xh = np.zeros((128, 1024), np.float32)
                if jq > 0:
                    xh[0:15] = hin_lat[b, r0 - 15:r0]
                if jq < 3:
                    xh[15:30] = hin_lat[b, r1:r1 + 15]
                m["xhalo"] = xh
                ch = np.zeros((128, 1024), np.float32)
                for r in range(94):
                    pos = 64 * jq - 15 + r
                    if 0 <= pos < 256:
                        ch[r] = hin_ctx[b, pos]
                m["cxh"] = ch
                m["edge"] = np.array([1.0 if jq > 0 else 0.0, 1.0 if jq < 3 else 0.0], np.float32)
                v = np.zeros((128, 1), np.float32)
                v[:64] = 1
                m["valid"] = v
                m["win"] = np.ascontiguousarray(inp["ab_w_in"][0][:, 0:1024])
                m["cw"] = np.ascontiguousarray(inp["conv_w"][0].T.reshape(4, 128, 31).transpose(1, 0, 2).reshape(128, 124))
                m["cvec"] = np.ascontiguousarray(np.concatenate([fop(inp["conv_b"][0], 4), fop(inp["conv_ln_g"][0], 4), fop(inp["conv_ln_b"][0], 4)], axis=1))
            else:
                m["mixidx"] = np.ascontiguousarray(np.stack([np.array([ag_row(jq * 256 + c2 * 128 + int(p_), h, 128, 1024) for p_ in pp]) for h in range(4) for c2 in range(2)], axis=1).astype(np.int32))
                m["valid"] = np.ones((128, 1), np.float32)
            maps.append(m)
    return maps


def gather_b(layer, results):
    L0 = layer == 0
    hl = np.zeros((2, 16384, 1024), np.float32)
    hc = np.zeros((2, 256, 1024), np.float32) if L0 else None
    for b in range(2):
        for jq in range(4):
            o = results[b * 4 + jq]["hout"]
            hl[b, jq * 4096:(jq + 1) * 4096] = o[:4096]
            if L0:
                hc[b, 64 * jq:64 * jq + 64] = o[4096:4160]
    return hl, hc


def build_l1a(kb, banks, x2out, x3in, n_lat_tiles=128, do_scan=True):
    kb.begin_stage("a1_")
    svec = kb.dram("svec", [128, 16], F32, "ExternalInput")
    adaw = kb.dram("adaw", [1024, 2048], F32, "ExternalInput")
    adab = kb.dram("adab", [128, 16], F32, "ExternalInput")
    g1 = kb.dram("g1", [128, 8], F32, "ExternalInput")
    w = kb.dram("w", [1024, 768], F32, "ExternalInput")
    waT = kb.dram("waT", [2, 16, 1024], F32, "ExternalInput")
    wa2 = kb.dram("wa2", [2, 16, 128], F32, "ExternalInput")
    small = kb.dram("small", [512], F32, "ExternalInput")
    proj = kb.dram("proj", [S + LC, 1024], F32)
    of_d = kb.dram("of_d", [S, 256], F32)

    def bfv(t):
        return t[:].bitcast(BF16)

    def dbl(name, shape, dt, n=2, es=None):
        return [kb.sb("%s%d" % (name, i), shape, dt, es) for i in range(n)]

    identb = kb.identity("identb", BF16)
    smallb = kb.sb("smallb", [128, 512], F32)
    kb.load("sp", smallb, smallb[:], small.h.partition_broadcast(128), small)
    zer = kb.sb("zer", [128, 128], F32)
    kb.op("pool", lambda e: e.memset(zer[:], 0.0), writes=[zer])

    def rstd_chain(stt, c_in, c_tmp, c_out, n, inv_n):
        kb.op("dve", lambda e: e.tensor_scalar(out=stt[:, c_tmp:c_tmp + n], in0=stt[:, c_in:c_in + n], scalar1=inv_n, scalar2=EPS, op0=ALU.mult, op1=ALU.add),
              reads=[stt], writes=[stt])
        kb.op("act", lambda e: e.activation(out=stt[:, c_tmp:c_tmp + n], in_=stt[:, c_tmp:c_tmp + n], func=AF.Sqrt), reads=[stt], writes=[stt])
        kb.op("dve", lambda e: e.reciprocal(out=stt[:, c_out:c_out + n], in_=stt[:, c_tmp:c_tmp + n]), reads=[stt], writes=[stt])

    wq = [kb.sb("wq%d" % j, [128, 8, 1024], BF16) for j in range(2)]
    bias = [kb.sb("bias%d" % j, [128, 1024], F32) for j in range(2)]
    pesA = ExitStack()
    s_sb = kb.sb("s_sb", [128, 16], F32, pesA)
    kb.load("sp", s_sb, s_sb[:], svec.h, svec)
    kb.op("act", lambda e: e.activation(out=s_sb[:], in_=s_sb[:], func=AF.Silu), reads=[s_sb], writes=[s_sb])
    adab_sb = kb.sb("adab_sb", [128, 16], F32, pesA)
    kb.load("sp", adab_sb, adab_sb[:], adab.h, adab)
    g1_sb = kb.sb("g1_sb", [128, 8], F32, pesA)
    kb.load("sp", g1_sb, g1_sb[:], g1.h, g1)
    mod = kb.sb("mod", [128, 16, 2], F32, pesA)
    gs = kb.sb("gs", [128, 8, 2], F32, pesA)
    w_sb = kb.sb("w_sb", [128, 8, 1024], F32, pesA)
    shiftbc = kb.sb("shiftbc", [128, 8, 128], F32, pesA)
    pm = banks[0]
    with ExitStack() as pes:
        adaw_sb = kb.sb("adaw_sb", [128, 8, 512], F32, pes)
        for v in range(4):
            kb.load("sp", adaw_sb, adaw_sb[:], adaw.h[:, v * 512:(v + 1) * 512].rearrange("(kc p) n -> p kc n", p=128), adaw)
            for oc in range(4):
                g = v * 4 + oc
                for kc in range(8):
                    kb.op("pe", lambda e: e.matmul(pm[:, g * 2:g * 2 + 2], lhsT=adaw_sb[:, kc, oc * 128:(oc + 1) * 128], rhs=s_sb[:, kc * 2:kc * 2 + 2],
                                                  start=(kc == 0), stop=(kc == 7)), reads=[adaw_sb, s_sb], writes=[pm])
        pm3 = pm[:, 0:32].rearrange("p (g j) -> p g j", j=2)
        for j in range(2):
            kb.op("dve", lambda e: e.tensor_tensor(out=mod[:, :, j], in0=pm3[:, :, j], in1=adab_sb[:], op=ALU.add), reads=[pm, adab_sb], writes=[mod])
            kb.op("dve", lambda e: e.scalar_tensor_tensor(out=gs[:, :, j], in0=mod[:, 8:16, j], scalar=1.0, in1=g1_sb[:], op0=ALU.add, op1=ALU.mult),
                  reads=[mod, g1_sb], writes=[gs])
        kb.load("sp", w_sb, w_sb[:, :, 0:768], w.h.rearrange("(kc p) n -> p kc n", p=128), w)
        waT_sb = [kb.sb("waT_sb%d" % d, [32, 1024], F32, pes) for d in range(2)]
        wa2_sb = [kb.sb("wa2_sb%d" % d, [32, 128], F32, pes) for d in range(2)]
        for d in range(2):
            kb.op("pool", lambda e: e.memset(waT_sb[d][:], 0.0), writes=[waT_sb[d]])
            kb.op("pool", lambda e: e.memset(wa2_sb[d][:], 0.0), writes=[wa2_sb[d]])
            kb.load("sp", waT_sb[d], waT_sb[d][0:16, :], waT.h[d], waT)
            kb.load("sp", wa2_sb[d], wa2_sb[d][0:16, :], wa2.h[d], wa2)
            for kc in range(8):
                pz = banks[1]
                kb.op("pe", lambda e: e.matmul(pz[:, 0:128], lhsT=waT_sb[d][:, kc * 128:(kc + 1) * 128], rhs=wa2_sb[d][:], start=True, stop=True),
                      reads=[waT_sb[d], wa2_sb[d]], writes=[pz])
                kb.op("dve", lambda e: e.tensor_copy(out=w_sb[:, kc, 768 + d * 128:768 + (d + 1) * 128], in_=pz[:, 0:128]), reads=[pz], writes=[w_sb])
        for j in range(2):
            for kc in range(8):
                kb.op("dve", lambda e: e.tensor_scalar(out=wq[j][:, kc, :], in0=w_sb[:, kc, :], scalar1=gs[:, kc, j:j + 1], scalar2=None, op0=ALU.mult),
                      reads=[w_sb, gs], writes=[wq[j]])
                kb.op("dve", lambda e: e.tensor_scalar(out=shiftbc[:, kc, :], in0=zer[:], scalar1=mod[:, kc, j:j + 1], scalar2=None, op0=ALU.add),
                      reads=[zer, mod], writes=[shiftbc])
            for half in range(2):
                pb = banks[2 + half]
                for kc in range(8):
                    kb.op("pe", lambda e: e.matmul(pb[:, :], lhsT=shiftbc[:, kc, :], rhs=w_sb[:, kc, half * 512:(half + 1) * 512], start=(kc == 0), stop=(kc == 7)),
                          reads=[shiftbc, w_sb], writes=[pb])
                kb.op("dve", lambda e: e.tensor_copy(out=bias[j][:, half * 512:(half + 1) * 512], in_=pb[:, :]), reads=[pb], writes=[bias[j]])
            kb.op("dve", lambda e: e.tensor_tensor(out=bias[j][:, 768:1024], in0=bias[j][:, 768:1024], in1=smallb[:, 0:256], op=ALU.add), reads=[bias[j], smallb], writes=[bias[j]])
        kb.barrier()
    kb.barrier()
    pesA.close()

    pesB = ExitStack()
    xt = dbl("xt", [128, 1024], F32, 2, pesB)
    junk = kb.sb("junk", [128, 1024], BF16, pesB)
    st1 = dbl("st1", [128, 4], F32, 2, pesB)
    xn = dbl("xn", [128, 1024], BF16, 2, pesB)
    xnT = dbl("xnT", [128, 1024], BF16, 2, pesB)
    pj = dbl("pj", [128, 1024], F32, 2, pesB)
    ez = dbl("ez", [128, 256], F32, 2, pesB)

    def proj_tile(i, src, row0, is_ctx, drow):
        p = i % 2
        j = 1 if is_ctx else 0
        if is_ctx:
            c = row0 // 128
            for hf in range(2):
                r = ag_row(4096, 2 * c + hf, 256, 4224)
                kb.load("sp", xt[p], xt[p][hf * 64:(hf + 1) * 64, :], x2out.h[r:r + 64, :], x2out)
                yield
        else:
            t = row0 // 128
            r = ag_row((t % 32) * 128, t // 32, 256, 4224)
            kb.load("sp", xt[p], xt[p][:], x2out.h[r:r + 128, :], x2out)
            yield
        kb.op("act", lambda e: e.activation(out=junk[:], in_=xt[p][:], func=AF.Square, accum_out=st1[p][:, 0:1]), reads=[xt[p]], writes=[junk, st1[p]])
        yield
        rstd_chain(st1[p], 0, 1, 2, 1, 1.0 / 1024)
        kb.op("act", lambda e: e.activation(out=xn[p][:], in_=xt[p][:], func=AF.Copy, scale=st1[p][:, 2:3]), reads=[xt[p], st1[p]], writes=[xn[p]])
        yield
        psT = banks[p]
        for kc in range(8):
            kb.op("pe", lambda e: e.transpose(out=bfv(psT)[:, kc * 128:(kc + 1) * 128], in_=xn[p][:, kc * 128:(kc + 1) * 128], identity=identb[:]),
                  reads=[xn[p], identb], writes=[psT])
            yield
        kb.op("dve", lambda e: e.tensor_copy(out=xnT[p][:], in_=bfv(psT)[:, 0:1024]), reads=[psT], writes=[xnT[p]])
        yield
        for half in range(2):
            pp = banks[2 + 2 * p + half]
            for kc in range(8):
                kb.op("pe", lambda e: e.matmul(pp[:, :], lhsT=xnT[p][:, kc * 128:(kc + 1) * 128], rhs=wq[j][:, kc, half * 512:(half + 1) * 512], start=(kc == 0), stop=(kc == 7)),
                      reads=[xnT[p], wq[j]], writes=[pp])
                yield
            kb.op("dve", lambda e: e.tensor_tensor(out=pj[p][:, half * 512:(half + 1) * 512], in0=pp[:, :], in1=bias[j][:, half * 512:(half + 1) * 512], op=ALU.add),
                  reads=[pp, bias[j]], writes=[pj[p]])
            yield
        kb.op("act", lambda e: e.activation(out=ez[p][:], in_=pj[p][:, 768:1024], func=AF.Exp, scale=-1.0), reads=[pj[p]], writes=[ez[p]])
        yield
        kb.op("pool", lambda e: e.tensor_scalar(out=ez[p][:], in0=ez[p][:], scalar1=1.0, scalar2=None, op0=ALU.add), reads=[ez[p]], writes=[ez[p]])
        yield
        kb.op("act", lambda e: e.activation(out=ez[p][:], in_=ez[p][:], func=AF.Ln), reads=[ez[p]], writes=[ez[p]])
        yield
        kb.op("pool", lambda e: e.tensor_scalar(out=pj[p][:, 768:1024], in0=ez[p][:], scalar1=-1.0 / 16, scalar2=None, op0=ALU.mult), reads=[ez[p]], writes=[pj[p]])
        yield
        kb.store("sp", proj, proj.h[drow:drow + 128, :], pj[p], pj[p][:])
        yield

    gens = []
    i = 0
    for c in range(2):
        gens.append(proj_tile(i, None, c * 128, True, S + c * 128))
        i += 1
    for t in range(n_lat_tiles):
        gens.append(proj_tile(i, None, t * 128, False, t * 128))
        i += 1
    interleave(gens, 2)
    kb.barrier()
    pesB.close()

    mask = []
    for d in range(2):
        mf = kb.sb("mask%d" % d, [128, 128], F32)
        kb.op("pool", lambda e: e.memset(mf[:], 1.0), writes=[mf])
        if d == 0:
            kb.op("pool", lambda e: e.affine_select(out=mf[:], in_=mf[:], pattern=[[1, 128]], compare_op=ALU.is_ge, fill=0.0, base=0, channel_multiplier=-1), reads=[mf], writes=[mf])
        else:
            kb.op("pool", lambda e: e.affine_select(out=mf[:], in_=mf[:], pattern=[[-1, 128]], compare_op=ALU.is_ge, fill=0.0, base=0, channel_multiplier=1), reads=[mf], writes=[mf])
        mask.append(mf)
    ones_f = kb.sb("ones_f", [128, 128], F32)
    kb.op("pool", lambda e: e.memset(ones_f[:], 1.0), writes=[ones_f])
    Sst = kb.sb("Sst", [128, 256], F32)
    Sb = dbl("Sb", [128, 256], BF16)
    pt = dbl("pt", [128, 1024], F32, 3)
    bc = dbl("bc", [128, 128], F32)
    eb = dbl("eb", [128, 128], F32)
    enb = dbl("enb", [128, 128], F32)
    dlt = dbl("dlt", [128, 128], F32)
    dec = dbl("dec", [128, 1], F32)
    qt = dbl("qt", [128, 128], BF16)
    ktl = dbl("ktl", [128, 128], BF16)
    kh = dbl("kh", [128, 128], BF16)
    vb = dbl("vb", [128, 256], BF16)
    qkT = dbl("qkT", [128, 256], BF16)
    attm = dbl("attm", [128, 128], BF16)
    ofs = dbl("ofs", [128, 256], F32)
    osum = dbl("osum", [128, 256], F32)
    fst = dbl("fst", [128, 4], F32)
    sg = dbl("sg", [128, 256], F32)
    ogb = dbl("ogb", [128, 256], BF16)
    ogT_sb = dbl("ogT_sb", [128, 2, 128], BF16)
    QS = 128.0 ** -0.5
    step = [0]

    def gla_prep(c, row, d):
        p = c % 2
        B = banks[4 * p:4 * p + 4]
        t_ = pt[c % 3]
        kb.load("sp", t_, t_[:], proj.h[row:row + 128, :], proj)
        yield
        la = t_[:, 768 + d * 128:768 + (d + 1) * 128]
        kb.op("pe", lambda e: e.matmul(B[0][:, 0:128], lhsT=mask[d][:], rhs=la, start=True, stop=True), reads=[mask[d], t_], writes=[B[0]])
        yield
        kb.op("pe", lambda e: e.matmul(B[0][:, 128:256], lhsT=ones_f[:], rhs=la, start=True, stop=True), reads=[ones_f, t_], writes=[B[0]])
        yield
        kb.op("pe", lambda e: e.matmul(B[0][:, 256:384], lhsT=la, rhs=ones_f[:], start=True, stop=True), reads=[ones_f, t_], writes=[B[0]])
        yield
        kb.op("act", lambda e: e.copy(out=bc[p][:], in_=B[0][:, 0:128]), reads=[B[0]], writes=[bc[p]])
        yield
        kb.op("act", lambda e: e.activation(out=eb[p][:], in_=B[0][:, 0:128], func=AF.Exp), reads=[B[0]], writes=[eb[p]])
        yield
        kb.op("act", lambda e: e.activation(out=enb[p][:], in_=B[0][:, 0:128], func=AF.Exp, scale=-1.0), reads=[B[0]], writes=[enb[p]])
        yield
        kb.op("dve", lambda e: e.tensor_tensor(out=dlt[p][:], in0=B[0][:, 128:256], in1=bc[p][:], op=ALU.subtract), reads=[B[0], bc[p]], writes=[dlt[p]])
        yield
        kb.op("act", lambda e: e.activation(out=dlt[p][:], in_=dlt[p][:], func=AF.Exp), reads=[dlt[p]], writes=[dlt[p]])
        yield
        kb.op("act", lambda e: e.activation(out=dec[p][:], in_=B[0][:, 256:257], func=AF.Exp), reads=[B[0]], writes=[dec[p]])
        yield
        kb.op("dve", lambda e: e.scalar_tensor_tensor(out=qt[p][:], in0=t_[:, 0:128], scalar=QS, in1=eb[p][:], op0=ALU.mult, op1=ALU.mult), reads=[t_, eb[p]], writes=[qt[p]])
        yield
        kb.op("dve", lambda e: e.tensor_tensor(out=ktl[p][:], in0=t_[:, 128:256], in1=enb[p][:], op=ALU.mult), reads=[t_, enb[p]], writes=[ktl[p]])
        yield
        kb.op("pool", lambda e: e.tensor_tensor(out=kh[p][:], in0=t_[:, 128:256], in1=dlt[p][:], op=ALU.mult), reads=[t_, dlt[p]], writes=[kh[p]])
        yield
        kb.op("pool", lambda e: e.tensor_copy(out=vb[p][:], in_=t_[:, 256:512]), reads=[t_], writes=[vb[p]])
        yield
        kb.op("pe", lambda e: e.transpose(out=bfv(B[1])[:, 0:128], in_=qt[p][:], identity=identb[:]), reads=[qt[p], identb], writes=[B[1]])
        yield
        kb.op("pe", lambda e: e.transpose(out=bfv(B[1])[:, 128:256], in_=ktl[p][:], identity=identb[:]), reads=[ktl[p], identb], writes=[B[1]])
        yield
        kb.op("act", lambda e: e.copy(out=qkT[p][:], in_=bfv(B[1])[:, 0:256]), reads=[B[1]], writes=[qkT[p]])
        yield
        kb.op("pe", lambda e: e.matmul(B[2][:, 0:128], lhsT=qkT[p][:, 128:256], rhs=qkT[p][:, 0:128], start=True, stop=True), reads=[qkT[p]], writes=[B[2]])
        yield
        kb.op("dve", lambda e: e.tensor_tensor(out=attm[p][:], in0=B[2][:, 0:128], in1=mask[d][:], op=ALU.mult), reads=[B[2], mask[d]], writes=[attm[p]])
        yield

    def gla_fin(c, d, out_mode, out_row):
        p = c % 2
        B = banks[4 * p:4 * p + 4]
        t_ = pt[c % 3]
        sb_cur = Sb[c % 2]
        sb_next = Sb[(c + 1) % 2]
        if out_mode is not None:
            kb.op("pe", lambda e: e.matmul(B[3][:, 0:256], lhsT=qkT[p][:, 0:128], rhs=sb_cur[:], start=True, stop=False), reads=[qkT[p], sb_cur], writes=[B[3]])
            yield
            kb.op("pe", lambda e: e.matmul(B[3][:, 0:256], lhsT=attm[p][:], rhs=vb[p][:], start=False, stop=True), reads=[attm[p], vb[p]], writes=[B[3]])
            yield
        kb.op("pe", lambda e: e.matmul(B[2][:, 128:384], lhsT=kh[p][:], rhs=vb[p][:], start=True, stop=True), reads=[kh[p], vb[p]], writes=[B[2]])
        yield
        kb.op("dve", lambda e: e.scalar_tensor_tensor(out=Sst[:], in0=Sst[:], scalar=dec[p][:, 0:1], in1=B[2][:, 128:384], op0=ALU.mult, op1=ALU.add),
              reads=[Sst, dec[p], B[2]], writes=[Sst])
        yield
        kb.op("act", lambda e: e.copy(out=sb_next[:], in_=Sst[:]), reads=[Sst], writes=[sb_next])
        yield
        if out_mode == "store":
            kb.op("act", lambda e: e.copy(out=ofs[p][:], in_=B[3][:, 0:256]), reads=[B[3]], writes=[ofs[p]])
            yield
            kb.store("sp", of_d, of_d.h[out_row:out_row + 128, :], ofs[p], ofs[p][:])
            yield
        elif out_mode == "final":
            kb.load("sp", ofs[p], ofs[p][:], of_d.h[out_row:out_row + 128, :], of_d)
            yield
            kb.op("dve", lambda e: e.tensor_tensor(out=osum[p][:], in0=B[3][:, 0:256], in1=ofs[p][:], op=ALU.add), reads=[B[3], ofs[p]], writes=[osum[p]])
            yield
            kb.op("act", lambda e: e.activation(out=sg[p][:], in_=osum[p][:], func=AF.Square, accum_out=fst[p][:, 0:1]), reads=[osum[p]], writes=[sg[p], fst[p]])
            yield
            rstd_chain(fst[p], 0, 1, 2, 1, 1.0 / 256)
            kb.op("dve", lambda e: e.scalar_tensor_tensor(out=osum[p][:], in0=osum[p][:], scalar=fst[p][:, 2:3], in1=smallb[:, 256:512], op0=ALU.mult, op1=ALU.mult),
                  reads=[osum[p], fst[p], smallb], writes=[osum[p]])
            yield
            kb.op("act", lambda e: e.activation(out=sg[p][:], in_=t_[:, 512:768], func=AF.Silu), reads=[t_], writes=[sg[p]])
            yield
            kb.op("dve", lambda e: e.tensor_tensor(out=ogb[p][:], in0=osum[p][:], in1=sg[p][:], op=ALU.mult), reads=[osum[p], sg[p]], writes=[ogb[p]])
            yield
            for hh in range(2):
                kb.op("pe", lambda e: e.transpose(out=bfv(B[1])[:, 256 + hh * 128:256 + (hh + 1) * 128], in_=ogb[p][:, hh * 128:(hh + 1) * 128], identity=identb[:]),
                      reads=[ogb[p], identb], writes=[B[1]])
                yield
            kb.op("act", lambda e: e.copy(out=ogT_sb[p][:].rearrange("p a b -> p (a b)"), in_=bfv(B[1])[:, 256:512]), reads=[B[1]], writes=[ogT_sb[p]])
            yield
            tq_, tc_ = (out_row // 128) // 32, ((out_row // 128) % 32) * 128
            kb.store("sp", x3in, x3in.h[tq_ * 256:(tq_ + 1) * 256, tc_:tc_ + 128].rearrange("(a p) n -> p a n", p=128), ogT_sb[p], ogT_sb[p][:])
            yield

    def reset_state(c):
        kb.op("pool", lambda e: e.memset(Sst[:], 0.0), writes=[Sst])
        kb.op("pool", lambda e: e.memset(Sb[c % 2][:], 0.0), writes=[Sb[c % 2]])

    def run_scan(chunks, c0):
        n = len(chunks)
        for _ in gla_prep(c0, chunks[0][0], chunks[0][1]):
            pass
        for k in range(n):
            row, d, om, orow = chunks[k]
            gens = [gla_fin(c0 + k, d, om, orow)]
            if k + 1 < n:
                gens.append(gla_prep(c0 + k + 1, chunks[k + 1][0], chunks[k + 1][1]))
            interleave(gens, 2)
        return c0 + n

    fwd = [(S + c * 128, 0, None, None) for c in range(2)] + [(t * 128, 0, "store", t * 128) for t in range(n_lat_tiles)]
    bwd = [(S + c * 128, 1, None, None) for c in (1, 0)] + [(t * 128, 1, "final", t * 128) for t in range(n_lat_tiles - 1, -1, -1)]
    reset_state(0)
    cn = run_scan(fwd, 0)
    kb.barrier()
    reset_state(cn)
    run_scan(bwd, cn)
    print("l1a instructions:", kb.n_ins, "sems:", len(kb.sems))
    kb.end_stage()


def fop(v, n):
    return np.ascontiguousarray(np.asarray(v, np.float32).reshape(n, 128).T)


def host_l1a(inp):
    maps = []
    wi = inp["gla_w_in"][0]
    for b in range(2):
        sv = np.stack([inp["c"][b], inp["c_ctx"]], -1).reshape(8, 128, 2).transpose(1, 0, 2).reshape(128, 16).astype(np.float32)
        for h in range(4):
            w = np.concatenate([wi[:, h * 128:(h + 1) * 128], wi[:, 512 + h * 128:512 + (h + 1) * 128], wi[:, 1024 + h * 256:1024 + (h + 1) * 256],
                                wi[:, 2048 + h * 256:2048 + (h + 1) * 256]], axis=1)
            waT = np.ascontiguousarray(wi[:, 3072:3104].T.reshape(2, 16, 1024))
            wa2 = np.ascontiguousarray(inp["gla_w_a2"][0][:, :, h * 128:(h + 1) * 128])
            small = np.concatenate([inp["gla_b_a2"][0][0, h * 128:(h + 1) * 128], inp["gla_b_a2"][0][1, h * 128:(h + 1) * 128], inp["gla_norm_g"][0]]).astype(np.float32)
            maps.append({"svec": np.ascontiguousarray(sv),
                         "adaw": np.ascontiguousarray(inp["ada_w"][1][:, 0:2048]), "adab": fop(inp["ada_b"][1][0:2048], 16), "g1": fop(inp["norm1_g"][1], 8),
                         "w": np.ascontiguousarray(w), "waT": waT, "wa2": wa2, "small": small})
    return maps


RG = [[0, 1, 2, 3], [4, 5, 6, 7]]


def build_all():
    kb = KB()
    banks = [kb.ps("bank%d" % i) for i in range(8)]
    x1in = kb.dram("x1in", [512, 4224], BF16)
    x1out = kb.dram("x1out", [2048, 4224], BF16)
    x2in = kb.dram("x2in", [4224, 1024], F32)
    x2out = kb.dram("x2out", [4 * 4224, 1024], F32)
    x3in = kb.dram("x3in", [1024, 4096], BF16)
    x3out = kb.dram("x3out", [4096, 4096], BF16)
    build_l0a(kb, banks, x1in)
    kb.all_gather(x1in, x1out, RG, 64)
    build_b(0, kb, banks, x1out, None, x2in)
    kb.all_gather(x2in, x2out, RG, 256)
    build_l1a(kb, banks, x2out, x3in)
    kb.all_gather(x3in, x3out, RG, 128)
    build_b(1, kb, banks, x3out, x2in, None)
    print("total instructions:", kb.n_ins, "sems:", len(kb.sems))
    return kb.finish()


def kernel(**inputs):
    inp = {k: np.asarray(v) for k, v in inputs.items()}
    parts = [("a0_", host_l0a(inp)), ("b0_", host_b(0, inp)), ("a1_", host_l1a(inp)), ("b1_", host_b(1, inp))]
    maps = []
    for c in range(8):
        m = {}
        for pre, ms in parts:
            for k, v in ms[c].items():
                m[pre + k] = v
        maps.append(m)
    nc = build_all()
    res = run_bass_kernel_spmd(nc, maps, core_ids=list(range(8)))
    out = np.zeros((2, 16384, 1024), np.float32)
    for b in range(2):
        for jq in range(4):
            out[b, jq * 4096:(jq + 1) * 4096] = np.asarray(res.results[b * 4 + jq]["b1_hout"])[:4096]
    return out
```

```python
import numpy as np
from contextlib import ExitStack
import concourse.bass as bass
import concourse.mybir as mybir
from concourse.bass_utils import run_bass_kernel_spmd
import ml_dtypes

F32 = mybir.dt.float32
BF16 = mybir.dt.bfloat16
I32 = mybir.dt.int32
AF = mybir.ActivationFunctionType
ALU = mybir.AluOpType
AX = mybir.AxisListType
NPBF16 = ml_dtypes.bfloat16


def interleave(gens, width):
    active = []
    it = iter(gens)
    while True:
        while len(active) < width:
            g = next(it, None)
            if g is None:
                break
            active.append(g)
        if not active:
            break
        for g in list(active):
            try:
                next(g)
            except StopIteration:
                active.remove(g)


def ag_row(i, rank, chunk_rows, total_rows, world=4):
    r0 = (i // chunk_rows) * chunk_rows
    n = min(chunk_rows, total_rows - r0)
    return world * r0 + rank * n + (i - r0)


class T:
    def __init__(self, h, name, kind):
        self.h = h
        self.name = name
        self.kind = kind
        self.w = None
        self.r = {}
        self.dkey = None

    def __getitem__(self, idx):
        return self.h[idx]


class KB:
    def __init__(self):
        self.nc = bass.Bass("TRN2", target_bir_lowering=False)
        nc = self.nc
        self.es = ExitStack()
        self.eng = {"pe": nc.tensor, "act": nc.scalar, "dve": nc.vector, "pool": nc.gpsimd, "sp": nc.sync}
        self.sems = {}
        self.cnt = {}
        self.seen = {e: {} for e in self.eng}
        for e in self.eng:
            self.sems[e] = self.es.enter_context(nc.semaphore("e_" + e))
            self.cnt[e] = 0
        self.issued = {}
        self.n_ins = 0
        self.outs = []
        self._uid = 0
        self.cur = self.es
        self.prefix = ""
        self.tiles = []
        self.free_dsems = []
        self.stage_tiles0 = 0

    def sb(self, name, shape, dt, es=None):
        h = (es or self.cur).enter_context(self.nc.sbuf_tensor(self.prefix + name, list(shape), dt))
        t = T(h, name, "sb")
        self.tiles.append(t)
        return t

    def ps(self, name, shape=(128, 512), dt=F32):
        h = self.es.enter_context(self.nc.psum_tensor(name, list(shape), dt))
        t = T(h, name, "ps")
        self.tiles.append(t)
        return t

    def dram(self, name, shape, dt, kind="Internal"):
        h = self.nc.dram_tensor(self.prefix + name, list(shape), dt, kind=kind)
        t = T(h.ap(), name, "dram")
        self.tiles.append(t)
        if kind == "ExternalOutput":
            self.outs.append(t)
        return t

    def _dsem(self, t):
        if t.dkey is None:
            self._uid += 1
            t.dkey = "d%d_%s" % (self._uid, t.name)
            if self.free_dsems:
                h, v = self.free_dsems.pop()
                self.sems[t.dkey] = h
                self.issued[t.dkey] = v
            else:
                self.sems[t.dkey] = self.es.enter_context(self.nc.semaphore(t.dkey))
                self.issued[t.dkey] = 0
        return t.dkey

    def begin_stage(self, prefix):
        self.prefix = prefix
        self.cur = ExitStack()
        self.stage_tiles0 = len(self.tiles)

    def end_stage(self):
        self.barrier()
        self.cur.close()
        self.cur = self.es
        for t in self.tiles[self.stage_tiles0:]:
            if t.kind == "sb" and t.dkey is not None:
                self.free_dsems.append((self.sems[t.dkey], self.issued[t.dkey]))
                del self.issued[t.dkey]
                del self.sems[t.dkey]
                t.dkey = None
        for t in self.tiles:
            t.w = None
            t.r = {}
        for e in self.eng:
            self._uid += 1
            self.sems[e] = self.es.enter_context(self.nc.semaphore("e%d_%s" % (self._uid, e)))
            self.cnt[e] = 0
        self.seen = {e: {} for e in self.eng}
        self.prefix = ""

    def all_gather(self, src, dst, groups, chunk_rows):
        self.barrier()
        self._uid += 1
        sem = self.es.enter_context(self.nc.semaphore("cc%d" % self._uid))
        R = src.h.shape[0]
        k = 0
        for r0 in range(0, R, chunk_rows):
            n = min(chunk_rows, R - r0)
            self.nc.gpsimd.collective_compute("AllGather", ALU.bypass, replica_groups=groups, ins=[src.h[r0:r0 + n, :]],
                                              outs=[dst.h[4 * r0:4 * r0 + 4 * n, :]]).then_inc(sem, 1)
            k += 1
        self.nc.gpsimd.wait_ge(sem, k)
        if not hasattr(self, "_fence"):
            self._fence = T(self.es.enter_context(self.nc.sbuf_tensor("cc_fence", [128, 8], F32)), "cc_fence", "sb")
            self.tiles.append(self._fence)
        f = self._fence
        self.op("pool", lambda e: e.memset(f[:], 0.0), writes=[f])
        for en in self.eng:
            if en != "pool":
                self._waits(en, {"pool": self.cnt["pool"]})
        self.n_ins += k + 1

    def _deps(self, en, reads, writes, is_dma=False):
        deps = {}

        def add(key, val, kind):
            if key == en:
                if en == "pe" or kind == "war":
                    return
            if is_dma and kind == "waw" and key in self.issued:
                return
            deps[key] = max(deps.get(key, 0), val)

        for t in reads:
            if t.w is not None:
                add(t.w[0], t.w[1], "raw")
            if t.kind == "ps":
                for k, v in t.r.items():
                    if k != en:
                        add(k, v, "rar")
        for t in writes:
            if t.w is not None:
                add(t.w[0], t.w[1], "waw")
            for k, v in t.r.items():
                add(k, v, "war")
        return deps

    def _waits(self, en, deps):
        e = self.eng[en]
        for key, val in deps.items():
            if key in self.issued:
                val = self.issued[key]
            if self.seen[en].get(key, 0) >= val:
                continue
            e.wait_ge(self.sems[key], val)
            self.seen[en][key] = val
            self.n_ins += 1

    def op(self, en, fn, reads=(), writes=()):
        self._waits(en, self._deps(en, reads, writes))
        ins = fn(self.eng[en])
        self.cnt[en] += 1
        self.n_ins += 1
        ins.then_inc(self.sems[en], 1)
        c = self.cnt[en]
        for t in reads:
            t.r[en] = c
        for t in writes:
            t.w = (en, c)
            t.r = {}
        return ins

    def dma(self, q, fn, reads=(), writes=()):
        self._waits(q, self._deps(q, reads, writes, is_dma=True))
        cand = [t for t in writes if t.kind != "dram"] or [t for t in reads if t.kind != "dram"] or list(writes) or list(reads)
        key = self._dsem(cand[0])
        ins = fn(self.eng[q])
        self.issued[key] += 16
        self.n_ins += 1
        ins.then_inc(self.sems[key], 16)
        v = self.issued[key]
        for t in reads:
            t.r[key] = v
        for t in writes:
            t.w = (key, v)
            t.r = {}
        return ins

    def load(self, q, dst_t, dst_ap, src_ap, src_t=None, **kw):
        return self.dma(q, lambda e: e.dma_start(out=dst_ap, in_=src_ap, **kw),
                        reads=[src_t] if src_t is not None else [], writes=[dst_t])

    def store(self, q, dst_t, dst_ap, src_t, src_ap, **kw):
        return self.dma(q, lambda e: e.dma_start(out=dst_ap, in_=src_ap, **kw), reads=[src_t], writes=[dst_t])

    def finish(self):
        deps = {}
        for t in self.outs:
            if t.w is not None:
                deps[t.w[0]] = max(deps.get(t.w[0], 0), t.w[1])
        self._waits("sp", deps)
        self.es.close()
        return self.nc

    def barrier(self):
        for en in self.eng:
            deps = {}
            for k in self.eng:
                if k != en and self.cnt[k] > 0:
                    deps[k] = self.cnt[k]
            for k, v in self.issued.items():
                if v > 0:
                    deps[k] = v
            self._waits(en, deps)

    def identity(self, name, dt):
        f = self.sb(name + "_f", [128, 128], F32)
        self.op("pool", lambda e: e.memset(f[:], 0.0), writes=[f])
        self.op("pool", lambda e: e.affine_select(out=f[:], in_=f[:], pattern=[[-1, 128]], compare_op=ALU.not_equal,
                                                  fill=1.0, base=0, channel_multiplier=1), reads=[f], writes=[f])
        if dt == F32:
            return f
        b = self.sb(name, [128, 128], dt)
        self.op("pool", lambda e: e.tensor_copy(out=b[:], in_=f[:]), reads=[f], writes=[b])
        return b

EPS = 1e-6
S = 16384
LC = 256

NKT = (S + LC) // 128
ATT_NSPLIT = 512


def build_l0a(kb, banks, x1in, n_groups=32, debug=False):
    kb.begin_stage("a0_")
    x = kb.dram("x", [S, 1024], F32, "ExternalInput")
    ctx = kb.dram("ctx", [LC, 1024], F32, "ExternalInput")
    svec = kb.dram("svec", [128, 16], F32, "ExternalInput")
    adaw = kb.dram("adaw", [1024, 2048], F32, "ExternalInput")
    adab = kb.dram("adab", [128, 16], F32, "ExternalInput")
    g1 = kb.dram("g1", [128, 8], F32, "ExternalInput")
    w = kb.dram("w", [1024, 384], F32, "ExternalInput")
    small = kb.dram("small", [640], F32, "ExternalInput")
    cos4 = kb.dram("cos4", [S, 256], F32, "ExternalInput")
    sin4 = kb.dram("sin4", [S, 256], F32, "ExternalInput")
    zpad = kb.sb("zpad", [128, 4, 64], BF16)
    kb.op("pool", lambda e: e.memset(zpad[:], 0.0), writes=[zpad])
    kb.store("sp", x1in, x1in.h[:, 4160:4224].rearrange("(q p) n -> p q n", p=128), zpad, zpad[:])

    def bfv(t):
        return t[:].bitcast(BF16)

    identb = kb.identity("identb", BF16)
    smallb = kb.sb("smallb", [128, 640], F32)
    kb.load("sp", smallb, smallb[:], small.h.partition_broadcast(128), small)

    tmp64 = kb.sb("tmp64", [128, 2, 64], F32)
    dots = kb.sb("dots", [128, 4], F32)
    kb.op("dve", lambda e: e.tensor_tensor(out=tmp64[:, 0, :], in0=smallb[:, 256:320], in1=smallb[:, 320:384], op=ALU.mult), reads=[smallb], writes=[tmp64])
    kb.op("dve", lambda e: e.tensor_tensor(out=tmp64[:, 1, :], in0=smallb[:, 384:448], in1=smallb[:, 448:512], op=ALU.mult), reads=[smallb], writes=[tmp64])
    kb.op("dve", lambda e: e.tensor_reduce(out=dots[:, 0:2], in_=tmp64[:], axis=AX.X, op=ALU.add), reads=[tmp64], writes=[dots])
    kb.op("act", lambda e: e.activation(out=dots[:, 2:4], in_=dots[:, 0:2], func=AF.Exp), reads=[dots], writes=[dots])
    neglam = kb.sb("neglam", [128, 1], F32)
    kb.op("dve", lambda e: e.scalar_tensor_tensor(out=neglam[:], in0=dots[:, 3:4], scalar=-0.2, in1=dots[:, 2:3], op0=ALU.add, op1=ALU.subtract), reads=[dots], writes=[neglam])
    subg_s = kb.sb("subg_s", [128, 128], F32)
    kb.op("dve", lambda e: e.tensor_scalar(out=subg_s[:], in0=smallb[:, 512:640], scalar1=0.8, scalar2=None, op0=ALU.mult), reads=[smallb], writes=[subg_s])

    s_sb = kb.sb("s_sb", [128, 16], F32)
    kb.load("sp", s_sb, s_sb[:], svec.h, svec)
    kb.op("act", lambda e: e.activation(out=s_sb[:], in_=s_sb[:], func=AF.Silu), reads=[s_sb], writes=[s_sb])
    adab_sb = kb.sb("adab_sb", [128, 16], F32)
    kb.load("sp", adab_sb, adab_sb[:], adab.h, adab)
    g1_sb = kb.sb("g1_sb", [128, 8], F32)
    kb.load("sp", g1_sb, g1_sb[:], g1.h, g1)
    mod = kb.sb("mod", [128, 16, 2], F32)
    gs = kb.sb("gs", [128, 8, 2], F32)
    zer = kb.sb("zer", [128, 128], F32)
    wq = [kb.sb("wq%d" % j, [128, 8, 384], BF16) for j in range(2)]
    bias = [kb.sb("bias%d" % j, [128, 384], F32) for j in range(2)]
    p0 = ExitStack()
    adaw_sb = kb.sb("adaw_sb", [128, 8, 512], F32, p0)
    pm = banks[0]
    for v in range(4):
        kb.load("sp", adaw_sb, adaw_sb[:], adaw.h[:, v * 512:(v + 1) * 512].rearrange("(kc p) n -> p kc n", p=128), adaw)
        for oc in range(4):
            g = v * 4 + oc
            for kc in range(8):
                kb.op("pe", lambda e: e.matmul(pm[:, g * 2:g * 2 + 2], lhsT=adaw_sb[:, kc, oc * 128:(oc + 1) * 128],
                                              rhs=s_sb[:, kc * 2:kc * 2 + 2], start=(kc == 0), stop=(kc == 7)),
                      reads=[adaw_sb, s_sb], writes=[pm])
    pm3 = pm[:, 0:32].rearrange("p (g j) -> p g j", j=2)
    for j in range(2):
        kb.op("dve", lambda e: e.tensor_tensor(out=mod[:, :, j], in0=pm3[:, :, j], in1=adab_sb[:], op=ALU.add), reads=[pm, adab_sb], writes=[mod])
        kb.op("dve", lambda e: e.scalar_tensor_tensor(out=gs[:, :, j], in0=mod[:, 8:16, j], scalar=1.0, in1=g1_sb[:], op0=ALU.add, op1=ALU.mult),
              reads=[mod, g1_sb], writes=[gs])

    w_sb = kb.sb("w_sb", [128, 8, 384], F32, p0)
    kb.load("sp", w_sb, w_sb[:], w.h.rearrange("(kc p) n -> p kc n", p=128), w)
    kb.op("pool", lambda e: e.memset(zer[:], 0.0), writes=[zer])
    shiftbc = kb.sb("shiftbc", [128, 8, 128], F32, p0)
    for j in range(2):
        for kc in range(8):
            kb.op("dve", lambda e: e.tensor_scalar(out=wq[j][:, kc, :], in0=w_sb[:, kc, :], scalar1=gs[:, kc, j:j + 1], scalar2=None, op0=ALU.mult),
                  reads=[w_sb, gs], writes=[wq[j]])
            kb.op("dve", lambda e: e.tensor_scalar(out=shiftbc[:, kc, :], in0=zer[:], scalar1=mod[:, kc, j:j + 1], scalar2=None, op0=ALU.add),
                  reads=[zer, mod], writes=[shiftbc])
        pb = banks[1]
        for kc in range(8):
            kb.op("pe", lambda e: e.matmul(pb[:, 0:384], lhsT=shiftbc[:, kc, :], rhs=w_sb[:, kc, :], start=(kc == 0), stop=(kc == 7)),
                  reads=[shiftbc, w_sb], writes=[pb])
        kb.op("dve", lambda e: e.tensor_copy(out=bias[j][:], in_=pb[:, 0:384]), reads=[pb], writes=[bias[j]])

    kb.barrier()
    p0.close()
    QT = kb.sb("QT", [128, S + LC], BF16)
    KTm = [kb.sb("KT%d" % m, [128, S + LC], BF16) for m in range(2)]
    kb.op("pool", lambda e: e.memset(KTm[0][64:128, :], 0.0), writes=[KTm[0]])
    kb.op("pool", lambda e: e.memset(KTm[1][0:64, :], 0.0), writes=[KTm[1]])
    Vx = kb.sb("Vx", [128, NKT, 129], BF16)
    kb.op("pool", lambda e: e.memset(Vx[:, :, 128:129], 1.0), writes=[Vx])

    def dbl(name, shape, dt, n=2, es=None):
        return [kb.sb("%s%d" % (name, i), shape, dt, es) for i in range(n)]

    p2 = ExitStack()

    xt = dbl("xt", [128, 1024], F32, 2, p2)
    junk = kb.sb("junk", [128, 1024], BF16, p2)
    st1 = dbl("st1", [128, 4], F32, 2, p2)
    xn = dbl("xn", [128, 1024], BF16, 2, p2)
    xnT = dbl("xnT", [128, 1024], BF16, 2, p2)
    qkv = dbl("qkv", [128, 384], F32, 2, p2)
    cs = dbl("cs", [128, 256], F32, 2, p2)
    sn = dbl("sn", [128, 256], F32, 2, p2)
    sq = dbl("sq", [128, 256], F32, 2, p2)
    st2 = dbl("st2", [128, 12], F32, 2, p2)
    qkn = dbl("qkn", [128, 256], F32, 2, p2)
    sw = dbl("sw", [128, 256], F32, 2, p2)
    t1 = dbl("t1", [128, 256], F32, 2, p2)
    rr = dbl("rr", [128, 256], BF16, 2, p2)

    def rstd_chain(stt, c_in, c_tmp, c_out, n, inv_n, srcs):
        kb.op("dve", lambda e: e.tensor_scalar(out=stt[:, c_tmp:c_tmp + n], in0=stt[:, c_in:c_in + n], scalar1=inv_n, scalar2=EPS, op0=ALU.mult, op1=ALU.add),
              reads=[stt], writes=[stt])
        kb.op("act", lambda e: e.activation(out=stt[:, c_tmp:c_tmp + n], in_=stt[:, c_tmp:c_tmp + n], func=AF.Sqrt), reads=[stt], writes=[stt])
        kb.op("dve", lambda e: e.reciprocal(out=stt[:, c_out:c_out + n], in_=stt[:, c_tmp:c_tmp + n]), reads=[stt], writes=[stt])

    def proj_tile(i, src, row0, is_ctx, qcol, kcol, kt):
        p = i % 2
        j = 1 if is_ctx else 0
        kb.load("sp", xt[p], xt[p][:], src.h[row0:row0 + 128, :], src)
        yield
        if not is_ctx:
            kb.load("pool", cs[p], cs[p][:], cos4.h[row0:row0 + 128, :], cos4)
            yield
            kb.load("pool", sn[p], sn[p][:], sin4.h[row0:row0 + 128, :], sin4)
            yield
        kb.op("act", lambda e: e.activation(out=junk[:], in_=xt[p][:], func=AF.Square, accum_out=st1[p][:, 0:1]), reads=[xt[p]], writes=[junk, st1[p]])
        yield
        rstd_chain(st1[p], 0, 1, 2, 1, 1.0 / 1024, None)
        kb.op("act", lambda e: e.activation(out=xn[p][:], in_=xt[p][:], func=AF.Copy, scale=st1[p][:, 2:3]), reads=[xt[p], st1[p]], writes=[xn[p]])
        yield
        psT = banks[p]
        for kc in range(8):
            kb.op("pe", lambda e: e.transpose(out=bfv(psT)[:, kc * 128:(kc + 1) * 128], in_=xn[p][:, kc * 128:(kc + 1) * 128], identity=identb[:]),
                  reads=[xn[p], identb], writes=[psT])
            yield
        kb.op("dve", lambda e: e.tensor_copy(out=xnT[p][:], in_=bfv(psT)[:, 0:1024]), reads=[psT], writes=[xnT[p]])
        yield
        pp = banks[2 + p]
        for kc in range(8):
            kb.op("pe", lambda e: e.matmul(pp[:, 0:384], lhsT=xnT[p][:, kc * 128:(kc + 1) * 128], rhs=wq[j][:, kc, :], start=(kc == 0), stop=(kc == 7)),
                  reads=[xnT[p], wq[j]], writes=[pp])
            yield
        kb.op("dve", lambda e: e.tensor_tensor(out=qkv[p][:], in0=pp[:, 0:384], in1=bias[j][:], op=ALU.add), reads=[pp, bias[j]], writes=[qkv[p]])
        yield
        kb.op("pool", lambda e: e.tensor_copy(out=Vx[:, kt, 0:128], in_=qkv[p][:, 256:384]), reads=[qkv[p]], writes=[Vx])
        yield
        kb.op("act", lambda e: e.activation(out=sq[p][:], in_=qkv[p][:, 0:256], func=AF.Square), reads=[qkv[p]], writes=[sq[p]])
        yield
        kb.op("dve", lambda e: e.tensor_reduce(out=st2[p][:, 0:4], in_=sq[p][:].rearrange("p (g d) -> p g d", g=4), axis=AX.X, op=ALU.add),
              reads=[sq[p]], writes=[st2[p]])
        yield
        rstd_chain(st2[p], 0, 4, 8, 4, 1.0 / 64, None)
        for g in range(4):
            kb.op("dve", lambda e: e.scalar_tensor_tensor(out=qkn[p][:, g * 64:(g + 1) * 64], in0=qkv[p][:, g * 64:(g + 1) * 64], scalar=st2[p][:, 8 + g:9 + g],
                                                          in1=smallb[:, g * 64:(g + 1) * 64], op0=ALU.mult, op1=ALU.mult),
                  reads=[qkv[p], st2[p], smallb], writes=[qkn[p]])
            yield
        if is_ctx:
            kb.op("pool", lambda e: e.tensor_copy(out=rr[p][:], in_=qkn[p][:]), reads=[qkn[p]], writes=[rr[p]])
            yield
        else:
            q5 = qkn[p][:].rearrange("p (a h d) -> p a h d", h=2, d=16)
            s5 = sw[p][:].rearrange("p (a h d) -> p a h d", h=2, d=16)
            kb.op("pool", lambda e: e.tensor_copy(out=s5[:, :, 0, :], in_=q5[:, :, 1, :]), reads=[qkn[p]], writes=[sw[p]])
            yield
            kb.op("pool", lambda e: e.tensor_copy(out=s5[:, :, 1, :], in_=q5[:, :, 0, :]), reads=[qkn[p]], writes=[sw[p]])
            yield
            kb.op("pool", lambda e: e.tensor_tensor(out=sw[p][:], in0=sw[p][:], in1=sn[p][:], op=ALU.mult), reads=[sw[p], sn[p]], writes=[sw[p]])
            yield
            kb.op("dve", lambda e: e.tensor_tensor(out=t1[p][:], in0=qkn[p][:], in1=cs[p][:], op=ALU.mult), reads=[qkn[p], cs[p]], writes=[t1[p]])
            yield
            kb.op("dve", lambda e: e.tensor_tensor(out=rr[p][:], in0=t1[p][:], in1=sw[p][:], op=ALU.add), reads=[t1[p], sw[p]], writes=[rr[p]])
            yield
        pq = banks[4 + p]
        for hh in range(2):
            kb.op("pe", lambda e: e.transpose(out=bfv(pq)[:, hh * 128:(hh + 1) * 128], in_=rr[p][:, hh * 128:(hh + 1) * 128], identity=identb[:]),
                  reads=[rr[p], identb], writes=[pq])
            yield
        kb.op("act", lambda e: e.copy(out=QT[:, qcol:qcol + 128], in_=bfv(pq)[:, 0:128]), reads=[pq], writes=[QT])
        yield
        kb.op("act", lambda e: e.copy(out=KTm[0][0:64, kcol:kcol + 128], in_=bfv(pq)[0:64, 128:256]), reads=[pq], writes=[KTm[0]])
        kb.op("act", lambda e: e.copy(out=KTm[1][64:128, kcol:kcol + 128], in_=bfv(pq)[64:128, 128:256]), reads=[pq], writes=[KTm[1]])
        yield

    gens = []
    i = 0
    for c in range(LC // 128):
        gens.append(proj_tile(i, ctx, c * 128, True, S + c * 128, c * 128, c))
        i += 1
    for t in range(S // 128):
        gens.append(proj_tile(i, x, t * 128, False, t * 128, LC + t * 128, LC // 128 + t))
        i += 1
    interleave(gens, 2)
    kb.barrier()
    p2.close()

    ST = banks[0:3]
    OT = [banks[4], banks[5]]
    PL = [banks[6], banks[7]]
    PS_ = banks[3]
    PT = dbl("pt", [128, 512], BF16, 4)
    Pacc = [kb.sb("pacc%d" % m, [128, 512], F32) for m in range(2)]
    ones_bb = kb.sb("ones_bb", [128, 128], BF16)
    kb.op("pool", lambda e: e.memset(ones_bb[:], 1.0), writes=[ones_bb])
    ones_ff = kb.sb("ones_ff", [128, 128], F32)
    kb.op("pool", lambda e: e.memset(ones_ff[:], 1.0), writes=[ones_ff])
    subg_col = kb.sb("subg_col", [128, 1], F32)
    kb.load("sp", subg_col, subg_col[:], small.h[512:640].rearrange("(p o) -> p o", o=1), small)
    kb.op("dve", lambda e: e.tensor_scalar(out=subg_col[:], in0=subg_col[:], scalar1=0.8, scalar2=None, op0=ALU.mult), reads=[subg_col], writes=[subg_col])
    rlb = dbl("rlb", [128, 512], F32)
    eo = dbl("eo", [128, 512], F32)
    esq = kb.sb("esq", [128, 512], F32)
    outT = dbl("outT", [128, 512], BF16)
    gcount = [0]
    NSPLIT = ATT_NSPLIT

    def attend(qc0, nq, kts):
        steps = [(m, idx, kt) for m in range(2) for idx, kt in enumerate(kts)]
        nk = len(kts)
        gi = gcount[0]
        gcount[0] += 1

        def score(s):
            m, idx, kt = steps[s]
            st = ST[s % 3]
            for c0 in range(0, nq, NSPLIT):
                kb.op("pe", lambda e: e.matmul(st[:, c0:min(nq, c0 + NSPLIT)], lhsT=KTm[m][:, kt * 128:(kt + 1) * 128], rhs=QT[:, qc0 + c0:qc0 + min(nq, c0 + NSPLIT)],
                                              start=True, stop=True), reads=[KTm[m], QT], writes=[st])

        used = {}

        def rest(s):
            m, idx, kt = steps[s]
            st = ST[s % 3]
            pt = PT[s % 4]
            kb.op("act", lambda e: e.activation(out=pt[:, 0:nq], in_=st[:, 0:nq], func=AF.Exp, scale=0.125), reads=[st], writes=[pt])
            for c0 in range(0, nq, NSPLIT):
                kb.op("pe", lambda e: e.matmul(OT[m][:, c0:min(nq, c0 + NSPLIT)], lhsT=Vx[:, kt, 0:128], rhs=pt[:, c0:min(nq, c0 + NSPLIT)], start=(idx == 0 and c0 == 0), stop=(idx == nk - 1),
                                              skip_group_check=True), reads=[pt, Vx], writes=[OT[m]])
            if s + 3 < len(steps):
                score(s + 3)
            if idx % 3 == 2:
                kb.op("pe", lambda e: e.matmul(PL[m][:, 0:nq], lhsT=ones_bb[:], rhs=pt[:, 0:nq], start=((m, "pe") not in used), stop=False), reads=[ones_bb, pt], writes=[PL[m]])
                used[(m, "pe")] = True
            elif (m, "dve") not in used:
                used[(m, "dve")] = True
                kb.op("dve", lambda e: e.tensor_copy(out=Pacc[m][:, 0:nq], in_=pt[:, 0:nq]), reads=[pt], writes=[Pacc[m]])
            else:
                kb.op("dve", lambda e: e.tensor_tensor(out=Pacc[m][:, 0:nq], in0=pt[:, 0:nq], in1=Pacc[m][:, 0:nq], op=ALU.add), reads=[pt, Pacc[m]], writes=[Pacc[m]])

        for s0 in range(min(3, len(steps))):
            score(s0)
        for s in range(len(steps)):
            rest(s)
        for m in range(2):
            kb.op("pe", lambda e: e.matmul(PL[m][:, 0:nq], lhsT=ones_ff[:], rhs=Pacc[m][:, 0:nq], start=((m, "pe") not in used), stop=True), reads=[ones_ff, Pacc[m]], writes=[PL[m]])
            kb.op("dve", lambda e: e.reciprocal(out=rlb[m][:, 0:nq], in_=PL[m][:, 0:nq]), reads=[PL[m]], writes=[rlb[m]])
            kb.op("dve", lambda e: e.tensor_tensor(out=eo[m][:, 0:nq], in0=OT[m][:, 0:nq], in1=rlb[m][:, 0:nq], op=ALU.mult), reads=[OT[m], rlb[m]], writes=[eo[m]])
        kb.op("dve", lambda e: e.scalar_tensor_tensor(out=eo[0][:, 0:nq], in0=eo[1][:, 0:nq], scalar=neglam[:, 0:1], in1=eo[0][:, 0:nq], op0=ALU.mult, op1=ALU.add),
              reads=[eo[1], neglam, eo[0]], writes=[eo[0]])
        kb.op("act", lambda e: e.activation(out=esq[:, 0:nq], in_=eo[0][:, 0:nq], func=AF.Square), reads=[eo[0]], writes=[esq])
        kb.op("pe", lambda e: e.matmul(PS_[:, 0:nq], lhsT=ones_ff[:], rhs=esq[:, 0:nq], start=True, stop=True), reads=[ones_ff, esq], writes=[PS_])
        kb.op("dve", lambda e: e.tensor_scalar(out=rlb[0][:, 0:nq], in0=PS_[:, 0:nq], scalar1=1.0 / 128, scalar2=EPS, op0=ALU.mult, op1=ALU.add), reads=[PS_], writes=[rlb[0]])
        kb.op("act", lambda e: e.activation(out=rlb[0][:, 0:nq], in_=rlb[0][:, 0:nq], func=AF.Sqrt), reads=[rlb[0]], writes=[rlb[0]])
        kb.op("dve", lambda e: e.reciprocal(out=rlb[1][:, 0:nq], in_=rlb[0][:, 0:nq]), reads=[rlb[0]], writes=[rlb[1]])
        ot = outT[gi % 2]
        kb.op("dve", lambda e: e.scalar_tensor_tensor(out=ot[:, 0:nq], in0=eo[0][:, 0:nq], scalar=subg_col[:, 0:1], in1=rlb[1][:, 0:nq], op0=ALU.mult, op1=ALU.mult),
              reads=[eo[0], subg_col, rlb[1]], writes=[ot])
        if qc0 >= S:
            for q in range(4):
                kb.store("sp", x1in, x1in.h[q * 128:(q + 1) * 128, 4096:4160], ot, ot[:, q * 64:(q + 1) * 64])
        else:
            q, col = qc0 // 4096, qc0 % 4096
            kb.store("sp", x1in, x1in.h[q * 128:(q + 1) * 128, col:col + nq], ot, ot[:, 0:nq])

    attend(S, LC, list(range(LC // 128)))
    for g in range(n_groups):
        attend(g * 512, 512, list(range(NKT)))
    print("l0a instructions:", kb.n_ins)
    kb.end_stage()


def rope_tables():
    half = 32
    inv = (10000.0 ** (-np.arange(0, half, 2, dtype=np.float32) / half)).astype(np.float32)
    t = np.arange(S)
    r = (t // 64).astype(np.float32)[:, None] * inv[None, :]
    c = (t % 64).astype(np.float32)[:, None] * inv[None, :]
    ang = np.concatenate([r, r, c, c], axis=-1).astype(np.float32)
    cos = np.cos(ang).astype(np.float32)
    sin = np.sin(ang).astype(np.float32)
    sgn = np.concatenate([-np.ones(16), np.ones(16), -np.ones(16), np.ones(16)]).astype(np.float32)
    sin = sin * sgn[None, :]
    return np.ascontiguousarray(np.tile(cos, (1, 4))), np.ascontiguousarray(np.tile(sin, (1, 4)))


def fop(v, n):
    return np.ascontiguousarray(np.asarray(v, np.float32).reshape(n, 128).T)


def host_l0a(inp):
    cos4, sin4 = rope_tables()
    maps = []
    wi = inp["ab_w_in"][0]
    for b in range(2):
        for h in range(4):
            sv = np.stack([inp["c"][b], inp["c_ctx"]], -1).reshape(8, 128, 2).transpose(1, 0, 2).reshape(128, 16)
            w = np.concatenate([wi[:, 1024 + h * 128:1024 + (h + 1) * 128], wi[:, 1536 + h * 128:1536 + (h + 1) * 128],
                                wi[:, 2048 + h * 128:2048 + (h + 1) * 128]], axis=1)
            qg, kg = inp["diff_qnorm_g"][0], inp["diff_knorm_g"][0]
            small = np.concatenate([qg, qg, kg, kg, inp["diff_lq1"][0], inp["diff_lk1"][0], inp["diff_lq2"][0], inp["diff_lk2"][0],
                                    inp["diff_subln_g"][0]]).astype(np.float32)
            maps.append({
                "x": np.ascontiguousarray(inp["x"][b]), "ctx": np.ascontiguousarray(inp["ctx"][b]),
                "svec": np.ascontiguousarray(sv.astype(np.float32)),
                "adaw": np.ascontiguousarray(inp["ada_w"][0][:, 0:2048]), "adab": fop(inp["ada_b"][0][0:2048], 16),
                "g1": fop(inp["norm1_g"][0], 8), "w": np.ascontiguousarray(w), "small": small, "cos4": cos4, "sin4": sin4,
            })
    return maps


BIG = 1.0e30


def build_b(layer, kb, banks, mixsrc, hin_t, hout_t, debug=False):
    L0 = (layer == 0)
    NTL = 32
    NT = NTL + (1 if L0 else 0)
    NTOK = NT * 128
    NTOKV = 4096 + (64 if L0 else 0)
    NB = (2 * NTOKV + 32 * 255 + 255) // 256
    NROWS = NB * 256
    NMIX = 4 if L0 else 8

    kb.begin_stage("b%d_" % layer)
    hin = hin_t if hin_t is not None else kb.dram("hin", [NTOK, 1024], F32, "ExternalInput")
    svec = kb.dram("svec", [128, 16], F32, "ExternalInput")
    adaw = kb.dram("adaw", [1024, 6144], F32, "ExternalInput")
    adabf = kb.dram("adabf", [128, 48], F32, "ExternalInput")
    adabr = kb.dram("adabr", [2048], F32, "ExternalInput")
    gfop = kb.dram("gfop", [128, 16], F32, "ExternalInput")
    mixidx = kb.dram("mixidx", [128, NMIX], I32, "ExternalInput")
    wout = kb.dram("wout", [1024, 1024], F32, "ExternalInput")
    rw = kb.dram("rw", [1024, 36], F32, "ExternalInput")
    rb = kb.dram("rb", [36], F32, "ExternalInput")
    w1t = kb.dram("w1t", [4096, 4096], F32, "ExternalInput")
    w3t = kb.dram("w3t", [4096, 4096], F32, "ExternalInput")
    w2t = kb.dram("w2t", [4096, 4096], F32, "ExternalInput")
    valid = kb.dram("valid", [128, 1], F32, "ExternalInput")
    if L0:
        xhalo = kb.dram("xhalo", [128, 1024], F32, "ExternalInput")
        cxh = kb.dram("cxh", [128, 1024], F32, "ExternalInput")
        edge = kb.dram("edge", [2], F32, "ExternalInput")
        win = kb.dram("win", [1024, 1024], F32, "ExternalInput")
        cw = kb.dram("cw", [128, 124], F32, "ExternalInput")
        cvec = kb.dram("cvec", [128, 12], F32, "ExternalInput")
    hout = hout_t if hout_t is not None else kb.dram("hout", [NTOK, 1024], F32, "ExternalOutput")
    hlm = kb.dram("hlm", [NTOK, 1024], F32)
    nl2d = kb.dram("nl2d", [NTOK, 1024], BF16)
    xs = kb.dram("xs", [NROWS + 128, 1024], BF16)
    ys = kb.dram("ys", [NROWS + 128, 1024], F32)

    def bfv(t):
        return t[:].bitcast(BF16)

    def dbl(name, shape, dt, n=2, es=None):
        return [kb.sb("%s%d" % (name, i), shape, dt, es) for i in range(n)]

    identb = kb.identity("identb", BF16)
    zer = kb.sb("zer", [128, 128], F32)
    kb.op("pool", lambda e: e.memset(zer[:], 0.0), writes=[zer])
    zerb = kb.sb("zerb", [128, 2048], BF16)
    kb.op("pool", lambda e: e.memset(zerb[:], 0.0), writes=[zerb])
    for a in range(0, NROWS // 128, 2):
        kb.store("pool", xs, xs.h[a * 128:(a + 2) * 128, :].rearrange("(a p) n -> p a n", p=128), zerb, zerb[:].rearrange("p (a n) -> p a n", a=2))

    OH = kb.sb("OH", [128, NT, 2, 32], F32)
    GT = kb.sb("GT", [128, NT, 2], F32)
    RK = kb.sb("RK", [128, NT, 2], F32)
    Rbc = kb.sb("Rbc", [128, 32], F32)
    DESTI = kb.sb("DESTI", [128, NT * 2], I32)
    WIDX = kb.sb("WIDX", [128, NB], I32)
    validt = kb.sb("validt", [128, 1], F32)
    gate_bc = [[kb.sb("gate_bc%d%d" % (j, w), [128, 1024], F32) for w in range(2)] for j in range(2)]
    mod = kb.sb("mod", [128, 48, 2], F32)
    gs1 = kb.sb("gs1", [128, 8, 2], F32)
    gs2 = kb.sb("gs2", [128, 8, 2], F32)
    s_sb = kb.sb("s_sb", [128, 16], F32)
    adabf_sb = kb.sb("adabf_sb", [128, 48], F32)
    gfop_sb = kb.sb("gfop_sb", [128, 16], F32)
    iop = kb.sb("iop", [128, 1], F32)
    blkst = kb.sb("blkst", [128, NB], F32)
    ltri_b = kb.sb("ltri_b", [128, 128], BF16)
    ones_b = kb.sb("ones_b", [128, 128], BF16)
    pesA = ExitStack()

    def rstd_chain(stt, c_in, c_tmp, c_out, n, inv_n):
        kb.op("dve", lambda e: e.tensor_scalar(out=stt[:, c_tmp:c_tmp + n], in0=stt[:, c_in:c_in + n], scalar1=inv_n, scalar2=EPS, op0=ALU.mult, op1=ALU.add),
              reads=[stt], writes=[stt])
        kb.op("act", lambda e: e.activation(out=stt[:, c_tmp:c_tmp + n], in_=stt[:, c_tmp:c_tmp + n], func=AF.Sqrt), reads=[stt], writes=[stt])
        kb.op("dve", lambda e: e.reciprocal(out=stt[:, c_out:c_out + n], in_=stt[:, c_tmp:c_tmp + n]), reads=[stt], writes=[stt])

    kb.load("sp", s_sb, s_sb[:], svec.h, svec)
    kb.op("act", lambda e: e.activation(out=s_sb[:], in_=s_sb[:], func=AF.Silu), reads=[s_sb], writes=[s_sb])
    kb.load("sp", adabf_sb, adabf_sb[:], adabf.h, adabf)
    kb.load("sp", gfop_sb, gfop_sb[:], gfop.h, gfop)
    kb.load("sp", validt, validt[:], valid.h, valid)
    pm = banks[0]
    with ExitStack() as pes:
        adabr_sb = kb.sb("adabr_sb", [128, 2048], F32, pes)
        kb.load("sp", adabr_sb, adabr_sb[:], adabr.h.partition_broadcast(128), adabr)
        s_bc = [kb.sb("s_bc%d" % j, [128, 8, 128], F32, pes) for j in range(2)]
        for j in range(2):
            for kc in range(8):
                kb.op("dve", lambda e: e.tensor_scalar(out=s_bc[j][:, kc, :], in0=zer[:, 0:128], scalar1=s_sb[:, kc * 2 + j:kc * 2 + j + 1], scalar2=None, op0=ALU.add),
                      reads=[zer, s_sb], writes=[s_bc[j]])
        adaw_sb = dbl("adaw_sb", [128, 8, 512], F32, 2, pes)
        for v in range(12):
            aw = adaw_sb[v % 2]
            kb.load("sp", aw, aw[:], adaw.h[:, v * 512:(v + 1) * 512].rearrange("(kc p) n -> p kc n", p=128), adaw)
            for oc in range(4):
                g = v * 4 + oc
                for kc in range(8):
                    kb.op("pe", lambda e: e.matmul(pm[:, g * 2:g * 2 + 2], lhsT=aw[:, kc, oc * 128:(oc + 1) * 128], rhs=s_sb[:, kc * 2:kc * 2 + 2],
                                                  start=(kc == 0), stop=(kc == 7)), reads=[aw, s_sb], writes=[pm])
            if v in (4, 5, 10, 11):
                which = 0 if v < 6 else 1
                half = v % 2
                for j in range(2):
                    pr = banks[1 + j]
                    for kc in range(8):
                        kb.op("pe", lambda e: e.matmul(pr[:, :], lhsT=s_bc[j][:, kc, :], rhs=aw[:, kc, :], start=(kc == 0), stop=(kc == 7)),
                              reads=[s_bc[j], aw], writes=[pr])
                    kb.op("dve", lambda e: e.tensor_tensor(out=gate_bc[j][which][:, half * 512:(half + 1) * 512], in0=pr[:, :],
                                                           in1=adabr_sb[:, which * 1024 + half * 512: which * 1024 + (half + 1) * 512], op=ALU.add),
                          reads=[pr, adabr_sb], writes=[gate_bc[j][which]])
        pm3 = pm[:, 0:96].rearrange("p (g j) -> p g j", j=2)
        for j in range(2):
            kb.op("dve", lambda e: e.tensor_tensor(out=mod[:, :, j], in0=pm3[:, :, j], in1=adabf_sb[:], op=ALU.add), reads=[pm, adabf_sb], writes=[mod])
        kb.barrier()
    for j in range(2):
        kb.op("dve", lambda e: e.scalar_tensor_tensor(out=gs1[:, :, j], in0=mod[:, 8:16, j], scalar=1.0, in1=gfop_sb[:, 0:8], op0=ALU.add, op1=ALU.mult),
              reads=[mod, gfop_sb], writes=[gs1])
        kb.op("dve", lambda e: e.scalar_tensor_tensor(out=gs2[:, :, j], in0=mod[:, 32:40, j], scalar=1.0, in1=gfop_sb[:, 8:16], op0=ALU.add, op1=ALU.mult),
              reads=[mod, gfop_sb], writes=[gs2])
    SH1, SH2 = 0, 24

    xn_b = dbl("xn_b", [128, 1024], BF16, 2, pesA)
    junk = kb.sb("junk", [128, 1024], BF16, pesA)
    stn = dbl("stn", [128, 4], F32, 2, pesA)

    def norm_T(i, xt_tile, gs, shoff, j, dstT, dcol, psT):
        p = i % 2
        kb.op("act", lambda e: e.activation(out=junk[:], in_=xt_tile[:], func=AF.Square, accum_out=stn[p][:, 0:1]), reads=[xt_tile], writes=[junk, stn[p]])
        rstd_chain(stn[p], 0, 1, 2, 1, 1.0 / 1024)
        kb.op("act", lambda e: e.activation(out=xn_b[p][:], in_=xt_tile[:], func=AF.Copy, scale=stn[p][:, 2:3]), reads=[xt_tile, stn[p]], writes=[xn_b[p]])
        for kc in range(8):
            kb.op("pe", lambda e: e.transpose(out=bfv(psT)[:, kc * 128:(kc + 1) * 128], in_=xn_b[p][:, kc * 128:(kc + 1) * 128], identity=identb[:]),
                  reads=[xn_b[p], identb], writes=[psT])
        for kc in range(8):
            kb.op("act", lambda e: e.activation(out=dstT[:, kc, dcol:dcol + 128], in_=bfv(psT)[:, kc * 128:(kc + 1) * 128], func=AF.Identity,
                                                scale=gs[:, kc, j:j + 1], bias=mod[:, shoff + kc, j:j + 1]), reads=[psT, gs, mod], writes=[dstT])

    xt = dbl("xt", [128, 1024], F32, 2, pesA)
    convT = kb.sb("convT", [128, 4, NTOK], BF16, pesA) if L0 else None

    if L0:
        with ExitStack() as pes:
            HW = 15 + 4096 + 15
            hT = kb.sb("hT", [128, 4, HW], F32, pes)
            hTc = kb.sb("hTc", [128, 4, 128], F32, pes)
            win_b = kb.sb("win_b", [128, 8, 1024], BF16, pes)
            stg = xt
            for kc in range(8):
                kb.load("sp", stg[kc % 2], stg[kc % 2][:], win.h[kc * 128:(kc + 1) * 128, :], win)
                kb.op("pool", lambda e: e.tensor_copy(out=win_b[:, kc, :], in_=stg[kc % 2][:]), reads=[stg[kc % 2]], writes=[win_b])
            cw_sb = kb.sb("cw_sb", [128, 4, 31], F32, pes)
            kb.load("sp", cw_sb, cw_sb[:], cw.h.rearrange("p (c t) -> p c t", c=4), cw)
            cvec_sb = kb.sb("cvec_sb", [128, 12], F32, pes)
            kb.load("sp", cvec_sb, cvec_sb[:], cvec.h, cvec)
            edge_sb = kb.sb("edge_sb", [128, 2], F32, pes)
            kb.load("sp", edge_sb, edge_sb[:], edge.h.partition_broadcast(128), edge)
            ones_s = kb.sb("ones_s", [128, 128], F32, pes)
            kb.op("pool", lambda e: e.memset(ones_s[:], 1.0 / 512), writes=[ones_s])
            nlT = dbl("nlT", [128, 8, 512], BF16, 1, pes) * 2
            sig = dbl("sig", [128, 512], F32, 2, pes)
            htmp = kb.sb("htmp", [128, 4, 128], F32, pes)

            def u_group(gi, nl, ncols, dst_fn):
                for cc in range(4):
                    pa, pg = banks[2], banks[3]
                    for kc in range(8):
                        kb.op("pe", lambda e: e.matmul(pa[:, 0:ncols], lhsT=win_b[:, kc, cc * 128:(cc + 1) * 128], rhs=nl[:, kc, 0:ncols], start=(kc == 0), stop=(kc == 7)),
                              reads=[win_b, nl], writes=[pa])
                    for kc in range(8):
                        kb.op("pe", lambda e: e.matmul(pg[:, 0:ncols], lhsT=win_b[:, kc, 512 + cc * 128:512 + (cc + 1) * 128], rhs=nl[:, kc, 0:ncols], start=(kc == 0), stop=(kc == 7)),
                              reads=[win_b, nl], writes=[pg])
                    sg = sig[cc % 2]
                    kb.op("act", lambda e: e.activation(out=sg[:, 0:ncols], in_=pg[:, 0:ncols], func=AF.Sigmoid), reads=[pg], writes=[sg])
                    dt_, dap = dst_fn(cc)
                    kb.op("dve", lambda e: e.tensor_tensor(out=dap, in0=pa[:, 0:ncols], in1=sg[:, 0:ncols], op=ALU.mult), reads=[pa, sg], writes=[dt_])

            ti = 0
            for g in range(8):
                nl = nlT[g % 2]
                for tt in range(4):
                    t = g * 4 + tt
                    kb.load("sp", xt[ti % 2], xt[ti % 2][:], hin.h[t * 128:(t + 1) * 128, :], hin)
                    norm_T(ti, xt[ti % 2], gs1, SH1, 0, nl, tt * 128, banks[ti % 2])
                    ti += 1
                u_group(g, nl, 512, lambda cc: (hT, hT[:, cc, 15 + g * 512:15 + (g + 1) * 512]))
            nl = nlT[0]
            kb.load("sp", xt[ti % 2], xt[ti % 2][:], xhalo.h, xhalo)
            norm_T(ti, xt[ti % 2], gs1, SH1, 0, nl, 0, banks[ti % 2])
            ti += 1
            u_group(8, nl, 128, lambda cc: (htmp, htmp[:, cc, :]))
            for cc in range(4):
                kb.op("dve", lambda e: e.tensor_scalar(out=hT[:, cc, 0:15], in0=htmp[:, cc, 0:15], scalar1=edge_sb[:, 0:1], scalar2=None, op0=ALU.mult),
                      reads=[htmp, edge_sb], writes=[hT])
                kb.op("dve", lambda e: e.tensor_scalar(out=hT[:, cc, 15 + 4096:HW], in0=htmp[:, cc, 15:30], scalar1=edge_sb[:, 1:2], scalar2=None, op0=ALU.mult),
                      reads=[htmp, edge_sb], writes=[hT])
            nl = nlT[1]
            kb.load("sp", xt[ti % 2], xt[ti % 2][:], cxh.h, cxh)
            norm_T(ti, xt[ti % 2], gs1, SH1, 1, nl, 0, banks[ti % 2])
            ti += 1
            u_group(9, nl, 128, lambda cc: (hTc, hTc[:, cc, :]))
            for cc in range(4):
                kb.op("dve", lambda e: e.tensor_scalar(out=hTc[:, cc, 0:15], in0=hTc[:, cc, 0:15], scalar1=edge_sb[:, 0:1], scalar2=None, op0=ALU.mult),
                      reads=[hTc, edge_sb], writes=[hTc])
                kb.op("dve", lambda e: e.tensor_scalar(out=hTc[:, cc, 79:94], in0=hTc[:, cc, 79:94], scalar1=edge_sb[:, 1:2], scalar2=None, op0=ALU.mult),
                      reads=[hTc, edge_sb], writes=[hTc])

            acc = [kb.sb("acc%d" % c, [128, 512], F32, pes) for c in range(4)]
            sqt = dbl("sqt", [128, 512], F32, 1, pes) * 2
            mean_sb = kb.sb("mean_sb", [128, 512], F32, pes)
            m2 = kb.sb("m2", [128, 512], F32, pes)
            rstd_bc = kb.sb("rstd_bc", [128, 512], F32, pes)
            tt_ = dbl("tt_", [128, 512], F32, 1, pes) * 2

            def conv_block(src, c0, n, out_c0):
                for tau in range(31):
                    for cc in range(4):
                        en = "dve"
                        if tau == 0:
                            kb.op(en, lambda e: e.tensor_scalar(out=acc[cc][:, 0:n], in0=src[:, cc, c0:c0 + n], scalar1=cw_sb[:, cc, 0:1], scalar2=cvec_sb[:, cc:cc + 1],
                                                                op0=ALU.mult, op1=ALU.add), reads=[src, cw_sb, cvec_sb], writes=[acc[cc]])
                        else:
                            kb.op(en, lambda e: e.scalar_tensor_tensor(out=acc[cc][:, 0:n], in0=src[:, cc, c0 + tau:c0 + tau + n], scalar=cw_sb[:, cc, tau:tau + 1],
                                                                       in1=acc[cc][:, 0:n], op0=ALU.mult, op1=ALU.add), reads=[src, cw_sb, acc[cc]], writes=[acc[cc]])
                pmean, pex2 = banks[4], banks[5]
                for cc in range(4):
                    kb.op("pe", lambda e: e.matmul(pmean[:, 0:n], lhsT=ones_s[:], rhs=acc[cc][:, 0:n], start=(cc == 0), stop=(cc == 3)), reads=[ones_s, acc[cc]], writes=[pmean])
                for cc in range(4):
                    sq_ = sqt[cc % 2]
                    kb.op("act", lambda e: e.activation(out=sq_[:, 0:n], in_=acc[cc][:, 0:n], func=AF.Square), reads=[acc[cc]], writes=[sq_])
                    kb.op("pe", lambda e: e.matmul(pex2[:, 0:n], lhsT=ones_s[:], rhs=sq_[:, 0:n], start=(cc == 0), stop=(cc == 3)), reads=[ones_s, sq_], writes=[pex2])
                kb.op("act", lambda e: e.copy(out=mean_sb[:, 0:n], in_=pmean[:, 0:n]), reads=[pmean], writes=[mean_sb])
                kb.op("pool", lambda e: e.tensor_tensor(out=m2[:, 0:n], in0=mean_sb[:, 0:n], in1=mean_sb[:, 0:n], op=ALU.mult), reads=[mean_sb], writes=[m2])
                kb.op("dve", lambda e: e.tensor_tensor(out=m2[:, 0:n], in0=pex2[:, 0:n], in1=m2[:, 0:n], op=ALU.subtract), reads=[pex2, m2], writes=[m2])
                kb.op("dve", lambda e: e.tensor_scalar(out=m2[:, 0:n], in0=m2[:, 0:n], scalar1=EPS, scalar2=None, op0=ALU.add), reads=[m2], writes=[m2])
                kb.op("act", lambda e: e.activation(out=m2[:, 0:n], in_=m2[:, 0:n], func=AF.Sqrt), reads=[m2], writes=[m2])
                kb.op("dve", lambda e: e.reciprocal(out=rstd_bc[:, 0:n], in_=m2[:, 0:n]), reads=[m2], writes=[rstd_bc])
                for cc in range(4):
                    t_ = tt_[cc % 2]
                    kb.op("dve", lambda e: e.tensor_tensor(out=t_[:, 0:n], in0=acc[cc][:, 0:n], in1=mean_sb[:, 0:n], op=ALU.subtract), reads=[acc[cc], mean_sb], writes=[t_])
                    kb.op("pool", lambda e: e.tensor_tensor(out=t_[:, 0:n], in0=t_[:, 0:n], in1=rstd_bc[:, 0:n], op=ALU.mult), reads=[t_, rstd_bc], writes=[t_])
                    kb.op("act", lambda e: e.activation(out=convT[:, cc, out_c0:out_c0 + n], in_=t_[:, 0:n], func=AF.Silu, scale=cvec_sb[:, 4 + cc:5 + cc],
                                                        bias=cvec_sb[:, 8 + cc:9 + cc]), reads=[t_, cvec_sb], writes=[convT])

            for tb in range(8):
                conv_block(hT, tb * 512, 512, tb * 512)
            conv_block(hTc, 0, 64, 4096)
            kb.op("pool", lambda e: e.memset(convT[:, :, 4096 + 64:4096 + 128], 0.0), writes=[convT])
            kb.barrier()

    mix_sb = kb.sb("mix_sb", [128, NMIX, NTOK], BF16, pesA)
    mixidx_sb = kb.sb("mixidx_sb", [128, NMIX], I32, pesA)
    kb.load("sp", mixidx_sb, mixidx_sb[:], mixidx.h, mixidx)
    for hh in range(NMIX):
        kb.dma("pool", lambda e: e.indirect_dma_start(out=mix_sb[:, hh, :], out_offset=None, in_=mixsrc.h[:, :],
                                                      in_offset=bass.IndirectOffsetOnAxis(ap=mixidx_sb[:, hh:hh + 1], axis=0)), reads=[mixidx_sb, mixsrc], writes=[mix_sb])
    wout_b = kb.sb("wout_b", [128, 8, 1024], BF16, pesA)
    rw_b = kb.sb("rw_b", [128, 8, 36], BF16, pesA)
    rb_bc = kb.sb("rb_bc", [128, 36], F32, pesA)
    kb.load("sp", rb_bc, rb_bc[:], rb.h.partition_broadcast(128), rb)
    kb.op("pool", lambda e: e.memset(Rbc[:], 0.0), writes=[Rbc])
    ltri = kb.sb("ltri", [128, 128], F32, pesA)
    kb.op("pool", lambda e: e.memset(ltri[:], 1.0), writes=[ltri])
    kb.op("pool", lambda e: e.affine_select(out=ltri[:], in_=ltri[:], pattern=[[1, 128]], compare_op=ALU.is_gt, fill=0.0, base=0, channel_multiplier=-1),
          reads=[ltri], writes=[ltri])
    kb.op("pool", lambda e: e.tensor_copy(out=ltri_b[:], in_=ltri[:]), reads=[ltri], writes=[ltri_b])
    kb.op("pool", lambda e: e.memset(ones_b[:], 1.0), writes=[ones_b])
    kb.op("pool", lambda e: e.iota(iop[:], pattern=[[0, 1]], base=0, channel_multiplier=1, allow_small_or_imprecise_dtypes=True), writes=[iop])
    kb.op("pool", lambda e: e.iota(blkst[:], pattern=[[256, NB]], base=0, channel_multiplier=0, allow_small_or_imprecise_dtypes=True), writes=[blkst])

    with ExitStack() as pes:
        stg = dbl("stg2", [128, 1024], F32, 2, pes)
        for kc in range(8):
            kb.load("sp", stg[kc % 2], stg[kc % 2][:], wout.h[kc * 128:(kc + 1) * 128, :], wout)
            kb.op("pool", lambda e: e.tensor_copy(out=wout_b[:, kc, :], in_=stg[kc % 2][:]), reads=[stg[kc % 2]], writes=[wout_b])
        rw_f = kb.sb("rw_f", [128, 8, 36], F32, pes)
        kb.load("sp", rw_f, rw_f[:], rw.h.rearrange("(kc p) n -> p kc n", p=128), rw)
        kb.op("pool", lambda e: e.tensor_copy(out=rw_b[:], in_=rw_f[:]), reads=[rw_f], writes=[rw_b])

        ytmp = dbl("ytmp", [128, 1024], F32, 2, pes)
        hl = dbl("hl", [128, 1024], F32, 2, pes)
        nl2T = dbl("nl2T", [128, 8, 128], BF16, 2, pes)
        nl2 = dbl("nl2", [128, 1024], BF16, 2, pes)
        lg = dbl("lg", [128, 36], F32, 2, pes)
        rt = dbl("rt", [128, 16], F32, 2, pes)
        lem = dbl("lem", [128, 32], F32, 2, pes)
        lem2 = dbl("lem2", [128, 32], F32, 2, pes)
        cb_ = dbl("cb_", [128, 32], BF16, 2, pes)
        rbase = dbl("rbase", [128, 32], F32, 2, pes)
        tmp32 = dbl("tmp32", [128, 2, 32], F32, 2, pes)
        ejunk = kb.sb("ejunk", [128, 4], F32, pes)

        for t in range(NT):
            p = t % 2
            j = 1 if (L0 and t == NT - 1) else 0
            kb.load("sp", xt[p], xt[p][:], hin.h[t * 128:(t + 1) * 128, :], hin)
            chunks = []
            if L0:
                for cc in range(4):
                    chunks.append((convT, convT[:, cc, t * 128:(t + 1) * 128]))
            for hh in range(NMIX):
                chunks.append((mix_sb, mix_sb[:, hh, t * 128:(t + 1) * 128]))
            for half in range(2):
                py = banks[half]
                for ci, (ct, cap) in enumerate(chunks):
                    kb.op("pe", lambda e: e.matmul(py[:, :], lhsT=cap, rhs=wout_b[:, ci, half * 512:(half + 1) * 512], start=(ci == 0), stop=(ci == 7)),
                          reads=[ct, wout_b], writes=[py])
                kb.op("dve", lambda e: e.tensor_tensor(out=ytmp[p][:, half * 512:(half + 1) * 512], in0=py[:, :], in1=gate_bc[j][0][:, half * 512:(half + 1) * 512], op=ALU.mult),
                      reads=[py, gate_bc[j][0]], writes=[ytmp[p]])
            kb.op("dve", lambda e: e.tensor_tensor(out=hl[p][:], in0=ytmp[p][:], in1=xt[p][:], op=ALU.add), reads=[ytmp[p], xt[p]], writes=[hl[p]])
            kb.store("sp", hlm, hlm.h[t * 128:(t + 1) * 128, :], hl[p], hl[p][:])
            norm_T(t, hl[p], gs2, SH2, j, nl2T[p], 0, banks[2])
            pl = banks[3]
            for kc in range(8):
                kb.op("pe", lambda e: e.matmul(pl[:, 0:36], lhsT=nl2T[p][:, kc, :], rhs=rw_b[:, kc, :], start=(kc == 0), stop=(kc == 7)), reads=[nl2T[p], rw_b], writes=[pl])
            kb.op("dve", lambda e: e.tensor_tensor(out=lg[p][:], in0=pl[:, 0:36], in1=rb_bc[:], op=ALU.add), reads=[pl, rb_bc], writes=[lg[p]])
            pbk = banks[4]
            for kc in range(8):
                kb.op("pe", lambda e: e.transpose(out=bfv(pbk)[:, kc * 128:(kc + 1) * 128], in_=nl2T[p][:, kc, :], identity=identb[:]), reads=[nl2T[p], identb], writes=[pbk])
            kb.op("act", lambda e: e.copy(out=nl2[p][:], in_=bfv(pbk)[:, 0:1024]), reads=[pbk], writes=[nl2[p]])
            kb.store("sp", nl2d, nl2d.h[t * 128:(t + 1) * 128, :], nl2[p], nl2[p][:])
            r_ = rt[p]
            kb.op("dve", lambda e: e.tensor_reduce(out=r_[:, 0:1], in_=lg[p][:, 0:4], axis=AX.X, op=ALU.max), reads=[lg[p]], writes=[r_])
            kb.op("dve", lambda e: e.tensor_scalar(out=r_[:, 1:5], in0=lg[p][:, 0:4], scalar1=r_[:, 0:1], scalar2=None, op0=ALU.is_equal), reads=[lg[p], r_], writes=[r_])
            kb.op("dve", lambda e: e.tensor_scalar(out=r_[:, 5:6], in0=r_[:, 0:1], scalar1=-1.0, scalar2=None, op0=ALU.mult), reads=[r_], writes=[r_])
            kb.op("act", lambda e: e.activation(out=ejunk[:], in_=lg[p][:, 0:4], func=AF.Exp, bias=r_[:, 5:6], accum_out=r_[:, 6:7]), reads=[lg[p], r_], writes=[ejunk, r_])
            kb.op("dve", lambda e: e.reciprocal(out=r_[:, 7:8], in_=r_[:, 6:7]), reads=[r_], writes=[r_])
            kb.op("dve", lambda e: e.tensor_scalar(out=r_[:, 8:12], in0=r_[:, 1:5], scalar1=-1.0, scalar2=BIG, op0=ALU.add, op1=ALU.mult), reads=[r_], writes=[r_])
            for g in range(4):
                kb.op("dve", lambda e: e.tensor_scalar(out=lem[p][:, g * 8:(g + 1) * 8], in0=lg[p][:, 4 + g * 8:4 + (g + 1) * 8], scalar1=r_[:, 8 + g:9 + g], scalar2=None, op0=ALU.add),
                      reads=[lg[p], r_], writes=[lem[p]])
            oh1 = OH[:, t, 0, :]
            oh2 = OH[:, t, 1, :]
            kb.op("dve", lambda e: e.tensor_reduce(out=r_[:, 12:13], in_=lem[p][:], axis=AX.X, op=ALU.max), reads=[lem[p]], writes=[r_])
            kb.op("dve", lambda e: e.tensor_scalar(out=oh1, in0=lem[p][:], scalar1=r_[:, 12:13], scalar2=None, op0=ALU.is_equal), reads=[lem[p], r_], writes=[OH])
            kb.op("dve", lambda e: e.scalar_tensor_tensor(out=lem2[p][:], in0=oh1, scalar=-BIG, in1=lem[p][:], op0=ALU.mult, op1=ALU.add), reads=[OH, lem[p]], writes=[lem2[p]])
            kb.op("dve", lambda e: e.tensor_reduce(out=r_[:, 13:14], in_=lem2[p][:], axis=AX.X, op=ALU.max), reads=[lem2[p]], writes=[r_])
            kb.op("dve", lambda e: e.tensor_scalar(out=oh2, in0=lem2[p][:], scalar1=r_[:, 13:14], scalar2=None, op0=ALU.is_equal), reads=[lem2[p], r_], writes=[OH])
            kb.op("dve", lambda e: e.tensor_tensor(out=r_[:, 14:15], in0=r_[:, 12:13], in1=r_[:, 13:14], op=ALU.subtract), reads=[r_], writes=[r_])
            kb.op("act", lambda e: e.activation(out=r_[:, 15:16], in_=r_[:, 14:15], func=AF.Sigmoid), reads=[r_], writes=[r_])
            kb.op("dve", lambda e: e.tensor_tensor(out=GT[:, t, 0:1], in0=r_[:, 15:16], in1=r_[:, 7:8], op=ALU.mult), reads=[r_], writes=[GT])
            kb.op("dve", lambda e: e.tensor_tensor(out=GT[:, t, 1:2], in0=r_[:, 7:8], in1=GT[:, t, 0:1], op=ALU.subtract), reads=[r_, GT], writes=[GT])
            if j == 1:
                kb.op("dve", lambda e: e.tensor_scalar(out=OH[:, t, :, :], in0=OH[:, t, :, :], scalar1=validt[:, 0:1], scalar2=None, op0=ALU.mult), reads=[OH, validt], writes=[OH])
            kb.op("dve", lambda e: e.tensor_tensor(out=cb_[p][:], in0=OH[:, t, 0, :], in1=OH[:, t, 1, :], op=ALU.add), reads=[OH], writes=[cb_[p]])
            pc = banks[5]
            kb.op("pe", lambda e: e.matmul(pc[:, 0:32], lhsT=ltri_b[:], rhs=cb_[p][:], start=True, stop=True), reads=[ltri_b, cb_[p]], writes=[pc])
            kb.op("dve", lambda e: e.tensor_tensor(out=rbase[p][:], in0=pc[:, 0:32], in1=Rbc[:], op=ALU.add), reads=[pc, Rbc], writes=[rbase[p]])
            pt_ = banks[6]
            kb.op("pe", lambda e: e.matmul(pt_[:, 0:32], lhsT=ones_b[:], rhs=cb_[p][:], start=True, stop=True), reads=[ones_b, cb_[p]], writes=[pt_])
            kb.op("dve", lambda e: e.tensor_tensor(out=Rbc[:], in0=pt_[:, 0:32], in1=Rbc[:], op=ALU.add), reads=[pt_, Rbc], writes=[Rbc])
            for k in range(2):
                kb.op("dve", lambda e: e.tensor_tensor(out=tmp32[p][:, k, :], in0=OH[:, t, k, :], in1=rbase[p][:], op=ALU.mult), reads=[OH, rbase[p]], writes=[tmp32[p]])
            kb.op("dve", lambda e: e.tensor_reduce(out=RK[:, t, :], in_=tmp32[p][:], axis=AX.X, op=ALU.add), reads=[tmp32[p]], writes=[RK])
        kb.barrier()
    pesA.close()
    pesD = ExitStack()

    cnt_i = kb.sb("cnt_i", [128, 32], I32, pesD)
    pcnt = kb.sb("pcnt", [128, 32], F32, pesD)
    pend = [kb.sb("pend%d" % i, [128, 32], F32, pesD) for i in range(2)]
    kb.op("dve", lambda e: e.tensor_scalar(out=pcnt[:], in0=Rbc[:], scalar1=255.0, scalar2=None, op0=ALU.add), reads=[Rbc], writes=[pcnt])
    kb.op("dve", lambda e: e.tensor_copy(out=cnt_i[:], in_=pcnt[:]), reads=[pcnt], writes=[cnt_i])
    kb.op("dve", lambda e: e.tensor_scalar(out=cnt_i[:], in0=cnt_i[:], scalar1=8, scalar2=8, op0=ALU.arith_shift_right, op1=ALU.logical_shift_left), reads=[cnt_i], writes=[cnt_i])
    kb.op("dve", lambda e: e.tensor_copy(out=pcnt[:], in_=cnt_i[:]), reads=[cnt_i], writes=[pcnt])
    kb.op("dve", lambda e: e.tensor_copy(out=pend[0][:], in_=pcnt[:]), reads=[pcnt], writes=[pend[0]])
    cur = 0
    for sft in (1, 2, 4, 8, 16):
        a, b = pend[cur], pend[1 - cur]
        kb.op("dve", lambda e: e.tensor_copy(out=b[:, 0:sft], in_=a[:, 0:sft]), reads=[a], writes=[b])
        kb.op("dve", lambda e: e.tensor_tensor(out=b[:, sft:32], in0=a[:, sft:32], in1=a[:, 0:32 - sft], op=ALU.add), reads=[a], writes=[b])
        cur = 1 - cur
    pendf = pend[cur]
    poff = kb.sb("poff", [128, 32], F32, pesD)
    kb.op("dve", lambda e: e.tensor_tensor(out=poff[:], in0=pendf[:], in1=pcnt[:], op=ALU.subtract), reads=[pendf, pcnt], writes=[poff])
    DEST = kb.sb("DEST", [128, NT, 2], F32, pesD)
    tmpd = kb.sb("tmpd", [128, NT * 2, 32], F32, pesD)
    for t in range(NT):
        for k in range(2):
            kb.op("dve", lambda e: e.tensor_tensor(out=tmpd[:, t * 2 + k, :], in0=OH[:, t, k, :], in1=poff[:], op=ALU.mult), reads=[OH, poff], writes=[tmpd])
    kb.op("dve", lambda e: e.tensor_reduce(out=DEST[:].rearrange("p t k -> p (t k)"), in_=tmpd[:], axis=AX.X, op=ALU.add), reads=[tmpd], writes=[DEST])
    kb.op("dve", lambda e: e.tensor_tensor(out=DEST[:], in0=DEST[:], in1=RK[:], op=ALU.add), reads=[DEST, RK], writes=[DEST])
    if L0:
        inval = kb.sb("inval", [128, 2], F32, pesD)
        kb.op("dve", lambda e: e.tensor_scalar(out=inval[:, 0:1], in0=validt[:], scalar1=-1.0, scalar2=-1.0, op0=ALU.add, op1=ALU.mult), reads=[validt], writes=[inval])
        kb.op("dve", lambda e: e.scalar_tensor_tensor(out=inval[:, 1:2], in0=iop[:], scalar=float(NROWS), in1=inval[:, 0:1], op0=ALU.add, op1=ALU.mult), reads=[iop, inval], writes=[inval])
        kb.op("dve", lambda e: e.tensor_scalar(out=DEST[:, NT - 1, :], in0=DEST[:, NT - 1, :], scalar1=validt[:, 0:1], scalar2=inval[:, 1:2], op0=ALU.mult, op1=ALU.add),
              reads=[DEST, validt, inval], writes=[DEST])
    kb.op("dve", lambda e: e.tensor_copy(out=DESTI[:], in_=DEST[:].rearrange("p t k -> p (t k)")), reads=[DEST], writes=[DESTI])
    eb = kb.sb("eb", [128, NB], F32, pesD)
    kb.op("pool", lambda e: e.memset(eb[:], 0.0), writes=[eb])
    for ee in range(32):
        kb.op("dve", lambda e: e.scalar_tensor_tensor(out=eb[:], in0=blkst[:], scalar=pendf[:, ee:ee + 1], in1=eb[:], op0=ALU.is_ge, op1=ALU.add), reads=[blkst, pendf, eb], writes=[eb])
    kb.op("dve", lambda e: e.tensor_scalar(out=eb[:], in0=eb[:], scalar1=31.0, scalar2=128.0, op0=ALU.min, op1=ALU.mult), reads=[eb], writes=[eb])
    kb.op("dve", lambda e: e.tensor_scalar(out=eb[:], in0=eb[:], scalar1=iop[:, 0:1], scalar2=None, op0=ALU.add), reads=[eb, iop], writes=[eb])
    kb.op("dve", lambda e: e.tensor_copy(out=WIDX[:], in_=eb[:]), reads=[eb], writes=[WIDX])

    srow = dbl("srow", [128, 1024], BF16, 3, pesD)
    for t in range(NT):
        sr = srow[t % 3]
        kb.load("sp", sr, sr[:], nl2d.h[t * 128:(t + 1) * 128, :], nl2d)
        for k in range(2):
            kb.dma("pool", lambda e: e.indirect_dma_start(out=xs.h[:, :], out_offset=bass.IndirectOffsetOnAxis(ap=DESTI[:, t * 2 + k:t * 2 + k + 1], axis=0),
                                                          in_=sr[:], in_offset=None), reads=[DESTI, sr], writes=[xs])
    kb.barrier()
    pesD.close()
    pesE = ExitStack()

    w1f = dbl("w1f", [128, 4096], F32, 2, pesE)
    w3f = dbl("w3f", [128, 4096], F32, 2, pesE)
    w2f = dbl("w2f", [128, 4096], F32, 2, pesE)
    w1b = dbl("w1b", [128, 8, 512], BF16, 1, pesE) * 2
    w3b = dbl("w3b", [128, 8, 512], BF16, 1, pesE) * 2
    w2b = dbl("w2b", [128, 4, 1024], BF16, 1, pesE) * 2
    xr = dbl("xr", [128, 2, 1024], BF16, 2, pesE)
    xsT = dbl("xsT", [128, 8, 256], BF16, 2, pesE)
    sl = dbl("sl", [128, 256], F32, 2, pesE)
    hhT = dbl("hhT", [128, 4, 256], BF16, 2, pesE)
    yo = dbl("yo", [128, 1024], F32, 2, pesE)
    for b in range(NB):
        p = b % 2
        for (tab, wf) in ((w1t, w1f[p]), (w3t, w3f[p]), (w2t, w2f[p])):
            kb.dma("pool", lambda e: e.indirect_dma_start(out=wf[:], out_offset=None, in_=tab.h[:, :], in_offset=bass.IndirectOffsetOnAxis(ap=WIDX[:, b:b + 1], axis=0)),
                   reads=[WIDX, tab], writes=[wf])
        kb.op("act", lambda e: e.copy(out=w1b[p][:].rearrange("p a b -> p (a b)"), in_=w1f[p][:]), reads=[w1f[p]], writes=[w1b[p]])
        kb.op("dve", lambda e: e.tensor_copy(out=w3b[p][:].rearrange("p a b -> p (a b)"), in_=w3f[p][:]), reads=[w3f[p]], writes=[w3b[p]])
        kb.op("act", lambda e: e.copy(out=w2b[p][:].rearrange("p a b -> p (a b)")[:, 0:2048], in_=w2f[p][:, 0:2048]), reads=[w2f[p]], writes=[w2b[p]])
        kb.op("dve", lambda e: e.tensor_copy(out=w2b[p][:].rearrange("p a b -> p (a b)")[:, 2048:4096], in_=w2f[p][:, 2048:4096]), reads=[w2f[p]], writes=[w2b[p]])
        kb.load("sp", xr[p], xr[p][:], xs.h[b * 256:(b + 1) * 256, :].rearrange("(a p) n -> p a n", p=128), xs)
        for sub in range(2):
            pT = banks[6]
            for kc in range(8):
                kb.op("pe", lambda e: e.transpose(out=bfv(pT)[:, kc * 128:(kc + 1) * 128], in_=xr[p][:, sub, kc * 128:(kc + 1) * 128], identity=identb[:]),
                      reads=[xr[p], identb], writes=[pT])
            kb.op("act", lambda e: e.copy(out=xsT[p][:, :, sub * 128:(sub + 1) * 128], in_=bfv(pT)[:, 0:1024].rearrange("p (a b) -> p a b", a=8)), reads=[pT], writes=[xsT[p]])
        for fc in range(4):
            ph1 = banks[0 + fc // 2]
            ph3 = banks[2 + fc // 2]
            c0 = (fc % 2) * 256
            for kc in range(8):
                kb.op("pe", lambda e: e.matmul(ph1[:, c0:c0 + 256], lhsT=w1b[p][:, kc, fc * 128:(fc + 1) * 128], rhs=xsT[p][:, kc, :], start=(kc == 0), stop=(kc == 7)),
                      reads=[w1b[p], xsT[p]], writes=[ph1])
            for kc in range(8):
                kb.op("pe", lambda e: e.matmul(ph3[:, c0:c0 + 256], lhsT=w3b[p][:, kc, fc * 128:(fc + 1) * 128], rhs=xsT[p][:, kc, :], start=(kc == 0), stop=(kc == 7)),
                      reads=[w3b[p], xsT[p]], writes=[ph3])
            s_ = sl[fc % 2]
            kb.op("act", lambda e: e.activation(out=s_[:], in_=ph1[:, c0:c0 + 256], func=AF.Silu), reads=[ph1], writes=[s_])
            kb.op("dve", lambda e: e.tensor_tensor(out=hhT[p][:, fc, :], in0=ph3[:, c0:c0 + 256], in1=s_[:], op=ALU.mult), reads=[ph3, s_], writes=[hhT[p]])
        for sub in range(2):
            y_ = yo[sub]
            for half in range(2):
                py = banks[4 + half]
                for fc in range(4):
                    kb.op("pe", lambda e: e.matmul(py[:, :], lhsT=hhT[p][:, fc, sub * 128:(sub + 1) * 128], rhs=w2b[p][:, fc, half * 512:(half + 1) * 512], start=(fc == 0), stop=(fc == 3)),
                          reads=[hhT[p], w2b[p]], writes=[py])
                if half == 0:
                    kb.op("act", lambda e: e.copy(out=y_[:, 0:512], in_=py[:, :]), reads=[py], writes=[y_])
                else:
                    kb.op("dve", lambda e: e.tensor_copy(out=y_[:, 512:1024], in_=py[:, :]), reads=[py], writes=[y_])
            kb.store("sp", ys, ys.h[b * 256 + sub * 128:b * 256 + (sub + 1) * 128, :], y_, y_[:])
    kb.barrier()
    pesE.close()
    pesF = ExitStack()

    y1 = dbl("y1", [128, 1024], F32, 2, pesF)
    y2 = dbl("y2", [128, 1024], F32, 2, pesF)
    hm = dbl("hm", [128, 1024], F32, 2, pesF)
    for t in range(NT):
        p = t % 2
        j = 1 if (L0 and t == NT - 1) else 0
        if j == 1:
            kb.op("pool", lambda e: e.memset(y1[p][:], 0.0), writes=[y1[p]])
            kb.op("pool", lambda e: e.memset(y2[p][:], 0.0), writes=[y2[p]])
        for k, yk in ((0, y1[p]), (1, y2[p])):
            kb.dma("pool", lambda e: e.indirect_dma_start(out=yk[:], out_offset=None, in_=ys.h[:, :], in_offset=bass.IndirectOffsetOnAxis(ap=DESTI[:, t * 2 + k:t * 2 + k + 1], axis=0)), reads=[DESTI, ys], writes=[yk])
        kb.load("sp", hm[p], hm[p][:], hlm.h[t * 128:(t + 1) * 128, :], hlm)
        kb.op("dve", lambda e: e.tensor_scalar(out=y1[p][:], in0=y1[p][:], scalar1=GT[:, t, 0:1], scalar2=None, op0=ALU.mult), reads=[y1[p], GT], writes=[y1[p]])
        kb.op("dve", lambda e: e.scalar_tensor_tensor(out=y1[p][:], in0=y2[p][:], scalar=GT[:, t, 1:2], in1=y1[p][:], op0=ALU.mult, op1=ALU.add), reads=[y2[p], GT, y1[p]], writes=[y1[p]])
        kb.op("dve", lambda e: e.tensor_tensor(out=y1[p][:], in0=y1[p][:], in1=gate_bc[j][1][:], op=ALU.mult), reads=[y1[p], gate_bc[j][1]], writes=[y1[p]])
        kb.op("dve", lambda e: e.tensor_tensor(out=hm[p][:], in0=hm[p][:], in1=y1[p][:], op=ALU.add), reads=[hm[p], y1[p]], writes=[hm[p]])
        kb.store("sp", hout, hout.h[t * 128:(t + 1) * 128, :], hm[p], hm[p][:])
    print("lb%d instructions:" % layer, kb.n_ins, "sems:", len(kb.sems))
    pesF.close()
    kb.end_stage()


def fop(v, n):
    return np.ascontiguousarray(np.asarray(v, np.float32).reshape(n, 128).T)


def moe_tables(inp, l):
    w1 = np.ascontiguousarray(inp["moe_w1"][l].reshape(32, 8, 128, 512).transpose(0, 2, 1, 3).reshape(4096, 4096))
    w3 = np.ascontiguousarray(inp["moe_w3"][l].reshape(32, 8, 128, 512).transpose(0, 2, 1, 3).reshape(4096, 4096))
    w2 = np.ascontiguousarray(inp["moe_w2"][l].reshape(32, 4, 128, 1024).transpose(0, 2, 1, 3).reshape(4096, 4096))
    return w1, w3, w2


def host_b(layer, inp):
    L0 = layer == 0
    l = layer
    w1, w3, w2 = moe_tables(inp, l)
    rw = np.ascontiguousarray(np.concatenate([inp["rg_w"][l], inp["re_w"][l]], axis=1).astype(np.float32))
    rb = np.concatenate([inp["rg_b"][l], inp["re_b"][l]]).astype(np.float32)
    adabr = np.concatenate([inp["ada_b"][l][2048:3072], inp["ada_b"][l][5120:6144]]).astype(np.float32)
    gfop = np.ascontiguousarray(np.concatenate([fop(inp["norm1_g"][l], 8), fop(inp["norm2_g"][l], 8)], axis=1))
    wout = np.ascontiguousarray(inp["ab_w_out"][0] if L0 else inp["gla_w_out"][0])
    hin_lat, hin_ctx = inp["x"], inp["ctx"]
    maps = []
    pp = np.arange(128, dtype=np.int32)
    for b in range(2):
        sv = np.stack([inp["c"][b], inp["c_ctx"]], -1).reshape(8, 128, 2).transpose(1, 0, 2).reshape(128, 16).astype(np.float32)
        for jq in range(4):
            r0, r1 = jq * 4096, (jq + 1) * 4096
            m = {"svec": np.ascontiguousarray(sv), "adaw": np.ascontiguousarray(inp["ada_w"][l]), "adabf": fop(inp["ada_b"][l], 48), "adabr": adabr, "gfop": gfop,
                 "wout": wout, "rw": rw, "rb": rb, "w1t": w1, "w3t": w3, "w2t": w2}
            if L0:
                cpad = np.zeros((128, 1024), np.float32)
                cpad[:64] = hin_ctx[b, 64 * jq:64 * jq + 64]
                m["hin"] = np.ascontiguousarray(np.concatenate([hin_lat[b, r0:r1], cpad], 0))
                m["mixidx"] = np.ascontiguousarray(np.stack([np.array([ag_row(jq * 128 + int(p_), h, 64, 512) for p_ in pp]) for h in range(4)], axis=1).astype(np.int32))
                xh = np.zeros((128, 1024), np.float32)
                if jq > 0:
                    xh[0:15] = hin_lat[b, r0 - 15:r0]
                if jq < 3:
                    xh[15:30] = hin_lat[b, r1:r1 + 15]
                m["xhalo"] = xh
                ch = np.zeros((128, 1024), np.float32)
                for r in range(94):
                    pos = 64 * jq - 15 + r
                    if 0 <= pos < 256:
                        ch[r] = hin_ctx[b, pos]
                m["cxh"] = ch
                m["edge"] = np.array([1.0 if jq > 0 else 0.0, 1.0 if jq < 3 else 0.0], np.float32)
                v = np.zeros((128, 1), np.float32)
                v[:64] = 1
                m["valid"] = v
                m["win"] = np.ascontiguousarray(inp["ab_w_in"][0][:, 0:1024])
                m["cw"] = np.ascontiguousarray(inp["conv_w"][0].T.reshape(4, 128, 31).transpose(1, 0, 2).reshape(128, 124))
                m["cvec"] = np.ascontiguousarray(np.concatenate([fop(inp["conv_b"][0], 4), fop(inp["conv_ln_g"][0], 4), fop(inp["conv_ln_b"][0], 4)], axis=1))
            else:
                m["mixidx"] = np.ascontiguousarray(np.stack([np.array([ag_row(jq * 256 + c2 * 128 + int(p_), h, 128, 1024) for p_ in pp]) for h in range(4) for c2 in range(2)], axis=1).astype(np.int32))
                m["valid"] = np.ones((128, 1), np.float32)
            maps.append(m)
    return maps


def gather_b(layer, results):
    L0 = layer == 0
    hl = np.zeros((2, 16384, 1024), np.float32)
    hc = np.zeros((2, 256, 1024), np.float32) if L0 else None
    for b in range(2):
        for jq in range(4):
            o = results[b * 4 + jq]["hout"]
            hl[b, jq * 4096:(jq + 1) * 4096] = o[:4096]
            if L0:
                hc[b, 64 * jq:64 * jq + 64] = o[4096:4160]
    return hl, hc


def build_l1a(kb, banks, x2out, x3in, n_lat_tiles=128, do_scan=True):
    kb.begin_stage("a1_")
    svec = kb.dram("svec", [128, 16], F32, "ExternalInput")
    adaw = kb.dram("adaw", [1024, 2048], F32, "ExternalInput")
    adab = kb.dram("adab", [128, 16], F32, "ExternalInput")
    g1 = kb.dram("g1", [128, 8], F32, "ExternalInput")
    w = kb.dram("w", [1024, 768], F32, "ExternalInput")
    waT = kb.dram("waT", [2, 16, 1024], F32, "ExternalInput")
    wa2 = kb.dram("wa2", [2, 16, 128], F32, "ExternalInput")
    small = kb.dram("small", [512], F32, "ExternalInput")
    proj = kb.dram("proj", [S + LC, 1024], F32)
    of_d = kb.dram("of_d", [S, 256], F32)

    def bfv(t):
        return t[:].bitcast(BF16)

    def dbl(name, shape, dt, n=2, es=None):
        return [kb.sb("%s%d" % (name, i), shape, dt, es) for i in range(n)]

    identb = kb.identity("identb", BF16)
    smallb = kb.sb("smallb", [128, 512], F32)
    kb.load("sp", smallb, smallb[:], small.h.partition_broadcast(128), small)
    zer = kb.sb("zer", [128, 128], F32)
    kb.op("pool", lambda e: e.memset(zer[:], 0.0), writes=[zer])

    def rstd_chain(stt, c_in, c_tmp, c_out, n, inv_n):
        kb.op("dve", lambda e: e.tensor_scalar(out=stt[:, c_tmp:c_tmp + n], in0=stt[:, c_in:c_in + n], scalar1=inv_n, scalar2=EPS, op0=ALU.mult, op1=ALU.add),
              reads=[stt], writes=[stt])
        kb.op("act", lambda e: e.activation(out=stt[:, c_tmp:c_tmp + n], in_=stt[:, c_tmp:c_tmp + n], func=AF.Sqrt), reads=[stt], writes=[stt])
        kb.op("dve", lambda e: e.reciprocal(out=stt[:, c_out:c_out + n], in_=stt[:, c_tmp:c_tmp + n]), reads=[stt], writes=[stt])

    wq = [kb.sb("wq%d" % j, [128, 8, 1024], BF16) for j in range(2)]
    bias = [kb.sb("bias%d" % j, [128, 1024], F32) for j in range(2)]
    pesA = ExitStack()
    s_sb = kb.sb("s_sb", [128, 16], F32, pesA)
    kb.load("sp", s_sb, s_sb[:], svec.h, svec)
    kb.op("act", lambda e: e.activation(out=s_sb[:], in_=s_sb[:], func=AF.Silu), reads=[s_sb], writes=[s_sb])
    adab_sb = kb.sb("adab_sb", [128, 16], F32, pesA)
    kb.load("sp", adab_sb, adab_sb[:], adab.h, adab)
    g1_sb = kb.sb("g1_sb", [128, 8], F32, pesA)
    kb.load("sp", g1_sb, g1_sb[:], g1.h, g1)
    mod = kb.sb("mod", [128, 16, 2], F32, pesA)
    gs = kb.sb("gs", [128, 8, 2], F32, pesA)
    w_sb = kb.sb("w_sb", [128, 8, 1024], F32, pesA)
    shiftbc = kb.sb("shiftbc", [128, 8, 128], F32, pesA)
    pm = banks[0]
    with ExitStack() as pes:
        adaw_sb = kb.sb("adaw_sb", [128, 8, 512], F32, pes)
        for v in range(4):
            kb.load("sp", adaw_sb, adaw_sb[:], adaw.h[:, v * 512:(v + 1) * 512].rearrange("(kc p) n -> p kc n", p=128), adaw)
            for oc in range(4):
                g = v * 4 + oc
                for kc in range(8):
                    kb.op("pe", lambda e: e.matmul(pm[:, g * 2:g * 2 + 2], lhsT=adaw_sb[:, kc, oc * 128:(oc + 1) * 128], rhs=s_sb[:, kc * 2:kc * 2 + 2],
                                                  start=(kc == 0), stop=(kc == 7)), reads=[adaw_sb, s_sb], writes=[pm])
        pm3 = pm[:, 0:32].rearrange("p (g j) -> p g j", j=2)
        for j in range(2):
            kb.op("dve", lambda e: e.tensor_tensor(out=mod[:, :, j], in0=pm3[:, :, j], in1=adab_sb[:], op=ALU.add), reads=[pm, adab_sb], writes=[mod])
            kb.op("dve", lambda e: e.scalar_tensor_tensor(out=gs[:, :, j], in0=mod[:, 8:16, j], scalar=1.0, in1=g1_sb[:], op0=ALU.add, op1=ALU.mult),
                  reads=[mod, g1_sb], writes=[gs])
        kb.load("sp", w_sb, w_sb[:, :, 0:768], w.h.rearrange("(kc p) n -> p kc n", p=128), w)
        waT_sb = [kb.sb("waT_sb%d" % d, [32, 1024], F32, pes) for d in range(2)]
        wa2_sb = [kb.sb("wa2_sb%d" % d, [32, 128], F32, pes) for d in range(2)]
        for d in range(2):
            kb.op("pool", lambda e: e.memset(waT_sb[d][:], 0.0), writes=[waT_sb[d]])
            kb.op("pool", lambda e: e.memset(wa2_sb[d][:], 0.0), writes=[wa2_sb[d]])
            kb.load("sp", waT_sb[d], waT_sb[d][0:16, :], waT.h[d], waT)
            kb.load("sp", wa2_sb[d], wa2_sb[d][0:16, :], wa2.h[d], wa2)
            for kc in range(8):
                pz = banks[1]
                kb.op("pe", lambda e: e.matmul(pz[:, 0:128], lhsT=waT_sb[d][:, kc * 128:(kc + 1) * 128], rhs=wa2_sb[d][:], start=True, stop=True),
                      reads=[waT_sb[d], wa2_sb[d]], writes=[pz])
                kb.op("dve", lambda e: e.tensor_copy(out=w_sb[:, kc, 768 + d * 128:768 + (d + 1) * 128], in_=pz[:, 0:128]), reads=[pz], writes=[w_sb])
        for j in range(2):
            for kc in range(8):
                kb.op("dve", lambda e: e.tensor_scalar(out=wq[j][:, kc, :], in0=w_sb[:, kc, :], scalar1=gs[:, kc, j:j + 1], scalar2=None, op0=ALU.mult),
                      reads=[w_sb, gs], writes=[wq[j]])
                kb.op("dve", lambda e: e.tensor_scalar(out=shiftbc[:, kc, :], in0=zer[:], scalar1=mod[:, kc, j:j + 1], scalar2=None, op0=ALU.add),
                      reads=[zer, mod], writes=[shiftbc])
            for half in range(2):
                pb = banks[2 + half]
                for kc in range(8):
                    kb.op("pe", lambda e: e.matmul(pb[:, :], lhsT=shiftbc[:, kc, :], rhs=w_sb[:, kc, half * 512:(half + 1) * 512], start=(kc == 0), stop=(kc == 7)),
                          reads=[shiftbc, w_sb], writes=[pb])
                kb.op("dve", lambda e: e.tensor_copy(out=bias[j][:, half * 512:(half + 1) * 512], in_=pb[:, :]), reads=[pb], writes=[bias[j]])
            kb.op("dve", lambda e: e.tensor_tensor(out=bias[j][:, 768:1024], in0=bias[j][:, 768:1024], in1=smallb[:, 0:256], op=ALU.add), reads=[bias[j], smallb], writes=[bias[j]])
        kb.barrier()
    kb.barrier()
    pesA.close()

    pesB = ExitStack()
    xt = dbl("xt", [128, 1024], F32, 2, pesB)
    junk = kb.sb("junk", [128, 1024], BF16, pesB)
    st1 = dbl("st1", [128, 4], F32, 2, pesB)
    xn = dbl("xn", [128, 1024], BF16, 2, pesB)
    xnT = dbl("xnT", [128, 1024], BF16, 2, pesB)
    pj = dbl("pj", [128, 1024], F32, 2, pesB)
    ez = dbl("ez", [128, 256], F32, 2, pesB)
    one_col = kb.sb("one_col", [128, 1], F32, pesB)
    kb.op("pool", lambda e: e.memset(one_col[:], 1.0), writes=[one_col])

    def proj_tile(i, src, row0, is_ctx, drow):
        p = i % 2
        j = 1 if is_ctx else 0
        if is_ctx:
            c = row0 // 128
            for hf in range(2):
                r = ag_row(4096, 2 * c + hf, 256, 4224)
                kb.load("sp", xt[p], xt[p][hf * 64:(hf + 1) * 64, :], x2out.h[r:r + 64, :], x2out)
                yield
        else:
            t = row0 // 128
            r = ag_row((t % 32) * 128, t // 32, 256, 4224)
            kb.load("sp", xt[p], xt[p][:], x2out.h[r:r + 128, :], x2out)
            yield
        kb.op("act", lambda e: e.activation(out=junk[:], in_=xt[p][:], func=AF.Square, accum_out=st1[p][:, 0:1]), reads=[xt[p]], writes=[junk, st1[p]])
        yield
        rstd_chain(st1[p], 0, 1, 2, 1, 1.0 / 1024)
        kb.op("act", lambda e: e.activation(out=xn[p][:], in_=xt[p][:], func=AF.Copy, scale=st1[p][:, 2:3]), reads=[xt[p], st1[p]], writes=[xn[p]])
        yield
        psT = banks[p]
        for kc in range(8):
            kb.op("pe", lambda e: e.transpose(out=bfv(psT)[:, kc * 128:(kc + 1) * 128], in_=xn[p][:, kc * 128:(kc + 1) * 128], identity=identb[:]),
                  reads=[xn[p], identb], writes=[psT])
            yield
        kb.op("dve", lambda e: e.tensor_copy(out=xnT[p][:], in_=bfv(psT)[:, 0:1024]), reads=[psT], writes=[xnT[p]])
        yield
        for half in range(2):
            pp = banks[2 + 2 * p + half]
            for kc in range(8):
                kb.op("pe", lambda e: e.matmul(pp[:, :], lhsT=xnT[p][:, kc * 128:(kc + 1) * 128], rhs=wq[j][:, kc, half * 512:(half + 1) * 512], start=(kc == 0), stop=(kc == 7)),
                      reads=[xnT[p], wq[j]], writes=[pp])
                yield
            kb.op("dve", lambda e: e.tensor_tensor(out=pj[p][:, half * 512:(half + 1) * 512], in0=pp[:, :], in1=bias[j][:, half * 512:(half + 1) * 512], op=ALU.add),
                  reads=[pp, bias[j]], writes=[pj[p]])
            yield
        kb.op("act", lambda e: e.activation(out=ez[p][:], in_=pj[p][:, 768:1024], func=AF.Exp, scale=-1.0), reads=[pj[p]], writes=[ez[p]])
        yield
        kb.op("act", lambda e: e.activation(out=pj[p][:, 768:1024], in_=ez[p][:], func=AF.Ln, bias=one_col[:, 0:1]), reads=[ez[p], one_col], writes=[pj[p]])
        yield
        kb.store("sp", proj, proj.h[drow:drow + 128, :], pj[p], pj[p][:])
        yield

    gens = []
    i = 0
    for c in range(2):
        gens.append(proj_tile(i, None, c * 128, True, S + c * 128))
        i += 1
    for t in range(n_lat_tiles):
        gens.append(proj_tile(i, None, t * 128, False, t * 128))
        i += 1
    interleave(gens, 2)
    kb.barrier()
    pesB.close()

    mask = []
    for d in range(2):
        mf = kb.sb("mask%d" % d, [128, 128], F32)
        kb.op("pool", lambda e: e.memset(mf[:], 1.0), writes=[mf])
        if d == 0:
            kb.op("pool", lambda e: e.affine_select(out=mf[:], in_=mf[:], pattern=[[1, 128]], compare_op=ALU.is_ge, fill=0.0, base=0, channel_multiplier=-1), reads=[mf], writes=[mf])
        else:
            kb.op("pool", lambda e: e.affine_select(out=mf[:], in_=mf[:], pattern=[[-1, 128]], compare_op=ALU.is_ge, fill=0.0, base=0, channel_multiplier=1), reads=[mf], writes=[mf])
        mask.append(mf)
    LS = -1.0 / 16
    maskS = []
    for d in range(2):
        ms_ = kb.sb("maskS%d" % d, [128, 128], F32)
        kb.op("dve", lambda e: e.tensor_scalar(out=ms_[:], in0=mask[d][:], scalar1=LS, scalar2=None, op0=ALU.mult), reads=[mask[d]], writes=[ms_])
        maskS.append(ms_)
    ones_f = kb.sb("ones_f", [128, 128], F32)
    kb.op("pool", lambda e: e.memset(ones_f[:], -1.0 / 16), writes=[ones_f])
    Sst = kb.sb("Sst", [128, 256], F32)
    Sb = dbl("Sb", [128, 256], BF16)
    pt = dbl("pt", [128, 1024], F32, 3)
    bc = dbl("bc", [128, 128], F32)
    eb = dbl("eb", [128, 128], F32)
    enb = dbl("enb", [128, 128], F32)
    dlt = dbl("dlt", [128, 128], F32)
    dec = dbl("dec", [128, 1], F32)
    qt = dbl("qt", [128, 128], BF16)
    ktl = dbl("ktl", [128, 128], BF16)
    kh = dbl("kh", [128, 128], BF16)
    vb = dbl("vb", [128, 256], BF16)
    qkT = dbl("qkT", [128, 256], BF16)
    attm = dbl("attm", [128, 128], BF16)
    ofs = dbl("ofs", [128, 256], F32)
    osum = dbl("osum", [128, 256], F32)
    fst = dbl("fst", [128, 4], F32)
    sg = dbl("sg", [128, 256], F32)
    ogb = dbl("ogb", [128, 256], BF16)
    ogT_sb = dbl("ogT_sb", [128, 2, 128], BF16)
    QS = 128.0 ** -0.5
    step = [0]

    def gla_prep(c, row, d):
        p = c % 2
        B = banks[4 * p:4 * p + 4]
        t_ = pt[c % 3]
        kb.load("sp", t_, t_[:], proj.h[row:row + 128, :], proj)
        yield
        la = t_[:, 768 + d * 128:768 + (d + 1) * 128]
        kb.op("pe", lambda e: e.matmul(B[0][:, 0:128], lhsT=maskS[d][:], rhs=la, start=True, stop=True), reads=[maskS[d], t_], writes=[B[0]])
        yield
        kb.op("pe", lambda e: e.matmul(B[0][:, 128:256], lhsT=ones_f[:], rhs=la, start=True, stop=True), reads=[ones_f, t_], writes=[B[0]])
        yield
        kb.op("pe", lambda e: e.matmul(B[0][:, 256:384], lhsT=la, rhs=ones_f[:], start=True, stop=True), reads=[ones_f, t_], writes=[B[0]])
        yield
        kb.op("act", lambda e: e.copy(out=bc[p][:], in_=B[0][:, 0:128]), reads=[B[0]], writes=[bc[p]])
        yield
        kb.op("act", lambda e: e.activation(out=eb[p][:], in_=B[0][:, 0:128], func=AF.Exp), reads=[B[0]], writes=[eb[p]])
        yield
        kb.op("act", lambda e: e.activation(out=enb[p][:], in_=B[0][:, 0:128], func=AF.Exp, scale=-1.0), reads=[B[0]], writes=[enb[p]])
        yield
        kb.op("dve", lambda e: e.tensor_tensor(out=dlt[p][:], in0=B[0][:, 128:256], in1=bc[p][:], op=ALU.subtract), reads=[B[0], bc[p]], writes=[dlt[p]])
        yield
        kb.op("act", lambda e: e.activation(out=dlt[p][:], in_=dlt[p][:], func=AF.Exp), reads=[dlt[p]], writes=[dlt[p]])
        yield
        kb.op("act", lambda e: e.activation(out=dec[p][:], in_=B[0][:, 256:257], func=AF.Exp), reads=[B[0]], writes=[dec[p]])
        yield
        kb.op("dve", lambda e: e.scalar_tensor_tensor(out=qt[p][:], in0=t_[:, 0:128], scalar=QS, in1=eb[p][:], op0=ALU.mult, op1=ALU.mult), reads=[t_, eb[p]], writes=[qt[p]])
        yield
        kb.op("dve", lambda e: e.tensor_tensor(out=ktl[p][:], in0=t_[:, 128:256], in1=enb[p][:], op=ALU.mult), reads=[t_, enb[p]], writes=[ktl[p]])
        yield
        kb.op("pool", lambda e: e.tensor_tensor(out=kh[p][:], in0=t_[:, 128:256], in1=dlt[p][:], op=ALU.mult), reads=[t_, dlt[p]], writes=[kh[p]])
        yield
        kb.op("pool", lambda e: e.tensor_copy(out=vb[p][:], in_=t_[:, 256:512]), reads=[t_], writes=[vb[p]])
        yield
        kb.op("pe", lambda e: e.transpose(out=bfv(B[1])[:, 0:128], in_=qt[p][:], identity=identb[:]), reads=[qt[p], identb], writes=[B[1]])
        yield
        kb.op("pe", lambda e: e.transpose(out=bfv(B[1])[:, 128:256], in_=ktl[p][:], identity=identb[:]), reads=[ktl[p], identb], writes=[B[1]])
        yield
        kb.op("act", lambda e: e.copy(out=qkT[p][:], in_=bfv(B[1])[:, 0:256]), reads=[B[1]], writes=[qkT[p]])
        yield
        kb.op("pe", lambda e: e.matmul(B[2][:, 0:128], lhsT=qkT[p][:, 128:256], rhs=qkT[p][:, 0:128], start=True, stop=True), reads=[qkT[p]], writes=[B[2]])
        yield
        kb.op("dve", lambda e: e.tensor_tensor(out=attm[p][:], in0=B[2][:, 0:128], in1=mask[d][:], op=ALU.mult), reads=[B[2], mask[d]], writes=[attm[p]])
        yield

    def gla_fin(c, d, out_mode, out_row):
        p = c % 2
        B = banks[4 * p:4 * p + 4]
        t_ = pt[c % 3]
        sb_cur = Sb[c % 2]
        sb_next = Sb[(c + 1) % 2]
        if out_mode is not None:
            kb.op("pe", lambda e: e.matmul(B[3][:, 0:256], lhsT=qkT[p][:, 0:128], rhs=sb_cur[:], start=True, stop=False), reads=[qkT[p], sb_cur], writes=[B[3]])
            yield
            kb.op("pe", lambda e: e.matmul(B[3][:, 0:256], lhsT=attm[p][:], rhs=vb[p][:], start=False, stop=True), reads=[attm[p], vb[p]], writes=[B[3]])
            yield
        kb.op("pe", lambda e: e.matmul(B[2][:, 128:384], lhsT=kh[p][:], rhs=vb[p][:], start=True, stop=True), reads=[kh[p], vb[p]], writes=[B[2]])
        yield
        kb.op("dve", lambda e: e.scalar_tensor_tensor(out=Sst[:], in0=Sst[:], scalar=dec[p][:, 0:1], in1=B[2][:, 128:384], op0=ALU.mult, op1=ALU.add),
              reads=[Sst, dec[p], B[2]], writes=[Sst])
        yield
        kb.op("act", lambda e: e.copy(out=sb_next[:], in_=Sst[:]), reads=[Sst], writes=[sb_next])
        yield
        if out_mode == "store":
            kb.op("act", lambda e: e.copy(out=ofs[p][:], in_=B[3][:, 0:256]), reads=[B[3]], writes=[ofs[p]])
            yield
            kb.store("sp", of_d, of_d.h[out_row:out_row + 128, :], ofs[p], ofs[p][:])
            yield
        elif out_mode == "final":
            kb.load("sp", ofs[p], ofs[p][:], of_d.h[out_row:out_row + 128, :], of_d)
            yield
            kb.op("dve", lambda e: e.tensor_tensor(out=osum[p][:], in0=B[3][:, 0:256], in1=ofs[p][:], op=ALU.add), reads=[B[3], ofs[p]], writes=[osum[p]])
            yield
            kb.op("act", lambda e: e.activation(out=sg[p][:], in_=osum[p][:], func=AF.Square, accum_out=fst[p][:, 0:1]), reads=[osum[p]], writes=[sg[p], fst[p]])
            yield
            rstd_chain(fst[p], 0, 1, 2, 1, 1.0 / 256)
            kb.op("dve", lambda e: e.scalar_tensor_tensor(out=osum[p][:], in0=osum[p][:], scalar=fst[p][:, 2:3], in1=smallb[:, 256:512], op0=ALU.mult, op1=ALU.mult),
                  reads=[osum[p], fst[p], smallb], writes=[osum[p]])
            yield
            kb.op("act", lambda e: e.activation(out=sg[p][:], in_=t_[:, 512:768], func=AF.Silu), reads=[t_], writes=[sg[p]])
            yield
            kb.op("dve", lambda e: e.tensor_tensor(out=ogb[p][:], in0=osum[p][:], in1=sg[p][:], op=ALU.mult), reads=[osum[p], sg[p]], writes=[ogb[p]])
            yield
            for hh in range(2):
                kb.op("pe", lambda e: e.transpose(out=bfv(B[1])[:, 256 + hh * 128:256 + (hh + 1) * 128], in_=ogb[p][:, hh * 128:(hh + 1) * 128], identity=identb[:]),
                      reads=[ogb[p], identb], writes=[B[1]])
                yield
            kb.op("act", lambda e: e.copy(out=ogT_sb[p][:].rearrange("p a b -> p (a b)"), in_=bfv(B[1])[:, 256:512]), reads=[B[1]], writes=[ogT_sb[p]])
            yield
            tq_, tc_ = (out_row // 128) // 32, ((out_row // 128) % 32) * 128
            kb.store("sp", x3in, x3in.h[tq_ * 256:(tq_ + 1) * 256, tc_:tc_ + 128].rearrange("(a p) n -> p a n", p=128), ogT_sb[p], ogT_sb[p][:])
            yield

    def reset_state(c):
        kb.op("pool", lambda e: e.memset(Sst[:], 0.0), writes=[Sst])
        kb.op("pool", lambda e: e.memset(Sb[c % 2][:], 0.0), writes=[Sb[c % 2]])

    def run_scan(chunks, c0):
        n = len(chunks)
        for _ in gla_prep(c0, chunks[0][0], chunks[0][1]):
            pass
        for k in range(n):
            row, d, om, orow = chunks[k]
            gens = [gla_fin(c0 + k, d, om, orow)]
            if k + 1 < n:
                gens.append(gla_prep(c0 + k + 1, chunks[k + 1][0], chunks[k + 1][1]))
            interleave(gens, 2)
        return c0 + n

    fwd = [(S + c * 128, 0, None, None) for c in range(2)] + [(t * 128, 0, "store", t * 128) for t in range(n_lat_tiles)]
    bwd = [(S + c * 128, 1, None, None) for c in (1, 0)] + [(t * 128, 1, "final", t * 128) for t in range(n_lat_tiles - 1, -1, -1)]
    reset_state(0)
    cn = run_scan(fwd, 0)
    kb.barrier()
    reset_state(cn)
    run_scan(bwd, cn)
    print("l1a instructions:", kb.n_ins, "sems:", len(kb.sems))
    kb.end_stage()


def fop(v, n):
    return np.ascontiguousarray(np.asarray(v, np.float32).reshape(n, 128).T)


def host_l1a(inp):
    maps = []
    wi = inp["gla_w_in"][0]
    for b in range(2):
        sv = np.stack([inp["c"][b], inp["c_ctx"]], -1).reshape(8, 128, 2).transpose(1, 0, 2).reshape(128, 16).astype(np.float32)
        for h in range(4):
            w = np.concatenate([wi[:, h * 128:(h + 1) * 128], wi[:, 512 + h * 128:512 + (h + 1) * 128], wi[:, 1024 + h * 256:1024 + (h + 1) * 256],
                                wi[:, 2048 + h * 256:2048 + (h + 1) * 256]], axis=1)
            waT = np.ascontiguousarray(wi[:, 3072:3104].T.reshape(2, 16, 1024))
            wa2 = np.ascontiguousarray(inp["gla_w_a2"][0][:, :, h * 128:(h + 1) * 128])
            small = np.concatenate([inp["gla_b_a2"][0][0, h * 128:(h + 1) * 128], inp["gla_b_a2"][0][1, h * 128:(h + 1) * 128], inp["gla_norm_g"][0]]).astype(np.float32)
            maps.append({"svec": np.ascontiguousarray(sv),
                         "adaw": np.ascontiguousarray(inp["ada_w"][1][:, 0:2048]), "adab": fop(inp["ada_b"][1][0:2048], 16), "g1": fop(inp["norm1_g"][1], 8),
                         "w": np.ascontiguousarray(w), "waT": waT, "wa2": wa2, "small": small})
    return maps


RG = [[0, 1, 2, 3], [4, 5, 6, 7]]


def build_all():
    kb = KB()
    banks = [kb.ps("bank%d" % i) for i in range(8)]
    x1in = kb.dram("x1in", [512, 4224], BF16)
    x1out = kb.dram("x1out", [2048, 4224], BF16)
    x2in = kb.dram("x2in", [4224, 1024], F32)
    x2out = kb.dram("x2out", [4 * 4224, 1024], F32)
    x3in = kb.dram("x3in", [1024, 4096], BF16)
    x3out = kb.dram("x3out", [4096, 4096], BF16)
    build_l0a(kb, banks, x1in)
    kb.all_gather(x1in, x1out, RG, 64)
    build_b(0, kb, banks, x1out, None, x2in)
    kb.all_gather(x2in, x2out, RG, 256)
    build_l1a(kb, banks, x2out, x3in)
    kb.all_gather(x3in, x3out, RG, 128)
    build_b(1, kb, banks, x3out, x2in, None)
    print("total instructions:", kb.n_ins, "sems:", len(kb.sems))
    return kb.finish()


def kernel(**inputs):
    inp = {k: np.asarray(v) for k, v in inputs.items()}
    parts = [("a0_", host_l0a(inp)), ("b0_", host_b(0, inp)), ("a1_", host_l1a(inp)), ("b1_", host_b(1, inp))]
    maps = []
    for c in range(8):
        m = {}
        for pre, ms in parts:
            for k, v in ms[c].items():
                m[pre + k] = v
        maps.append(m)
    nc = build_all()
    res = run_bass_kernel_spmd(nc, maps, core_ids=list(range(8)))
    out = np.zeros((2, 16384, 1024), np.float32)
    for b in range(2):
        for jq in range(4):
            out[b, jq * 4096:(jq + 1) * 4096] = np.asarray(res.results[b * 4 + jq]["b1_hout"])[:4096]
    return out
```

```python
import numpy as np
from contextlib import ExitStack
import concourse.bass as bass
import concourse.mybir as mybir
from concourse.bass_utils import run_bass_kernel_spmd
import ml_dtypes

F32 = mybir.dt.float32
BF16 = mybir.dt.bfloat16
I32 = mybir.dt.int32
AF = mybir.ActivationFunctionType
ALU = mybir.AluOpType
AX = mybir.AxisListType
NPBF16 = ml_dtypes.bfloat16


def interleave(gens, width):
    active = []
    it = iter(gens)
    while True:
        while len(active) < width:
            g = next(it, None)
            if g is None:
                break
            active.append(g)
        if not active:
            break
        for g in list(active):
            try:
                next(g)
            except StopIteration:
                active.remove(g)


def ag_row(i, rank, chunk_rows, total_rows, world=4):
    r0 = (i // chunk_rows) * chunk_rows
    n = min(chunk_rows, total_rows - r0)
    return world * r0 + rank * n + (i - r0)


class T:
    def __init__(self, h, name, kind):
        self.h = h
        self.name = name
        self.kind = kind
        self.w = None
        self.r = {}
        self.dkey = None

    def __getitem__(self, idx):
        return self.h[idx]


class KB:
    def __init__(self):
        self.nc = bass.Bass("TRN2", target_bir_lowering=False)
        nc = self.nc
        self.es = ExitStack()
        self.eng = {"pe": nc.tensor, "act": nc.scalar, "dve": nc.vector, "pool": nc.gpsimd, "sp": nc.sync}
        self.sems = {}
        self.cnt = {}
        self.seen = {e: {} for e in self.eng}
        for e in self.eng:
            self.sems[e] = self.es.enter_context(nc.semaphore("e_" + e))
            self.cnt[e] = 0
        self.issued = {}
        self.n_ins = 0
        self.outs = []
        self._uid = 0
        self.cur = self.es
        self.prefix = ""
        self.tiles = []
        self.free_dsems = []
        self.stage_tiles0 = 0

    def sb(self, name, shape, dt, es=None):
        h = (es or self.cur).enter_context(self.nc.sbuf_tensor(self.prefix + name, list(shape), dt))
        t = T(h, name, "sb")
        self.tiles.append(t)
        return t

    def ps(self, name, shape=(128, 512), dt=F32):
        h = self.es.enter_context(self.nc.psum_tensor(name, list(shape), dt))
        t = T(h, name, "ps")
        self.tiles.append(t)
        return t

    def dram(self, name, shape, dt, kind="Internal"):
        h = self.nc.dram_tensor(self.prefix + name, list(shape), dt, kind=kind)
        t = T(h.ap(), name, "dram")
        self.tiles.append(t)
        if kind == "ExternalOutput":
            self.outs.append(t)
        return t

    def _dsem(self, t):
        if t.dkey is None:
            self._uid += 1
            t.dkey = "d%d_%s" % (self._uid, t.name)
            if self.free_dsems:
                h, v = self.free_dsems.pop()
                self.sems[t.dkey] = h
                self.issued[t.dkey] = v
            else:
                self.sems[t.dkey] = self.es.enter_context(self.nc.semaphore(t.dkey))
                self.issued[t.dkey] = 0
        return t.dkey

    def begin_stage(self, prefix):
        self.prefix = prefix
        self.cur = ExitStack()
        self.stage_tiles0 = len(self.tiles)

    def end_stage(self):
        self.barrier()
        self.cur.close()
        self.cur = self.es
        for t in self.tiles[self.stage_tiles0:]:
            if t.kind == "sb" and t.dkey is not None:
                self.free_dsems.append((self.sems[t.dkey], self.issued[t.dkey]))
                del self.issued[t.dkey]
                del self.sems[t.dkey]
                t.dkey = None
        for t in self.tiles:
            t.w = None
            t.r = {}
        for e in self.eng:
            self._uid += 1
            self.sems[e] = self.es.enter_context(self.nc.semaphore("e%d_%s" % (self._uid, e)))
            self.cnt[e] = 0
        self.seen = {e: {} for e in self.eng}
        self.prefix = ""

    def all_gather(self, src, dst, groups, chunk_rows):
        self.barrier()
        self._uid += 1
        sem = self.es.enter_context(self.nc.semaphore("cc%d" % self._uid))
        R = src.h.shape[0]
        k = 0
        for r0 in range(0, R, chunk_rows):
            n = min(chunk_rows, R - r0)
            self.nc.gpsimd.collective_compute("AllGather", ALU.bypass, replica_groups=groups, ins=[src.h[r0:r0 + n, :]],
                                              outs=[dst.h[4 * r0:4 * r0 + 4 * n, :]]).then_inc(sem, 1)
            k += 1
        self.nc.gpsimd.wait_ge(sem, k)
        if not hasattr(self, "_fence"):
            self._fence = T(self.es.enter_context(self.nc.sbuf_tensor("cc_fence", [128, 8], F32)), "cc_fence", "sb")
            self.tiles.append(self._fence)
        f = self._fence
        self.op("pool", lambda e: e.memset(f[:], 0.0), writes=[f])
        for en in self.eng:
            if en != "pool":
                self._waits(en, {"pool": self.cnt["pool"]})
        self.n_ins += k + 1

    def _deps(self, en, reads, writes, is_dma=False):
        deps = {}

        def add(key, val, kind):
            if key == en:
                if en == "pe" or kind == "war":
                    return
            if is_dma and kind == "waw" and key in self.issued:
                return
            deps[key] = max(deps.get(key, 0), val)

        for t in reads:
            if t.w is not None:
                add(t.w[0], t.w[1], "raw")
            if t.kind == "ps":
                for k, v in t.r.items():
                    if k != en:
                        add(k, v, "rar")
        for t in writes:
            if t.w is not None:
                add(t.w[0], t.w[1], "waw")
            for k, v in t.r.items():
                add(k, v, "war")
        return deps

    def _waits(self, en, deps):
        e = self.eng[en]
        for key, val in deps.items():
            if key in self.issued:
                val = self.issued[key]
            if self.seen[en].get(key, 0) >= val:
                continue
            e.wait_ge(self.sems[key], val)
            self.seen[en][key] = val
            self.n_ins += 1

    def op(self, en, fn, reads=(), writes=()):
        self._waits(en, self._deps(en, reads, writes))
        ins = fn(self.eng[en])
        self.cnt[en] += 1
        self.n_ins += 1
        ins.then_inc(self.sems[en], 1)
        c = self.cnt[en]
        for t in reads:
            t.r[en] = c
        for t in writes:
            t.w = (en, c)
            t.r = {}
        return ins

    def dma(self, q, fn, reads=(), writes=()):
        self._waits(q, self._deps(q, reads, writes, is_dma=True))
        cand = [t for t in writes if t.kind != "dram"] or [t for t in reads if t.kind != "dram"] or list(writes) or list(reads)
        key = self._dsem(cand[0])
        ins = fn(self.eng[q])
        self.issued[key] += 16
        self.n_ins += 1
        ins.then_inc(self.sems[key], 16)
        v = self.issued[key]
        for t in reads:
            t.r[key] = v
        for t in writes:
            t.w = (key, v)
            t.r = {}
        return ins

    def load(self, q, dst_t, dst_ap, src_ap, src_t=None, **kw):
        return self.dma(q, lambda e: e.dma_start(out=dst_ap, in_=src_ap, **kw),
                        reads=[src_t] if src_t is not None else [], writes=[dst_t])

    def store(self, q, dst_t, dst_ap, src_t, src_ap, **kw):
        return self.dma(q, lambda e: e.dma_start(out=dst_ap, in_=src_ap, **kw), reads=[src_t], writes=[dst_t])

    def finish(self):
        deps = {}
        for t in self.outs:
            if t.w is not None:
                deps[t.w[0]] = max(deps.get(t.w[0], 0), t.w[1])
        self._waits("sp", deps)
        self.es.close()
        return self.nc

    def barrier(self):
        for en in self.eng:
            deps = {}
            for k in self.eng:
                if k != en and self.cnt[k] > 0:
                    deps[k] = self.cnt[k]
            for k, v in self.issued.items():
                if v > 0:
                    deps[k] = v
            self._waits(en, deps)

    def identity(self, name, dt):
        f = self.sb(name + "_f", [128, 128], F32)
        self.op("pool", lambda e: e.memset(f[:], 0.0), writes=[f])
        self.op("pool", lambda e: e.affine_select(out=f[:], in_=f[:], pattern=[[-1, 128]], compare_op=ALU.not_equal,
                                                  fill=1.0, base=0, channel_multiplier=1), reads=[f], writes=[f])
        if dt == F32:
            return f
        b = self.sb(name, [128, 128], dt)
        self.op("pool", lambda e: e.tensor_copy(out=b[:], in_=f[:]), reads=[f], writes=[b])
        return b

EPS = 1e-6
S = 16384
LC = 256

NKT = (S + LC) // 128
ATT_NSPLIT = 512


def build_l0a(kb, banks, x1in, n_groups=32, debug=False):
    kb.begin_stage("a0_")
    x = kb.dram("x", [S, 1024], F32, "ExternalInput")
    ctx = kb.dram("ctx", [LC, 1024], F32, "ExternalInput")
    svec = kb.dram("svec", [128, 16], F32, "ExternalInput")
    adaw = kb.dram("adaw", [1024, 2048], F32, "ExternalInput")
    adab = kb.dram("adab", [128, 16], F32, "ExternalInput")
    g1 = kb.dram("g1", [128, 8], F32, "ExternalInput")
    w = kb.dram("w", [1024, 384], F32, "ExternalInput")
    small = kb.dram("small", [640], F32, "ExternalInput")
    cos4 = kb.dram("cos4", [S, 256], F32, "ExternalInput")
    sin4 = kb.dram("sin4", [S, 256], F32, "ExternalInput")
    zpad = kb.sb("zpad", [128, 4, 64], BF16)
    kb.op("pool", lambda e: e.memset(zpad[:], 0.0), writes=[zpad])
    kb.store("sp", x1in, x1in.h[:, 4160:4224].rearrange("(q p) n -> p q n", p=128), zpad, zpad[:])

    def bfv(t):
        return t[:].bitcast(BF16)

    identb = kb.identity("identb", BF16)
    smallb = kb.sb("smallb", [128, 640], F32)
    kb.load("sp", smallb, smallb[:], small.h.partition_broadcast(128), small)

    tmp64 = kb.sb("tmp64", [128, 2, 64], F32)
    dots = kb.sb("dots", [128, 4], F32)
    kb.op("dve", lambda e: e.tensor_tensor(out=tmp64[:, 0, :], in0=smallb[:, 256:320], in1=smallb[:, 320:384], op=ALU.mult), reads=[smallb], writes=[tmp64])
    kb.op("dve", lambda e: e.tensor_tensor(out=tmp64[:, 1, :], in0=smallb[:, 384:448], in1=smallb[:, 448:512], op=ALU.mult), reads=[smallb], writes=[tmp64])
    kb.op("dve", lambda e: e.tensor_reduce(out=dots[:, 0:2], in_=tmp64[:], axis=AX.X, op=ALU.add), reads=[tmp64], writes=[dots])
    kb.op("act", lambda e: e.activation(out=dots[:, 2:4], in_=dots[:, 0:2], func=AF.Exp), reads=[dots], writes=[dots])
    neglam = kb.sb("neglam", [128, 1], F32)
    kb.op("dve", lambda e: e.scalar_tensor_tensor(out=neglam[:], in0=dots[:, 3:4], scalar=-0.2, in1=dots[:, 2:3], op0=ALU.add, op1=ALU.subtract), reads=[dots], writes=[neglam])
    subg_s = kb.sb("subg_s", [128, 128], F32)
    kb.op("dve", lambda e: e.tensor_scalar(out=subg_s[:], in0=smallb[:, 512:640], scalar1=0.8, scalar2=None, op0=ALU.mult), reads=[smallb], writes=[subg_s])

    s_sb = kb.sb("s_sb", [128, 16], F32)
    kb.load("sp", s_sb, s_sb[:], svec.h, svec)
    kb.op("act", lambda e: e.activation(out=s_sb[:], in_=s_sb[:], func=AF.Silu), reads=[s_sb], writes=[s_sb])
    adab_sb = kb.sb("adab_sb", [128, 16], F32)
    kb.load("sp", adab_sb, adab_sb[:], adab.h, adab)
    g1_sb = kb.sb("g1_sb", [128, 8], F32)
    kb.load("sp", g1_sb, g1_sb[:], g1.h, g1)
    mod = kb.sb("mod", [128, 16, 2], F32)
    gs = kb.sb("gs", [128, 8, 2], F32)
    zer = kb.sb("zer", [128, 128], F32)
    wq = [kb.sb("wq%d" % j, [128, 8, 384], BF16) for j in range(2)]
    bias = [kb.sb("bias%d" % j, [128, 384], F32) for j in range(2)]
    p0 = ExitStack()
    adaw_sb = kb.sb("adaw_sb", [128, 8, 512], F32, p0)
    pm = banks[0]
    for v in range(4):
        kb.load("sp", adaw_sb, adaw_sb[:], adaw.h[:, v * 512:(v + 1) * 512].rearrange("(kc p) n -> p kc n", p=128), adaw)
        for oc in range(4):
            g = v * 4 + oc
            for kc in range(8):
                kb.op("pe", lambda e: e.matmul(pm[:, g * 2:g * 2 + 2], lhsT=adaw_sb[:, kc, oc * 128:(oc + 1) * 128],
                                              rhs=s_sb[:, kc * 2:kc * 2 + 2], start=(kc == 0), stop=(kc == 7)),
                      reads=[adaw_sb, s_sb], writes=[pm])
    pm3 = pm[:, 0:32].rearrange("p (g j) -> p g j", j=2)
    for j in range(2):
        kb.op("dve", lambda e: e.tensor_tensor(out=mod[:, :, j], in0=pm3[:, :, j], in1=adab_sb[:], op=ALU.add), reads=[pm, adab_sb], writes=[mod])
        kb.op("dve", lambda e: e.scalar_tensor_tensor(out=gs[:, :, j], in0=mod[:, 8:16, j], scalar=1.0, in1=g1_sb[:], op0=ALU.add, op1=ALU.mult),
              reads=[mod, g1_sb], writes=[gs])

    w_sb = kb.sb("w_sb", [128, 8, 384], F32, p0)
    kb.load("sp", w_sb, w_sb[:], w.h.rearrange("(kc p) n -> p kc n", p=128), w)
    kb.op("pool", lambda e: e.memset(zer[:], 0.0), writes=[zer])
    shiftbc = kb.sb("shiftbc", [128, 8, 128], F32, p0)
    for j in range(2):
        for kc in range(8):
            kb.op("dve", lambda e: e.tensor_scalar(out=wq[j][:, kc, :], in0=w_sb[:, kc, :], scalar1=gs[:, kc, j:j + 1], scalar2=None, op0=ALU.mult),
                  reads=[w_sb, gs], writes=[wq[j]])
            kb.op("dve", lambda e: e.tensor_scalar(out=shiftbc[:, kc, :], in0=zer[:], scalar1=mod[:, kc, j:j + 1], scalar2=None, op0=ALU.add),
                  reads=[zer, mod], writes=[shiftbc])
        pb = banks[1]
        for kc in range(8):
            kb.op("pe", lambda e: e.matmul(pb[:, 0:384], lhsT=shiftbc[:, kc, :], rhs=w_sb[:, kc, :], start=(kc == 0), stop=(kc == 7)),
                  reads=[shiftbc, w_sb], writes=[pb])
        kb.op("dve", lambda e: e.tensor_copy(out=bias[j][:], in_=pb[:, 0:384]), reads=[pb], writes=[bias[j]])

    kb.barrier()
    p0.close()
    QT = kb.sb("QT", [128, S + LC], BF16)
    KTm = [kb.sb("KT%d" % m, [128, S + LC], BF16) for m in range(2)]
    kb.op("pool", lambda e: e.memset(KTm[0][64:128, :], 0.0), writes=[KTm[0]])
    kb.op("pool", lambda e: e.memset(KTm[1][0:64, :], 0.0), writes=[KTm[1]])
    Vx = kb.sb("Vx", [128, NKT, 129], BF16)
    kb.op("pool", lambda e: e.memset(Vx[:, :, 128:129], 1.0), writes=[Vx])

    def dbl(name, shape, dt, n=2, es=None):
        return [kb.sb("%s%d" % (name, i), shape, dt, es) for i in range(n)]

    p2 = ExitStack()

    xt = dbl("xt", [128, 1024], F32, 2, p2)
    junk = kb.sb("junk", [128, 1024], BF16, p2)
    st1 = dbl("st1", [128, 4], F32, 2, p2)
    xn = dbl("xn", [128, 1024], BF16, 2, p2)
    xnT = dbl("xnT", [128, 1024], BF16, 2, p2)
    qkv = dbl("qkv", [128, 384], F32, 2, p2)
    cs = dbl("cs", [128, 256], F32, 2, p2)
    sn = dbl("sn", [128, 256], F32, 2, p2)
    sq = dbl("sq", [128, 256], F32, 2, p2)
    st2 = dbl("st2", [128, 12], F32, 2, p2)
    qkn = dbl("qkn", [128, 256], F32, 2, p2)
    sw = dbl("sw", [128, 256], F32, 2, p2)
    t1 = dbl("t1", [128, 256], F32, 2, p2)
    rr = dbl("rr", [128, 256], BF16, 2, p2)

    def rstd_chain(stt, c_in, c_tmp, c_out, n, inv_n, srcs):
        kb.op("dve", lambda e: e.tensor_scalar(out=stt[:, c_tmp:c_tmp + n], in0=stt[:, c_in:c_in + n], scalar1=inv_n, scalar2=EPS, op0=ALU.mult, op1=ALU.add),
              reads=[stt], writes=[stt])
        kb.op("act", lambda e: e.activation(out=stt[:, c_tmp:c_tmp + n], in_=stt[:, c_tmp:c_tmp + n], func=AF.Sqrt), reads=[stt], writes=[stt])
        kb.op("dve", lambda e: e.reciprocal(out=stt[:, c_out:c_out + n], in_=stt[:, c_tmp:c_tmp + n]), reads=[stt], writes=[stt])

    def proj_tile(i, src, row0, is_ctx, qcol, kcol, kt):
        p = i % 2
        j = 1 if is_ctx else 0
        kb.load("sp", xt[p], xt[p][:], src.h[row0:row0 + 128, :], src)
        yield
        if not is_ctx:
            kb.load("pool", cs[p], cs[p][:], cos4.h[row0:row0 + 128, :], cos4)
            yield
            kb.load("pool", sn[p], sn[p][:], sin4.h[row0:row0 + 128, :], sin4)
            yield
        kb.op("act", lambda e: e.activation(out=junk[:], in_=xt[p][:], func=AF.Square, accum_out=st1[p][:, 0:1]), reads=[xt[p]], writes=[junk, st1[p]])
        yield
        rstd_chain(st1[p], 0, 1, 2, 1, 1.0 / 1024, None)
        kb.op("act", lambda e: e.activation(out=xn[p][:], in_=xt[p][:], func=AF.Copy, scale=st1[p][:, 2:3]), reads=[xt[p], st1[p]], writes=[xn[p]])
        yield
        psT = banks[p]
        for kc in range(8):
            kb.op("pe", lambda e: e.transpose(out=bfv(psT)[:, kc * 128:(kc + 1) * 128], in_=xn[p][:, kc * 128:(kc + 1) * 128], identity=identb[:]),
                  reads=[xn[p], identb], writes=[psT])
            yield
        kb.op("dve", lambda e: e.tensor_copy(out=xnT[p][:], in_=bfv(psT)[:, 0:1024]), reads=[psT], writes=[xnT[p]])
        yield
        pp = banks[2 + p]
        for kc in range(8):
            kb.op("pe", lambda e: e.matmul(pp[:, 0:384], lhsT=xnT[p][:, kc * 128:(kc + 1) * 128], rhs=wq[j][:, kc, :], start=(kc == 0), stop=(kc == 7)),
                  reads=[xnT[p], wq[j]], writes=[pp])
            yield
        kb.op("dve", lambda e: e.tensor_tensor(out=qkv[p][:], in0=pp[:, 0:384], in1=bias[j][:], op=ALU.add), reads=[pp, bias[j]], writes=[qkv[p]])
        yield
        kb.op("pool", lambda e: e.tensor_copy(out=Vx[:, kt, 0:128], in_=qkv[p][:, 256:384]), reads=[qkv[p]], writes=[Vx])
        yield
        kb.op("act", lambda e: e.activation(out=sq[p][:], in_=qkv[p][:, 0:256], func=AF.Square), reads=[qkv[p]], writes=[sq[p]])
        yield
        kb.op("dve", lambda e: e.tensor_reduce(out=st2[p][:, 0:4], in_=sq[p][:].rearrange("p (g d) -> p g d", g=4), axis=AX.X, op=ALU.add),
              reads=[sq[p]], writes=[st2[p]])
        yield
        rstd_chain(st2[p], 0, 4, 8, 4, 1.0 / 64, None)
        for g in range(4):
            kb.op("dve", lambda e: e.scalar_tensor_tensor(out=qkn[p][:, g * 64:(g + 1) * 64], in0=qkv[p][:, g * 64:(g + 1) * 64], scalar=st2[p][:, 8 + g:9 + g],
                                                          in1=smallb[:, g * 64:(g + 1) * 64], op0=ALU.mult, op1=ALU.mult),
                  reads=[qkv[p], st2[p], smallb], writes=[qkn[p]])
            yield
        if is_ctx:
            kb.op("pool", lambda e: e.tensor_copy(out=rr[p][:], in_=qkn[p][:]), reads=[qkn[p]], writes=[rr[p]])
            yield
        else:
            q5 = qkn[p][:].rearrange("p (a h d) -> p a h d", h=2, d=16)
            s5 = sw[p][:].rearrange("p (a h d) -> p a h d", h=2, d=16)
            kb.op("pool", lambda e: e.tensor_copy(out=s5[:, :, 0, :], in_=q5[:, :, 1, :]), reads=[qkn[p]], writes=[sw[p]])
            yield
            kb.op("pool", lambda e: e.tensor_copy(out=s5[:, :, 1, :], in_=q5[:, :, 0, :]), reads=[qkn[p]], writes=[sw[p]])
            yield
            kb.op("pool", lambda e: e.tensor_tensor(out=sw[p][:], in0=sw[p][:], in1=sn[p][:], op=ALU.mult), reads=[sw[p], sn[p]], writes=[sw[p]])
            yield
            kb.op("dve", lambda e: e.tensor_tensor(out=t1[p][:], in0=qkn[p][:], in1=cs[p][:], op=ALU.mult), reads=[qkn[p], cs[p]], writes=[t1[p]])
            yield
            kb.op("dve", lambda e: e.tensor_tensor(out=rr[p][:], in0=t1[p][:], in1=sw[p][:], op=ALU.add), reads=[t1[p], sw[p]], writes=[rr[p]])
            yield
        pq = banks[4 + p]
        for hh in range(2):
            kb.op("pe", lambda e: e.transpose(out=bfv(pq)[:, hh * 128:(hh + 1) * 128], in_=rr[p][:, hh * 128:(hh + 1) * 128], identity=identb[:]),
                  reads=[rr[p], identb], writes=[pq])
            yield
        kb.op("act", lambda e: e.copy(out=QT[:, qcol:qcol + 128], in_=bfv(pq)[:, 0:128]), reads=[pq], writes=[QT])
        yield
        kb.op("act", lambda e: e.copy(out=KTm[0][0:64, kcol:kcol + 128], in_=bfv(pq)[0:64, 128:256]), reads=[pq], writes=[KTm[0]])
        kb.op("act", lambda e: e.copy(out=KTm[1][64:128, kcol:kcol + 128], in_=bfv(pq)[64:128, 128:256]), reads=[pq], writes=[KTm[1]])
        yield

    gens = []
    i = 0
    for c in range(LC // 128):
        gens.append(proj_tile(i, ctx, c * 128, True, S + c * 128, c * 128, c))
        i += 1
    for t in range(S // 128):
        gens.append(proj_tile(i, x, t * 128, False, t * 128, LC + t * 128, LC // 128 + t))
        i += 1
    interleave(gens, 2)
    kb.barrier()
    p2.close()

    ST = banks[0:3]
    OT = [banks[4], banks[5]]
    PL = [banks[6], banks[7]]
    PS_ = banks[3]
    PT = dbl("pt", [128, 512], BF16, 4)
    Pacc = [kb.sb("pacc%d" % m, [128, 512], F32) for m in range(2)]
    ones_bb = kb.sb("ones_bb", [128, 128], BF16)
    kb.op("pool", lambda e: e.memset(ones_bb[:], 1.0), writes=[ones_bb])
    ones_ff = kb.sb("ones_ff", [128, 128], F32)
    kb.op("pool", lambda e: e.memset(ones_ff[:], 1.0), writes=[ones_ff])
    subg_col = kb.sb("subg_col", [128, 1], F32)
    kb.load("sp", subg_col, subg_col[:], small.h[512:640].rearrange("(p o) -> p o", o=1), small)
    kb.op("dve", lambda e: e.tensor_scalar(out=subg_col[:], in0=subg_col[:], scalar1=0.8, scalar2=None, op0=ALU.mult), reads=[subg_col], writes=[subg_col])
    rlb = dbl("rlb", [128, 512], F32)
    eo = dbl("eo", [128, 512], F32)
    esq = kb.sb("esq", [128, 512], F32)
    outT = dbl("outT", [128, 512], BF16)
    gcount = [0]
    NSPLIT = ATT_NSPLIT

    def attend(qc0, nq, kts):
        steps = [(m, idx, kt) for m in range(2) for idx, kt in enumerate(kts)]
        nk = len(kts)
        gi = gcount[0]
        gcount[0] += 1

        def score(s):
            m, idx, kt = steps[s]
            st = ST[s % 3]
            for c0 in range(0, nq, NSPLIT):
                kb.op("pe", lambda e: e.matmul(st[:, c0:min(nq, c0 + NSPLIT)], lhsT=KTm[m][:, kt * 128:(kt + 1) * 128], rhs=QT[:, qc0 + c0:qc0 + min(nq, c0 + NSPLIT)],
                                              start=True, stop=True), reads=[KTm[m], QT], writes=[st])

        used = {}

        def rest(s):
            m, idx, kt = steps[s]
            st = ST[s % 3]
            pt = PT[s % 4]
            kb.op("act", lambda e: e.activation(out=pt[:, 0:nq], in_=st[:, 0:nq], func=AF.Exp, scale=0.125), reads=[st], writes=[pt])
            for c0 in range(0, nq, NSPLIT):
                kb.op("pe", lambda e: e.matmul(OT[m][:, c0:min(nq, c0 + NSPLIT)], lhsT=Vx[:, kt, 0:128], rhs=pt[:, c0:min(nq, c0 + NSPLIT)], start=(idx == 0 and c0 == 0), stop=(idx == nk - 1),
                                              skip_group_check=True), reads=[pt, Vx], writes=[OT[m]])
            if s + 3 < len(steps):
                score(s + 3)
            if idx % 3 == 2:
                kb.op("pe", lambda e: e.matmul(PL[m][:, 0:nq], lhsT=ones_bb[:], rhs=pt[:, 0:nq], start=((m, "pe") not in used), stop=False), reads=[ones_bb, pt], writes=[PL[m]])
                used[(m, "pe")] = True
            elif (m, "dve") not in used:
                used[(m, "dve")] = True
                kb.op("dve", lambda e: e.tensor_copy(out=Pacc[m][:, 0:nq], in_=pt[:, 0:nq]), reads=[pt], writes=[Pacc[m]])
            else:
                kb.op("dve", lambda e: e.tensor_tensor(out=Pacc[m][:, 0:nq], in0=pt[:, 0:nq], in1=Pacc[m][:, 0:nq], op=ALU.add), reads=[pt, Pacc[m]], writes=[Pacc[m]])

        for s0 in range(min(3, len(steps))):
            score(s0)
        for s in range(len(steps)):
            rest(s)
        for m in range(2):
            kb.op("pe", lambda e: e.matmul(PL[m][:, 0:nq], lhsT=ones_ff[:], rhs=Pacc[m][:, 0:nq], start=((m, "pe") not in used), stop=True), reads=[ones_ff, Pacc[m]], writes=[PL[m]])
            kb.op("dve", lambda e: e.reciprocal(out=rlb[m][:, 0:nq], in_=PL[m][:, 0:nq]), reads=[PL[m]], writes=[rlb[m]])
            kb.op("dve", lambda e: e.tensor_tensor(out=eo[m][:, 0:nq], in0=OT[m][:, 0:nq], in1=rlb[m][:, 0:nq], op=ALU.mult), reads=[OT[m], rlb[m]], writes=[eo[m]])
        kb.op("dve", lambda e: e.scalar_tensor_tensor(out=eo[0][:, 0:nq], in0=eo[1][:, 0:nq], scalar=neglam[:, 0:1], in1=eo[0][:, 0:nq], op0=ALU.mult, op1=ALU.add),
              reads=[eo[1], neglam, eo[0]], writes=[eo[0]])
        kb.op("act", lambda e: e.activation(out=esq[:, 0:nq], in_=eo[0][:, 0:nq], func=AF.Square), reads=[eo[0]], writes=[esq])
        kb.op("pe", lambda e: e.matmul(PS_[:, 0:nq], lhsT=ones_ff[:], rhs=esq[:, 0:nq], start=True, stop=True), reads=[ones_ff, esq], writes=[PS_])
        kb.op("dve", lambda e: e.tensor_scalar(out=rlb[0][:, 0:nq], in0=PS_[:, 0:nq], scalar1=1.0 / 128, scalar2=EPS, op0=ALU.mult, op1=ALU.add), reads=[PS_], writes=[rlb[0]])
        kb.op("act", lambda e: e.activation(out=rlb[0][:, 0:nq], in_=rlb[0][:, 0:nq], func=AF.Sqrt), reads=[rlb[0]], writes=[rlb[0]])
        kb.op("dve", lambda e: e.reciprocal(out=rlb[1][:, 0:nq], in_=rlb[0][:, 0:nq]), reads=[rlb[0]], writes=[rlb[1]])
        ot = outT[gi % 2]
        kb.op("dve", lambda e: e.scalar_tensor_tensor(out=ot[:, 0:nq], in0=eo[0][:, 0:nq], scalar=subg_col[:, 0:1], in1=rlb[1][:, 0:nq], op0=ALU.mult, op1=ALU.mult),
              reads=[eo[0], subg_col, rlb[1]], writes=[ot])
        if qc0 >= S:
            for q in range(4):
                kb.store("sp", x1in, x1in.h[q * 128:(q + 1) * 128, 4096:4160], ot, ot[:, q * 64:(q + 1) * 64])
        else:
            q, col = qc0 // 4096, qc0 % 4096
            kb.store("sp", x1in, x1in.h[q * 128:(q + 1) * 128, col:col + nq], ot, ot[:, 0:nq])

    attend(S, LC, list(range(LC // 128)))
    for g in range(n_groups):
        attend(g * 512, 512, list(range(NKT)))
    print("l0a instructions:", kb.n_ins)
    kb.end_stage()


def rope_tables():
    half = 32
    inv = (10000.0 ** (-np.arange(0, half, 2, dtype=np.float32) / half)).astype(np.float32)
    t = np.arange(S)
    r = (t // 64).astype(np.float32)[:, None] * inv[None, :]
    c = (t % 64).astype(np.float32)[:, None] * inv[None, :]
    ang = np.concatenate([r, r, c, c], axis=-1).astype(np.float32)
    cos = np.cos(ang).astype(np.float32)
    sin = np.sin(ang).astype(np.float32)
    sgn = np.concatenate([-np.ones(16), np.ones(16), -np.ones(16), np.ones(16)]).astype(np.float32)
    sin = sin * sgn[None, :]
    return np.ascontiguousarray(np.tile(cos, (1, 4))), np.ascontiguousarray(np.tile(sin, (1, 4)))


def fop(v, n):
    return np.ascontiguousarray(np.asarray(v, np.float32).reshape(n, 128).T)


def host_l0a(inp):
    cos4, sin4 = rope_tables()
    maps = []
    wi = inp["ab_w_in"][0]
    for b in range(2):
        for h in range(4):
            sv = np.stack([inp["c"][b], inp["c_ctx"]], -1).reshape(8, 128, 2).transpose(1, 0, 2).reshape(128, 16)
            w = np.concatenate([wi[:, 1024 + h * 128:1024 + (h + 1) * 128], wi[:, 1536 + h * 128:1536 + (h + 1) * 128],
                                wi[:, 2048 + h * 128:2048 + (h + 1) * 128]], axis=1)
            qg, kg = inp["diff_qnorm_g"][0], inp["diff_knorm_g"][0]
            small = np.concatenate([qg, qg, kg, kg, inp["diff_lq1"][0], inp["diff_lk1"][0], inp["diff_lq2"][0], inp["diff_lk2"][0],
                                    inp["diff_subln_g"][0]]).astype(np.float32)
            maps.append({
                "x": np.ascontiguousarray(inp["x"][b]), "ctx": np.ascontiguousarray(inp["ctx"][b]),
                "svec": np.ascontiguousarray(sv.astype(np.float32)),
                "adaw": np.ascontiguousarray(inp["ada_w"][0][:, 0:2048]), "adab": fop(inp["ada_b"][0][0:2048], 16),
                "g1": fop(inp["norm1_g"][0], 8), "w": np.ascontiguousarray(w), "small": small, "cos4": cos4, "sin4": sin4,
            })
    return maps


BIG = 1.0e30


def build_b(layer, kb, banks, mixsrc, hin_t, hout_t, debug=False):
    L0 = (layer == 0)
    NTL = 32
    NT = NTL + (1 if L0 else 0)
    NTOK = NT * 128
    NTOKV = 4096 + (64 if L0 else 0)
    NB = (2 * NTOKV + 32 * 255 + 255) // 256
    NROWS = NB * 256
    NMIX = 4 if L0 else 8

    kb.begin_stage("b%d_" % layer)
    hin = hin_t if hin_t is not None else kb.dram("hin", [NTOK, 1024], F32, "ExternalInput")
    svec = kb.dram("svec", [128, 16], F32, "ExternalInput")
    adaw = kb.dram("adaw", [1024, 6144], F32, "ExternalInput")
    adabf = kb.dram("adabf", [128, 48], F32, "ExternalInput")
    adabr = kb.dram("adabr", [2048], F32, "ExternalInput")
    gfop = kb.dram("gfop", [128, 16], F32, "ExternalInput")
    mixidx = kb.dram("mixidx", [128, NMIX], I32, "ExternalInput")
    wout = kb.dram("wout", [1024, 1024], F32, "ExternalInput")
    rw = kb.dram("rw", [1024, 36], F32, "ExternalInput")
    rb = kb.dram("rb", [36], F32, "ExternalInput")
    w1t = kb.dram("w1t", [4096, 4096], F32, "ExternalInput")
    w3t = kb.dram("w3t", [4096, 4096], F32, "ExternalInput")
    w2t = kb.dram("w2t", [4096, 4096], F32, "ExternalInput")
    valid = kb.dram("valid", [128, 1], F32, "ExternalInput")
    if L0:
        xhalo = kb.dram("xhalo", [128, 1024], F32, "ExternalInput")
        cxh = kb.dram("cxh", [128, 1024], F32, "ExternalInput")
        edge = kb.dram("edge", [2], F32, "ExternalInput")
        win = kb.dram("win", [1024, 1024], F32, "ExternalInput")
        cw = kb.dram("cw", [128, 124], F32, "ExternalInput")
        cvec = kb.dram("cvec", [128, 12], F32, "ExternalInput")
    hout = hout_t if hout_t is not None else kb.dram("hout", [NTOK, 1024], F32, "ExternalOutput")
    hlm = kb.dram("hlm", [NTOK, 1024], F32)
    nl2d = kb.dram("nl2d", [NTOK, 1024], BF16)
    xs = kb.dram("xs", [NROWS + 128, 1024], BF16)
    ys = kb.dram("ys", [NROWS + 128, 1024], F32)

    def bfv(t):
        return t[:].bitcast(BF16)

    def dbl(name, shape, dt, n=2, es=None):
        return [kb.sb("%s%d" % (name, i), shape, dt, es) for i in range(n)]

    identb = kb.identity("identb", BF16)
    zer = kb.sb("zer", [128, 128], F32)
    kb.op("pool", lambda e: e.memset(zer[:], 0.0), writes=[zer])
    zerb = kb.sb("zerb", [128, 2048], BF16)
    kb.op("pool", lambda e: e.memset(zerb[:], 0.0), writes=[zerb])
    for a in range(0, NROWS // 128, 2):
        kb.store("pool", xs, xs.h[a * 128:(a + 2) * 128, :].rearrange("(a p) n -> p a n", p=128), zerb, zerb[:].rearrange("p (a n) -> p a n", a=2))

    OH = kb.sb("OH", [128, NT, 2, 32], F32)
    GT = kb.sb("GT", [128, NT, 2], F32)
    RK = kb.sb("RK", [128, NT, 2], F32)
    Rbc = kb.sb("Rbc", [128, 32], F32)
    DESTI = kb.sb("DESTI", [128, NT * 2], I32)
    WIDX = kb.sb("WIDX", [128, NB], I32)
    validt = kb.sb("validt", [128, 1], F32)
    gate_bc = [[kb.sb("gate_bc%d%d" % (j, w), [128, 1024], F32) for w in range(2)] for j in range(2)]
    mod = kb.sb("mod", [128, 48, 2], F32)
    gs1 = kb.sb("gs1", [128, 8, 2], F32)
    gs2 = kb.sb("gs2", [128, 8, 2], F32)
    s_sb = kb.sb("s_sb", [128, 16], F32)
    adabf_sb = kb.sb("adabf_sb", [128, 48], F32)
    gfop_sb = kb.sb("gfop_sb", [128, 16], F32)
    iop = kb.sb("iop", [128, 1], F32)
    blkst = kb.sb("blkst", [128, NB], F32)
    ltri_b = kb.sb("ltri_b", [128, 128], BF16)
    ones_b = kb.sb("ones_b", [128, 128], BF16)
    pesA = ExitStack()

    def rstd_chain(stt, c_in, c_tmp, c_out, n, inv_n):
        kb.op("dve", lambda e: e.tensor_scalar(out=stt[:, c_tmp:c_tmp + n], in0=stt[:, c_in:c_in + n], scalar1=inv_n, scalar2=EPS, op0=ALU.mult, op1=ALU.add),
              reads=[stt], writes=[stt])
        kb.op("act", lambda e: e.activation(out=stt[:, c_tmp:c_tmp + n], in_=stt[:, c_tmp:c_tmp + n], func=AF.Sqrt), reads=[stt], writes=[stt])
        kb.op("dve", lambda e: e.reciprocal(out=stt[:, c_out:c_out + n], in_=stt[:, c_tmp:c_tmp + n]), reads=[stt], writes=[stt])

    kb.load("sp", s_sb, s_sb[:], svec.h, svec)
    kb.op("act", lambda e: e.activation(out=s_sb[:], in_=s_sb[:], func=AF.Silu), reads=[s_sb], writes=[s_sb])
    kb.load("sp", adabf_sb, adabf_sb[:], adabf.h, adabf)
    kb.load("sp", gfop_sb, gfop_sb[:], gfop.h, gfop)
    kb.load("sp", validt, validt[:], valid.h, valid)
    pm = banks[0]
    with ExitStack() as pes:
        adabr_sb = kb.sb("adabr_sb", [128, 2048], F32, pes)
        kb.load("sp", adabr_sb, adabr_sb[:], adabr.h.partition_broadcast(128), adabr)
        s_bc = [kb.sb("s_bc%d" % j, [128, 8, 128], F32, pes) for j in range(2)]
        for j in range(2):
            for kc in range(8):
                kb.op("dve", lambda e: e.tensor_scalar(out=s_bc[j][:, kc, :], in0=zer[:, 0:128], scalar1=s_sb[:, kc * 2 + j:kc * 2 + j + 1], scalar2=None, op0=ALU.add),
                      reads=[zer, s_sb], writes=[s_bc[j]])
        adaw_sb = dbl("adaw_sb", [128, 8, 512], F32, 2, pes)
        for v in range(12):
            aw = adaw_sb[v % 2]
            kb.load("sp", aw, aw[:], adaw.h[:, v * 512:(v + 1) * 512].rearrange("(kc p) n -> p kc n", p=128), adaw)
            for oc in range(4):
                g = v * 4 + oc
                for kc in range(8):
                    kb.op("pe", lambda e: e.matmul(pm[:, g * 2:g * 2 + 2], lhsT=aw[:, kc, oc * 128:(oc + 1) * 128], rhs=s_sb[:, kc * 2:kc * 2 + 2],
                                                  start=(kc == 0), stop=(kc == 7)), reads=[aw, s_sb], writes=[pm])
            if v in (4, 5, 10, 11):
                which = 0 if v < 6 else 1
                half = v % 2
                for j in range(2):
                    pr = banks[1 + j]
                    for kc in range(8):
                        kb.op("pe", lambda e: e.matmul(pr[:, :], lhsT=s_bc[j][:, kc, :], rhs=aw[:, kc, :], start=(kc == 0), stop=(kc == 7)),
                              reads=[s_bc[j], aw], writes=[pr])
                    kb.op("dve", lambda e: e.tensor_tensor(out=gate_bc[j][which][:, half * 512:(half + 1) * 512], in0=pr[:, :],
                                                           in1=adabr_sb[:, which * 1024 + half * 512: which * 1024 + (half + 1) * 512], op=ALU.add),
                          reads=[pr, adabr_sb], writes=[gate_bc[j][which]])
        pm3 = pm[:, 0:96].rearrange("p (g j) -> p g j", j=2)
        for j in range(2):
            kb.op("dve", lambda e: e.tensor_tensor(out=mod[:, :, j], in0=pm3[:, :, j], in1=adabf_sb[:], op=ALU.add), reads=[pm, adabf_sb], writes=[mod])
        kb.barrier()
    for j in range(2):
        kb.op("dve", lambda e: e.scalar_tensor_tensor(out=gs1[:, :, j], in0=mod[:, 8:16, j], scalar=1.0, in1=gfop_sb[:, 0:8], op0=ALU.add, op1=ALU.mult),
              reads=[mod, gfop_sb], writes=[gs1])
        kb.op("dve", lambda e: e.scalar_tensor_tensor(out=gs2[:, :, j], in0=mod[:, 32:40, j], scalar=1.0, in1=gfop_sb[:, 8:16], op0=ALU.add, op1=ALU.mult),
              reads=[mod, gfop_sb], writes=[gs2])
    SH1, SH2 = 0, 24

    xn_b = dbl("xn_b", [128, 1024], BF16, 2, pesA)
    junk = kb.sb("junk", [128, 1024], BF16, pesA)
    stn = dbl("stn", [128, 4], F32, 2, pesA)

    def norm_T(i, xt_tile, gs, shoff, j, dstT, dcol, psT):
        p = i % 2
        kb.op("act", lambda e: e.activation(out=junk[:], in_=xt_tile[:], func=AF.Square, accum_out=stn[p][:, 0:1]), reads=[xt_tile], writes=[junk, stn[p]])
        rstd_chain(stn[p], 0, 1, 2, 1, 1.0 / 1024)
        kb.op("act", lambda e: e.activation(out=xn_b[p][:], in_=xt_tile[:], func=AF.Copy, scale=stn[p][:, 2:3]), reads=[xt_tile, stn[p]], writes=[xn_b[p]])
        for kc in range(8):
            kb.op("pe", lambda e: e.transpose(out=bfv(psT)[:, kc * 128:(kc + 1) * 128], in_=xn_b[p][:, kc * 128:(kc + 1) * 128], identity=identb[:]),
                  reads=[xn_b[p], identb], writes=[psT])
        for kc in range(8):
            kb.op("act", lambda e: e.activation(out=dstT[:, kc, dcol:dcol + 128], in_=bfv(psT)[:, kc * 128:(kc + 1) * 128], func=AF.Identity,
                                                scale=gs[:, kc, j:j + 1], bias=mod[:, shoff + kc, j:j + 1]), reads=[psT, gs, mod], writes=[dstT])

    xt = dbl("xt", [128, 1024], F32, 2, pesA)
    convT = kb.sb("convT", [128, 4, NTOK], BF16, pesA) if L0 else None

    if L0:
        with ExitStack() as pes:
            HW = 15 + 4096 + 15
            hT = kb.sb("hT", [128, 4, HW], F32, pes)
            hTc = kb.sb("hTc", [128, 4, 128], F32, pes)
            win_b = kb.sb("win_b", [128, 8, 1024], BF16, pes)
            stg = xt
            for kc in range(8):
                kb.load("sp", stg[kc % 2], stg[kc % 2][:], win.h[kc * 128:(kc + 1) * 128, :], win)
                kb.op("pool", lambda e: e.tensor_copy(out=win_b[:, kc, :], in_=stg[kc % 2][:]), reads=[stg[kc % 2]], writes=[win_b])
            cw_sb = kb.sb("cw_sb", [128, 4, 31], F32, pes)
            kb.load("sp", cw_sb, cw_sb[:], cw.h.rearrange("p (c t) -> p c t", c=4), cw)
            cvec_sb = kb.sb("cvec_sb", [128, 12], F32, pes)
            kb.load("sp", cvec_sb, cvec_sb[:], cvec.h, cvec)
            edge_sb = kb.sb("edge_sb", [128, 2], F32, pes)
            kb.load("sp", edge_sb, edge_sb[:], edge.h.partition_broadcast(128), edge)
            ones_s = kb.sb("ones_s", [128, 128], F32, pes)
            kb.op("pool", lambda e: e.memset(ones_s[:], 1.0 / 512), writes=[ones_s])
            nlT = dbl("nlT", [128, 8, 512], BF16, 1, pes) * 2
            sig = dbl("sig", [128, 512], F32, 2, pes)
            htmp = kb.sb("htmp", [128, 4, 128], F32, pes)

            def u_group(gi, nl, ncols, dst_fn):
                for cc in range(4):
                    pa, pg = banks[2], banks[3]
                    for kc in range(8):
                        kb.op("pe", lambda e: e.matmul(pa[:, 0:ncols], lhsT=win_b[:, kc, cc * 128:(cc + 1) * 128], rhs=nl[:, kc, 0:ncols], start=(kc == 0), stop=(kc == 7)),
                              reads=[win_b, nl], writes=[pa])
                    for kc in range(8):
                        kb.op("pe", lambda e: e.matmul(pg[:, 0:ncols], lhsT=win_b[:, kc, 512 + cc * 128:512 + (cc + 1) * 128], rhs=nl[:, kc, 0:ncols], start=(kc == 0), stop=(kc == 7)),
                              reads=[win_b, nl], writes=[pg])
                    sg = sig[cc % 2]
                    kb.op("act", lambda e: e.activation(out=sg[:, 0:ncols], in_=pg[:, 0:ncols], func=AF.Sigmoid), reads=[pg], writes=[sg])
                    dt_, dap = dst_fn(cc)
                    kb.op("dve", lambda e: e.tensor_tensor(out=dap, in0=pa[:, 0:ncols], in1=sg[:, 0:ncols], op=ALU.mult), reads=[pa, sg], writes=[dt_])

            ti = 0
            for g in range(8):
                nl = nlT[g % 2]
                for tt in range(4):
                    t = g * 4 + tt
                    kb.load("sp", xt[ti % 2], xt[ti % 2][:], hin.h[t * 128:(t + 1) * 128, :], hin)
                    norm_T(ti, xt[ti % 2], gs1, SH1, 0, nl, tt * 128, banks[ti % 2])
                    ti += 1
                u_group(g, nl, 512, lambda cc: (hT, hT[:, cc, 15 + g * 512:15 + (g + 1) * 512]))
            nl = nlT[0]
            kb.load("sp", xt[ti % 2], xt[ti % 2][:], xhalo.h, xhalo)
            norm_T(ti, xt[ti % 2], gs1, SH1, 0, nl, 0, banks[ti % 2])
            ti += 1
            u_group(8, nl, 128, lambda cc: (htmp, htmp[:, cc, :]))
            for cc in range(4):
                kb.op("dve", lambda e: e.tensor_scalar(out=hT[:, cc, 0:15], in0=htmp[:, cc, 0:15], scalar1=edge_sb[:, 0:1], scalar2=None, op0=ALU.mult),
                      reads=[htmp, edge_sb], writes=[hT])
                kb.op("dve", lambda e: e.tensor_scalar(out=hT[:, cc, 15 + 4096:HW], in0=htmp[:, cc, 15:30], scalar1=edge_sb[:, 1:2], scalar2=None, op0=ALU.mult),
                      reads=[htmp, edge_sb], writes=[hT])
            nl = nlT[1]
            kb.load("sp", xt[ti % 2], xt[ti % 2][:], cxh.h, cxh)
            norm_T(ti, xt[ti % 2], gs1, SH1, 1, nl, 0, banks[ti % 2])
            ti += 1
            u_group(9, nl, 128, lambda cc: (hTc, hTc[:, cc, :]))
            for cc in range(4):
                kb.op("dve", lambda e: e.tensor_scalar(out=hTc[:, cc, 0:15], in0=hTc[:, cc, 0:15], scalar1=edge_sb[:, 0:1], scalar2=None, op0=ALU.mult),
                      reads=[hTc, edge_sb], writes=[hTc])
                kb.op("dve", lambda e: e.tensor_scalar(out=hTc[:, cc, 79:94], in0=hTc[:, cc, 79:94], scalar1=edge_sb[:, 1:2], scalar2=None, op0=ALU.mult),
                      reads=[hTc, edge_sb], writes=[hTc])

            acc = [kb.sb("acc%d" % c, [128, 512], F32, pes) for c in range(4)]
            sqt = dbl("sqt", [128, 512], F32, 1, pes) * 2
            mean_sb = kb.sb("mean_sb", [128, 512], F32, pes)
            m2 = kb.sb("m2", [128, 512], F32, pes)
            rstd_bc = kb.sb("rstd_bc", [128, 512], F32, pes)
            tt_ = dbl("tt_", [128, 512], F32, 1, pes) * 2

            def conv_block(src, c0, n, out_c0):
                for tau in range(31):
                    for cc in range(4):
                        en = "dve"
                        if tau == 0:
                            kb.op(en, lambda e: e.tensor_scalar(out=acc[cc][:, 0:n], in0=src[:, cc, c0:c0 + n], scalar1=cw_sb[:, cc, 0:1], scalar2=cvec_sb[:, cc:cc + 1],
                                                                op0=ALU.mult, op1=ALU.add), reads=[src, cw_sb, cvec_sb], writes=[acc[cc]])
                        else:
                            kb.op(en, lambda e: e.scalar_tensor_tensor(out=acc[cc][:, 0:n], in0=src[:, cc, c0 + tau:c0 + tau + n], scalar=cw_sb[:, cc, tau:tau + 1],
                                                                       in1=acc[cc][:, 0:n], op0=ALU.mult, op1=ALU.add), reads=[src, cw_sb, acc[cc]], writes=[acc[cc]])
                pmean, pex2 = banks[4], banks[5]
                for cc in range(4):
                    kb.op("pe", lambda e: e.matmul(pmean[:, 0:n], lhsT=ones_s[:], rhs=acc[cc][:, 0:n], start=(cc == 0), stop=(cc == 3)), reads=[ones_s, acc[cc]], writes=[pmean])
                for cc in range(4):
                    sq_ = sqt[cc % 2]
                    kb.op("act", lambda e: e.activation(out=sq_[:, 0:n], in_=acc[cc][:, 0:n], func=AF.Square), reads=[acc[cc]], writes=[sq_])
                    kb.op("pe", lambda e: e.matmul(pex2[:, 0:n], lhsT=ones_s[:], rhs=sq_[:, 0:n], start=(cc == 0), stop=(cc == 3)), reads=[ones_s, sq_], writes=[pex2])
                kb.op("act", lambda e: e.copy(out=mean_sb[:, 0:n], in_=pmean[:, 0:n]), reads=[pmean], writes=[mean_sb])
                kb.op("pool", lambda e: e.tensor_tensor(out=m2[:, 0:n], in0=mean_sb[:, 0:n], in1=mean_sb[:, 0:n], op=ALU.mult), reads=[mean_sb], writes=[m2])
                kb.op("dve", lambda e: e.tensor_tensor(out=m2[:, 0:n], in0=pex2[:, 0:n], in1=m2[:, 0:n], op=ALU.subtract), reads=[pex2, m2], writes=[m2])
                kb.op("dve", lambda e: e.tensor_scalar(out=m2[:, 0:n], in0=m2[:, 0:n], scalar1=EPS, scalar2=None, op0=ALU.add), reads=[m2], writes=[m2])
                kb.op("act", lambda e: e.activation(out=m2[:, 0:n], in_=m2[:, 0:n], func=AF.Sqrt), reads=[m2], writes=[m2])
                kb.op("dve", lambda e: e.reciprocal(out=rstd_bc[:, 0:n], in_=m2[:, 0:n]), reads=[m2], writes=[rstd_bc])
                for cc in range(4):
                    t_ = tt_[cc % 2]
                    kb.op("dve", lambda e: e.tensor_tensor(out=t_[:, 0:n], in0=acc[cc][:, 0:n], in1=mean_sb[:, 0:n], op=ALU.subtract), reads=[acc[cc], mean_sb], writes=[t_])
                    kb.op("pool", lambda e: e.tensor_tensor(out=t_[:, 0:n], in0=t_[:, 0:n], in1=rstd_bc[:, 0:n], op=ALU.mult), reads=[t_, rstd_bc], writes=[t_])
                    kb.op("act", lambda e: e.activation(out=convT[:, cc, out_c0:out_c0 + n], in_=t_[:, 0:n], func=AF.Silu, scale=cvec_sb[:, 4 + cc:5 + cc],
                                                        bias=cvec_sb[:, 8 + cc:9 + cc]), reads=[t_, cvec_sb], writes=[convT])

            for tb in range(8):
                conv_block(hT, tb * 512, 512, tb * 512)
            conv_block(hTc, 0, 64, 4096)
            kb.op("pool", lambda e: e.memset(convT[:, :, 4096 + 64:4096 + 128], 0.0), writes=[convT])
            kb.barrier()

    mix_sb = kb.sb("mix_sb", [128, NMIX, NTOK], BF16, pesA)
    mixidx_sb = kb.sb("mixidx_sb", [128, NMIX], I32, pesA)
    kb.load("sp", mixidx_sb, mixidx_sb[:], mixidx.h, mixidx)
    for hh in range(NMIX):
        kb.dma("pool", lambda e: e.indirect_dma_start(out=mix_sb[:, hh, :], out_offset=None, in_=mixsrc.h[:, :],
                                                      in_offset=bass.IndirectOffsetOnAxis(ap=mixidx_sb[:, hh:hh + 1], axis=0)), reads=[mixidx_sb, mixsrc], writes=[mix_sb])
    wout_b = kb.sb("wout_b", [128, 8, 1024], BF16, pesA)
    rw_b = kb.sb("rw_b", [128, 8, 36], BF16, pesA)
    rb_bc = kb.sb("rb_bc", [128, 36], F32, pesA)
    kb.load("sp", rb_bc, rb_bc[:], rb.h.partition_broadcast(128), rb)
    kb.op("pool", lambda e: e.memset(Rbc[:], 0.0), writes=[Rbc])
    ltri = kb.sb("ltri", [128, 128], F32, pesA)
    kb.op("pool", lambda e: e.memset(ltri[:], 1.0), writes=[ltri])
    kb.op("pool", lambda e: e.affine_select(out=ltri[:], in_=ltri[:], pattern=[[1, 128]], compare_op=ALU.is_gt, fill=0.0, base=0, channel_multiplier=-1),
          reads=[ltri], writes=[ltri])
    kb.op("pool", lambda e: e.tensor_copy(out=ltri_b[:], in_=ltri[:]), reads=[ltri], writes=[ltri_b])
    kb.op("pool", lambda e: e.memset(ones_b[:], 1.0), writes=[ones_b])
    kb.op("pool", lambda e: e.iota(iop[:], pattern=[[0, 1]], base=0, channel_multiplier=1, allow_small_or_imprecise_dtypes=True), writes=[iop])
    kb.op("pool", lambda e: e.iota(blkst[:], pattern=[[256, NB]], base=0, channel_multiplier=0, allow_small_or_imprecise_dtypes=True), writes=[blkst])

    with ExitStack() as pes:
        stg = dbl("stg2", [128, 1024], F32, 2, pes)
        for kc in range(8):
            kb.load("sp", stg[kc % 2], stg[kc % 2][:], wout.h[kc * 128:(kc + 1) * 128, :], wout)
            kb.op("pool", lambda e: e.tensor_copy(out=wout_b[:, kc, :], in_=stg[kc % 2][:]), reads=[stg[kc % 2]], writes=[wout_b])
        rw_f = kb.sb("rw_f", [128, 8, 36], F32, pes)
        kb.load("sp", rw_f, rw_f[:], rw.h.rearrange("(kc p) n -> p kc n", p=128), rw)
        kb.op("pool", lambda e: e.tensor_copy(out=rw_b[:], in_=rw_f[:]), reads=[rw_f], writes=[rw_b])

        ytmp = dbl("ytmp", [128, 1024], F32, 2, pes)
        hl = dbl("hl", [128, 1024], F32, 2, pes)
        nl2T = dbl("nl2T", [128, 8, 128], BF16, 2, pes)
        nl2 = dbl("nl2", [128, 1024], BF16, 2, pes)
        lg = dbl("lg", [128, 36], F32, 2, pes)
        rt = dbl("rt", [128, 16], F32, 2, pes)
        lem = dbl("lem", [128, 32], F32, 2, pes)
        lem2 = dbl("lem2", [128, 32], F32, 2, pes)
        cb_ = dbl("cb_", [128, 32], BF16, 2, pes)
        rbase = dbl("rbase", [128, 32], F32, 2, pes)
        tmp32 = dbl("tmp32", [128, 2, 32], F32, 2, pes)
        ejunk = kb.sb("ejunk", [128, 4], F32, pes)

        for t in range(NT):
            p = t % 2
            j = 1 if (L0 and t == NT - 1) else 0
            kb.load("sp", xt[p], xt[p][:], hin.h[t * 128:(t + 1) * 128, :], hin)
            chunks = []
            if L0:
                for cc in range(4):
                    chunks.append((convT, convT[:, cc, t * 128:(t + 1) * 128]))
            for hh in range(NMIX):
                chunks.append((mix_sb, mix_sb[:, hh, t * 128:(t + 1) * 128]))
            for half in range(2):
                py = banks[half]
                for ci, (ct, cap) in enumerate(chunks):
                    kb.op("pe", lambda e: e.matmul(py[:, :], lhsT=cap, rhs=wout_b[:, ci, half * 512:(half + 1) * 512], start=(ci == 0), stop=(ci == 7)),
                          reads=[ct, wout_b], writes=[py])
                kb.op("dve", lambda e: e.tensor_tensor(out=ytmp[p][:, half * 512:(half + 1) * 512], in0=py[:, :], in1=gate_bc[j][0][:, half * 512:(half + 1) * 512], op=ALU.mult),
                      reads=[py, gate_bc[j][0]], writes=[ytmp[p]])
            kb.op("dve", lambda e: e.tensor_tensor(out=hl[p][:], in0=ytmp[p][:], in1=xt[p][:], op=ALU.add), reads=[ytmp[p], xt[p]], writes=[hl[p]])
            kb.store("sp", hlm, hlm.h[t * 128:(t + 1) * 128, :], hl[p], hl[p][:])
            norm_T(t, hl[p], gs2, SH2, j, nl2T[p], 0, banks[2])
            pl = banks[3]
            for kc in range(8):
                kb.op("pe", lambda e: e.matmul(pl[:, 0:36], lhsT=nl2T[p][:, kc, :], rhs=rw_b[:, kc, :], start=(kc == 0), stop=(kc == 7)), reads=[nl2T[p], rw_b], writes=[pl])
            kb.op("dve", lambda e: e.tensor_tensor(out=lg[p][:], in0=pl[:, 0:36], in1=rb_bc[:], op=ALU.add), reads=[pl, rb_bc], writes=[lg[p]])
            pbk = banks[4]
            for kc in range(8):
                kb.op("pe", lambda e: e.transpose(out=bfv(pbk)[:, kc * 128:(kc + 1) * 128], in_=nl2T[p][:, kc, :], identity=identb[:]), reads=[nl2T[p], identb], writes=[pbk])
            kb.op("act", lambda e: e.copy(out=nl2[p][:], in_=bfv(pbk)[:, 0:1024]), reads=[pbk], writes=[nl2[p]])
            kb.store("sp", nl2d, nl2d.h[t * 128:(t + 1) * 128, :], nl2[p], nl2[p][:])
            r_ = rt[p]
            kb.op("dve", lambda e: e.tensor_reduce(out=r_[:, 0:1], in_=lg[p][:, 0:4], axis=AX.X, op=ALU.max), reads=[lg[p]], writes=[r_])
            kb.op("dve", lambda e: e.tensor_scalar(out=r_[:, 1:5], in0=lg[p][:, 0:4], scalar1=r_[:, 0:1], scalar2=None, op0=ALU.is_equal), reads=[lg[p], r_], writes=[r_])
            kb.op("dve", lambda e: e.tensor_scalar(out=r_[:, 5:6], in0=r_[:, 0:1], scalar1=-1.0, scalar2=None, op0=ALU.mult), reads=[r_], writes=[r_])
            kb.op("act", lambda e: e.activation(out=ejunk[:], in_=lg[p][:, 0:4], func=AF.Exp, bias=r_[:, 5:6], accum_out=r_[:, 6:7]), reads=[lg[p], r_], writes=[ejunk, r_])
            kb.op("dve", lambda e: e.reciprocal(out=r_[:, 7:8], in_=r_[:, 6:7]), reads=[r_], writes=[r_])
            kb.op("dve", lambda e: e.tensor_scalar(out=r_[:, 8:12], in0=r_[:, 1:5], scalar1=-1.0, scalar2=BIG, op0=ALU.add, op1=ALU.mult), reads=[r_], writes=[r_])
            for g in range(4):
                kb.op("dve", lambda e: e.tensor_scalar(out=lem[p][:, g * 8:(g + 1) * 8], in0=lg[p][:, 4 + g * 8:4 + (g + 1) * 8], scalar1=r_[:, 8 + g:9 + g], scalar2=None, op0=ALU.add),
                      reads=[lg[p], r_], writes=[lem[p]])
            oh1 = OH[:, t, 0, :]
            oh2 = OH[:, t, 1, :]
            kb.op("dve", lambda e: e.tensor_reduce(out=r_[:, 12:13], in_=lem[p][:], axis=AX.X, op=ALU.max), reads=[lem[p]], writes=[r_])
            kb.op("dve", lambda e: e.tensor_scalar(out=oh1, in0=lem[p][:], scalar1=r_[:, 12:13], scalar2=None, op0=ALU.is_equal), reads=[lem[p], r_], writes=[OH])
            kb.op("dve", lambda e: e.scalar_tensor_tensor(out=lem2[p][:], in0=oh1, scalar=-BIG, in1=lem[p][:], op0=ALU.mult, op1=ALU.add), reads=[OH, lem[p]], writes=[lem2[p]])
            kb.op("dve", lambda e: e.tensor_reduce(out=r_[:, 13:14], in_=lem2[p][:], axis=AX.X, op=ALU.max), reads=[lem2[p]], writes=[r_])
            kb.op("dve", lambda e: e.tensor_scalar(out=oh2, in0=lem2[p][:], scalar1=r_[:, 13:14], scalar2=None, op0=ALU.is_equal), reads=[lem2[p], r_], writes=[OH])
            kb.op("dve", lambda e: e.tensor_tensor(out=r_[:, 14:15], in0=r_[:, 12:13], in1=r_[:, 13:14], op=ALU.subtract), reads=[r_], writes=[r_])
            kb.op("act", lambda e: e.activation(out=r_[:, 15:16], in_=r_[:, 14:15], func=AF.Sigmoid), reads=[r_], writes=[r_])
            kb.op("dve", lambda e: e.tensor_tensor(out=GT[:, t, 0:1], in0=r_[:, 15:16], in1=r_[:, 7:8], op=ALU.mult), reads=[r_], writes=[GT])
            kb.op("dve", lambda e: e.tensor_tensor(out=GT[:, t, 1:2], in0=r_[:, 7:8], in1=GT[:, t, 0:1], op=ALU.subtract), reads=[r_, GT], writes=[GT])
            if j == 1:
                kb.op("dve", lambda e: e.tensor_scalar(out=OH[:, t, :, :], in0=OH[:, t, :, :], scalar1=validt[:, 0:1], scalar2=None, op0=ALU.mult), reads=[OH, validt], writes=[OH])
            kb.op("dve", lambda e: e.tensor_tensor(out=cb_[p][:], in0=OH[:, t, 0, :], in1=OH[:, t, 1, :], op=ALU.add), reads=[OH], writes=[cb_[p]])
            pc = banks[5]
            kb.op("pe", lambda e: e.matmul(pc[:, 0:32], lhsT=ltri_b[:], rhs=cb_[p][:], start=True, stop=True), reads=[ltri_b, cb_[p]], writes=[pc])
            kb.op("dve", lambda e: e.tensor_tensor(out=rbase[p][:], in0=pc[:, 0:32], in1=Rbc[:], op=ALU.add), reads=[pc, Rbc], writes=[rbase[p]])
            pt_ = banks[6]
            kb.op("pe", lambda e: e.matmul(pt_[:, 0:32], lhsT=ones_b[:], rhs=cb_[p][:], start=True, stop=True), reads=[ones_b, cb_[p]], writes=[pt_])
            kb.op("dve", lambda e: e.tensor_tensor(out=Rbc[:], in0=pt_[:, 0:32], in1=Rbc[:], op=ALU.add), reads=[pt_, Rbc], writes=[Rbc])
            for k in range(2):
                kb.op("dve", lambda e: e.tensor_tensor(out=tmp32[p][:, k, :], in0=OH[:, t, k, :], in1=rbase[p][:], op=ALU.mult), reads=[OH, rbase[p]], writes=[tmp32[p]])
            kb.op("dve", lambda e: e.tensor_reduce(out=RK[:, t, :], in_=tmp32[p][:], axis=AX.X, op=ALU.add), reads=[tmp32[p]], writes=[RK])
        kb.barrier()
    pesA.close()
    pesD = ExitStack()

    cnt_i = kb.sb("cnt_i", [128, 32], I32, pesD)
    pcnt = kb.sb("pcnt", [128, 32], F32, pesD)
    pend = [kb.sb("pend%d" % i, [128, 32], F32, pesD) for i in range(2)]
    kb.op("dve", lambda e: e.tensor_scalar(out=pcnt[:], in0=Rbc[:], scalar1=255.0, scalar2=None, op0=ALU.add), reads=[Rbc], writes=[pcnt])
    kb.op("dve", lambda e: e.tensor_copy(out=cnt_i[:], in_=pcnt[:]), reads=[pcnt], writes=[cnt_i])
    kb.op("dve", lambda e: e.tensor_scalar(out=cnt_i[:], in0=cnt_i[:], scalar1=8, scalar2=8, op0=ALU.arith_shift_right, op1=ALU.logical_shift_left), reads=[cnt_i], writes=[cnt_i])
    kb.op("dve", lambda e: e.tensor_copy(out=pcnt[:], in_=cnt_i[:]), reads=[cnt_i], writes=[pcnt])
    kb.op("dve", lambda e: e.tensor_copy(out=pend[0][:], in_=pcnt[:]), reads=[pcnt], writes=[pend[0]])
    cur = 0
    for sft in (1, 2, 4, 8, 16):
        a, b = pend[cur], pend[1 - cur]
        kb.op("dve", lambda e: e.tensor_copy(out=b[:, 0:sft], in_=a[:, 0:sft]), reads=[a], writes=[b])
        kb.op("dve", lambda e: e.tensor_tensor(out=b[:, sft:32], in0=a[:, sft:32], in1=a[:, 0:32 - sft], op=ALU.add), reads=[a], writes=[b])
        cur = 1 - cur
    pendf = pend[cur]
    poff = kb.sb("poff", [128, 32], F32, pesD)
    kb.op("dve", lambda e: e.tensor_tensor(out=poff[:], in0=pendf[:], in1=pcnt[:], op=ALU.subtract), reads=[pendf, pcnt], writes=[poff])
    DEST = kb.sb("DEST", [128, NT, 2], F32, pesD)
    tmpd = kb.sb("tmpd", [128, NT * 2, 32], F32, pesD)
    for t in range(NT):
        for k in range(2):
            kb.op("dve", lambda e: e.tensor_tensor(out=tmpd[:, t * 2 + k, :], in0=OH[:, t, k, :], in1=poff[:], op=ALU.mult), reads=[OH, poff], writes=[tmpd])
    kb.op("dve", lambda e: e.tensor_reduce(out=DEST[:].rearrange("p t k -> p (t k)"), in_=tmpd[:], axis=AX.X, op=ALU.add), reads=[tmpd], writes=[DEST])
    kb.op("dve", lambda e: e.tensor_tensor(out=DEST[:], in0=DEST[:], in1=RK[:], op=ALU.add), reads=[DEST, RK], writes=[DEST])
    if L0:
        inval = kb.sb("inval", [128, 2], F32, pesD)
        kb.op("dve", lambda e: e.tensor_scalar(out=inval[:, 0:1], in0=validt[:], scalar1=-1.0, scalar2=-1.0, op0=ALU.add, op1=ALU.mult), reads=[validt], writes=[inval])
        kb.op("dve", lambda e: e.scalar_tensor_tensor(out=inval[:, 1:2], in0=iop[:], scalar=float(NROWS), in1=inval[:, 0:1], op0=ALU.add, op1=ALU.mult), reads=[iop, inval], writes=[inval])
        kb.op("dve", lambda e: e.tensor_scalar(out=DEST[:, NT - 1, :], in0=DEST[:, NT - 1, :], scalar1=validt[:, 0:1], scalar2=inval[:, 1:2], op0=ALU.mult, op1=ALU.add),
              reads=[DEST, validt, inval], writes=[DEST])
    kb.op("dve", lambda e: e.tensor_copy(out=DESTI[:], in_=DEST[:].rearrange("p t k -> p (t k)")), reads=[DEST], writes=[DESTI])
    eb = kb.sb("eb", [128, NB], F32, pesD)
    kb.op("pool", lambda e: e.memset(eb[:], 0.0), writes=[eb])
    for ee in range(32):
        kb.op("dve", lambda e: e.scalar_tensor_tensor(out=eb[:], in0=blkst[:], scalar=pendf[:, ee:ee + 1], in1=eb[:], op0=ALU.is_ge, op1=ALU.add), reads=[blkst, pendf, eb], writes=[eb])
    kb.op("dve", lambda e: e.tensor_scalar(out=eb[:], in0=eb[:], scalar1=31.0, scalar2=128.0, op0=ALU.min, op1=ALU.mult), reads=[eb], writes=[eb])
    flag = kb.sb("flag", [128, NB], F32, pesD)
    kb.op("pool", lambda e: e.memset(flag[:], 1.0), writes=[flag])
    kb.op("dve", lambda e: e.tensor_tensor(out=flag[:, 1:NB], in0=eb[:, 1:NB], in1=eb[:, 0:NB - 1], op=ALU.not_equal), reads=[eb], writes=[flag])
    kb.op("dve", lambda e: e.tensor_scalar(out=flag[:], in0=flag[:], scalar1=-1.0, scalar2=-1.0e9, op0=ALU.add, op1=ALU.mult), reads=[flag], writes=[flag])
    kb.op("dve", lambda e: e.tensor_scalar(out=eb[:], in0=eb[:], scalar1=iop[:, 0:1], scalar2=None, op0=ALU.add), reads=[eb, iop], writes=[eb])
    kb.op("dve", lambda e: e.tensor_tensor(out=eb[:], in0=eb[:], in1=flag[:], op=ALU.add), reads=[eb, flag], writes=[eb])
    kb.op("dve", lambda e: e.tensor_copy(out=WIDX[:], in_=eb[:]), reads=[eb], writes=[WIDX])

    srow = dbl("srow", [128, 1024], BF16, 3, pesD)
    for t in range(NT):
        sr = srow[t % 3]
        kb.load("sp", sr, sr[:], nl2d.h[t * 128:(t + 1) * 128, :], nl2d)
        for k in range(2):
            kb.dma("pool", lambda e: e.indirect_dma_start(out=xs.h[:, :], out_offset=bass.IndirectOffsetOnAxis(ap=DESTI[:, t * 2 + k:t * 2 + k + 1], axis=0),
                                                          in_=sr[:], in_offset=None), reads=[DESTI, sr], writes=[xs])
    kb.barrier()
    pesD.close()
    pesE = ExitStack()

    w1f = dbl("w1f", [128, 4096], F32, 1, pesE) * 2
    w3f = dbl("w3f", [128, 4096], F32, 1, pesE) * 2
    w2f = dbl("w2f", [128, 4096], F32, 1, pesE) * 2
    w1b = dbl("w1b", [128, 8, 512], BF16, 2, pesE)
    w3b = dbl("w3b", [128, 8, 512], BF16, 2, pesE)
    w2b = dbl("w2b", [128, 4, 1024], BF16, 2, pesE)
    xr = dbl("xr", [128, 2, 1024], BF16, 2, pesE)
    xsT = dbl("xsT", [128, 8, 256], BF16, 2, pesE)
    sl = dbl("sl", [128, 256], F32, 2, pesE)
    hhT = dbl("hhT", [128, 4, 256], BF16, 2, pesE)
    yo = dbl("yo", [128, 1024], F32, 2, pesE)
    wreg = kb.nc.gpsimd.to_reg(4095)
    for b in range(NB):
        p = b % 2
        for (tab, wf) in ((w1t, w1f[p]), (w3t, w3f[p]), (w2t, w2f[p])):
            kb.dma("pool", lambda e: e.indirect_dma_start(out=wf[:], out_offset=None, in_=tab.h[:, :], in_offset=bass.IndirectOffsetOnAxis(ap=WIDX[:, b:b + 1], axis=0),
                                                          bounds_check=wreg, oob_is_err=False), reads=[WIDX, tab], writes=[wf])
        kb.op("act", lambda e: e.copy(out=w1b[p][:].rearrange("p a b -> p (a b)"), in_=w1f[p][:]), reads=[w1f[p]], writes=[w1b[p]])
        kb.op("dve", lambda e: e.tensor_copy(out=w3b[p][:].rearrange("p a b -> p (a b)"), in_=w3f[p][:]), reads=[w3f[p]], writes=[w3b[p]])
        kb.op("act", lambda e: e.copy(out=w2b[p][:].rearrange("p a b -> p (a b)")[:, 0:2048], in_=w2f[p][:, 0:2048]), reads=[w2f[p]], writes=[w2b[p]])
        kb.op("dve", lambda e: e.tensor_copy(out=w2b[p][:].rearrange("p a b -> p (a b)")[:, 2048:4096], in_=w2f[p][:, 2048:4096]), reads=[w2f[p]], writes=[w2b[p]])
        kb.load("sp", xr[p], xr[p][:], xs.h[b * 256:(b + 1) * 256, :].rearrange("(a p) n -> p a n", p=128), xs)
        for sub in range(2):
            pT = banks[6]
            for kc in range(8):
                kb.op("pe", lambda e: e.transpose(out=bfv(pT)[:, kc * 128:(kc + 1) * 128], in_=xr[p][:, sub, kc * 128:(kc + 1) * 128], identity=identb[:]),
                      reads=[xr[p], identb], writes=[pT])
            kb.op("act", lambda e: e.copy(out=xsT[p][:, :, sub * 128:(sub + 1) * 128], in_=bfv(pT)[:, 0:1024].rearrange("p (a b) -> p a b", a=8)), reads=[pT], writes=[xsT[p]])
        for fc in range(4):
            ph1 = banks[0 + fc // 2]
            ph3 = banks[2 + fc // 2]
            c0 = (fc % 2) * 256
            for kc in range(8):
                kb.op("pe", lambda e: e.matmul(ph1[:, c0:c0 + 256], lhsT=w1b[p][:, kc, fc * 128:(fc + 1) * 128], rhs=xsT[p][:, kc, :], start=(kc == 0), stop=(kc == 7)),
                      reads=[w1b[p], xsT[p]], writes=[ph1])
            for kc in range(8):
                kb.op("pe", lambda e: e.matmul(ph3[:, c0:c0 + 256], lhsT=w3b[p][:, kc, fc * 128:(fc + 1) * 128], rhs=xsT[p][:, kc, :], start=(kc == 0), stop=(kc == 7)),
                      reads=[w3b[p], xsT[p]], writes=[ph3])
            s_ = sl[fc % 2]
            kb.op("act", lambda e: e.activation(out=s_[:], in_=ph1[:, c0:c0 + 256], func=AF.Silu), reads=[ph1], writes=[s_])
            kb.op("dve", lambda e: e.tensor_tensor(out=hhT[p][:, fc, :], in0=ph3[:, c0:c0 + 256], in1=s_[:], op=ALU.mult), reads=[ph3, s_], writes=[hhT[p]])
        for sub in range(2):
            y_ = yo[sub]
            for half in range(2):
                py = banks[4 + half]
                for fc in range(4):
                    kb.op("pe", lambda e: e.matmul(py[:, :], lhsT=hhT[p][:, fc, sub * 128:(sub + 1) * 128], rhs=w2b[p][:, fc, half * 512:(half + 1) * 512], start=(fc == 0), stop=(fc == 3)),
                          reads=[hhT[p], w2b[p]], writes=[py])
                if half == 0:
                    kb.op("act", lambda e: e.copy(out=y_[:, 0:512], in_=py[:, :]), reads=[py], writes=[y_])
                else:
                    kb.op("dve", lambda e: e.tensor_copy(out=y_[:, 512:1024], in_=py[:, :]), reads=[py], writes=[y_])
            kb.store("sp", ys, ys.h[b * 256 + sub * 128:b * 256 + (sub + 1) * 128, :], y_, y_[:])
    kb.barrier()
    pesE.close()
    pesF = ExitStack()

    y1 = dbl("y1", [128, 1024], F32, 2, pesF)
    y2 = dbl("y2", [128, 1024], F32, 2, pesF)
    hm = dbl("hm", [128, 1024], F32, 2, pesF)
    for t in range(NT):
        p = t % 2
        j = 1 if (L0 and t == NT - 1) else 0
        if j == 1:
            kb.op("pool", lambda e: e.memset(y1[p][:], 0.0), writes=[y1[p]])
            kb.op("pool", lambda e: e.memset(y2[p][:], 0.0), writes=[y2[p]])
        for k, yk in ((0, y1[p]), (1, y2[p])):
            kb.dma("pool", lambda e: e.indirect_dma_start(out=yk[:], out_offset=None, in_=ys.h[:, :], in_offset=bass.IndirectOffsetOnAxis(ap=DESTI[:, t * 2 + k:t * 2 + k + 1], axis=0)), reads=[DESTI, ys], writes=[yk])
        kb.load("sp", hm[p], hm[p][:], hlm.h[t * 128:(t + 1) * 128, :], hlm)
        kb.op("dve", lambda e: e.tensor_scalar(out=y1[p][:], in0=y1[p][:], scalar1=GT[:, t, 0:1], scalar2=None, op0=ALU.mult), reads=[y1[p], GT], writes=[y1[p]])
        kb.op("dve", lambda e: e.scalar_tensor_tensor(out=y1[p][:], in0=y2[p][:], scalar=GT[:, t, 1:2], in1=y1[p][:], op0=ALU.mult, op1=ALU.add), reads=[y2[p], GT, y1[p]], writes=[y1[p]])
        kb.op("dve", lambda e: e.tensor_tensor(out=y1[p][:], in0=y1[p][:], in1=gate_bc[j][1][:], op=ALU.mult), reads=[y1[p], gate_bc[j][1]], writes=[y1[p]])
        kb.op("dve", lambda e: e.tensor_tensor(out=hm[p][:], in0=hm[p][:], in1=y1[p][:], op=ALU.add), reads=[hm[p], y1[p]], writes=[hm[p]])
        kb.store("sp", hout, hout.h[t * 128:(t + 1) * 128, :], hm[p], hm[p][:])
    print("lb%d instructions:" % layer, kb.n_ins, "sems:", len(kb.sems))
    pesF.close()
    kb.end_stage()


def fop(v, n):
    return np.ascontiguousarray(np.asarray(v, np.float32).reshape(n, 128).T)


def moe_tables(inp, l):
    w1 = np.ascontiguousarray(inp["moe_w1"][l].reshape(32, 8, 128, 512).transpose(0, 2, 1, 3).reshape(4096, 4096))
    w3 = np.ascontiguousarray(inp["moe_w3"][l].reshape(32, 8, 128, 512).transpose(0, 2, 1, 3).reshape(4096, 4096))
    w2 = np.ascontiguousarray(inp["moe_w2"][l].reshape(32, 4, 128, 1024).transpose(0, 2, 1, 3).reshape(4096, 4096))
    return w1, w3, w2


def host_b(layer, inp):
    L0 = layer == 0
    l = layer
    w1, w3, w2 = moe_tables(inp, l)
    rw = np.ascontiguousarray(np.concatenate([inp["rg_w"][l], inp["re_w"][l]], axis=1).astype(np.float32))
    rb = np.concatenate([inp["rg_b"][l], inp["re_b"][l]]).astype(np.float32)
    adabr = np.concatenate([inp["ada_b"][l][2048:3072], inp["ada_b"][l][5120:6144]]).astype(np.float32)
    gfop = np.ascontiguousarray(np.concatenate([fop(inp["norm1_g"][l], 8), fop(inp["norm2_g"][l], 8)], axis=1))
    wout = np.ascontiguousarray(inp["ab_w_out"][0] if L0 else inp["gla_w_out"][0])
    hin_lat, hin_ctx = inp["x"], inp["ctx"]
    maps = []
    pp = np.arange(128, dtype=np.int32)
    for b in range(2):
        sv = np.stack([inp["c"][b], inp["c_ctx"]], -1).reshape(8, 128, 2).transpose(1, 0, 2).reshape(128, 16).astype(np.float32)
        for jq in range(4):
            r0, r1 = jq * 4096, (jq + 1) * 4096
            m = {"svec": np.ascontiguousarray(sv), "adaw": np.ascontiguousarray(inp["ada_w"][l]), "adabf": fop(inp["ada_b"][l], 48), "adabr": adabr, "gfop": gfop,
                 "wout": wout, "rw": rw, "rb": rb, "w1t": w1, "w3t": w3, "w2t": w2}
            if L0:
                cpad = np.zeros((128, 1024), np.float32)
                cpad[:64] = hin_ctx[b, 64 * jq:64 * jq + 64]
                m["hin"] = np.ascontiguousarray(np.concatenate([hin_lat[b, r0:r1], cpad], 0))
                m["mixidx"] = np.ascontiguousarray(np.stack([np.array([ag_row(jq * 128 + int(p_), h, 64, 512) for p_ in pp]) for h in range(4)], axis=1).astype(np.int32))
                xh = np.zeros((128, 1024), np.float32)
                if jq > 0:
                    xh[0:15] = hin_lat[b, r0 - 15:r0]
                if jq < 3:
                    xh[15:30] = hin_lat[b, r1:r1 + 15]
                m["xhalo"] = xh
                ch = np.zeros((128, 1024), np.float32)
                for r in range(94):
                    pos = 64 * jq - 15 + r
                    if 0 <= pos < 256:
                        ch[r] = hin_ctx[b, pos]
                m["cxh"] = ch
                m["edge"] = np.array([1.0 if jq > 0 else 0.0, 1.0 if jq < 3 else 0.0], np.float32)
                v = np.zeros((128, 1), np.float32)
                v[:64] = 1
                m["valid"] = v
                m["win"] = np.ascontiguousarray(inp["ab_w_in"][0][:, 0:1024])
                m["cw"] = np.ascontiguousarray(inp["conv_w"][0].T.reshape(4, 128, 31).transpose(1, 0, 2).reshape(128, 124))
                m["cvec"] = np.ascontiguousarray(np.concatenate([fop(inp["conv_b"][0], 4), fop(inp["conv_ln_g"][0], 4), fop(inp["conv_ln_b"][0], 4)], axis=1))
            else:
                m["mixidx"] = np.ascontiguousarray(np.stack([np.array([ag_row(jq * 256 + c2 * 128 + int(p_), h, 128, 1024) for p_ in pp]) for h in range(4) for c2 in range(2)], axis=1).astype(np.int32))
                m["valid"] = np.ones((128, 1), np.float32)
            maps.append(m)
    return maps


def gather_b(layer, results):
    L0 = layer == 0
    hl = np.zeros((2, 16384, 1024), np.float32)
    hc = np.zeros((2, 256, 1024), np.float32) if L0 else None
    for b in range(2):
        for jq in range(4):
            o = results[b * 4 + jq]["hout"]
            hl[b, jq * 4096:(jq + 1) * 4096] = o[:4096]
            if L0:
                hc[b, 64 * jq:64 * jq + 64] = o[4096:4160]
    return hl, hc


def build_l1a(kb, banks, x2out, x3in, n_lat_tiles=128, do_scan=True):
    kb.begin_stage("a1_")
    svec = kb.dram("svec", [128, 16], F32, "ExternalInput")
    adaw = kb.dram("adaw", [1024, 2048], F32, "ExternalInput")
    adab = kb.dram("adab", [128, 16], F32, "ExternalInput")
    g1 = kb.dram("g1", [128, 8], F32, "ExternalInput")
    w = kb.dram("w", [1024, 768], F32, "ExternalInput")
    waT = kb.dram("waT", [2, 16, 1024], F32, "ExternalInput")
    wa2 = kb.dram("wa2", [2, 16, 128], F32, "ExternalInput")
    small = kb.dram("small", [512], F32, "ExternalInput")
    proj = kb.dram("proj", [S + LC, 1024], F32)
    of_d = kb.dram("of_d", [S, 256], F32)

    def bfv(t):
        return t[:].bitcast(BF16)

    def dbl(name, shape, dt, n=2, es=None):
        return [kb.sb("%s%d" % (name, i), shape, dt, es) for i in range(n)]

    identb = kb.identity("identb", BF16)
    smallb = kb.sb("smallb", [128, 512], F32)
    kb.load("sp", smallb, smallb[:], small.h.partition_broadcast(128), small)
    zer = kb.sb("zer", [128, 128], F32)
    kb.op("pool", lambda e: e.memset(zer[:], 0.0), writes=[zer])

    def rstd_chain(stt, c_in, c_tmp, c_out, n, inv_n):
        kb.op("dve", lambda e: e.tensor_scalar(out=stt[:, c_tmp:c_tmp + n], in0=stt[:, c_in:c_in + n], scalar1=inv_n, scalar2=EPS, op0=ALU.mult, op1=ALU.add),
              reads=[stt], writes=[stt])
        kb.op("act", lambda e: e.activation(out=stt[:, c_tmp:c_tmp + n], in_=stt[:, c_tmp:c_tmp + n], func=AF.Sqrt), reads=[stt], writes=[stt])
        kb.op("dve", lambda e: e.reciprocal(out=stt[:, c_out:c_out + n], in_=stt[:, c_tmp:c_tmp + n]), reads=[stt], writes=[stt])

    wq = [kb.sb("wq%d" % j, [128, 8, 1024], BF16) for j in range(2)]
    bias = [kb.sb("bias%d" % j, [128, 1024], F32) for j in range(2)]
    pesA = ExitStack()
    s_sb = kb.sb("s_sb", [128, 16], F32, pesA)
    kb.load("sp", s_sb, s_sb[:], svec.h, svec)
    kb.op("act", lambda e: e.activation(out=s_sb[:], in_=s_sb[:], func=AF.Silu), reads=[s_sb], writes=[s_sb])
    adab_sb = kb.sb("adab_sb", [128, 16], F32, pesA)
    kb.load("sp", adab_sb, adab_sb[:], adab.h, adab)
    g1_sb = kb.sb("g1_sb", [128, 8], F32, pesA)
    kb.load("sp", g1_sb, g1_sb[:], g1.h, g1)
    mod = kb.sb("mod", [128, 16, 2], F32, pesA)
    gs = kb.sb("gs", [128, 8, 2], F32, pesA)
    w_sb = kb.sb("w_sb", [128, 8, 1024], F32, pesA)
    shiftbc = kb.sb("shiftbc", [128, 8, 128], F32, pesA)
    pm = banks[0]
    with ExitStack() as pes:
        adaw_sb = kb.sb("adaw_sb", [128, 8, 512], F32, pes)
        for v in range(4):
            kb.load("sp", adaw_sb, adaw_sb[:], adaw.h[:, v * 512:(v + 1) * 512].rearrange("(kc p) n -> p kc n", p=128), adaw)
            for oc in range(4):
                g = v * 4 + oc
                for kc in range(8):
                    kb.op("pe", lambda e: e.matmul(pm[:, g * 2:g * 2 + 2], lhsT=adaw_sb[:, kc, oc * 128:(oc + 1) * 128], rhs=s_sb[:, kc * 2:kc * 2 + 2],
                                                  start=(kc == 0), stop=(kc == 7)), reads=[adaw_sb, s_sb], writes=[pm])
        pm3 = pm[:, 0:32].rearrange("p (g j) -> p g j", j=2)
        for j in range(2):
            kb.op("dve", lambda e: e.tensor_tensor(out=mod[:, :, j], in0=pm3[:, :, j], in1=adab_sb[:], op=ALU.add), reads=[pm, adab_sb], writes=[mod])
            kb.op("dve", lambda e: e.scalar_tensor_tensor(out=gs[:, :, j], in0=mod[:, 8:16, j], scalar=1.0, in1=g1_sb[:], op0=ALU.add, op1=ALU.mult),
                  reads=[mod, g1_sb], writes=[gs])
        kb.load("sp", w_sb, w_sb[:, :, 0:768], w.h.rearrange("(kc p) n -> p kc n", p=128), w)
        waT_sb = [kb.sb("waT_sb%d" % d, [32, 1024], F32, pes) for d in range(2)]
        wa2_sb = [kb.sb("wa2_sb%d" % d, [32, 128], F32, pes) for d in range(2)]
        for d in range(2):
            kb.op("pool", lambda e: e.memset(waT_sb[d][:], 0.0), writes=[waT_sb[d]])
            kb.op("pool", lambda e: e.memset(wa2_sb[d][:], 0.0), writes=[wa2_sb[d]])
            kb.load("sp", waT_sb[d], waT_sb[d][0:16, :], waT.h[d], waT)
            kb.load("sp", wa2_sb[d], wa2_sb[d][0:16, :], wa2.h[d], wa2)
            for kc in range(8):
                pz = banks[1]
                kb.op("pe", lambda e: e.matmul(pz[:, 0:128], lhsT=waT_sb[d][:, kc * 128:(kc + 1) * 128], rhs=wa2_sb[d][:], start=True, stop=True),
                      reads=[waT_sb[d], wa2_sb[d]], writes=[pz])
                kb.op("dve", lambda e: e.tensor_copy(out=w_sb[:, kc, 768 + d * 128:768 + (d + 1) * 128], in_=pz[:, 0:128]), reads=[pz], writes=[w_sb])
        for j in range(2):
            for kc in range(8):
                kb.op("dve", lambda e: e.tensor_scalar(out=wq[j][:, kc, :], in0=w_sb[:, kc, :], scalar1=gs[:, kc, j:j + 1], scalar2=None, op0=ALU.mult),
                      reads=[w_sb, gs], writes=[wq[j]])
                kb.op("dve", lambda e: e.tensor_scalar(out=shiftbc[:, kc, :], in0=zer[:], scalar1=mod[:, kc, j:j + 1], scalar2=None, op0=ALU.add),
                      reads=[zer, mod], writes=[shiftbc])
            for half in range(2):
                pb = banks[2 + half]
                for kc in range(8):
                    kb.op("pe", lambda e: e.matmul(pb[:, :], lhsT=shiftbc[:, kc, :], rhs=w_sb[:, kc, half * 512:(half + 1) * 512], start=(kc == 0), stop=(kc == 7)),
                          reads=[shiftbc, w_sb], writes=[pb])
                kb.op("dve", lambda e: e.tensor_copy(out=bias[j][:, half * 512:(half + 1) * 512], in_=pb[:, :]), reads=[pb], writes=[bias[j]])
            kb.op("dve", lambda e: e.tensor_tensor(out=bias[j][:, 768:1024], in0=bias[j][:, 768:1024], in1=smallb[:, 0:256], op=ALU.add), reads=[bias[j], smallb], writes=[bias[j]])
        kb.barrier()
    kb.barrier()
    pesA.close()

    pesB = ExitStack()
    xt = dbl("xt", [128, 1024], F32, 2, pesB)
    junk = kb.sb("junk", [128, 1024], BF16, pesB)
    st1 = dbl("st1", [128, 4], F32, 2, pesB)
    xn = dbl("xn", [128, 1024], BF16, 2, pesB)
    xnT = dbl("xnT", [128, 1024], BF16, 2, pesB)
    pj = dbl("pj", [128, 1024], F32, 2, pesB)
    ez = dbl("ez", [128, 256], F32, 2, pesB)
    one_col = kb.sb("one_col", [128, 1], F32, pesB)
    kb.op("pool", lambda e: e.memset(one_col[:], 1.0), writes=[one_col])

    def proj_tile(i, src, row0, is_ctx, drow):
        p = i % 2
        j = 1 if is_ctx else 0
        if is_ctx:
            c = row0 // 128
            for hf in range(2):
                r = ag_row(4096, 2 * c + hf, 256, 4224)
                kb.load("sp", xt[p], xt[p][hf * 64:(hf + 1) * 64, :], x2out.h[r:r + 64, :], x2out)
                yield
        else:
            t = row0 // 128
            r = ag_row((t % 32) * 128, t // 32, 256, 4224)
            kb.load("sp", xt[p], xt[p][:], x2out.h[r:r + 128, :], x2out)
            yield
        kb.op("act", lambda e: e.activation(out=junk[:], in_=xt[p][:], func=AF.Square, accum_out=st1[p][:, 0:1]), reads=[xt[p]], writes=[junk, st1[p]])
        yield
        rstd_chain(st1[p], 0, 1, 2, 1, 1.0 / 1024)
        kb.op("act", lambda e: e.activation(out=xn[p][:], in_=xt[p][:], func=AF.Copy, scale=st1[p][:, 2:3]), reads=[xt[p], st1[p]], writes=[xn[p]])
        yield
        psT = banks[p]
        for kc in range(8):
            kb.op("pe", lambda e: e.transpose(out=bfv(psT)[:, kc * 128:(kc + 1) * 128], in_=xn[p][:, kc * 128:(kc + 1) * 128], identity=identb[:]),
                  reads=[xn[p], identb], writes=[psT])
            yield
        kb.op("dve", lambda e: e.tensor_copy(out=xnT[p][:], in_=bfv(psT)[:, 0:1024]), reads=[psT], writes=[xnT[p]])
        yield
        for half in range(2):
            pp = banks[2 + 2 * p + half]
            for kc in range(8):
                kb.op("pe", lambda e: e.matmul(pp[:, :], lhsT=xnT[p][:, kc * 128:(kc + 1) * 128], rhs=wq[j][:, kc, half * 512:(half + 1) * 512], start=(kc == 0), stop=(kc == 7)),
                      reads=[xnT[p], wq[j]], writes=[pp])
                yield
            kb.op("dve", lambda e: e.tensor_tensor(out=pj[p][:, half * 512:(half + 1) * 512], in0=pp[:, :], in1=bias[j][:, half * 512:(half + 1) * 512], op=ALU.add),
                  reads=[pp, bias[j]], writes=[pj[p]])
            yield
        kb.op("act", lambda e: e.activation(out=ez[p][:], in_=pj[p][:, 768:1024], func=AF.Exp, scale=-1.0), reads=[pj[p]], writes=[ez[p]])
        yield
        kb.op("act", lambda e: e.activation(out=pj[p][:, 768:1024], in_=ez[p][:], func=AF.Ln, bias=one_col[:, 0:1]), reads=[ez[p], one_col], writes=[pj[p]])
        yield
        kb.store("sp", proj, proj.h[drow:drow + 128, :], pj[p], pj[p][:])
        yield

    gens = []
    i = 0
    for c in range(2):
        gens.append(proj_tile(i, None, c * 128, True, S + c * 128))
        i += 1
    for t in range(n_lat_tiles):
        gens.append(proj_tile(i, None, t * 128, False, t * 128))
        i += 1
    interleave(gens, 2)
    kb.barrier()
    pesB.close()

    mask = []
    for d in range(2):
        mf = kb.sb("mask%d" % d, [128, 128], F32)
        kb.op("pool", lambda e: e.memset(mf[:], 1.0), writes=[mf])
        if d == 0:
            kb.op("pool", lambda e: e.affine_select(out=mf[:], in_=mf[:], pattern=[[1, 128]], compare_op=ALU.is_ge, fill=0.0, base=0, channel_multiplier=-1), reads=[mf], writes=[mf])
        else:
            kb.op("pool", lambda e: e.affine_select(out=mf[:], in_=mf[:], pattern=[[-1, 128]], compare_op=ALU.is_ge, fill=0.0, base=0, channel_multiplier=1), reads=[mf], writes=[mf])
        mask.append(mf)
    LS = -1.0 / 16
    maskS = []
    for d in range(2):
        ms_ = kb.sb("maskS%d" % d, [128, 128], F32)
        kb.op("dve", lambda e: e.tensor_scalar(out=ms_[:], in0=mask[d][:], scalar1=LS, scalar2=None, op0=ALU.mult), reads=[mask[d]], writes=[ms_])
        maskS.append(ms_)
    ones_f = kb.sb("ones_f", [128, 128], F32)
    kb.op("pool", lambda e: e.memset(ones_f[:], -1.0 / 16), writes=[ones_f])
    Sst = kb.sb("Sst", [128, 256], F32)
    Sb = dbl("Sb", [128, 256], BF16)
    pt = dbl("pt", [128, 1024], F32, 3)
    bc = dbl("bc", [128, 128], F32)
    eb = dbl("eb", [128, 128], F32)
    enb = dbl("enb", [128, 128], F32)
    dlt = dbl("dlt", [128, 128], F32)
    dec = dbl("dec", [128, 1], F32)
    qt = dbl("qt", [128, 128], BF16)
    ktl = dbl("ktl", [128, 128], BF16)
    kh = dbl("kh", [128, 128], BF16)
    vb = dbl("vb", [128, 256], BF16)
    qkT = dbl("qkT", [128, 256], BF16)
    attm = dbl("attm", [128, 128], BF16)
    ofs = dbl("ofs", [128, 256], F32)
    osum = dbl("osum", [128, 256], F32)
    fst = dbl("fst", [128, 4], F32)
    sg = dbl("sg", [128, 256], F32)
    ogb = dbl("ogb", [128, 256], BF16)
    ogT_sb = dbl("ogT_sb", [128, 2, 128], BF16)
    QS = 128.0 ** -0.5
    step = [0]

    def gla_prep(c, row, d):
        p = c % 2
        B = banks[4 * p:4 * p + 4]
        t_ = pt[c % 3]
        kb.load("sp", t_, t_[:], proj.h[row:row + 128, :], proj)
        yield
        la = t_[:, 768 + d * 128:768 + (d + 1) * 128]
        kb.op("pe", lambda e: e.matmul(B[0][:, 0:128], lhsT=maskS[d][:], rhs=la, start=True, stop=True), reads=[maskS[d], t_], writes=[B[0]])
        yield
        kb.op("pe", lambda e: e.matmul(B[0][:, 128:256], lhsT=ones_f[:], rhs=la, start=True, stop=True), reads=[ones_f, t_], writes=[B[0]])
        yield
        kb.op("pe", lambda e: e.matmul(B[0][:, 256:384], lhsT=la, rhs=ones_f[:], start=True, stop=True), reads=[ones_f, t_], writes=[B[0]])
        yield
        kb.op("act", lambda e: e.copy(out=bc[p][:], in_=B[0][:, 0:128]), reads=[B[0]], writes=[bc[p]])
        yield
        kb.op("act", lambda e: e.activation(out=eb[p][:], in_=B[0][:, 0:128], func=AF.Exp), reads=[B[0]], writes=[eb[p]])
        yield
        kb.op("act", lambda e: e.activation(out=enb[p][:], in_=B[0][:, 0:128], func=AF.Exp, scale=-1.0), reads=[B[0]], writes=[enb[p]])
        yield
        kb.op("dve", lambda e: e.tensor_tensor(out=dlt[p][:], in0=B[0][:, 128:256], in1=bc[p][:], op=ALU.subtract), reads=[B[0], bc[p]], writes=[dlt[p]])
        yield
        kb.op("act", lambda e: e.activation(out=dlt[p][:], in_=dlt[p][:], func=AF.Exp), reads=[dlt[p]], writes=[dlt[p]])
        yield
        kb.op("act", lambda e: e.activation(out=dec[p][:], in_=B[0][:, 256:257], func=AF.Exp), reads=[B[0]], writes=[dec[p]])
        yield
        kb.op("dve", lambda e: e.scalar_tensor_tensor(out=qt[p][:], in0=t_[:, 0:128], scalar=QS, in1=eb[p][:], op0=ALU.mult, op1=ALU.mult), reads=[t_, eb[p]], writes=[qt[p]])
        yield
        kb.op("dve", lambda e: e.tensor_tensor(out=ktl[p][:], in0=t_[:, 128:256], in1=enb[p][:], op=ALU.mult), reads=[t_, enb[p]], writes=[ktl[p]])
        yield
        kb.op("pool", lambda e: e.tensor_tensor(out=kh[p][:], in0=t_[:, 128:256], in1=dlt[p][:], op=ALU.mult), reads=[t_, dlt[p]], writes=[kh[p]])
        yield
        kb.op("pool", lambda e: e.tensor_copy(out=vb[p][:], in_=t_[:, 256:512]), reads=[t_], writes=[vb[p]])
        yield
        kb.op("pe", lambda e: e.transpose(out=bfv(B[1])[:, 0:128], in_=qt[p][:], identity=identb[:]), reads=[qt[p], identb], writes=[B[1]])
        yield
        kb.op("pe", lambda e: e.transpose(out=bfv(B[1])[:, 128:256], in_=ktl[p][:], identity=identb[:]), reads=[ktl[p], identb], writes=[B[1]])
        yield
        kb.op("act", lambda e: e.copy(out=qkT[p][:], in_=bfv(B[1])[:, 0:256]), reads=[B[1]], writes=[qkT[p]])
        yield
        kb.op("pe", lambda e: e.matmul(B[2][:, 0:128], lhsT=qkT[p][:, 128:256], rhs=qkT[p][:, 0:128], start=True, stop=True), reads=[qkT[p]], writes=[B[2]])
        yield
        kb.op("dve", lambda e: e.tensor_tensor(out=attm[p][:], in0=B[2][:, 0:128], in1=mask[d][:], op=ALU.mult), reads=[B[2], mask[d]], writes=[attm[p]])
        yield

    def gla_fin(c, d, out_mode, out_row):
        p = c % 2
        B = banks[4 * p:4 * p + 4]
        t_ = pt[c % 3]
        sb_cur = Sb[c % 2]
        sb_next = Sb[(c + 1) % 2]
        if out_mode is not None:
            kb.op("pe", lambda e: e.matmul(B[3][:, 0:256], lhsT=qkT[p][:, 0:128], rhs=sb_cur[:], start=True, stop=False), reads=[qkT[p], sb_cur], writes=[B[3]])
            yield
            kb.op("pe", lambda e: e.matmul(B[3][:, 0:256], lhsT=attm[p][:], rhs=vb[p][:], start=False, stop=True), reads=[attm[p], vb[p]], writes=[B[3]])
            yield
        kb.op("pe", lambda e: e.matmul(B[2][:, 128:384], lhsT=kh[p][:], rhs=vb[p][:], start=True, stop=True), reads=[kh[p], vb[p]], writes=[B[2]])
        yield
        kb.op("dve", lambda e: e.scalar_tensor_tensor(out=Sst[:], in0=Sst[:], scalar=dec[p][:, 0:1], in1=B[2][:, 128:384], op0=ALU.mult, op1=ALU.add),
              reads=[Sst, dec[p], B[2]], writes=[Sst])
        yield
        kb.op("act", lambda e: e.copy(out=sb_next[:], in_=Sst[:]), reads=[Sst], writes=[sb_next])
        yield
        if out_mode == "store":
            kb.op("act", lambda e: e.copy(out=ofs[p][:], in_=B[3][:, 0:256]), reads=[B[3]], writes=[ofs[p]])
            yield
            kb.store("sp", of_d, of_d.h[out_row:out_row + 128, :], ofs[p], ofs[p][:])
            yield
        elif out_mode == "final":
            kb.load("sp", ofs[p], ofs[p][:], of_d.h[out_row:out_row + 128, :], of_d)
            yield
            kb.op("dve", lambda e: e.tensor_tensor(out=osum[p][:], in0=B[3][:, 0:256], in1=ofs[p][:], op=ALU.add), reads=[B[3], ofs[p]], writes=[osum[p]])
            yield
            kb.op("act", lambda e: e.activation(out=sg[p][:], in_=osum[p][:], func=AF.Square, accum_out=fst[p][:, 0:1]), reads=[osum[p]], writes=[sg[p], fst[p]])
            yield
            rstd_chain(fst[p], 0, 1, 2, 1, 1.0 / 256)
            kb.op("dve", lambda e: e.scalar_tensor_tensor(out=osum[p][:], in0=osum[p][:], scalar=fst[p][:, 2:3], in1=smallb[:, 256:512], op0=ALU.mult, op1=ALU.mult),
                  reads=[osum[p], fst[p], smallb], writes=[osum[p]])
            yield
            kb.op("act", lambda e: e.activation(out=sg[p][:], in_=t_[:, 512:768], func=AF.Silu), reads=[t_], writes=[sg[p]])
            yield
            kb.op("dve", lambda e: e.tensor_tensor(out=ogb[p][:], in0=osum[p][:], in1=sg[p][:], op=ALU.mult), reads=[osum[p], sg[p]], writes=[ogb[p]])
            yield
            for hh in range(2):
                kb.op("pe", lambda e: e.transpose(out=bfv(B[1])[:, 256 + hh * 128:256 + (hh + 1) * 128], in_=ogb[p][:, hh * 128:(hh + 1) * 128], identity=identb[:]),
                      reads=[ogb[p], identb], writes=[B[1]])
                yield
            kb.op("act", lambda e: e.copy(out=ogT_sb[p][:].rearrange("p a b -> p (a b)"), in_=bfv(B[1])[:, 256:512]), reads=[B[1]], writes=[ogT_sb[p]])
            yield
            tq_, tc_ = (out_row // 128) // 32, ((out_row // 128) % 32) * 128
            kb.store("sp", x3in, x3in.h[tq_ * 256:(tq_ + 1) * 256, tc_:tc_ + 128].rearrange("(a p) n -> p a n", p=128), ogT_sb[p], ogT_sb[p][:])
            yield

    def reset_state(c):
        kb.op("pool", lambda e: e.memset(Sst[:], 0.0), writes=[Sst])
        kb.op("pool", lambda e: e.memset(Sb[c % 2][:], 0.0), writes=[Sb[c % 2]])

    def run_scan(chunks, c0):
        n = len(chunks)
        for _ in gla_prep(c0, chunks[0][0], chunks[0][1]):
            pass
        for k in range(n):
            row, d, om, orow = chunks[k]
            gens = [gla_fin(c0 + k, d, om, orow)]
            if k + 1 < n:
                gens.append(gla_prep(c0 + k + 1, chunks[k + 1][0], chunks[k + 1][1]))
            interleave(gens, 2)
        return c0 + n

    fwd = [(S + c * 128, 0, None, None) for c in range(2)] + [(t * 128, 0, "store", t * 128) for t in range(n_lat_tiles)]
    bwd = [(S + c * 128, 1, None, None) for c in (1, 0)] + [(t * 128, 1, "final", t * 128) for t in range(n_lat_tiles - 1, -1, -1)]
    reset_state(0)
    cn = run_scan(fwd, 0)
    kb.barrier()
    reset_state(cn)
    run_scan(bwd, cn)
    print("l1a instructions:", kb.n_ins, "sems:", len(kb.sems))
    kb.end_stage()


def fop(v, n):
    return np.ascontiguousarray(np.asarray(v, np.float32).reshape(n, 128).T)


def host_l1a(inp):
    maps = []
    wi = inp["gla_w_in"][0]
    for b in range(2):
        sv = np.stack([inp["c"][b], inp["c_ctx"]], -1).reshape(8, 128, 2).transpose(1, 0, 2).reshape(128, 16).astype(np.float32)
        for h in range(4):
            w = np.concatenate([wi[:, h * 128:(h + 1) * 128], wi[:, 512 + h * 128:512 + (h + 1) * 128], wi[:, 1024 + h * 256:1024 + (h + 1) * 256],
                                wi[:, 2048 + h * 256:2048 + (h + 1) * 256]], axis=1)
            waT = np.ascontiguousarray(wi[:, 3072:3104].T.reshape(2, 16, 1024))
            wa2 = np.ascontiguousarray(inp["gla_w_a2"][0][:, :, h * 128:(h + 1) * 128])
            small = np.concatenate([inp["gla_b_a2"][0][0, h * 128:(h + 1) * 128], inp["gla_b_a2"][0][1, h * 128:(h + 1) * 128], inp["gla_norm_g"][0]]).astype(np.float32)
            maps.append({"svec": np.ascontiguousarray(sv),
                         "adaw": np.ascontiguousarray(inp["ada_w"][1][:, 0:2048]), "adab": fop(inp["ada_b"][1][0:2048], 16), "g1": fop(inp["norm1_g"][1], 8),
                         "w": np.ascontiguousarray(w), "waT": waT, "wa2": wa2, "small": small})
    return maps


RG = [[0, 1, 2, 3], [4, 5, 6, 7]]


def build_all():
    kb = KB()
    banks = [kb.ps("bank%d" % i) for i in range(8)]
    x1in = kb.dram("x1in", [512, 4224], BF16)
    x1out = kb.dram("x1out", [2048, 4224], BF16)
    x2in = kb.dram("x2in", [4224, 1024], F32)
    x2out = kb.dram("x2out", [4 * 4224, 1024], F32)
    x3in = kb.dram("x3in", [1024, 4096], BF16)
    x3out = kb.dram("x3out", [4096, 4096], BF16)
    build_l0a(kb, banks, x1in)
    kb.all_gather(x1in, x1out, RG, 64)
    build_b(0, kb, banks, x1out, None, x2in)
    kb.all_gather(x2in, x2out, RG, 256)
    build_l1a(kb, banks, x2out, x3in)
    kb.all_gather(x3in, x3out, RG, 128)
    build_b(1, kb, banks, x3out, x2in, None)
    print("total instructions:", kb.n_ins, "sems:", len(kb.sems))
    return kb.finish()


def kernel(**inputs):
    inp = {k: np.asarray(v) for k, v in inputs.items()}
    parts = [("a0_", host_l0a(inp)), ("b0_", host_b(0, inp)), ("a1_", host_l1a(inp)), ("b1_", host_b(1, inp))]
    maps = []
    for c in range(8):
        m = {}
        for pre, ms in parts:
            for k, v in ms[c].items():
                m[pre + k] = v
        maps.append(m)
    nc = build_all()
    res = run_bass_kernel_spmd(nc, maps, core_ids=list(range(8)))
    out = np.zeros((2, 16384, 1024), np.float32)
    for b in range(2):
        for jq in range(4):
            out[b, jq * 4096:(jq + 1) * 4096] = np.asarray(res.results[b * 4 + jq]["b1_hout"])[:4096]
    return out
```

```python
import numpy as np
from contextlib import ExitStack
import concourse.bass as bass
import concourse.mybir as mybir
from concourse.bass_utils import run_bass_kernel_spmd
import ml_dtypes

F32 = mybir.dt.float32
BF16 = mybir.dt.bfloat16
I32 = mybir.dt.int32
AF = mybir.ActivationFunctionType
ALU = mybir.AluOpType
AX = mybir.AxisListType
NPBF16 = ml_dtypes.bfloat16


def interleave(gens, width):
    active = []
    it = iter(gens)
    while True:
        while len(active) < width:
            g = next(it, None)
            if g is None:
                break
            active.append(g)
        if not active:
            break
        for g in list(active):
            try:
                next(g)
            except StopIteration:
                active.remove(g)


def ag_row(i, rank, chunk_rows, total_rows, world=4):
    r0 = (i // chunk_rows) * chunk_rows
    n = min(chunk_rows, total_rows - r0)
    return world * r0 + rank * n + (i - r0)


class T:
    def __init__(self, h, name, kind):
        self.h = h
        self.name = name
        self.kind = kind
        self.w = None
        self.r = {}
        self.dkey = None

    def __getitem__(self, idx):
        return self.h[idx]


class KB:
    def __init__(self):
        self.nc = bass.Bass("TRN2", target_bir_lowering=False)
        nc = self.nc
        self.es = ExitStack()
        self.eng = {"pe": nc.tensor, "act": nc.scalar, "dve": nc.vector, "pool": nc.gpsimd, "sp": nc.sync}
        self.sems = {}
        self.cnt = {}
        self.seen = {e: {} for e in self.eng}
        for e in self.eng:
            self.sems[e] = self.es.enter_context(nc.semaphore("e_" + e))
            self.cnt[e] = 0
        self.issued = {}
        self.n_ins = 0
        self.outs = []
        self._uid = 0
        self.cur = self.es
        self.prefix = ""
        self.tiles = []
        self.free_dsems = []
        self.stage_tiles0 = 0

    def sb(self, name, shape, dt, es=None):
        h = (es or self.cur).enter_context(self.nc.sbuf_tensor(self.prefix + name, list(shape), dt))
        t = T(h, name, "sb")
        self.tiles.append(t)
        return t

    def ps(self, name, shape=(128, 512), dt=F32):
        h = self.es.enter_context(self.nc.psum_tensor(name, list(shape), dt))
        t = T(h, name, "ps")
        self.tiles.append(t)
        return t

    def dram(self, name, shape, dt, kind="Internal"):
        h = self.nc.dram_tensor(self.prefix + name, list(shape), dt, kind=kind)
        t = T(h.ap(), name, "dram")
        self.tiles.append(t)
        if kind == "ExternalOutput":
            self.outs.append(t)
        return t

    def _dsem(self, t):
        if t.dkey is None:
            self._uid += 1
            t.dkey = "d%d_%s" % (self._uid, t.name)
            if self.free_dsems:
                h, v = self.free_dsems.pop()
                self.sems[t.dkey] = h
                self.issued[t.dkey] = v
            else:
                self.sems[t.dkey] = self.es.enter_context(self.nc.semaphore(t.dkey))
                self.issued[t.dkey] = 0
        return t.dkey

    def begin_stage(self, prefix):
        self.prefix = prefix
        self.cur = ExitStack()
        self.stage_tiles0 = len(self.tiles)

    def end_stage(self):
        self.barrier()
        self.cur.close()
        self.cur = self.es
        for t in self.tiles[self.stage_tiles0:]:
            if t.kind == "sb" and t.dkey is not None:
                self.free_dsems.append((self.sems[t.dkey], self.issued[t.dkey]))
                del self.issued[t.dkey]
                del self.sems[t.dkey]
                t.dkey = None
        for t in self.tiles:
            t.w = None
            t.r = {}
        for e in self.eng:
            self._uid += 1
            self.sems[e] = self.es.enter_context(self.nc.semaphore("e%d_%s" % (self._uid, e)))
            self.cnt[e] = 0
        self.seen = {e: {} for e in self.eng}
        self.prefix = ""

    def all_gather(self, src, dst, groups, chunk_rows):
        self.barrier()
        self._uid += 1
        sem = self.es.enter_context(self.nc.semaphore("cc%d" % self._uid))
        R = src.h.shape[0]
        k = 0
        for r0 in range(0, R, chunk_rows):
            n = min(chunk_rows, R - r0)
            self.nc.gpsimd.collective_compute("AllGather", ALU.bypass, replica_groups=groups, ins=[src.h[r0:r0 + n, :]],
                                              outs=[dst.h[4 * r0:4 * r0 + 4 * n, :]]).then_inc(sem, 1)
            k += 1
        self.nc.gpsimd.wait_ge(sem, k)
        if not hasattr(self, "_fence"):
            self._fence = T(self.es.enter_context(self.nc.sbuf_tensor("cc_fence", [128, 8], F32)), "cc_fence", "sb")
            self.tiles.append(self._fence)
        f = self._fence
        self.op("pool", lambda e: e.memset(f[:], 0.0), writes=[f])
        for en in self.eng:
            if en != "pool":
                self._waits(en, {"pool": self.cnt["pool"]})
        self.n_ins += k + 1

    def _deps(self, en, reads, writes, is_dma=False):
        deps = {}

        def add(key, val, kind):
            if key == en:
                if en == "pe" or kind == "war":
                    return
            if is_dma and kind == "waw" and key in self.issued:
                return
            deps[key] = max(deps.get(key, 0), val)

        for t in reads:
            if t.w is not None:
                add(t.w[0], t.w[1], "raw")
            if t.kind == "ps":
                for k, v in t.r.items():
                    if k != en:
                        add(k, v, "rar")
        for t in writes:
            if t.w is not None:
                add(t.w[0], t.w[1], "waw")
            for k, v in t.r.items():
                add(k, v, "war")
        return deps

    def _waits(self, en, deps):
        e = self.eng[en]
        for key, val in deps.items():
            if key in self.issued:
                val = self.issued[key]
            if self.seen[en].get(key, 0) >= val:
                continue
            e.wait_ge(self.sems[key], val)
            self.seen[en][key] = val
            self.n_ins += 1

    def op(self, en, fn, reads=(), writes=()):
        self._waits(en, self._deps(en, reads, writes))
        ins = fn(self.eng[en])
        self.cnt[en] += 1
        self.n_ins += 1
        ins.then_inc(self.sems[en], 1)
        c = self.cnt[en]
        for t in reads:
            t.r[en] = c
        for t in writes:
            t.w = (en, c)
            t.r = {}
        return ins

    def dma(self, q, fn, reads=(), writes=()):
        self._waits(q, self._deps(q, reads, writes, is_dma=True))
        cand = [t for t in writes if t.kind != "dram"] or [t for t in reads if t.kind != "dram"] or list(writes) or list(reads)
        key = self._dsem(cand[0])
        ins = fn(self.eng[q])
        self.issued[key] += 16
        self.n_ins += 1
        ins.then_inc(self.sems[key], 16)
        v = self.issued[key]
        for t in reads:
            t.r[key] = v
        for t in writes:
            t.w = (key, v)
            t.r = {}
        return ins

    def load(self, q, dst_t, dst_ap, src_ap, src_t=None, **kw):
        return self.dma(q, lambda e: e.dma_start(out=dst_ap, in_=src_ap, **kw),
                        reads=[src_t] if src_t is not None else [], writes=[dst_t])

    def store(self, q, dst_t, dst_ap, src_t, src_ap, **kw):
        return self.dma(q, lambda e: e.dma_start(out=dst_ap, in_=src_ap, **kw), reads=[src_t], writes=[dst_t])

    def finish(self):
        deps = {}
        for t in self.outs:
            if t.w is not None:
                deps[t.w[0]] = max(deps.get(t.w[0], 0), t.w[1])
        self._waits("sp", deps)
        self.es.close()
        return self.nc

    def barrier(self):
        for en in self.eng:
            deps = {}
            for k in self.eng:
                if k != en and self.cnt[k] > 0:
                    deps[k] = self.cnt[k]
            for k, v in self.issued.items():
                if v > 0:
                    deps[k] = v
            self._waits(en, deps)

    def identity(self, name, dt):
        f = self.sb(name + "_f", [128, 128], F32)
        self.op("pool", lambda e: e.memset(f[:], 0.0), writes=[f])
        self.op("pool", lambda e: e.affine_select(out=f[:], in_=f[:], pattern=[[-1, 128]], compare_op=ALU.not_equal,
                                                  fill=1.0, base=0, channel_multiplier=1), reads=[f], writes=[f])
        if dt == F32:
            return f
        b = self.sb(name, [128, 128], dt)
        self.op("pool", lambda e: e.tensor_copy(out=b[:], in_=f[:]), reads=[f], writes=[b])
        return b

EPS = 1e-6
S = 16384
LC = 256

NKT = (S + LC) // 128
ATT_NSPLIT = 512


def build_l0a(kb, banks, x1in, n_groups=32, debug=False):
    kb.begin_stage("a0_")
    x = kb.dram("x", [S, 1024], F32, "ExternalInput")
    ctx = kb.dram("ctx", [LC, 1024], F32, "ExternalInput")
    svec = kb.dram("svec", [128, 16], F32, "ExternalInput")
    adaw = kb.dram("adaw", [1024, 2048], F32, "ExternalInput")
    adab = kb.dram("adab", [128, 16], F32, "ExternalInput")
    g1 = kb.dram("g1", [128, 8], F32, "ExternalInput")
    w = kb.dram("w", [1024, 384], F32, "ExternalInput")
    small = kb.dram("small", [640], F32, "ExternalInput")
    cos4 = kb.dram("cos4", [S, 256], F32, "ExternalInput")
    sin4 = kb.dram("sin4", [S, 256], F32, "ExternalInput")
    zpad = kb.sb("zpad", [128, 4, 64], BF16)
    kb.op("pool", lambda e: e.memset(zpad[:], 0.0), writes=[zpad])
    kb.store("sp", x1in, x1in.h[:, 4160:4224].rearrange("(q p) n -> p q n", p=128), zpad, zpad[:])

    def bfv(t):
        return t[:].bitcast(BF16)

    identb = kb.identity("identb", BF16)
    smallb = kb.sb("smallb", [128, 640], F32)
    kb.load("sp", smallb, smallb[:], small.h.partition_broadcast(128), small)

    tmp64 = kb.sb("tmp64", [128, 2, 64], F32)
    dots = kb.sb("dots", [128, 4], F32)
    kb.op("dve", lambda e: e.tensor_tensor(out=tmp64[:, 0, :], in0=smallb[:, 256:320], in1=smallb[:, 320:384], op=ALU.mult), reads=[smallb], writes=[tmp64])
    kb.op("dve", lambda e: e.tensor_tensor(out=tmp64[:, 1, :], in0=smallb[:, 384:448], in1=smallb[:, 448:512], op=ALU.mult), reads=[smallb], writes=[tmp64])
    kb.op("dve", lambda e: e.tensor_reduce(out=dots[:, 0:2], in_=tmp64[:], axis=AX.X, op=ALU.add), reads=[tmp64], writes=[dots])
    kb.op("act", lambda e: e.activation(out=dots[:, 2:4], in_=dots[:, 0:2], func=AF.Exp), reads=[dots], writes=[dots])
    neglam = kb.sb("neglam", [128, 1], F32)
    kb.op("dve", lambda e: e.scalar_tensor_tensor(out=neglam[:], in0=dots[:, 3:4], scalar=-0.2, in1=dots[:, 2:3], op0=ALU.add, op1=ALU.subtract), reads=[dots], writes=[neglam])
    subg_s = kb.sb("subg_s", [128, 128], F32)
    kb.op("dve", lambda e: e.tensor_scalar(out=subg_s[:], in0=smallb[:, 512:640], scalar1=0.8, scalar2=None, op0=ALU.mult), reads=[smallb], writes=[subg_s])

    s_sb = kb.sb("s_sb", [128, 16], F32)
    kb.load("sp", s_sb, s_sb[:], svec.h, svec)
    kb.op("act", lambda e: e.activation(out=s_sb[:], in_=s_sb[:], func=AF.Silu), reads=[s_sb], writes=[s_sb])
    adab_sb = kb.sb("adab_sb", [128, 16], F32)
    kb.load("sp", adab_sb, adab_sb[:], adab.h, adab)
    g1_sb = kb.sb("g1_sb", [128, 8], F32)
    kb.load("sp", g1_sb, g1_sb[:], g1.h, g1)
    mod = kb.sb("mod", [128, 16, 2], F32)
    gs = kb.sb("gs", [128, 8, 2], F32)
    zer = kb.sb("zer", [128, 128], F32)
    wq = [kb.sb("wq%d" % j, [128, 8, 384], BF16) for j in range(2)]
    bias = [kb.sb("bias%d" % j, [128, 384], F32) for j in range(2)]
    p0 = ExitStack()
    adaw_sb = kb.sb("adaw_sb", [128, 8, 512], F32, p0)
    pm = banks[0]
    for v in range(4):
        kb.load("sp", adaw_sb, adaw_sb[:], adaw.h[:, v * 512:(v + 1) * 512].rearrange("(kc p) n -> p kc n", p=128), adaw)
        for oc in range(4):
            g = v * 4 + oc
            for kc in range(8):
                kb.op("pe", lambda e: e.matmul(pm[:, g * 2:g * 2 + 2], lhsT=adaw_sb[:, kc, oc * 128:(oc + 1) * 128],
                                              rhs=s_sb[:, kc * 2:kc * 2 + 2], start=(kc == 0), stop=(kc == 7)),
                      reads=[adaw_sb, s_sb], writes=[pm])
    pm3 = pm[:, 0:32].rearrange("p (g j) -> p g j", j=2)
    for j in range(2):
        kb.op("dve", lambda e: e.tensor_tensor(out=mod[:, :, j], in0=pm3[:, :, j], in1=adab_sb[:], op=ALU.add), reads=[pm, adab_sb], writes=[mod])
        kb.op("dve", lambda e: e.scalar_tensor_tensor(out=gs[:, :, j], in0=mod[:, 8:16, j], scalar=1.0, in1=g1_sb[:], op0=ALU.add, op1=ALU.mult),
              reads=[mod, g1_sb], writes=[gs])

    w_sb = kb.sb("w_sb", [128, 8, 384], F32, p0)
    kb.load("sp", w_sb, w_sb[:], w.h.rearrange("(kc p) n -> p kc n", p=128), w)
    kb.op("pool", lambda e: e.memset(zer[:], 0.0), writes=[zer])
    shiftbc = kb.sb("shiftbc", [128, 8, 128], F32, p0)
    for j in range(2):
        for kc in range(8):
            kb.op("dve", lambda e: e.tensor_scalar(out=wq[j][:, kc, :], in0=w_sb[:, kc, :], scalar1=gs[:, kc, j:j + 1], scalar2=None, op0=ALU.mult),
                  reads=[w_sb, gs], writes=[wq[j]])
            kb.op("dve", lambda e: e.tensor_scalar(out=shiftbc[:, kc, :], in0=zer[:], scalar1=mod[:, kc, j:j + 1], scalar2=None, op0=ALU.add),
                  reads=[zer, mod], writes=[shiftbc])
        pb = banks[1]
        for kc in range(8):
            kb.op("pe", lambda e: e.matmul(pb[:, 0:384], lhsT=shiftbc[:, kc, :], rhs=w_sb[:, kc, :], start=(kc == 0), stop=(kc == 7)),
                  reads=[shiftbc, w_sb], writes=[pb])
        kb.op("dve", lambda e: e.tensor_copy(out=bias[j][:], in_=pb[:, 0:384]), reads=[pb], writes=[bias[j]])

    kb.barrier()
    p0.close()
    QT = kb.sb("QT", [128, S + LC], BF16)
    KTm = [kb.sb("KT%d" % m, [128, S + LC], BF16) for m in range(2)]
    kb.op("pool", lambda e: e.memset(KTm[0][64:128, :], 0.0), writes=[KTm[0]])
    kb.op("pool", lambda e: e.memset(KTm[1][0:64, :], 0.0), writes=[KTm[1]])
    Vx = kb.sb("Vx", [128, NKT, 129], BF16)
    kb.op("pool", lambda e: e.memset(Vx[:, :, 128:129], 1.0), writes=[Vx])

    def dbl(name, shape, dt, n=2, es=None):
        return [kb.sb("%s%d" % (name, i), shape, dt, es) for i in range(n)]

    p2 = ExitStack()

    xt = dbl("xt", [128, 1024], F32, 4, p2)
    junk = kb.sb("junk", [128, 1024], BF16, p2)
    st1 = dbl("st1", [128, 4], F32, 2, p2)
    xn = dbl("xn", [128, 1024], BF16, 2, p2)
    xnT = dbl("xnT", [128, 1024], BF16, 2, p2)
    qkv = dbl("qkv", [128, 384], F32, 2, p2)
    cs = dbl("cs", [128, 256], F32, 2, p2)
    sn = dbl("sn", [128, 256], F32, 2, p2)
    sq = dbl("sq", [128, 256], F32, 2, p2)
    st2 = dbl("st2", [128, 12], F32, 2, p2)
    qkn = dbl("qkn", [128, 256], F32, 2, p2)
    sw = dbl("sw", [128, 256], F32, 2, p2)
    t1 = dbl("t1", [128, 256], F32, 2, p2)
    rr = dbl("rr", [128, 256], BF16, 2, p2)

    def rstd_chain(stt, c_in, c_tmp, c_out, n, inv_n, srcs):
        kb.op("dve", lambda e: e.tensor_scalar(out=stt[:, c_tmp:c_tmp + n], in0=stt[:, c_in:c_in + n], scalar1=inv_n, scalar2=EPS, op0=ALU.mult, op1=ALU.add),
              reads=[stt], writes=[stt])
        kb.op("act", lambda e: e.activation(out=stt[:, c_tmp:c_tmp + n], in_=stt[:, c_tmp:c_tmp + n], func=AF.Sqrt), reads=[stt], writes=[stt])
        kb.op("dve", lambda e: e.reciprocal(out=stt[:, c_out:c_out + n], in_=stt[:, c_tmp:c_tmp + n]), reads=[stt], writes=[stt])

    tile_args = []

    def do_load(i):
        _, src, row0 = tile_args[i][0:3]
        kb.load("sp", xt[i % 4], xt[i % 4][:], src.h[row0:row0 + 128, :], src)

    def proj_tile(i, src, row0, is_ctx, qcol, kcol, kt):
        p = i % 2
        j = 1 if is_ctx else 0
        if i + 2 < len(tile_args):
            do_load(i + 2)
        yield
        if not is_ctx:
            kb.load("pool", cs[p], cs[p][:], cos4.h[row0:row0 + 128, :], cos4)
            yield
            kb.load("pool", sn[p], sn[p][:], sin4.h[row0:row0 + 128, :], sin4)
            yield
        kb.op("act", lambda e: e.activation(out=junk[:], in_=xt[i % 4][:], func=AF.Square, accum_out=st1[p][:, 0:1]), reads=[xt[i % 4]], writes=[junk, st1[p]])
        yield
        rstd_chain(st1[p], 0, 1, 2, 1, 1.0 / 1024, None)
        kb.op("act", lambda e: e.activation(out=xn[p][:], in_=xt[i % 4][:], func=AF.Copy, scale=st1[p][:, 2:3]), reads=[xt[i % 4], st1[p]], writes=[xn[p]])
        yield
        psT = banks[p]
        for kc in range(8):
            kb.op("pe", lambda e: e.transpose(out=bfv(psT)[:, kc * 128:(kc + 1) * 128], in_=xn[p][:, kc * 128:(kc + 1) * 128], identity=identb[:]),
                  reads=[xn[p], identb], writes=[psT])
            yield
        kb.op("dve", lambda e: e.tensor_copy(out=xnT[p][:], in_=bfv(psT)[:, 0:1024]), reads=[psT], writes=[xnT[p]])
        yield
        pp = banks[2 + p]
        for kc in range(8):
            kb.op("pe", lambda e: e.matmul(pp[:, 0:384], lhsT=xnT[p][:, kc * 128:(kc + 1) * 128], rhs=wq[j][:, kc, :], start=(kc == 0), stop=(kc == 7)),
                  reads=[xnT[p], wq[j]], writes=[pp])
            yield
        kb.op("dve", lambda e: e.tensor_tensor(out=qkv[p][:], in0=pp[:, 0:384], in1=bias[j][:], op=ALU.add), reads=[pp, bias[j]], writes=[qkv[p]])
        yield
        kb.op("pool", lambda e: e.tensor_copy(out=Vx[:, kt, 0:128], in_=qkv[p][:, 256:384]), reads=[qkv[p]], writes=[Vx])
        yield
        kb.op("act", lambda e: e.activation(out=sq[p][:], in_=qkv[p][:, 0:256], func=AF.Square), reads=[qkv[p]], writes=[sq[p]])
        yield
        kb.op("dve", lambda e: e.tensor_reduce(out=st2[p][:, 0:4], in_=sq[p][:].rearrange("p (g d) -> p g d", g=4), axis=AX.X, op=ALU.add),
              reads=[sq[p]], writes=[st2[p]])
        yield
        rstd_chain(st2[p], 0, 4, 8, 4, 1.0 / 64, None)
        for g in range(4):
            kb.op("dve", lambda e: e.scalar_tensor_tensor(out=qkn[p][:, g * 64:(g + 1) * 64], in0=qkv[p][:, g * 64:(g + 1) * 64], scalar=st2[p][:, 8 + g:9 + g],
                                                          in1=smallb[:, g * 64:(g + 1) * 64], op0=ALU.mult, op1=ALU.mult),
                  reads=[qkv[p], st2[p], smallb], writes=[qkn[p]])
            yield
        if is_ctx:
            kb.op("pool", lambda e: e.tensor_copy(out=rr[p][:], in_=qkn[p][:]), reads=[qkn[p]], writes=[rr[p]])
            yield
        else:
            q5 = qkn[p][:].rearrange("p (a h d) -> p a h d", h=2, d=16)
            s5 = sw[p][:].rearrange("p (a h d) -> p a h d", h=2, d=16)
            kb.op("pool", lambda e: e.tensor_copy(out=s5[:, :, 0, :], in_=q5[:, :, 1, :]), reads=[qkn[p]], writes=[sw[p]])
            yield
            kb.op("pool", lambda e: e.tensor_copy(out=s5[:, :, 1, :], in_=q5[:, :, 0, :]), reads=[qkn[p]], writes=[sw[p]])
            yield
            kb.op("pool", lambda e: e.tensor_tensor(out=sw[p][:], in0=sw[p][:], in1=sn[p][:], op=ALU.mult), reads=[sw[p], sn[p]], writes=[sw[p]])
            yield
            kb.op("dve", lambda e: e.tensor_tensor(out=t1[p][:], in0=qkn[p][:], in1=cs[p][:], op=ALU.mult), reads=[qkn[p], cs[p]], writes=[t1[p]])
            yield
            kb.op("dve", lambda e: e.tensor_tensor(out=rr[p][:], in0=t1[p][:], in1=sw[p][:], op=ALU.add), reads=[t1[p], sw[p]], writes=[rr[p]])
            yield
        pq = banks[4 + p]
        for hh in range(2):
            kb.op("pe", lambda e: e.transpose(out=bfv(pq)[:, hh * 128:(hh + 1) * 128], in_=rr[p][:, hh * 128:(hh + 1) * 128], identity=identb[:]),
                  reads=[rr[p], identb], writes=[pq])
            yield
        kb.op("act", lambda e: e.copy(out=QT[:, qcol:qcol + 128], in_=bfv(pq)[:, 0:128]), reads=[pq], writes=[QT])
        yield
        kb.op("act", lambda e: e.copy(out=KTm[0][0:64, kcol:kcol + 128], in_=bfv(pq)[0:64, 128:256]), reads=[pq], writes=[KTm[0]])
        kb.op("act", lambda e: e.copy(out=KTm[1][64:128, kcol:kcol + 128], in_=bfv(pq)[64:128, 128:256]), reads=[pq], writes=[KTm[1]])
        yield

    i = 0
    for c in range(LC // 128):
        tile_args.append((i, ctx, c * 128, True, S + c * 128, c * 128, c))
        i += 1
    for t in range(S // 128):
        tile_args.append((i, x, t * 128, False, t * 128, LC + t * 128, LC // 128 + t))
        i += 1
    do_load(0)
    do_load(1)
    gens = [proj_tile(*a_) for a_ in tile_args]
    interleave(gens, 2)
    kb.barrier()
    p2.close()

    ST = banks[0:3]
    OT = [banks[4], banks[5]]
    PL = [banks[6], banks[7]]
    PS_ = banks[3]
    PT = dbl("pt", [128, 512], BF16, 4)
    Pacc = [kb.sb("pacc%d" % m, [128, 512], F32) for m in range(2)]
    ones_bb = kb.sb("ones_bb", [128, 128], BF16)
    kb.op("pool", lambda e: e.memset(ones_bb[:], 1.0), writes=[ones_bb])
    ones_ff = kb.sb("ones_ff", [128, 128], F32)
    kb.op("pool", lambda e: e.memset(ones_ff[:], 1.0), writes=[ones_ff])
    subg_col = kb.sb("subg_col", [128, 1], F32)
    kb.load("sp", subg_col, subg_col[:], small.h[512:640].rearrange("(p o) -> p o", o=1), small)
    kb.op("dve", lambda e: e.tensor_scalar(out=subg_col[:], in0=subg_col[:], scalar1=0.8, scalar2=None, op0=ALU.mult), reads=[subg_col], writes=[subg_col])
    rlb = dbl("rlb", [128, 512], F32)
    eo = dbl("eo", [128, 512], F32)
    esq = kb.sb("esq", [128, 512], F32)
    outT = dbl("outT", [128, 512], BF16)
    gcount = [0]
    NSPLIT = ATT_NSPLIT

    def attend(qc0, nq, kts):
        steps = [(m, idx, kt) for m in range(2) for idx, kt in enumerate(kts)]
        nk = len(kts)
        gi = gcount[0]
        gcount[0] += 1

        def score(s):
            m, idx, kt = steps[s]
            st = ST[s % 3]
            for c0 in range(0, nq, NSPLIT):
                kb.op("pe", lambda e: e.matmul(st[:, c0:min(nq, c0 + NSPLIT)], lhsT=KTm[m][:, kt * 128:(kt + 1) * 128], rhs=QT[:, qc0 + c0:qc0 + min(nq, c0 + NSPLIT)],
                                              start=True, stop=True), reads=[KTm[m], QT], writes=[st])

        used = {}

        def rest(s):
            m, idx, kt = steps[s]
            st = ST[s % 3]
            pt = PT[s % 4]
            kb.op("act", lambda e: e.activation(out=pt[:, 0:nq], in_=st[:, 0:nq], func=AF.Exp, scale=0.125), reads=[st], writes=[pt])
            for c0 in range(0, nq, NSPLIT):
                kb.op("pe", lambda e: e.matmul(OT[m][:, c0:min(nq, c0 + NSPLIT)], lhsT=Vx[:, kt, 0:128], rhs=pt[:, c0:min(nq, c0 + NSPLIT)], start=(idx == 0 and c0 == 0), stop=(idx == nk - 1),
                                              skip_group_check=True), reads=[pt, Vx], writes=[OT[m]])
            if s + 3 < len(steps):
                score(s + 3)
            if idx % 3 == 2:
                kb.op("pe", lambda e: e.matmul(PL[m][:, 0:nq], lhsT=ones_bb[:], rhs=pt[:, 0:nq], start=((m, "pe") not in used), stop=False), reads=[ones_bb, pt], writes=[PL[m]])
                used[(m, "pe")] = True
            elif (m, "dve") not in used:
                used[(m, "dve")] = True
                kb.op("dve", lambda e: e.tensor_copy(out=Pacc[m][:, 0:nq], in_=pt[:, 0:nq]), reads=[pt], writes=[Pacc[m]])
            else:
                kb.op("dve", lambda e: e.tensor_tensor(out=Pacc[m][:, 0:nq], in0=pt[:, 0:nq], in1=Pacc[m][:, 0:nq], op=ALU.add), reads=[pt, Pacc[m]], writes=[Pacc[m]])

        for s0 in range(min(3, len(steps))):
            score(s0)
        for s in range(len(steps)):
            rest(s)
        for m in range(2):
            kb.op("pe", lambda e: e.matmul(PL[m][:, 0:nq], lhsT=ones_ff[:], rhs=Pacc[m][:, 0:nq], start=((m, "pe") not in used), stop=True), reads=[ones_ff, Pacc[m]], writes=[PL[m]])
            kb.op("dve", lambda e: e.reciprocal(out=rlb[m][:, 0:nq], in_=PL[m][:, 0:nq]), reads=[PL[m]], writes=[rlb[m]])
            kb.op("dve", lambda e: e.tensor_tensor(out=eo[m][:, 0:nq], in0=OT[m][:, 0:nq], in1=rlb[m][:, 0:nq], op=ALU.mult), reads=[OT[m], rlb[m]], writes=[eo[m]])
        kb.op("dve", lambda e: e.scalar_tensor_tensor(out=eo[0][:, 0:nq], in0=eo[1][:, 0:nq], scalar=neglam[:, 0:1], in1=eo[0][:, 0:nq], op0=ALU.mult, op1=ALU.add),
              reads=[eo[1], neglam, eo[0]], writes=[eo[0]])
        kb.op("act", lambda e: e.activation(out=esq[:, 0:nq], in_=eo[0][:, 0:nq], func=AF.Square), reads=[eo[0]], writes=[esq])
        kb.op("pe", lambda e: e.matmul(PS_[:, 0:nq], lhsT=ones_ff[:], rhs=esq[:, 0:nq], start=True, stop=True), reads=[ones_ff, esq], writes=[PS_])
        kb.op("dve", lambda e: e.tensor_scalar(out=rlb[0][:, 0:nq], in0=PS_[:, 0:nq], scalar1=1.0 / 128, scalar2=EPS, op0=ALU.mult, op1=ALU.add), reads=[PS_], writes=[rlb[0]])
        kb.op("act", lambda e: e.activation(out=rlb[0][:, 0:nq], in_=rlb[0][:, 0:nq], func=AF.Sqrt), reads=[rlb[0]], writes=[rlb[0]])
        kb.op("dve", lambda e: e.reciprocal(out=rlb[1][:, 0:nq], in_=rlb[0][:, 0:nq]), reads=[rlb[0]], writes=[rlb[1]])
        ot = outT[gi % 2]
        kb.op("dve", lambda e: e.scalar_tensor_tensor(out=ot[:, 0:nq], in0=eo[0][:, 0:nq], scalar=subg_col[:, 0:1], in1=rlb[1][:, 0:nq], op0=ALU.mult, op1=ALU.mult),
              reads=[eo[0], subg_col, rlb[1]], writes=[ot])
        if qc0 >= S:
            for q in range(4):
                kb.store("sp", x1in, x1in.h[q * 128:(q + 1) * 128, 4096:4160], ot, ot[:, q * 64:(q + 1) * 64])
        else:
            q, col = qc0 // 4096, qc0 % 4096
            kb.store("sp", x1in, x1in.h[q * 128:(q + 1) * 128, col:col + nq], ot, ot[:, 0:nq])

    attend(S, LC, list(range(LC // 128)))
    for g in range(n_groups):
        attend(g * 512, 512, list(range(NKT)))
    print("l0a instructions:", kb.n_ins)
    kb.end_stage()


def rope_tables():
    half = 32
    inv = (10000.0 ** (-np.arange(0, half, 2, dtype=np.float32) / half)).astype(np.float32)
    t = np.arange(S)
    r = (t // 64).astype(np.float32)[:, None] * inv[None, :]
    c = (t % 64).astype(np.float32)[:, None] * inv[None, :]
    ang = np.concatenate([r, r, c, c], axis=-1).astype(np.float32)
    cos = np.cos(ang).astype(np.float32)
    sin = np.sin(ang).astype(np.float32)
    sgn = np.concatenate([-np.ones(16), np.ones(16), -np.ones(16), np.ones(16)]).astype(np.float32)
    sin = sin * sgn[None, :]
    return np.ascontiguousarray(np.tile(cos, (1, 4))), np.ascontiguousarray(np.tile(sin, (1, 4)))


def fop(v, n):
    return np.ascontiguousarray(np.asarray(v, np.float32).reshape(n, 128).T)


def host_l0a(inp):
    cos4, sin4 = rope_tables()
    maps = []
    wi = inp["ab_w_in"][0]
    for b in range(2):
        for h in range(4):
            sv = np.stack([inp["c"][b], inp["c_ctx"]], -1).reshape(8, 128, 2).transpose(1, 0, 2).reshape(128, 16)
            w = np.concatenate([wi[:, 1024 + h * 128:1024 + (h + 1) * 128], wi[:, 1536 + h * 128:1536 + (h + 1) * 128],
                                wi[:, 2048 + h * 128:2048 + (h + 1) * 128]], axis=1)
            qg, kg = inp["diff_qnorm_g"][0], inp["diff_knorm_g"][0]
            small = np.concatenate([qg, qg, kg, kg, inp["diff_lq1"][0], inp["diff_lk1"][0], inp["diff_lq2"][0], inp["diff_lk2"][0],
                                    inp["diff_subln_g"][0]]).astype(np.float32)
            maps.append({
                "x": np.ascontiguousarray(inp["x"][b]), "ctx": np.ascontiguousarray(inp["ctx"][b]),
                "svec": np.ascontiguousarray(sv.astype(np.float32)),
                "adaw": np.ascontiguousarray(inp["ada_w"][0][:, 0:2048]), "adab": fop(inp["ada_b"][0][0:2048], 16),
                "g1": fop(inp["norm1_g"][0], 8), "w": np.ascontiguousarray(w), "small": small, "cos4": cos4, "sin4": sin4,
            })
    return maps


BIG = 1.0e30


def build_b(layer, kb, banks, mixsrc, hin_t, hout_t, debug=False):
    L0 = (layer == 0)
    NTL = 32
    NT = NTL + (1 if L0 else 0)
    NTOK = NT * 128
    NTOKV = 4096 + (64 if L0 else 0)
    NB = (2 * NTOKV + 32 * 255 + 255) // 256
    NROWS = NB * 256
    NMIX = 4 if L0 else 8

    kb.begin_stage("b%d_" % layer)
    hin = hin_t if hin_t is not None else kb.dram("hin", [NTOK, 1024], F32, "ExternalInput")
    svec = kb.dram("svec", [128, 16], F32, "ExternalInput")
    adaw = kb.dram("adaw", [1024, 6144], F32, "ExternalInput")
    adabf = kb.dram("adabf", [128, 48], F32, "ExternalInput")
    adabr = kb.dram("adabr", [2048], F32, "ExternalInput")
    gfop = kb.dram("gfop", [128, 16], F32, "ExternalInput")
    mixidx = kb.dram("mixidx", [128, NMIX], I32, "ExternalInput")
    wout = kb.dram("wout", [1024, 1024], F32, "ExternalInput")
    rw = kb.dram("rw", [1024, 36], F32, "ExternalInput")
    rb = kb.dram("rb", [36], F32, "ExternalInput")
    w1t = kb.dram("w1t", [4096, 4096], F32, "ExternalInput")
    w3t = kb.dram("w3t", [4096, 4096], F32, "ExternalInput")
    w2t = kb.dram("w2t", [4096, 4096], F32, "ExternalInput")
    valid = kb.dram("valid", [128, 1], F32, "ExternalInput")
    if L0:
        xhalo = kb.dram("xhalo", [128, 1024], F32, "ExternalInput")
        cxh = kb.dram("cxh", [128, 1024], F32, "ExternalInput")
        edge = kb.dram("edge", [2], F32, "ExternalInput")
        win = kb.dram("win", [1024, 1024], F32, "ExternalInput")
        cw = kb.dram("cw", [128, 124], F32, "ExternalInput")
        cvec = kb.dram("cvec", [128, 12], F32, "ExternalInput")
    hout = hout_t if hout_t is not None else kb.dram("hout", [NTOK, 1024], F32, "ExternalOutput")
    hlm = kb.dram("hlm", [NTOK, 1024], F32)
    nl2d = kb.dram("nl2d", [NTOK, 1024], BF16)
    xs = kb.dram("xs", [NROWS + 128, 1024], BF16)
    ys = kb.dram("ys", [NROWS + 128, 1024], F32)

    def bfv(t):
        return t[:].bitcast(BF16)

    def dbl(name, shape, dt, n=2, es=None):
        return [kb.sb("%s%d" % (name, i), shape, dt, es) for i in range(n)]

    identb = kb.identity("identb", BF16)
    zer = kb.sb("zer", [128, 128], F32)
    kb.op("pool", lambda e: e.memset(zer[:], 0.0), writes=[zer])
    zerb = kb.sb("zerb", [128, 2048], BF16)
    kb.op("pool", lambda e: e.memset(zerb[:], 0.0), writes=[zerb])
    for a in range(0, NROWS // 128, 2):
        kb.store("pool", xs, xs.h[a * 128:(a + 2) * 128, :].rearrange("(a p) n -> p a n", p=128), zerb, zerb[:].rearrange("p (a n) -> p a n", a=2))

    OH = kb.sb("OH", [128, NT, 2, 32], F32)
    GT = kb.sb("GT", [128, NT, 2], F32)
    RK = kb.sb("RK", [128, NT, 2], F32)
    Rbc = kb.sb("Rbc", [128, 32], F32)
    DESTI = kb.sb("DESTI", [128, NT * 2], I32)
    WIDX = kb.sb("WIDX", [128, NB], I32)
    validt = kb.sb("validt", [128, 1], F32)
    gate_bc = [[kb.sb("gate_bc%d%d" % (j, w), [128, 1024], F32) for w in range(2)] for j in range(2)]
    mod = kb.sb("mod", [128, 48, 2], F32)
    gs1 = kb.sb("gs1", [128, 8, 2], F32)
    gs2 = kb.sb("gs2", [128, 8, 2], F32)
    s_sb = kb.sb("s_sb", [128, 16], F32)
    adabf_sb = kb.sb("adabf_sb", [128, 48], F32)
    gfop_sb = kb.sb("gfop_sb", [128, 16], F32)
    iop = kb.sb("iop", [128, 1], F32)
    blkst = kb.sb("blkst", [128, NB], F32)
    ltri_b = kb.sb("ltri_b", [128, 128], BF16)
    ones_b = kb.sb("ones_b", [128, 128], BF16)
    pesA = ExitStack()

    def rstd_chain(stt, c_in, c_tmp, c_out, n, inv_n):
        kb.op("dve", lambda e: e.tensor_scalar(out=stt[:, c_tmp:c_tmp + n], in0=stt[:, c_in:c_in + n], scalar1=inv_n, scalar2=EPS, op0=ALU.mult, op1=ALU.add),
              reads=[stt], writes=[stt])
        kb.op("act", lambda e: e.activation(out=stt[:, c_tmp:c_tmp + n], in_=stt[:, c_tmp:c_tmp + n], func=AF.Sqrt), reads=[stt], writes=[stt])
        kb.op("dve", lambda e: e.reciprocal(out=stt[:, c_out:c_out + n], in_=stt[:, c_tmp:c_tmp + n]), reads=[stt], writes=[stt])

    kb.load("sp", s_sb, s_sb[:], svec.h, svec)
    kb.op("act", lambda e: e.activation(out=s_sb[:], in_=s_sb[:], func=AF.Silu), reads=[s_sb], writes=[s_sb])
    kb.load("sp", adabf_sb, adabf_sb[:], adabf.h, adabf)
    kb.load("sp", gfop_sb, gfop_sb[:], gfop.h, gfop)
    kb.load("sp", validt, validt[:], valid.h, valid)
    pm = banks[0]
    with ExitStack() as pes:
        adabr_sb = kb.sb("adabr_sb", [128, 2048], F32, pes)
        kb.load("sp", adabr_sb, adabr_sb[:], adabr.h.partition_broadcast(128), adabr)
        s_bc = [kb.sb("s_bc%d" % j, [128, 8, 128], F32, pes) for j in range(2)]
        for j in range(2):
            for kc in range(8):
                kb.op("dve", lambda e: e.tensor_scalar(out=s_bc[j][:, kc, :], in0=zer[:, 0:128], scalar1=s_sb[:, kc * 2 + j:kc * 2 + j + 1], scalar2=None, op0=ALU.add),
                      reads=[zer, s_sb], writes=[s_bc[j]])
        adaw_sb = dbl("adaw_sb", [128, 8, 512], F32, 2, pes)
        for v in range(12):
            aw = adaw_sb[v % 2]
            kb.load("sp", aw, aw[:], adaw.h[:, v * 512:(v + 1) * 512].rearrange("(kc p) n -> p kc n", p=128), adaw)
            for oc in range(4):
                g = v * 4 + oc
                for kc in range(8):
                    kb.op("pe", lambda e: e.matmul(pm[:, g * 2:g * 2 + 2], lhsT=aw[:, kc, oc * 128:(oc + 1) * 128], rhs=s_sb[:, kc * 2:kc * 2 + 2],
                                                  start=(kc == 0), stop=(kc == 7)), reads=[aw, s_sb], writes=[pm])
            if v in (4, 5, 10, 11):
                which = 0 if v < 6 else 1
                half = v % 2
                for j in range(2):
                    pr = banks[1 + j]
                    for kc in range(8):
                        kb.op("pe", lambda e: e.matmul(pr[:, :], lhsT=s_bc[j][:, kc, :], rhs=aw[:, kc, :], start=(kc == 0), stop=(kc == 7)),
                              reads=[s_bc[j], aw], writes=[pr])
                    kb.op("dve", lambda e: e.tensor_tensor(out=gate_bc[j][which][:, half * 512:(half + 1) * 512], in0=pr[:, :],
                                                           in1=adabr_sb[:, which * 1024 + half * 512: which * 1024 + (half + 1) * 512], op=ALU.add),
                          reads=[pr, adabr_sb], writes=[gate_bc[j][which]])
        pm3 = pm[:, 0:96].rearrange("p (g j) -> p g j", j=2)
        for j in range(2):
            kb.op("dve", lambda e: e.tensor_tensor(out=mod[:, :, j], in0=pm3[:, :, j], in1=adabf_sb[:], op=ALU.add), reads=[pm, adabf_sb], writes=[mod])
        kb.barrier()
    for j in range(2):
        kb.op("dve", lambda e: e.scalar_tensor_tensor(out=gs1[:, :, j], in0=mod[:, 8:16, j], scalar=1.0, in1=gfop_sb[:, 0:8], op0=ALU.add, op1=ALU.mult),
              reads=[mod, gfop_sb], writes=[gs1])
        kb.op("dve", lambda e: e.scalar_tensor_tensor(out=gs2[:, :, j], in0=mod[:, 32:40, j], scalar=1.0, in1=gfop_sb[:, 8:16], op0=ALU.add, op1=ALU.mult),
              reads=[mod, gfop_sb], writes=[gs2])
    SH1, SH2 = 0, 24

    xn_b = dbl("xn_b", [128, 1024], BF16, 2, pesA)
    junk = kb.sb("junk", [128, 1024], BF16, pesA)
    stn = dbl("stn", [128, 4], F32, 2, pesA)

    def norm_T(i, xt_tile, gs, shoff, j, dstT, dcol, psT):
        p = i % 2
        kb.op("act", lambda e: e.activation(out=junk[:], in_=xt_tile[:], func=AF.Square, accum_out=stn[p][:, 0:1]), reads=[xt_tile], writes=[junk, stn[p]])
        rstd_chain(stn[p], 0, 1, 2, 1, 1.0 / 1024)
        kb.op("act", lambda e: e.activation(out=xn_b[p][:], in_=xt_tile[:], func=AF.Copy, scale=stn[p][:, 2:3]), reads=[xt_tile, stn[p]], writes=[xn_b[p]])
        for kc in range(8):
            kb.op("pe", lambda e: e.transpose(out=bfv(psT)[:, kc * 128:(kc + 1) * 128], in_=xn_b[p][:, kc * 128:(kc + 1) * 128], identity=identb[:]),
                  reads=[xn_b[p], identb], writes=[psT])
        for kc in range(8):
            kb.op("act", lambda e: e.activation(out=dstT[:, kc, dcol:dcol + 128], in_=bfv(psT)[:, kc * 128:(kc + 1) * 128], func=AF.Identity,
                                                scale=gs[:, kc, j:j + 1], bias=mod[:, shoff + kc, j:j + 1]), reads=[psT, gs, mod], writes=[dstT])

    xt = dbl("xt", [128, 1024], F32, 2, pesA)
    convT = kb.sb("convT", [128, 4, NTOK], BF16, pesA) if L0 else None

    if L0:
        with ExitStack() as pes:
            HW = 15 + 4096 + 15
            hT = kb.sb("hT", [128, 4, HW], F32, pes)
            hTc = kb.sb("hTc", [128, 4, 128], F32, pes)
            win_b = kb.sb("win_b", [128, 8, 1024], BF16, pes)
            stg = xt
            for kc in range(8):
                kb.load("sp", stg[kc % 2], stg[kc % 2][:], win.h[kc * 128:(kc + 1) * 128, :], win)
                kb.op("pool", lambda e: e.tensor_copy(out=win_b[:, kc, :], in_=stg[kc % 2][:]), reads=[stg[kc % 2]], writes=[win_b])
            cw_sb = kb.sb("cw_sb", [128, 4, 31], F32, pes)
            kb.load("sp", cw_sb, cw_sb[:], cw.h.rearrange("p (c t) -> p c t", c=4), cw)
            cvec_sb = kb.sb("cvec_sb", [128, 12], F32, pes)
            kb.load("sp", cvec_sb, cvec_sb[:], cvec.h, cvec)
            edge_sb = kb.sb("edge_sb", [128, 2], F32, pes)
            kb.load("sp", edge_sb, edge_sb[:], edge.h.partition_broadcast(128), edge)
            ones_s = kb.sb("ones_s", [128, 128], F32, pes)
            kb.op("pool", lambda e: e.memset(ones_s[:], 1.0 / 512), writes=[ones_s])
            nlT = dbl("nlT", [128, 8, 512], BF16, 1, pes) * 2
            sig = dbl("sig", [128, 512], F32, 2, pes)
            htmp = kb.sb("htmp", [128, 4, 128], F32, pes)

            def u_group(gi, nl, ncols, dst_fn):
                for cc in range(4):
                    pa, pg = banks[2], banks[3]
                    for kc in range(8):
                        kb.op("pe", lambda e: e.matmul(pa[:, 0:ncols], lhsT=win_b[:, kc, cc * 128:(cc + 1) * 128], rhs=nl[:, kc, 0:ncols], start=(kc == 0), stop=(kc == 7)),
                              reads=[win_b, nl], writes=[pa])
                    for kc in range(8):
                        kb.op("pe", lambda e: e.matmul(pg[:, 0:ncols], lhsT=win_b[:, kc, 512 + cc * 128:512 + (cc + 1) * 128], rhs=nl[:, kc, 0:ncols], start=(kc == 0), stop=(kc == 7)),
                              reads=[win_b, nl], writes=[pg])
                    sg = sig[cc % 2]
                    kb.op("act", lambda e: e.activation(out=sg[:, 0:ncols], in_=pg[:, 0:ncols], func=AF.Sigmoid), reads=[pg], writes=[sg])
                    dt_, dap = dst_fn(cc)
                    kb.op("dve", lambda e: e.tensor_tensor(out=dap, in0=pa[:, 0:ncols], in1=sg[:, 0:ncols], op=ALU.mult), reads=[pa, sg], writes=[dt_])

            ti = 0
            for g in range(8):
                nl = nlT[g % 2]
                for tt in range(4):
                    t = g * 4 + tt
                    kb.load("sp", xt[ti % 2], xt[ti % 2][:], hin.h[t * 128:(t + 1) * 128, :], hin)
                    norm_T(ti, xt[ti % 2], gs1, SH1, 0, nl, tt * 128, banks[ti % 2])
                    ti += 1
                u_group(g, nl, 512, lambda cc: (hT, hT[:, cc, 15 + g * 512:15 + (g + 1) * 512]))
            nl = nlT[0]
            kb.load("sp", xt[ti % 2], xt[ti % 2][:], xhalo.h, xhalo)
            norm_T(ti, xt[ti % 2], gs1, SH1, 0, nl, 0, banks[ti % 2])
            ti += 1
            u_group(8, nl, 128, lambda cc: (htmp, htmp[:, cc, :]))
            for cc in range(4):
                kb.op("dve", lambda e: e.tensor_scalar(out=hT[:, cc, 0:15], in0=htmp[:, cc, 0:15], scalar1=edge_sb[:, 0:1], scalar2=None, op0=ALU.mult),
                      reads=[htmp, edge_sb], writes=[hT])
                kb.op("dve", lambda e: e.tensor_scalar(out=hT[:, cc, 15 + 4096:HW], in0=htmp[:, cc, 15:30], scalar1=edge_sb[:, 1:2], scalar2=None, op0=ALU.mult),
                      reads=[htmp, edge_sb], writes=[hT])
            nl = nlT[1]
            kb.load("sp", xt[ti % 2], xt[ti % 2][:], cxh.h, cxh)
            norm_T(ti, xt[ti % 2], gs1, SH1, 1, nl, 0, banks[ti % 2])
            ti += 1
            u_group(9, nl, 128, lambda cc: (hTc, hTc[:, cc, :]))
            for cc in range(4):
                kb.op("dve", lambda e: e.tensor_scalar(out=hTc[:, cc, 0:15], in0=hTc[:, cc, 0:15], scalar1=edge_sb[:, 0:1], scalar2=None, op0=ALU.mult),
                      reads=[hTc, edge_sb], writes=[hTc])
                kb.op("dve", lambda e: e.tensor_scalar(out=hTc[:, cc, 79:94], in0=hTc[:, cc, 79:94], scalar1=edge_sb[:, 1:2], scalar2=None, op0=ALU.mult),
                      reads=[hTc, edge_sb], writes=[hTc])

            acc = [kb.sb("acc%d" % c, [128, 512], F32, pes) for c in range(4)]
            sqt = dbl("sqt", [128, 512], F32, 1, pes) * 2
            mean_sb = kb.sb("mean_sb", [128, 512], F32, pes)
            m2 = kb.sb("m2", [128, 512], F32, pes)
            rstd_bc = kb.sb("rstd_bc", [128, 512], F32, pes)
            tt_ = dbl("tt_", [128, 512], F32, 1, pes) * 2

            def conv_block(src, c0, n, out_c0):
                for tau in range(31):
                    for cc in range(4):
                        en = "dve"
                        if tau == 0:
                            kb.op(en, lambda e: e.tensor_scalar(out=acc[cc][:, 0:n], in0=src[:, cc, c0:c0 + n], scalar1=cw_sb[:, cc, 0:1], scalar2=cvec_sb[:, cc:cc + 1],
                                                                op0=ALU.mult, op1=ALU.add), reads=[src, cw_sb, cvec_sb], writes=[acc[cc]])
                        else:
                            kb.op(en, lambda e: e.scalar_tensor_tensor(out=acc[cc][:, 0:n], in0=src[:, cc, c0 + tau:c0 + tau + n], scalar=cw_sb[:, cc, tau:tau + 1],
                                                                       in1=acc[cc][:, 0:n], op0=ALU.mult, op1=ALU.add), reads=[src, cw_sb, acc[cc]], writes=[acc[cc]])
                pmean, pex2 = banks[4], banks[5]
                for cc in range(4):
                    kb.op("pe", lambda e: e.matmul(pmean[:, 0:n], lhsT=ones_s[:], rhs=acc[cc][:, 0:n], start=(cc == 0), stop=(cc == 3)), reads=[ones_s, acc[cc]], writes=[pmean])
                for cc in range(4):
                    sq_ = sqt[cc % 2]
                    kb.op("act", lambda e: e.activation(out=sq_[:, 0:n], in_=acc[cc][:, 0:n], func=AF.Square), reads=[acc[cc]], writes=[sq_])
                    kb.op("pe", lambda e: e.matmul(pex2[:, 0:n], lhsT=ones_s[:], rhs=sq_[:, 0:n], start=(cc == 0), stop=(cc == 3)), reads=[ones_s, sq_], writes=[pex2])
                kb.op("act", lambda e: e.copy(out=mean_sb[:, 0:n], in_=pmean[:, 0:n]), reads=[pmean], writes=[mean_sb])
                kb.op("pool", lambda e: e.tensor_tensor(out=m2[:, 0:n], in0=mean_sb[:, 0:n], in1=mean_sb[:, 0:n], op=ALU.mult), reads=[mean_sb], writes=[m2])
                kb.op("dve", lambda e: e.tensor_tensor(out=m2[:, 0:n], in0=pex2[:, 0:n], in1=m2[:, 0:n], op=ALU.subtract), reads=[pex2, m2], writes=[m2])
                kb.op("dve", lambda e: e.tensor_scalar(out=m2[:, 0:n], in0=m2[:, 0:n], scalar1=EPS, scalar2=None, op0=ALU.add), reads=[m2], writes=[m2])
                kb.op("act", lambda e: e.activation(out=m2[:, 0:n], in_=m2[:, 0:n], func=AF.Sqrt), reads=[m2], writes=[m2])
                kb.op("dve", lambda e: e.reciprocal(out=rstd_bc[:, 0:n], in_=m2[:, 0:n]), reads=[m2], writes=[rstd_bc])
                for cc in range(4):
                    t_ = tt_[cc % 2]
                    kb.op("dve", lambda e: e.tensor_tensor(out=t_[:, 0:n], in0=acc[cc][:, 0:n], in1=mean_sb[:, 0:n], op=ALU.subtract), reads=[acc[cc], mean_sb], writes=[t_])
                    kb.op("pool", lambda e: e.tensor_tensor(out=t_[:, 0:n], in0=t_[:, 0:n], in1=rstd_bc[:, 0:n], op=ALU.mult), reads=[t_, rstd_bc], writes=[t_])
                    kb.op("act", lambda e: e.activation(out=convT[:, cc, out_c0:out_c0 + n], in_=t_[:, 0:n], func=AF.Silu, scale=cvec_sb[:, 4 + cc:5 + cc],
                                                        bias=cvec_sb[:, 8 + cc:9 + cc]), reads=[t_, cvec_sb], writes=[convT])

            for tb in range(8):
                conv_block(hT, tb * 512, 512, tb * 512)
            conv_block(hTc, 0, 64, 4096)
            kb.op("pool", lambda e: e.memset(convT[:, :, 4096 + 64:4096 + 128], 0.0), writes=[convT])
            kb.barrier()

    mix_sb = kb.sb("mix_sb", [128, NMIX, NTOK], BF16, pesA)
    mixidx_sb = kb.sb("mixidx_sb", [128, NMIX], I32, pesA)
    kb.load("sp", mixidx_sb, mixidx_sb[:], mixidx.h, mixidx)
    for hh in range(NMIX):
        kb.dma("pool", lambda e: e.indirect_dma_start(out=mix_sb[:, hh, :], out_offset=None, in_=mixsrc.h[:, :],
                                                      in_offset=bass.IndirectOffsetOnAxis(ap=mixidx_sb[:, hh:hh + 1], axis=0)), reads=[mixidx_sb, mixsrc], writes=[mix_sb])
    wout_b = kb.sb("wout_b", [128, 8, 1024], BF16, pesA)
    rw_b = kb.sb("rw_b", [128, 8, 36], BF16, pesA)
    rb_bc = kb.sb("rb_bc", [128, 36], F32, pesA)
    kb.load("sp", rb_bc, rb_bc[:], rb.h.partition_broadcast(128), rb)
    kb.op("pool", lambda e: e.memset(Rbc[:], 0.0), writes=[Rbc])
    ltri = kb.sb("ltri", [128, 128], F32, pesA)
    kb.op("pool", lambda e: e.memset(ltri[:], 1.0), writes=[ltri])
    kb.op("pool", lambda e: e.affine_select(out=ltri[:], in_=ltri[:], pattern=[[1, 128]], compare_op=ALU.is_gt, fill=0.0, base=0, channel_multiplier=-1),
          reads=[ltri], writes=[ltri])
    kb.op("pool", lambda e: e.tensor_copy(out=ltri_b[:], in_=ltri[:]), reads=[ltri], writes=[ltri_b])
    kb.op("pool", lambda e: e.memset(ones_b[:], 1.0), writes=[ones_b])
    kb.op("pool", lambda e: e.iota(iop[:], pattern=[[0, 1]], base=0, channel_multiplier=1, allow_small_or_imprecise_dtypes=True), writes=[iop])
    kb.op("pool", lambda e: e.iota(blkst[:], pattern=[[256, NB]], base=0, channel_multiplier=0, allow_small_or_imprecise_dtypes=True), writes=[blkst])

    with ExitStack() as pes:
        stg = dbl("stg2", [128, 1024], F32, 2, pes)
        for kc in range(8):
            kb.load("sp", stg[kc % 2], stg[kc % 2][:], wout.h[kc * 128:(kc + 1) * 128, :], wout)
            kb.op("pool", lambda e: e.tensor_copy(out=wout_b[:, kc, :], in_=stg[kc % 2][:]), reads=[stg[kc % 2]], writes=[wout_b])
        rw_f = kb.sb("rw_f", [128, 8, 36], F32, pes)
        kb.load("sp", rw_f, rw_f[:], rw.h.rearrange("(kc p) n -> p kc n", p=128), rw)
        kb.op("pool", lambda e: e.tensor_copy(out=rw_b[:], in_=rw_f[:]), reads=[rw_f], writes=[rw_b])

        ytmp = dbl("ytmp", [128, 1024], F32, 2, pes)
        hl = dbl("hl", [128, 1024], F32, 2, pes)
        nl2T = dbl("nl2T", [128, 8, 128], BF16, 2, pes)
        nl2 = dbl("nl2", [128, 1024], BF16, 2, pes)
        lg = dbl("lg", [128, 36], F32, 2, pes)
        rt = dbl("rt", [128, 16], F32, 2, pes)
        lem = dbl("lem", [128, 32], F32, 2, pes)
        lem2 = dbl("lem2", [128, 32], F32, 2, pes)
        cb_ = dbl("cb_", [128, 32], BF16, 2, pes)
        rbase = dbl("rbase", [128, 32], F32, 2, pes)
        tmp32 = dbl("tmp32", [128, 2, 32], F32, 2, pes)
        ejunk = kb.sb("ejunk", [128, 4], F32, pes)

        for t in range(NT):
            p = t % 2
            j = 1 if (L0 and t == NT - 1) else 0
            kb.load("sp", xt[p], xt[p][:], hin.h[t * 128:(t + 1) * 128, :], hin)
            chunks = []
            if L0:
                for cc in range(4):
                    chunks.append((convT, convT[:, cc, t * 128:(t + 1) * 128]))
            for hh in range(NMIX):
                chunks.append((mix_sb, mix_sb[:, hh, t * 128:(t + 1) * 128]))
            for half in range(2):
                py = banks[half]
                for ci, (ct, cap) in enumerate(chunks):
                    kb.op("pe", lambda e: e.matmul(py[:, :], lhsT=cap, rhs=wout_b[:, ci, half * 512:(half + 1) * 512], start=(ci == 0), stop=(ci == 7)),
                          reads=[ct, wout_b], writes=[py])
                kb.op("dve", lambda e: e.tensor_tensor(out=ytmp[p][:, half * 512:(half + 1) * 512], in0=py[:, :], in1=gate_bc[j][0][:, half * 512:(half + 1) * 512], op=ALU.mult),
                      reads=[py, gate_bc[j][0]], writes=[ytmp[p]])
            kb.op("dve", lambda e: e.tensor_tensor(out=hl[p][:], in0=ytmp[p][:], in1=xt[p][:], op=ALU.add), reads=[ytmp[p], xt[p]], writes=[hl[p]])
            kb.store("sp", hlm, hlm.h[t * 128:(t + 1) * 128, :], hl[p], hl[p][:])
            norm_T(t, hl[p], gs2, SH2, j, nl2T[p], 0, banks[2])
            pl = banks[3]
            for kc in range(8):
                kb.op("pe", lambda e: e.matmul(pl[:, 0:36], lhsT=nl2T[p][:, kc, :], rhs=rw_b[:, kc, :], start=(kc == 0), stop=(kc == 7)), reads=[nl2T[p], rw_b], writes=[pl])
            kb.op("dve", lambda e: e.tensor_tensor(out=lg[p][:], in0=pl[:, 0:36], in1=rb_bc[:], op=ALU.add), reads=[pl, rb_bc], writes=[lg[p]])
            pbk = banks[4]
            for kc in range(8):
                kb.op("pe", lambda e: e.transpose(out=bfv(pbk)[:, kc * 128:(kc + 1) * 128], in_=nl2T[p][:, kc, :], identity=identb[:]), reads=[nl2T[p], identb], writes=[pbk])
            kb.op("act", lambda e: e.copy(out=nl2[p][:], in_=bfv(pbk)[:, 0:1024]), reads=[pbk], writes=[nl2[p]])
            kb.store("sp", nl2d, nl2d.h[t * 128:(t + 1) * 128, :], nl2[p], nl2[p][:])
            r_ = rt[p]
            kb.op("dve", lambda e: e.tensor_reduce(out=r_[:, 0:1], in_=lg[p][:, 0:4], axis=AX.X, op=ALU.max), reads=[lg[p]], writes=[r_])
            kb.op("dve", lambda e: e.tensor_scalar(out=r_[:, 1:5], in0=lg[p][:, 0:4], scalar1=r_[:, 0:1], scalar2=None, op0=ALU.is_equal), reads=[lg[p], r_], writes=[r_])
            kb.op("dve", lambda e: e.tensor_scalar(out=r_[:, 5:6], in0=r_[:, 0:1], scalar1=-1.0, scalar2=None, op0=ALU.mult), reads=[r_], writes=[r_])
            kb.op("act", lambda e: e.activation(out=ejunk[:], in_=lg[p][:, 0:4], func=AF.Exp, bias=r_[:, 5:6], accum_out=r_[:, 6:7]), reads=[lg[p], r_], writes=[ejunk, r_])
            kb.op("dve", lambda e: e.reciprocal(out=r_[:, 7:8], in_=r_[:, 6:7]), reads=[r_], writes=[r_])
            kb.op("dve", lambda e: e.tensor_scalar(out=r_[:, 8:12], in0=r_[:, 1:5], scalar1=-1.0, scalar2=BIG, op0=ALU.add, op1=ALU.mult), reads=[r_], writes=[r_])
            for g in range(4):
                kb.op("dve", lambda e: e.tensor_scalar(out=lem[p][:, g * 8:(g + 1) * 8], in0=lg[p][:, 4 + g * 8:4 + (g + 1) * 8], scalar1=r_[:, 8 + g:9 + g], scalar2=None, op0=ALU.add),
                      reads=[lg[p], r_], writes=[lem[p]])
            oh1 = OH[:, t, 0, :]
            oh2 = OH[:, t, 1, :]
            kb.op("dve", lambda e: e.tensor_reduce(out=r_[:, 12:13], in_=lem[p][:], axis=AX.X, op=ALU.max), reads=[lem[p]], writes=[r_])
            kb.op("dve", lambda e: e.tensor_scalar(out=oh1, in0=lem[p][:], scalar1=r_[:, 12:13], scalar2=None, op0=ALU.is_equal), reads=[lem[p], r_], writes=[OH])
            kb.op("dve", lambda e: e.scalar_tensor_tensor(out=lem2[p][:], in0=oh1, scalar=-BIG, in1=lem[p][:], op0=ALU.mult, op1=ALU.add), reads=[OH, lem[p]], writes=[lem2[p]])
            kb.op("dve", lambda e: e.tensor_reduce(out=r_[:, 13:14], in_=lem2[p][:], axis=AX.X, op=ALU.max), reads=[lem2[p]], writes=[r_])
            kb.op("dve", lambda e: e.tensor_scalar(out=oh2, in0=lem2[p][:], scalar1=r_[:, 13:14], scalar2=None, op0=ALU.is_equal), reads=[lem2[p], r_], writes=[OH])
            kb.op("dve", lambda e: e.tensor_tensor(out=r_[:, 14:15], in0=r_[:, 12:13], in1=r_[:, 13:14], op=ALU.subtract), reads=[r_], writes=[r_])
            kb.op("act", lambda e: e.activation(out=r_[:, 15:16], in_=r_[:, 14:15], func=AF.Sigmoid), reads=[r_], writes=[r_])
            kb.op("dve", lambda e: e.tensor_tensor(out=GT[:, t, 0:1], in0=r_[:, 15:16], in1=r_[:, 7:8], op=ALU.mult), reads=[r_], writes=[GT])
            kb.op("dve", lambda e: e.tensor_tensor(out=GT[:, t, 1:2], in0=r_[:, 7:8], in1=GT[:, t, 0:1], op=ALU.subtract), reads=[r_, GT], writes=[GT])
            if j == 1:
                kb.op("dve", lambda e: e.tensor_scalar(out=OH[:, t, :, :], in0=OH[:, t, :, :], scalar1=validt[:, 0:1], scalar2=None, op0=ALU.mult), reads=[OH, validt], writes=[OH])
            kb.op("dve", lambda e: e.tensor_tensor(out=cb_[p][:], in0=OH[:, t, 0, :], in1=OH[:, t, 1, :], op=ALU.add), reads=[OH], writes=[cb_[p]])
            pc = banks[5]
            kb.op("pe", lambda e: e.matmul(pc[:, 0:32], lhsT=ltri_b[:], rhs=cb_[p][:], start=True, stop=True), reads=[ltri_b, cb_[p]], writes=[pc])
            kb.op("dve", lambda e: e.tensor_tensor(out=rbase[p][:], in0=pc[:, 0:32], in1=Rbc[:], op=ALU.add), reads=[pc, Rbc], writes=[rbase[p]])
            pt_ = banks[6]
            kb.op("pe", lambda e: e.matmul(pt_[:, 0:32], lhsT=ones_b[:], rhs=cb_[p][:], start=True, stop=True), reads=[ones_b, cb_[p]], writes=[pt_])
            kb.op("dve", lambda e: e.tensor_tensor(out=Rbc[:], in0=pt_[:, 0:32], in1=Rbc[:], op=ALU.add), reads=[pt_, Rbc], writes=[Rbc])
            for k in range(2):
                kb.op("dve", lambda e: e.tensor_tensor(out=tmp32[p][:, k, :], in0=OH[:, t, k, :], in1=rbase[p][:], op=ALU.mult), reads=[OH, rbase[p]], writes=[tmp32[p]])
            kb.op("dve", lambda e: e.tensor_reduce(out=RK[:, t, :], in_=tmp32[p][:], axis=AX.X, op=ALU.add), reads=[tmp32[p]], writes=[RK])
        kb.barrier()
    pesA.close()
    pesD = ExitStack()

    cnt_i = kb.sb("cnt_i", [128, 32], I32, pesD)
    pcnt = kb.sb("pcnt", [128, 32], F32, pesD)
    pend = [kb.sb("pend%d" % i, [128, 32], F32, pesD) for i in range(2)]
    kb.op("dve", lambda e: e.tensor_scalar(out=pcnt[:], in0=Rbc[:], scalar1=255.0, scalar2=None, op0=ALU.add), reads=[Rbc], writes=[pcnt])
    kb.op("dve", lambda e: e.tensor_copy(out=cnt_i[:], in_=pcnt[:]), reads=[pcnt], writes=[cnt_i])
    kb.op("dve", lambda e: e.tensor_scalar(out=cnt_i[:], in0=cnt_i[:], scalar1=8, scalar2=8, op0=ALU.arith_shift_right, op1=ALU.logical_shift_left), reads=[cnt_i], writes=[cnt_i])
    kb.op("dve", lambda e: e.tensor_copy(out=pcnt[:], in_=cnt_i[:]), reads=[cnt_i], writes=[pcnt])
    kb.op("dve", lambda e: e.tensor_copy(out=pend[0][:], in_=pcnt[:]), reads=[pcnt], writes=[pend[0]])
    cur = 0
    for sft in (1, 2, 4, 8, 16):
        a, b = pend[cur], pend[1 - cur]
        kb.op("dve", lambda e: e.tensor_copy(out=b[:, 0:sft], in_=a[:, 0:sft]), reads=[a], writes=[b])
        kb.op("dve", lambda e: e.tensor_tensor(out=b[:, sft:32], in0=a[:, sft:32], in1=a[:, 0:32 - sft], op=ALU.add), reads=[a], writes=[b])
        cur = 1 - cur
    pendf = pend[cur]
    poff = kb.sb("poff", [128, 32], F32, pesD)
    kb.op("dve", lambda e: e.tensor_tensor(out=poff[:], in0=pendf[:], in1=pcnt[:], op=ALU.subtract), reads=[pendf, pcnt], writes=[poff])
    DEST = kb.sb("DEST", [128, NT, 2], F32, pesD)
    tmpd = kb.sb("tmpd", [128, NT * 2, 32], F32, pesD)
    for t in range(NT):
        for k in range(2):
            kb.op("dve", lambda e: e.tensor_tensor(out=tmpd[:, t * 2 + k, :], in0=OH[:, t, k, :], in1=poff[:], op=ALU.mult), reads=[OH, poff], writes=[tmpd])
    kb.op("dve", lambda e: e.tensor_reduce(out=DEST[:].rearrange("p t k -> p (t k)"), in_=tmpd[:], axis=AX.X, op=ALU.add), reads=[tmpd], writes=[DEST])
    kb.op("dve", lambda e: e.tensor_tensor(out=DEST[:], in0=DEST[:], in1=RK[:], op=ALU.add), reads=[DEST, RK], writes=[DEST])
    if L0:
        inval = kb.sb("inval", [128, 2], F32, pesD)
        kb.op("dve", lambda e: e.tensor_scalar(out=inval[:, 0:1], in0=validt[:], scalar1=-1.0, scalar2=-1.0, op0=ALU.add, op1=ALU.mult), reads=[validt], writes=[inval])
        kb.op("dve", lambda e: e.scalar_tensor_tensor(out=inval[:, 1:2], in0=iop[:], scalar=float(NROWS), in1=inval[:, 0:1], op0=ALU.add, op1=ALU.mult), reads=[iop, inval], writes=[inval])
        kb.op("dve", lambda e: e.tensor_scalar(out=DEST[:, NT - 1, :], in0=DEST[:, NT - 1, :], scalar1=validt[:, 0:1], scalar2=inval[:, 1:2], op0=ALU.mult, op1=ALU.add),
              reads=[DEST, validt, inval], writes=[DEST])
    kb.op("dve", lambda e: e.tensor_copy(out=DESTI[:], in_=DEST[:].rearrange("p t k -> p (t k)")), reads=[DEST], writes=[DESTI])
    eb = kb.sb("eb", [128, NB], F32, pesD)
    kb.op("pool", lambda e: e.memset(eb[:], 0.0), writes=[eb])
    for ee in range(32):
        kb.op("dve", lambda e: e.scalar_tensor_tensor(out=eb[:], in0=blkst[:], scalar=pendf[:, ee:ee + 1], in1=eb[:], op0=ALU.is_ge, op1=ALU.add), reads=[blkst, pendf, eb], writes=[eb])
    kb.op("dve", lambda e: e.tensor_scalar(out=eb[:], in0=eb[:], scalar1=31.0, scalar2=128.0, op0=ALU.min, op1=ALU.mult), reads=[eb], writes=[eb])
    flag = kb.sb("flag", [128, NB], F32, pesD)
    kb.op("pool", lambda e: e.memset(flag[:], 1.0), writes=[flag])
    kb.op("dve", lambda e: e.tensor_tensor(out=flag[:, 1:NB], in0=eb[:, 1:NB], in1=eb[:, 0:NB - 1], op=ALU.not_equal), reads=[eb], writes=[flag])
    kb.op("dve", lambda e: e.tensor_scalar(out=flag[:], in0=flag[:], scalar1=-1.0, scalar2=-1.0e9, op0=ALU.add, op1=ALU.mult), reads=[flag], writes=[flag])
    kb.op("dve", lambda e: e.tensor_scalar(out=eb[:], in0=eb[:], scalar1=iop[:, 0:1], scalar2=None, op0=ALU.add), reads=[eb, iop], writes=[eb])
    kb.op("dve", lambda e: e.tensor_tensor(out=eb[:], in0=eb[:], in1=flag[:], op=ALU.add), reads=[eb, flag], writes=[eb])
    kb.op("dve", lambda e: e.tensor_copy(out=WIDX[:], in_=eb[:]), reads=[eb], writes=[WIDX])

    srow = dbl("srow", [128, 1024], BF16, 3, pesD)
    for t in range(NT):
        sr = srow[t % 3]
        kb.load("sp", sr, sr[:], nl2d.h[t * 128:(t + 1) * 128, :], nl2d)
        for k in range(2):
            kb.dma("pool", lambda e: e.indirect_dma_start(out=xs.h[:, :], out_offset=bass.IndirectOffsetOnAxis(ap=DESTI[:, t * 2 + k:t * 2 + k + 1], axis=0),
                                                          in_=sr[:], in_offset=None), reads=[DESTI, sr], writes=[xs])
    kb.barrier()
    pesD.close()
    pesE = ExitStack()

    w1f = dbl("w1f", [128, 4096], F32, 1, pesE) * 2
    w3f = dbl("w3f", [128, 4096], F32, 1, pesE) * 2
    w2f = dbl("w2f", [128, 4096], F32, 1, pesE) * 2
    w1b = dbl("w1b", [128, 8, 512], BF16, 2, pesE)
    w3b = dbl("w3b", [128, 8, 512], BF16, 2, pesE)
    w2b = dbl("w2b", [128, 4, 1024], BF16, 2, pesE)
    xr = dbl("xr", [128, 2, 1024], BF16, 2, pesE)
    xsT = dbl("xsT", [128, 8, 256], BF16, 2, pesE)
    sl = dbl("sl", [128, 256], F32, 2, pesE)
    hhT = dbl("hhT", [128, 4, 256], BF16, 2, pesE)
    yo = dbl("yo", [128, 1024], F32, 2, pesE)
    wreg = kb.nc.gpsimd.to_reg(4095)
    for b in range(NB):
        p = b % 2
        for (tab, wf) in ((w1t, w1f[p]), (w3t, w3f[p]), (w2t, w2f[p])):
            kb.dma("pool", lambda e: e.indirect_dma_start(out=wf[:], out_offset=None, in_=tab.h[:, :], in_offset=bass.IndirectOffsetOnAxis(ap=WIDX[:, b:b + 1], axis=0),
                                                          bounds_check=wreg, oob_is_err=False), reads=[WIDX, tab], writes=[wf])
        kb.op("act", lambda e: e.copy(out=w1b[p][:].rearrange("p a b -> p (a b)"), in_=w1f[p][:]), reads=[w1f[p]], writes=[w1b[p]])
        kb.op("dve", lambda e: e.tensor_copy(out=w3b[p][:].rearrange("p a b -> p (a b)"), in_=w3f[p][:]), reads=[w3f[p]], writes=[w3b[p]])
        kb.op("act", lambda e: e.copy(out=w2b[p][:].rearrange("p a b -> p (a b)")[:, 0:2048], in_=w2f[p][:, 0:2048]), reads=[w2f[p]], writes=[w2b[p]])
        kb.op("dve", lambda e: e.tensor_copy(out=w2b[p][:].rearrange("p a b -> p (a b)")[:, 2048:4096], in_=w2f[p][:, 2048:4096]), reads=[w2f[p]], writes=[w2b[p]])
        kb.load("sp", xr[p], xr[p][:], xs.h[b * 256:(b + 1) * 256, :].rearrange("(a p) n -> p a n", p=128), xs)
        for sub in range(2):
            pT = banks[6]
            for kc in range(8):
                kb.op("pe", lambda e: e.transpose(out=bfv(pT)[:, kc * 128:(kc + 1) * 128], in_=xr[p][:, sub, kc * 128:(kc + 1) * 128], identity=identb[:]),
                      reads=[xr[p], identb], writes=[pT])
            kb.op("act", lambda e: e.copy(out=xsT[p][:, :, sub * 128:(sub + 1) * 128], in_=bfv(pT)[:, 0:1024].rearrange("p (a b) -> p a b", a=8)), reads=[pT], writes=[xsT[p]])
        for fc in range(4):
            ph1 = banks[0 + fc // 2]
            ph3 = banks[2 + fc // 2]
            c0 = (fc % 2) * 256
            for kc in range(8):
                kb.op("pe", lambda e: e.matmul(ph1[:, c0:c0 + 256], lhsT=w1b[p][:, kc, fc * 128:(fc + 1) * 128], rhs=xsT[p][:, kc, :], start=(kc == 0), stop=(kc == 7)),
                      reads=[w1b[p], xsT[p]], writes=[ph1])
            for kc in range(8):
                kb.op("pe", lambda e: e.matmul(ph3[:, c0:c0 + 256], lhsT=w3b[p][:, kc, fc * 128:(fc + 1) * 128], rhs=xsT[p][:, kc, :], start=(kc == 0), stop=(kc == 7)),
                      reads=[w3b[p], xsT[p]], writes=[ph3])
            s_ = sl[fc % 2]
            kb.op("act", lambda e: e.activation(out=s_[:], in_=ph1[:, c0:c0 + 256], func=AF.Silu), reads=[ph1], writes=[s_])
            kb.op("dve", lambda e: e.tensor_tensor(out=hhT[p][:, fc, :], in0=ph3[:, c0:c0 + 256], in1=s_[:], op=ALU.mult), reads=[ph3, s_], writes=[hhT[p]])
        for sub in range(2):
            y_ = yo[sub]
            for half in range(2):
                py = banks[4 + half]
                for fc in range(4):
                    kb.op("pe", lambda e: e.matmul(py[:, :], lhsT=hhT[p][:, fc, sub * 128:(sub + 1) * 128], rhs=w2b[p][:, fc, half * 512:(half + 1) * 512], start=(fc == 0), stop=(fc == 3)),
                          reads=[hhT[p], w2b[p]], writes=[py])
                if half == 0:
                    kb.op("act", lambda e: e.copy(out=y_[:, 0:512], in_=py[:, :]), reads=[py], writes=[y_])
                else:
                    kb.op("dve", lambda e: e.tensor_copy(out=y_[:, 512:1024], in_=py[:, :]), reads=[py], writes=[y_])
            kb.store("sp", ys, ys.h[b * 256 + sub * 128:b * 256 + (sub + 1) * 128, :], y_, y_[:])
    kb.barrier()
    pesE.close()
    pesF = ExitStack()

    y1 = dbl("y1", [128, 1024], F32, 2, pesF)
    y2 = dbl("y2", [128, 1024], F32, 2, pesF)
    hm = dbl("hm", [128, 1024], F32, 2, pesF)
    for t in range(NT):
        p = t % 2
        j = 1 if (L0 and t == NT - 1) else 0
        if j == 1:
            kb.op("pool", lambda e: e.memset(y1[p][:], 0.0), writes=[y1[p]])
            kb.op("pool", lambda e: e.memset(y2[p][:], 0.0), writes=[y2[p]])
        for k, yk in ((0, y1[p]), (1, y2[p])):
            kb.dma("pool", lambda e: e.indirect_dma_start(out=yk[:], out_offset=None, in_=ys.h[:, :], in_offset=bass.IndirectOffsetOnAxis(ap=DESTI[:, t * 2 + k:t * 2 + k + 1], axis=0)), reads=[DESTI, ys], writes=[yk])
        kb.load("sp", hm[p], hm[p][:], hlm.h[t * 128:(t + 1) * 128, :], hlm)
        kb.op("dve", lambda e: e.tensor_scalar(out=y1[p][:], in0=y1[p][:], scalar1=GT[:, t, 0:1], scalar2=None, op0=ALU.mult), reads=[y1[p], GT], writes=[y1[p]])
        kb.op("dve", lambda e: e.scalar_tensor_tensor(out=y1[p][:], in0=y2[p][:], scalar=GT[:, t, 1:2], in1=y1[p][:], op0=ALU.mult, op1=ALU.add), reads=[y2[p], GT, y1[p]], writes=[y1[p]])
        kb.op("dve", lambda e: e.tensor_tensor(out=y1[p][:], in0=y1[p][:], in1=gate_bc[j][1][:], op=ALU.mult), reads=[y1[p], gate_bc[j][1]], writes=[y1[p]])
        kb.op("dve", lambda e: e.tensor_tensor(out=hm[p][:], in0=hm[p][:], in1=y1[p][:], op=ALU.add), reads=[hm[p], y1[p]], writes=[hm[p]])
        kb.store("sp", hout, hout.h[t * 128:(t + 1) * 128, :], hm[p], hm[p][:])
    print("lb%d instructions:" % layer, kb.n_ins, "sems:", len(kb.sems))
    pesF.close()
    kb.end_stage()


def fop(v, n):
    return np.ascontiguousarray(np.asarray(v, np.float32).reshape(n, 128).T)


def moe_tables(inp, l):
    w1 = np.ascontiguousarray(inp["moe_w1"][l].reshape(32, 8, 128, 512).transpose(0, 2, 1, 3).reshape(4096, 4096))
    w3 = np.ascontiguousarray(inp["moe_w3"][l].reshape(32, 8, 128, 512).transpose(0, 2, 1, 3).reshape(4096, 4096))
    w2 = np.ascontiguousarray(inp["moe_w2"][l].reshape(32, 4, 128, 1024).transpose(0, 2, 1, 3).reshape(4096, 4096))
    return w1, w3, w2


def host_b(layer, inp):
    L0 = layer == 0
    l = layer
    w1, w3, w2 = moe_tables(inp, l)
    rw = np.ascontiguousarray(np.concatenate([inp["rg_w"][l], inp["re_w"][l]], axis=1).astype(np.float32))
    rb = np.concatenate([inp["rg_b"][l], inp["re_b"][l]]).astype(np.float32)
    adabr = np.concatenate([inp["ada_b"][l][2048:3072], inp["ada_b"][l][5120:6144]]).astype(np.float32)
    gfop = np.ascontiguousarray(np.concatenate([fop(inp["norm1_g"][l], 8), fop(inp["norm2_g"][l], 8)], axis=1))
    wout = np.ascontiguousarray(inp["ab_w_out"][0] if L0 else inp["gla_w_out"][0])
    hin_lat, hin_ctx = inp["x"], inp["ctx"]
    maps = []
    pp = np.arange(128, dtype=np.int32)
    for b in range(2):
        sv = np.stack([inp["c"][b], inp["c_ctx"]], -1).reshape(8, 128, 2).transpose(1, 0, 2).reshape(128, 16).astype(np.float32)
        for jq in range(4):
            r0, r1 = jq * 4096, (jq + 1) * 4096
            m = {"svec": np.ascontiguousarray(sv), "adaw": np.ascontiguousarray(inp["ada_w"][l]), "adabf": fop(inp["ada_b"][l], 48), "adabr": adabr, "gfop": gfop,
                 "wout": wout, "rw": rw, "rb": rb, "w1t": w1, "w3t": w3, "w2t": w2}
            if L0:
                cpad = np.zeros((128, 1024), np.float32)
                cpad[:64] = hin_ctx[b, 64 * jq:64 * jq + 64]
                m["hin"] = np.ascontiguousarray(np.concatenate([hin_lat[b, r0:r1], cpad], 0))
                m["mixidx"] = np.ascontiguousarray(np.stack([np.array([ag_row(jq * 128 + int(p_), h, 64, 512) for p_ in pp]) for h in range(4)], axis=1).astype(np.int32))
                xh = np.zeros((128, 1024), np.float32)
                if jq > 0:
                    xh[0:15] = hin_lat[b, r0 - 15:r0]
                if jq < 3:
                    xh[15:30] = hin_lat[b, r1:r1 + 15]
                m["xhalo"] = xh
                ch = np.zeros((128, 1024), np.float32)
                for r in range(94):
                    pos = 64 * jq - 15 + r
                    if 0 <= pos < 256:
                        ch[r] = hin_ctx[b, pos]
                m["cxh"] = ch
                m["edge"] = np.array([1.0 if jq > 0 else 0.0, 1.0 if jq < 3 else 0.0], np.float32)
                v = np.zeros((128, 1), np.float32)
                v[:64] = 1
                m["valid"] = v
                m["win"] = np.ascontiguousarray(inp["ab_w_in"][0][:, 0:1024])
                m["cw"] = np.ascontiguousarray(inp["conv_w"][0].T.reshape(4, 128, 31).transpose(1, 0, 2).reshape(128, 124))
                m["cvec"] = np.ascontiguousarray(np.concatenate([fop(inp["conv_b"][0], 4), fop(inp["conv_ln_g"][0], 4), fop(inp["conv_ln_b"][0], 4)], axis=1))
            else:
                m["mixidx"] = np.ascontiguousarray(np.stack([np.array([ag_row(jq * 256 + c2 * 128 + int(p_), h, 128, 1024) for p_ in pp]) for h in range(4) for c2 in range(2)], axis=1).astype(np.int32))
                m["valid"] = np.ones((128, 1), np.float32)
            maps.append(m)
    return maps


def gather_b(layer, results):
    L0 = layer == 0
    hl = np.zeros((2, 16384, 1024), np.float32)
    hc = np.zeros((2, 256, 1024), np.float32) if L0 else None
    for b in range(2):
        for jq in range(4):
            o = results[b * 4 + jq]["hout"]
            hl[b, jq * 4096:(jq + 1) * 4096] = o[:4096]
            if L0:
                hc[b, 64 * jq:64 * jq + 64] = o[4096:4160]
    return hl, hc


def build_l1a(kb, banks, x2out, x3in, n_lat_tiles=128, do_scan=True):
    kb.begin_stage("a1_")
    svec = kb.dram("svec", [128, 16], F32, "ExternalInput")
    adaw = kb.dram("adaw", [1024, 2048], F32, "ExternalInput")
    adab = kb.dram("adab", [128, 16], F32, "ExternalInput")
    g1 = kb.dram("g1", [128, 8], F32, "ExternalInput")
    w = kb.dram("w", [1024, 768], F32, "ExternalInput")
    waT = kb.dram("waT", [2, 16, 1024], F32, "ExternalInput")
    wa2 = kb.dram("wa2", [2, 16, 128], F32, "ExternalInput")
    small = kb.dram("small", [512], F32, "ExternalInput")
    proj = kb.dram("proj", [S + LC, 1024], F32)
    of_d = kb.dram("of_d", [S, 256], F32)

    def bfv(t):
        return t[:].bitcast(BF16)

    def dbl(name, shape, dt, n=2, es=None):
        return [kb.sb("%s%d" % (name, i), shape, dt, es) for i in range(n)]

    identb = kb.identity("identb", BF16)
    smallb = kb.sb("smallb", [128, 512], F32)
    kb.load("sp", smallb, smallb[:], small.h.partition_broadcast(128), small)
    zer = kb.sb("zer", [128, 128], F32)
    kb.op("pool", lambda e: e.memset(zer[:], 0.0), writes=[zer])

    def rstd_chain(stt, c_in, c_tmp, c_out, n, inv_n):
        kb.op("dve", lambda e: e.tensor_scalar(out=stt[:, c_tmp:c_tmp + n], in0=stt[:, c_in:c_in + n], scalar1=inv_n, scalar2=EPS, op0=ALU.mult, op1=ALU.add),
              reads=[stt], writes=[stt])
        kb.op("act", lambda e: e.activation(out=stt[:, c_tmp:c_tmp + n], in_=stt[:, c_tmp:c_tmp + n], func=AF.Sqrt), reads=[stt], writes=[stt])
        kb.op("dve", lambda e: e.reciprocal(out=stt[:, c_out:c_out + n], in_=stt[:, c_tmp:c_tmp + n]), reads=[stt], writes=[stt])

    wq = [kb.sb("wq%d" % j, [128, 8, 1024], BF16) for j in range(2)]
    bias = [kb.sb("bias%d" % j, [128, 1024], F32) for j in range(2)]
    pesA = ExitStack()
    s_sb = kb.sb("s_sb", [128, 16], F32, pesA)
    kb.load("sp", s_sb, s_sb[:], svec.h, svec)
    kb.op("act", lambda e: e.activation(out=s_sb[:], in_=s_sb[:], func=AF.Silu), reads=[s_sb], writes=[s_sb])
    adab_sb = kb.sb("adab_sb", [128, 16], F32, pesA)
    kb.load("sp", adab_sb, adab_sb[:], adab.h, adab)
    g1_sb = kb.sb("g1_sb", [128, 8], F32, pesA)
    kb.load("sp", g1_sb, g1_sb[:], g1.h, g1)
    mod = kb.sb("mod", [128, 16, 2], F32, pesA)
    gs = kb.sb("gs", [128, 8, 2], F32, pesA)
    w_sb = kb.sb("w_sb", [128, 8, 1024], F32, pesA)
    shiftbc = kb.sb("shiftbc", [128, 8, 128], F32, pesA)
    pm = banks[0]
    with ExitStack() as pes:
        adaw_sb = kb.sb("adaw_sb", [128, 8, 512], F32, pes)
        for v in range(4):
            kb.load("sp", adaw_sb, adaw_sb[:], adaw.h[:, v * 512:(v + 1) * 512].rearrange("(kc p) n -> p kc n", p=128), adaw)
            for oc in range(4):
                g = v * 4 + oc
                for kc in range(8):
                    kb.op("pe", lambda e: e.matmul(pm[:, g * 2:g * 2 + 2], lhsT=adaw_sb[:, kc, oc * 128:(oc + 1) * 128], rhs=s_sb[:, kc * 2:kc * 2 + 2],
                                                  start=(kc == 0), stop=(kc == 7)), reads=[adaw_sb, s_sb], writes=[pm])
        pm3 = pm[:, 0:32].rearrange("p (g j) -> p g j", j=2)
        for j in range(2):
            kb.op("dve", lambda e: e.tensor_tensor(out=mod[:, :, j], in0=pm3[:, :, j], in1=adab_sb[:], op=ALU.add), reads=[pm, adab_sb], writes=[mod])
            kb.op("dve", lambda e: e.scalar_tensor_tensor(out=gs[:, :, j], in0=mod[:, 8:16, j], scalar=1.0, in1=g1_sb[:], op0=ALU.add, op1=ALU.mult),
                  reads=[mod, g1_sb], writes=[gs])
        kb.load("sp", w_sb, w_sb[:, :, 0:768], w.h.rearrange("(kc p) n -> p kc n", p=128), w)
        waT_sb = [kb.sb("waT_sb%d" % d, [32, 1024], F32, pes) for d in range(2)]
        wa2_sb = [kb.sb("wa2_sb%d" % d, [32, 128], F32, pes) for d in range(2)]
        for d in range(2):
            kb.op("pool", lambda e: e.memset(waT_sb[d][:], 0.0), writes=[waT_sb[d]])
            kb.op("pool", lambda e: e.memset(wa2_sb[d][:], 0.0), writes=[wa2_sb[d]])
            kb.load("sp", waT_sb[d], waT_sb[d][0:16, :], waT.h[d], waT)
            kb.load("sp", wa2_sb[d], wa2_sb[d][0:16, :], wa2.h[d], wa2)
            for kc in range(8):
                pz = banks[1]
                kb.op("pe", lambda e: e.matmul(pz[:, 0:128], lhsT=waT_sb[d][:, kc * 128:(kc + 1) * 128], rhs=wa2_sb[d][:], start=True, stop=True),
                      reads=[waT_sb[d], wa2_sb[d]], writes=[pz])
                kb.op("dve", lambda e: e.tensor_copy(out=w_sb[:, kc, 768 + d * 128:768 + (d + 1) * 128], in_=pz[:, 0:128]), reads=[pz], writes=[w_sb])
        for j in range(2):
            for kc in range(8):
                kb.op("dve", lambda e: e.tensor_scalar(out=wq[j][:, kc, :], in0=w_sb[:, kc, :], scalar1=gs[:, kc, j:j + 1], scalar2=None, op0=ALU.mult),
                      reads=[w_sb, gs], writes=[wq[j]])
                kb.op("dve", lambda e: e.tensor_scalar(out=shiftbc[:, kc, :], in0=zer[:], scalar1=mod[:, kc, j:j + 1], scalar2=None, op0=ALU.add),
                      reads=[zer, mod], writes=[shiftbc])
            for half in range(2):
                pb = banks[2 + half]
                for kc in range(8):
                    kb.op("pe", lambda e: e.matmul(pb[:, :], lhsT=shiftbc[:, kc, :], rhs=w_sb[:, kc, half * 512:(half + 1) * 512], start=(kc == 0), stop=(kc == 7)),
                          reads=[shiftbc, w_sb], writes=[pb])
                kb.op("dve", lambda e: e.tensor_copy(out=bias[j][:, half * 512:(half + 1) * 512], in_=pb[:, :]), reads=[pb], writes=[bias[j]])
            kb.op("dve", lambda e: e.tensor_tensor(out=bias[j][:, 768:1024], in0=bias[j][:, 768:1024], in1=smallb[:, 0:256], op=ALU.add), reads=[bias[j], smallb], writes=[bias[j]])
        kb.barrier()
    kb.barrier()
    pesA.close()

    pesB = ExitStack()
    xt = dbl("xt", [128, 1024], F32, 4, pesB)
    junk = kb.sb("junk", [128, 1024], BF16, pesB)
    st1 = dbl("st1", [128, 4], F32, 2, pesB)
    xn = dbl("xn", [128, 1024], BF16, 2, pesB)
    xnT = dbl("xnT", [128, 1024], BF16, 2, pesB)
    pj = dbl("pj", [128, 1024], F32, 2, pesB)
    ez = dbl("ez", [128, 256], F32, 2, pesB)
    one_col = kb.sb("one_col", [128, 1], F32, pesB)
    kb.op("pool", lambda e: e.memset(one_col[:], 1.0), writes=[one_col])

    tile_args = []

    def do_load(i):
        _, _, row0, is_ctx, _ = tile_args[i]
        xb = xt[i % 4]
        if is_ctx:
            c = row0 // 128
            for hf in range(2):
                r = ag_row(4096, 2 * c + hf, 256, 4224)
                kb.load("sp", xb, xb[hf * 64:(hf + 1) * 64, :], x2out.h[r:r + 64, :], x2out)
        else:
            t = row0 // 128
            r = ag_row((t % 32) * 128, t // 32, 256, 4224)
            kb.load("sp", xb, xb[:], x2out.h[r:r + 128, :], x2out)

    def proj_tile(i, src, row0, is_ctx, drow):
        p = i % 2
        j = 1 if is_ctx else 0
        if i + 2 < len(tile_args):
            do_load(i + 2)
        yield
        kb.op("act", lambda e: e.activation(out=junk[:], in_=xt[i % 4][:], func=AF.Square, accum_out=st1[p][:, 0:1]), reads=[xt[i % 4]], writes=[junk, st1[p]])
        yield
        rstd_chain(st1[p], 0, 1, 2, 1, 1.0 / 1024)
        kb.op("act", lambda e: e.activation(out=xn[p][:], in_=xt[i % 4][:], func=AF.Copy, scale=st1[p][:, 2:3]), reads=[xt[i % 4], st1[p]], writes=[xn[p]])
        yield
        psT = banks[p]
        for kc in range(8):
            kb.op("pe", lambda e: e.transpose(out=bfv(psT)[:, kc * 128:(kc + 1) * 128], in_=xn[p][:, kc * 128:(kc + 1) * 128], identity=identb[:]),
                  reads=[xn[p], identb], writes=[psT])
            yield
        kb.op("dve", lambda e: e.tensor_copy(out=xnT[p][:], in_=bfv(psT)[:, 0:1024]), reads=[psT], writes=[xnT[p]])
        yield
        for half in range(2):
            pp = banks[2 + 2 * p + half]
            for kc in range(8):
                kb.op("pe", lambda e: e.matmul(pp[:, :], lhsT=xnT[p][:, kc * 128:(kc + 1) * 128], rhs=wq[j][:, kc, half * 512:(half + 1) * 512], start=(kc == 0), stop=(kc == 7)),
                      reads=[xnT[p], wq[j]], writes=[pp])
                yield
            kb.op("dve", lambda e: e.tensor_tensor(out=pj[p][:, half * 512:(half + 1) * 512], in0=pp[:, :], in1=bias[j][:, half * 512:(half + 1) * 512], op=ALU.add),
                  reads=[pp, bias[j]], writes=[pj[p]])
            yield
        kb.op("act", lambda e: e.activation(out=ez[p][:], in_=pj[p][:, 768:1024], func=AF.Exp, scale=-1.0), reads=[pj[p]], writes=[ez[p]])
        yield
        kb.op("act", lambda e: e.activation(out=pj[p][:, 768:1024], in_=ez[p][:], func=AF.Ln, bias=one_col[:, 0:1]), reads=[ez[p], one_col], writes=[pj[p]])
        yield
        kb.store("sp", proj, proj.h[drow:drow + 128, :], pj[p], pj[p][:])
        yield

    i = 0
    for c in range(2):
        tile_args.append((i, None, c * 128, True, S + c * 128))
        i += 1
    for t in range(n_lat_tiles):
        tile_args.append((i, None, t * 128, False, t * 128))
        i += 1
    do_load(0)
    do_load(1)
    gens = [proj_tile(*a_) for a_ in tile_args]
    interleave(gens, 2)
    kb.barrier()
    pesB.close()

    mask = []
    for d in range(2):
        mf = kb.sb("mask%d" % d, [128, 128], F32)
        kb.op("pool", lambda e: e.memset(mf[:], 1.0), writes=[mf])
        if d == 0:
            kb.op("pool", lambda e: e.affine_select(out=mf[:], in_=mf[:], pattern=[[1, 128]], compare_op=ALU.is_ge, fill=0.0, base=0, channel_multiplier=-1), reads=[mf], writes=[mf])
        else:
            kb.op("pool", lambda e: e.affine_select(out=mf[:], in_=mf[:], pattern=[[-1, 128]], compare_op=ALU.is_ge, fill=0.0, base=0, channel_multiplier=1), reads=[mf], writes=[mf])
        mask.append(mf)
    LS = -1.0 / 16
    maskS = []
    for d in range(2):
        ms_ = kb.sb("maskS%d" % d, [128, 128], F32)
        kb.op("dve", lambda e: e.tensor_scalar(out=ms_[:], in0=mask[d][:], scalar1=LS, scalar2=None, op0=ALU.mult), reads=[mask[d]], writes=[ms_])
        maskS.append(ms_)
    ones_f = kb.sb("ones_f", [128, 128], F32)
    kb.op("pool", lambda e: e.memset(ones_f[:], -1.0 / 16), writes=[ones_f])
    Sst = kb.sb("Sst", [128, 256], F32)
    Sb = dbl("Sb", [128, 256], BF16)
    pt = dbl("pt", [128, 1024], F32, 3)
    bc = dbl("bc", [128, 128], F32)
    eb = dbl("eb", [128, 128], F32)
    enb = dbl("enb", [128, 128], F32)
    dlt = dbl("dlt", [128, 128], F32)
    dec = dbl("dec", [128, 1], F32)
    qt = dbl("qt", [128, 128], BF16)
    ktl = dbl("ktl", [128, 128], BF16)
    kh = dbl("kh", [128, 128], BF16)
    vb = dbl("vb", [128, 256], BF16)
    qkT = dbl("qkT", [128, 256], BF16)
    attm = dbl("attm", [128, 128], BF16)
    ofs = dbl("ofs", [128, 256], F32)
    osum = dbl("osum", [128, 256], F32)
    fst = dbl("fst", [128, 4], F32)
    sg = dbl("sg", [128, 256], F32)
    ogb = dbl("ogb", [128, 256], BF16)
    ogT_sb = dbl("ogT_sb", [128, 2, 128], BF16)
    QS = 128.0 ** -0.5
    step = [0]

    def gla_prep(c, row, d):
        p = c % 2
        B = banks[4 * p:4 * p + 4]
        t_ = pt[c % 3]
        kb.load("sp", t_, t_[:], proj.h[row:row + 128, :], proj)
        yield
        la = t_[:, 768 + d * 128:768 + (d + 1) * 128]
        kb.op("pe", lambda e: e.matmul(B[0][:, 0:128], lhsT=maskS[d][:], rhs=la, start=True, stop=True), reads=[maskS[d], t_], writes=[B[0]])
        yield
        kb.op("pe", lambda e: e.matmul(B[0][:, 128:256], lhsT=ones_f[:], rhs=la, start=True, stop=True), reads=[ones_f, t_], writes=[B[0]])
        yield
        kb.op("pe", lambda e: e.matmul(B[0][:, 256:384], lhsT=la, rhs=ones_f[:], start=True, stop=True), reads=[ones_f, t_], writes=[B[0]])
        yield
        kb.op("act", lambda e: e.copy(out=bc[p][:], in_=B[0][:, 0:128]), reads=[B[0]], writes=[bc[p]])
        yield
        kb.op("act", lambda e: e.activation(out=eb[p][:], in_=B[0][:, 0:128], func=AF.Exp), reads=[B[0]], writes=[eb[p]])
        yield
        kb.op("act", lambda e: e.activation(out=enb[p][:], in_=B[0][:, 0:128], func=AF.Exp, scale=-1.0), reads=[B[0]], writes=[enb[p]])
        yield
        kb.op("dve", lambda e: e.tensor_tensor(out=dlt[p][:], in0=B[0][:, 128:256], in1=bc[p][:], op=ALU.subtract), reads=[B[0], bc[p]], writes=[dlt[p]])
        yield
        kb.op("act", lambda e: e.activation(out=dlt[p][:], in_=dlt[p][:], func=AF.Exp), reads=[dlt[p]], writes=[dlt[p]])
        yield
        kb.op("act", lambda e: e.activation(out=dec[p][:], in_=B[0][:, 256:257], func=AF.Exp), reads=[B[0]], writes=[dec[p]])
        yield
        kb.op("dve", lambda e: e.scalar_tensor_tensor(out=qt[p][:], in0=t_[:, 0:128], scalar=QS, in1=eb[p][:], op0=ALU.mult, op1=ALU.mult), reads=[t_, eb[p]], writes=[qt[p]])
        yield
        kb.op("dve", lambda e: e.tensor_tensor(out=ktl[p][:], in0=t_[:, 128:256], in1=enb[p][:], op=ALU.mult), reads=[t_, enb[p]], writes=[ktl[p]])
        yield
        kb.op("pool", lambda e: e.tensor_tensor(out=kh[p][:], in0=t_[:, 128:256], in1=dlt[p][:], op=ALU.mult), reads=[t_, dlt[p]], writes=[kh[p]])
        yield
        kb.op("pool", lambda e: e.tensor_copy(out=vb[p][:], in_=t_[:, 256:512]), reads=[t_], writes=[vb[p]])
        yield
        kb.op("pe", lambda e: e.transpose(out=bfv(B[1])[:, 0:128], in_=qt[p][:], identity=identb[:]), reads=[qt[p], identb], writes=[B[1]])
        yield
        kb.op("pe", lambda e: e.transpose(out=bfv(B[1])[:, 128:256], in_=ktl[p][:], identity=identb[:]), reads=[ktl[p], identb], writes=[B[1]])
        yield
        kb.op("act", lambda e: e.copy(out=qkT[p][:], in_=bfv(B[1])[:, 0:256]), reads=[B[1]], writes=[qkT[p]])
        yield
        kb.op("pe", lambda e: e.matmul(B[2][:, 0:128], lhsT=qkT[p][:, 128:256], rhs=qkT[p][:, 0:128], start=True, stop=True), reads=[qkT[p]], writes=[B[2]])
        yield
        kb.op("dve", lambda e: e.tensor_tensor(out=attm[p][:], in0=B[2][:, 0:128], in1=mask[d][:], op=ALU.mult), reads=[B[2], mask[d]], writes=[attm[p]])
        yield

    def gla_fin(c, d, out_mode, out_row):
        p = c % 2
        B = banks[4 * p:4 * p + 4]
        t_ = pt[c % 3]
        sb_cur = Sb[c % 2]
        sb_next = Sb[(c + 1) % 2]
        if out_mode is not None:
            kb.op("pe", lambda e: e.matmul(B[3][:, 0:256], lhsT=qkT[p][:, 0:128], rhs=sb_cur[:], start=True, stop=False), reads=[qkT[p], sb_cur], writes=[B[3]])
            yield
            kb.op("pe", lambda e: e.matmul(B[3][:, 0:256], lhsT=attm[p][:], rhs=vb[p][:], start=False, stop=True), reads=[attm[p], vb[p]], writes=[B[3]])
            yield
        kb.op("pe", lambda e: e.matmul(B[2][:, 128:384], lhsT=kh[p][:], rhs=vb[p][:], start=True, stop=True), reads=[kh[p], vb[p]], writes=[B[2]])
        yield
        kb.op("dve", lambda e: e.scalar_tensor_tensor(out=Sst[:], in0=Sst[:], scalar=dec[p][:, 0:1], in1=B[2][:, 128:384], op0=ALU.mult, op1=ALU.add),
              reads=[Sst, dec[p], B[2]], writes=[Sst])
        yield
        kb.op("act", lambda e: e.copy(out=sb_next[:], in_=Sst[:]), reads=[Sst], writes=[sb_next])
        yield
        if out_mode == "store":
            kb.op("act", lambda e: e.copy(out=ofs[p][:], in_=B[3][:, 0:256]), reads=[B[3]], writes=[ofs[p]])
            yield
            kb.store("sp", of_d, of_d.h[out_row:out_row + 128, :], ofs[p], ofs[p][:])
            yield
        elif out_mode == "final":
            kb.load("sp", ofs[p], ofs[p][:], of_d.h[out_row:out_row + 128, :], of_d)
            yield
            kb.op("dve", lambda e: e.tensor_tensor(out=osum[p][:], in0=B[3][:, 0:256], in1=ofs[p][:], op=ALU.add), reads=[B[3], ofs[p]], writes=[osum[p]])
            yield
            kb.op("act", lambda e: e.activation(out=sg[p][:], in_=osum[p][:], func=AF.Square, accum_out=fst[p][:, 0:1]), reads=[osum[p]], writes=[sg[p], fst[p]])
            yield
            rstd_chain(fst[p], 0, 1, 2, 1, 1.0 / 256)
            kb.op("dve", lambda e: e.scalar_tensor_tensor(out=osum[p][:], in0=osum[p][:], scalar=fst[p][:, 2:3], in1=smallb[:, 256:512], op0=ALU.mult, op1=ALU.mult),
                  reads=[osum[p], fst[p], smallb], writes=[osum[p]])
            yield
            kb.op("act", lambda e: e.activation(out=sg[p][:], in_=t_[:, 512:768], func=AF.Silu), reads=[t_], writes=[sg[p]])
            yield
            kb.op("dve", lambda e: e.tensor_tensor(out=ogb[p][:], in0=osum[p][:], in1=sg[p][:], op=ALU.mult), reads=[osum[p], sg[p]], writes=[ogb[p]])
            yield
            for hh in range(2):
                kb.op("pe", lambda e: e.transpose(out=bfv(B[1])[:, 256 + hh * 128:256 + (hh + 1) * 128], in_=ogb[p][:, hh * 128:(hh + 1) * 128], identity=identb[:]),
                      reads=[ogb[p], identb], writes=[B[1]])
                yield
            kb.op("act", lambda e: e.copy(out=ogT_sb[p][:].rearrange("p a b -> p (a b)"), in_=bfv(B[1])[:, 256:512]), reads=[B[1]], writes=[ogT_sb[p]])
            yield
            tq_, tc_ = (out_row // 128) // 32, ((out_row // 128) % 32) * 128
            kb.store("sp", x3in, x3in.h[tq_ * 256:(tq_ + 1) * 256, tc_:tc_ + 128].rearrange("(a p) n -> p a n", p=128), ogT_sb[p], ogT_sb[p][:])
            yield

    def reset_state(c):
        kb.op("pool", lambda e: e.memset(Sst[:], 0.0), writes=[Sst])
        kb.op("pool", lambda e: e.memset(Sb[c % 2][:], 0.0), writes=[Sb[c % 2]])

    def run_scan(chunks, c0):
        n = len(chunks)
        for _ in gla_prep(c0, chunks[0][0], chunks[0][1]):
            pass
        for k in range(n):
            row, d, om, orow = chunks[k]
            gens = [gla_fin(c0 + k, d, om, orow)]
            if k + 1 < n:
                gens.append(gla_prep(c0 + k + 1, chunks[k + 1][0], chunks[k + 1][1]))
            interleave(gens, 2)
        return c0 + n

    fwd = [(S + c * 128, 0, None, None) for c in range(2)] + [(t * 128, 0, "store", t * 128) for t in range(n_lat_tiles)]
    bwd = [(S + c * 128, 1, None, None) for c in (1, 0)] + [(t * 128, 1, "final", t * 128) for t in range(n_lat_tiles - 1, -1, -1)]
    reset_state(0)
    cn = run_scan(fwd, 0)
    kb.barrier()
    reset_state(cn)
    run_scan(bwd, cn)
    print("l1a instructions:", kb.n_ins, "sems:", len(kb.sems))
    kb.end_stage()


def fop(v, n):
    return np.ascontiguousarray(np.asarray(v, np.float32).reshape(n, 128).T)


def host_l1a(inp):
    maps = []
    wi = inp["gla_w_in"][0]
    for b in range(2):
        sv = np.stack([inp["c"][b], inp["c_ctx"]], -1).reshape(8, 128, 2).transpose(1, 0, 2).reshape(128, 16).astype(np.float32)
        for h in range(4):
            w = np.concatenate([wi[:, h * 128:(h + 1) * 128], wi[:, 512 + h * 128:512 + (h + 1) * 128], wi[:, 1024 + h * 256:1024 + (h + 1) * 256],
                                wi[:, 2048 + h * 256:2048 + (h + 1) * 256]], axis=1)
            waT = np.ascontiguousarray(wi[:, 3072:3104].T.reshape(2, 16, 1024))
            wa2 = np.ascontiguousarray(inp["gla_w_a2"][0][:, :, h * 128:(h + 1) * 128])
            small = np.concatenate([inp["gla_b_a2"][0][0, h * 128:(h + 1) * 128], inp["gla_b_a2"][0][1, h * 128:(h + 1) * 128], inp["gla_norm_g"][0]]).astype(np.float32)
            maps.append({"svec": np.ascontiguousarray(sv),
                         "adaw": np.ascontiguousarray(inp["ada_w"][1][:, 0:2048]), "adab": fop(inp["ada_b"][1][0:2048], 16), "g1": fop(inp["norm1_g"][1], 8),
                         "w": np.ascontiguousarray(w), "waT": waT, "wa2": wa2, "small": small})
    return maps


RG = [[0, 1, 2, 3], [4, 5, 6, 7]]


def build_all():
    kb = KB()
    banks = [kb.ps("bank%d" % i) for i in range(8)]
    x1in = kb.dram("x1in", [512, 4224], BF16)
    x1out = kb.dram("x1out", [2048, 4224], BF16)
    x2in = kb.dram("x2in", [4224, 1024], F32)
    x2out = kb.dram("x2out", [4 * 4224, 1024], F32)
    x3in = kb.dram("x3in", [1024, 4096], BF16)
    x3out = kb.dram("x3out", [4096, 4096], BF16)
    build_l0a(kb, banks, x1in)
    kb.all_gather(x1in, x1out, RG, 64)
    build_b(0, kb, banks, x1out, None, x2in)
    kb.all_gather(x2in, x2out, RG, 256)
    build_l1a(kb, banks, x2out, x3in)
    kb.all_gather(x3in, x3out, RG, 128)
    build_b(1, kb, banks, x3out, x2in, None)
    print("total instructions:", kb.n_ins, "sems:", len(kb.sems))
    return kb.finish()


def kernel(**inputs):
    inp = {k: np.asarray(v) for k, v in inputs.items()}
    parts = [("a0_", host_l0a(inp)), ("b0_", host_b(0, inp)), ("a1_", host_l1a(inp)), ("b1_", host_b(1, inp))]
    maps = []
    for c in range(8):
        m = {}
        for pre, ms in parts:
            for k, v in ms[c].items():
                m[pre + k] = v
        maps.append(m)
    nc = build_all()
    res = run_bass_kernel_spmd(nc, maps, core_ids=list(range(8)))
    out = np.zeros((2, 16384, 1024), np.float32)
    for b in range(2):
        for jq in range(4):
            out[b, jq * 4096:(jq + 1) * 4096] = np.asarray(res.results[b * 4 + jq]["b1_hout"])[:4096]
    return out
```

```python
import numpy as np
from contextlib import ExitStack
import concourse.bass as bass
import concourse.mybir as mybir
from concourse.bass_utils import run_bass_kernel_spmd
import ml_dtypes

F32 = mybir.dt.float32
BF16 = mybir.dt.bfloat16
I32 = mybir.dt.int32
AF = mybir.ActivationFunctionType
ALU = mybir.AluOpType
AX = mybir.AxisListType
NPBF16 = ml_dtypes.bfloat16


def interleave(gens, width):
    active = []
    it = iter(gens)
    while True:
        while len(active) < width:
            g = next(it, None)
            if g is None:
                break
            active.append(g)
        if not active:
            break
        for g in list(active):
            try:
                next(g)
            except StopIteration:
                active.remove(g)


def ag_row(i, rank, chunk_rows, total_rows, world=4):
    r0 = (i // chunk_rows) * chunk_rows
    n = min(chunk_rows, total_rows - r0)
    return world * r0 + rank * n + (i - r0)


class T:
    def __init__(self, h, name, kind):
        self.h = h
        self.name = name
        self.kind = kind
        self.w = None
        self.r = {}
        self.dkey = None

    def __getitem__(self, idx):
        return self.h[idx]


class KB:
    def __init__(self):
        self.nc = bass.Bass("TRN2", target_bir_lowering=False)
        nc = self.nc
        self.es = ExitStack()
        self.eng = {"pe": nc.tensor, "act": nc.scalar, "dve": nc.vector, "pool": nc.gpsimd, "sp": nc.sync}
        self.sems = {}
        self.cnt = {}
        self.seen = {e: {} for e in self.eng}
        for e in self.eng:
            self.sems[e] = self.es.enter_context(nc.semaphore("e_" + e))
            self.cnt[e] = 0
        self.issued = {}
        self.n_ins = 0
        self.outs = []
        self._uid = 0
        self.cur = self.es
        self.prefix = ""
        self.tiles = []
        self.free_dsems = []
        self.stage_tiles0 = 0

    def sb(self, name, shape, dt, es=None):
        h = (es or self.cur).enter_context(self.nc.sbuf_tensor(self.prefix + name, list(shape), dt))
        t = T(h, name, "sb")
        self.tiles.append(t)
        return t

    def ps(self, name, shape=(128, 512), dt=F32):
        h = self.es.enter_context(self.nc.psum_tensor(name, list(shape), dt))
        t = T(h, name, "ps")
        self.tiles.append(t)
        return t

    def dram(self, name, shape, dt, kind="Internal"):
        h = self.nc.dram_tensor(self.prefix + name, list(shape), dt, kind=kind)
        t = T(h.ap(), name, "dram")
        self.tiles.append(t)
        if kind == "ExternalOutput":
            self.outs.append(t)
        return t

    def _dsem(self, t):
        if t.dkey is None:
            self._uid += 1
            t.dkey = "d%d_%s" % (self._uid, t.name)
            if self.free_dsems:
                h, v = self.free_dsems.pop()
                self.sems[t.dkey] = h
                self.issued[t.dkey] = v
            else:
                self.sems[t.dkey] = self.es.enter_context(self.nc.semaphore(t.dkey))
                self.issued[t.dkey] = 0
        return t.dkey

    def begin_stage(self, prefix):
        self.prefix = prefix
        self.cur = ExitStack()
        self.stage_tiles0 = len(self.tiles)

    def end_stage(self):
        self.barrier()
        self.cur.close()
        self.cur = self.es
        for t in self.tiles[self.stage_tiles0:]:
            if t.kind == "sb" and t.dkey is not None:
                self.free_dsems.append((self.sems[t.dkey], self.issued[t.dkey]))
                del self.issued[t.dkey]
                del self.sems[t.dkey]
                t.dkey = None
        for t in self.tiles:
            t.w = None
            t.r = {}
        for e in self.eng:
            self._uid += 1
            self.sems[e] = self.es.enter_context(self.nc.semaphore("e%d_%s" % (self._uid, e)))
            self.cnt[e] = 0
        self.seen = {e: {} for e in self.eng}
        self.prefix = ""

    def all_gather(self, src, dst, groups, chunk_rows):
        self.barrier()
        self._uid += 1
        sem = self.es.enter_context(self.nc.semaphore("cc%d" % self._uid))
        R = src.h.shape[0]
        k = 0
        for r0 in range(0, R, chunk_rows):
            n = min(chunk_rows, R - r0)
            self.nc.gpsimd.collective_compute("AllGather", ALU.bypass, replica_groups=groups, ins=[src.h[r0:r0 + n, :]],
                                              outs=[dst.h[4 * r0:4 * r0 + 4 * n, :]]).then_inc(sem, 1)
            k += 1
        self.nc.gpsimd.wait_ge(sem, k)
        if not hasattr(self, "_fence"):
            self._fence = T(self.es.enter_context(self.nc.sbuf_tensor("cc_fence", [128, 8], F32)), "cc_fence", "sb")
            self.tiles.append(self._fence)
        f = self._fence
        self.op("pool", lambda e: e.memset(f[:], 0.0), writes=[f])
        for en in self.eng:
            if en != "pool":
                self._waits(en, {"pool": self.cnt["pool"]})
        self.n_ins += k + 1

    def _deps(self, en, reads, writes, is_dma=False):
        deps = {}

        def add(key, val, kind):
            if key == en and not is_dma:
                if en == "pe" or kind == "war":
                    return
            if is_dma and kind == "waw" and key in self.issued:
                return
            deps[key] = max(deps.get(key, 0), val)

        for t in reads:
            if t.w is not None:
                add(t.w[0], t.w[1], "raw")
            if t.kind == "ps":
                for k, v in t.r.items():
                    if k != en:
                        add(k, v, "rar")
        for t in writes:
            if t.w is not None:
                add(t.w[0], t.w[1], "waw")
            for k, v in t.r.items():
                add(k, v, "war")
        return deps

    def _waits(self, en, deps):
        e = self.eng[en]
        for key, val in deps.items():
            if key in self.issued:
                val = self.issued[key]
            if self.seen[en].get(key, 0) >= val:
                continue
            e.wait_ge(self.sems[key], val)
            self.seen[en][key] = val
            self.n_ins += 1

    def op(self, en, fn, reads=(), writes=()):
        self._waits(en, self._deps(en, reads, writes))
        ins = fn(self.eng[en])
        self.cnt[en] += 1
        self.n_ins += 1
        ins.then_inc(self.sems[en], 1)
        c = self.cnt[en]
        for t in reads:
            t.r[en] = c
        for t in writes:
            t.w = (en, c)
            t.r = {}
        return ins

    def dma(self, q, fn, reads=(), writes=()):
        self._waits(q, self._deps(q, reads, writes, is_dma=True))
        cand = [t for t in writes if t.kind != "dram"] or [t for t in reads if t.kind != "dram"] or list(writes) or list(reads)
        key = self._dsem(cand[0])
        ins = fn(self.eng[q])
        self.issued[key] += 16
        self.n_ins += 1
        ins.then_inc(self.sems[key], 16)
        v = self.issued[key]
        for t in reads:
            t.r[key] = v
        for t in writes:
            t.w = (key, v)
            t.r = {}
        return ins

    def load(self, q, dst_t, dst_ap, src_ap, src_t=None, **kw):
        return self.dma(q, lambda e: e.dma_start(out=dst_ap, in_=src_ap, **kw),
                        reads=[src_t] if src_t is not None else [], writes=[dst_t])

    def store(self, q, dst_t, dst_ap, src_t, src_ap, **kw):
        return self.dma(q, lambda e: e.dma_start(out=dst_ap, in_=src_ap, **kw), reads=[src_t], writes=[dst_t])

    def finish(self):
        deps = {}
        for t in self.outs:
            if t.w is not None:
                deps[t.w[0]] = max(deps.get(t.w[0], 0), t.w[1])
        self._waits("sp", deps)
        self.es.close()
        return self.nc

    def barrier(self):
        for en in self.eng:
            deps = {}
            for k in self.eng:
                if k != en and self.cnt[k] > 0:
                    deps[k] = self.cnt[k]
            for k, v in self.issued.items():
                if v > 0:
                    deps[k] = v
            self._waits(en, deps)

    def identity(self, name, dt):
        f = self.sb(name + "_f", [128, 128], F32)
        self.op("pool", lambda e: e.memset(f[:], 0.0), writes=[f])
        self.op("pool", lambda e: e.affine_select(out=f[:], in_=f[:], pattern=[[-1, 128]], compare_op=ALU.not_equal,
                                                  fill=1.0, base=0, channel_multiplier=1), reads=[f], writes=[f])
        if dt == F32:
            return f
        b = self.sb(name, [128, 128], dt)
        self.op("pool", lambda e: e.tensor_copy(out=b[:], in_=f[:]), reads=[f], writes=[b])
        return b

EPS = 1e-6
S = 16384
LC = 256

NKT = (S + LC) // 128
ATT_NSPLIT = 512


def build_l0a(kb, banks, x1in, n_groups=32, debug=False):
    kb.begin_stage("a0_")
    x = kb.dram("x", [S, 1024], F32, "ExternalInput")
    ctx = kb.dram("ctx", [LC, 1024], F32, "ExternalInput")
    svec = kb.dram("svec", [128, 16], F32, "ExternalInput")
    adaw = kb.dram("adaw", [1024, 2048], F32, "ExternalInput")
    adab = kb.dram("adab", [128, 16], F32, "ExternalInput")
    g1 = kb.dram("g1", [128, 8], F32, "ExternalInput")
    w = kb.dram("w", [1024, 384], F32, "ExternalInput")
    small = kb.dram("small", [640], F32, "ExternalInput")
    cos4 = kb.dram("cos4", [S, 256], F32, "ExternalInput")
    sin4 = kb.dram("sin4", [S, 256], F32, "ExternalInput")
    zpad = kb.sb("zpad", [128, 4, 64], BF16)
    kb.op("pool", lambda e: e.memset(zpad[:], 0.0), writes=[zpad])
    kb.store("sp", x1in, x1in.h[:, 4160:4224].rearrange("(q p) n -> p q n", p=128), zpad, zpad[:])

    def bfv(t):
        return t[:].bitcast(BF16)

    identb = kb.identity("identb", BF16)
    smallb = kb.sb("smallb", [128, 640], F32)
    kb.load("sp", smallb, smallb[:], small.h.partition_broadcast(128), small)

    tmp64 = kb.sb("tmp64", [128, 2, 64], F32)
    dots = kb.sb("dots", [128, 4], F32)
    kb.op("dve", lambda e: e.tensor_tensor(out=tmp64[:, 0, :], in0=smallb[:, 256:320], in1=smallb[:, 320:384], op=ALU.mult), reads=[smallb], writes=[tmp64])
    kb.op("dve", lambda e: e.tensor_tensor(out=tmp64[:, 1, :], in0=smallb[:, 384:448], in1=smallb[:, 448:512], op=ALU.mult), reads=[smallb], writes=[tmp64])
    kb.op("dve", lambda e: e.tensor_reduce(out=dots[:, 0:2], in_=tmp64[:], axis=AX.X, op=ALU.add), reads=[tmp64], writes=[dots])
    kb.op("act", lambda e: e.activation(out=dots[:, 2:4], in_=dots[:, 0:2], func=AF.Exp), reads=[dots], writes=[dots])
    neglam = kb.sb("neglam", [128, 1], F32)
    kb.op("dve", lambda e: e.scalar_tensor_tensor(out=neglam[:], in0=dots[:, 3:4], scalar=-0.2, in1=dots[:, 2:3], op0=ALU.add, op1=ALU.subtract), reads=[dots], writes=[neglam])
    subg_s = kb.sb("subg_s", [128, 128], F32)
    kb.op("dve", lambda e: e.tensor_scalar(out=subg_s[:], in0=smallb[:, 512:640], scalar1=0.8, scalar2=None, op0=ALU.mult), reads=[smallb], writes=[subg_s])

    s_sb = kb.sb("s_sb", [128, 16], F32)
    kb.load("sp", s_sb, s_sb[:], svec.h, svec)
    kb.op("act", lambda e: e.activation(out=s_sb[:], in_=s_sb[:], func=AF.Silu), reads=[s_sb], writes=[s_sb])
    adab_sb = kb.sb("adab_sb", [128, 16], F32)
    kb.load("sp", adab_sb, adab_sb[:], adab.h, adab)
    g1_sb = kb.sb("g1_sb", [128, 8], F32)
    kb.load("sp", g1_sb, g1_sb[:], g1.h, g1)
    mod = kb.sb("mod", [128, 16, 2], F32)
    gs = kb.sb("gs", [128, 8, 2], F32)
    zer = kb.sb("zer", [128, 128], F32)
    wq = [kb.sb("wq%d" % j, [128, 8, 384], BF16) for j in range(2)]
    bias = [kb.sb("bias%d" % j, [128, 384], F32) for j in range(2)]
    p0 = ExitStack()
    adaw_sb = kb.sb("adaw_sb", [128, 8, 512], F32, p0)
    pm = banks[0]
    for v in range(4):
        kb.load("sp", adaw_sb, adaw_sb[:], adaw.h[:, v * 512:(v + 1) * 512].rearrange("(kc p) n -> p kc n", p=128), adaw)
        for oc in range(4):
            g = v * 4 + oc
            for kc in range(8):
                kb.op("pe", lambda e: e.matmul(pm[:, g * 2:g * 2 + 2], lhsT=adaw_sb[:, kc, oc * 128:(oc + 1) * 128],
                                              rhs=s_sb[:, kc * 2:kc * 2 + 2], start=(kc == 0), stop=(kc == 7)),
                      reads=[adaw_sb, s_sb], writes=[pm])
    pm3 = pm[:, 0:32].rearrange("p (g j) -> p g j", j=2)
    for j in range(2):
        kb.op("dve", lambda e: e.tensor_tensor(out=mod[:, :, j], in0=pm3[:, :, j], in1=adab_sb[:], op=ALU.add), reads=[pm, adab_sb], writes=[mod])
        kb.op("dve", lambda e: e.scalar_tensor_tensor(out=gs[:, :, j], in0=mod[:, 8:16, j], scalar=1.0, in1=g1_sb[:], op0=ALU.add, op1=ALU.mult),
              reads=[mod, g1_sb], writes=[gs])

    w_sb = kb.sb("w_sb", [128, 8, 384], F32, p0)
    kb.load("sp", w_sb, w_sb[:], w.h.rearrange("(kc p) n -> p kc n", p=128), w)
    kb.op("pool", lambda e: e.memset(zer[:], 0.0), writes=[zer])
    shiftbc = kb.sb("shiftbc", [128, 8, 128], F32, p0)
    for j in range(2):
        for kc in range(8):
            kb.op("dve", lambda e: e.tensor_scalar(out=wq[j][:, kc, :], in0=w_sb[:, kc, :], scalar1=gs[:, kc, j:j + 1], scalar2=None, op0=ALU.mult),
                  reads=[w_sb, gs], writes=[wq[j]])
            kb.op("dve", lambda e: e.tensor_scalar(out=shiftbc[:, kc, :], in0=zer[:], scalar1=mod[:, kc, j:j + 1], scalar2=None, op0=ALU.add),
                  reads=[zer, mod], writes=[shiftbc])
        pb = banks[1]
        for kc in range(8):
            kb.op("pe", lambda e: e.matmul(pb[:, 0:384], lhsT=shiftbc[:, kc, :], rhs=w_sb[:, kc, :], start=(kc == 0), stop=(kc == 7)),
                  reads=[shiftbc, w_sb], writes=[pb])
        kb.op("dve", lambda e: e.tensor_copy(out=bias[j][:], in_=pb[:, 0:384]), reads=[pb], writes=[bias[j]])

    kb.barrier()
    p0.close()
    QT = kb.sb("QT", [128, S + LC], BF16)
    KTm = [kb.sb("KT%d" % m, [128, S + LC], BF16) for m in range(2)]
    kb.op("pool", lambda e: e.memset(KTm[0][64:128, :], 0.0), writes=[KTm[0]])
    kb.op("pool", lambda e: e.memset(KTm[1][0:64, :], 0.0), writes=[KTm[1]])
    Vx = kb.sb("Vx", [128, NKT, 129], BF16)
    kb.op("pool", lambda e: e.memset(Vx[:, :, 128:129], 1.0), writes=[Vx])

    def dbl(name, shape, dt, n=2, es=None):
        return [kb.sb("%s%d" % (name, i), shape, dt, es) for i in range(n)]

    p2 = ExitStack()

    xt = dbl("xt", [128, 1024], F32, 4, p2)
    junk = kb.sb("junk", [128, 1024], BF16, p2)
    st1 = dbl("st1", [128, 4], F32, 2, p2)
    xn = dbl("xn", [128, 1024], BF16, 2, p2)
    xnT = dbl("xnT", [128, 1024], BF16, 2, p2)
    qkv = dbl("qkv", [128, 384], F32, 2, p2)
    cs = dbl("cs", [128, 256], F32, 2, p2)
    sn = dbl("sn", [128, 256], F32, 2, p2)
    sq = dbl("sq", [128, 256], F32, 2, p2)
    st2 = dbl("st2", [128, 12], F32, 2, p2)
    qkn = dbl("qkn", [128, 256], F32, 2, p2)
    sw = dbl("sw", [128, 256], F32, 2, p2)
    t1 = dbl("t1", [128, 256], F32, 2, p2)
    rr = dbl("rr", [128, 256], BF16, 2, p2)

    def rstd_chain(stt, c_in, c_tmp, c_out, n, inv_n, srcs):
        kb.op("dve", lambda e: e.tensor_scalar(out=stt[:, c_tmp:c_tmp + n], in0=stt[:, c_in:c_in + n], scalar1=inv_n, scalar2=EPS, op0=ALU.mult, op1=ALU.add),
              reads=[stt], writes=[stt])
        kb.op("act", lambda e: e.activation(out=stt[:, c_tmp:c_tmp + n], in_=stt[:, c_tmp:c_tmp + n], func=AF.Sqrt), reads=[stt], writes=[stt])
        kb.op("dve", lambda e: e.reciprocal(out=stt[:, c_out:c_out + n], in_=stt[:, c_tmp:c_tmp + n]), reads=[stt], writes=[stt])

    tile_args = []

    def do_load(i):
        _, src, row0 = tile_args[i][0:3]
        kb.load("sp", xt[i % 4], xt[i % 4][:], src.h[row0:row0 + 128, :], src)

    def proj_tile(i, src, row0, is_ctx, qcol, kcol, kt):
        p = i % 2
        j = 1 if is_ctx else 0
        if i + 2 < len(tile_args):
            do_load(i + 2)
        yield
        if not is_ctx:
            kb.load("pool", cs[p], cs[p][:], cos4.h[row0:row0 + 128, :], cos4)
            yield
            kb.load("pool", sn[p], sn[p][:], sin4.h[row0:row0 + 128, :], sin4)
            yield
        kb.op("act", lambda e: e.activation(out=junk[:], in_=xt[i % 4][:], func=AF.Square, accum_out=st1[p][:, 0:1]), reads=[xt[i % 4]], writes=[junk, st1[p]])
        yield
        rstd_chain(st1[p], 0, 1, 2, 1, 1.0 / 1024, None)
        kb.op("act", lambda e: e.activation(out=xn[p][:], in_=xt[i % 4][:], func=AF.Copy, scale=st1[p][:, 2:3]), reads=[xt[i % 4], st1[p]], writes=[xn[p]])
        yield
        psT = banks[p]
        for kc in range(8):
            kb.op("pe", lambda e: e.transpose(out=bfv(psT)[:, kc * 128:(kc + 1) * 128], in_=xn[p][:, kc * 128:(kc + 1) * 128], identity=identb[:]),
                  reads=[xn[p], identb], writes=[psT])
            yield
        kb.op("dve", lambda e: e.tensor_copy(out=xnT[p][:], in_=bfv(psT)[:, 0:1024]), reads=[psT], writes=[xnT[p]])
        yield
        pp = banks[2 + p]
        for kc in range(8):
            kb.op("pe", lambda e: e.matmul(pp[:, 0:384], lhsT=xnT[p][:, kc * 128:(kc + 1) * 128], rhs=wq[j][:, kc, :], start=(kc == 0), stop=(kc == 7)),
                  reads=[xnT[p], wq[j]], writes=[pp])
            yield
        kb.op("dve", lambda e: e.tensor_tensor(out=qkv[p][:], in0=pp[:, 0:384], in1=bias[j][:], op=ALU.add), reads=[pp, bias[j]], writes=[qkv[p]])
        yield
        kb.op("pool", lambda e: e.tensor_copy(out=Vx[:, kt, 0:128], in_=qkv[p][:, 256:384]), reads=[qkv[p]], writes=[Vx])
        yield
        kb.op("act", lambda e: e.activation(out=sq[p][:], in_=qkv[p][:, 0:256], func=AF.Square), reads=[qkv[p]], writes=[sq[p]])
        yield
        kb.op("dve", lambda e: e.tensor_reduce(out=st2[p][:, 0:4], in_=sq[p][:].rearrange("p (g d) -> p g d", g=4), axis=AX.X, op=ALU.add),
              reads=[sq[p]], writes=[st2[p]])
        yield
        rstd_chain(st2[p], 0, 4, 8, 4, 1.0 / 64, None)
        for g in range(4):
            kb.op("dve", lambda e: e.scalar_tensor_tensor(out=qkn[p][:, g * 64:(g + 1) * 64], in0=qkv[p][:, g * 64:(g + 1) * 64], scalar=st2[p][:, 8 + g:9 + g],
                                                          in1=smallb[:, g * 64:(g + 1) * 64], op0=ALU.mult, op1=ALU.mult),
                  reads=[qkv[p], st2[p], smallb], writes=[qkn[p]])
            yield
        if is_ctx:
            kb.op("pool", lambda e: e.tensor_copy(out=rr[p][:], in_=qkn[p][:]), reads=[qkn[p]], writes=[rr[p]])
            yield
        else:
            q5 = qkn[p][:].rearrange("p (a h d) -> p a h d", h=2, d=16)
            s5 = sw[p][:].rearrange("p (a h d) -> p a h d", h=2, d=16)
            kb.op("pool", lambda e: e.tensor_copy(out=s5[:, :, 0, :], in_=q5[:, :, 1, :]), reads=[qkn[p]], writes=[sw[p]])
            yield
            kb.op("pool", lambda e: e.tensor_copy(out=s5[:, :, 1, :], in_=q5[:, :, 0, :]), reads=[qkn[p]], writes=[sw[p]])
            yield
            kb.op("pool", lambda e: e.tensor_tensor(out=sw[p][:], in0=sw[p][:], in1=sn[p][:], op=ALU.mult), reads=[sw[p], sn[p]], writes=[sw[p]])
            yield
            kb.op("dve", lambda e: e.tensor_tensor(out=t1[p][:], in0=qkn[p][:], in1=cs[p][:], op=ALU.mult), reads=[qkn[p], cs[p]], writes=[t1[p]])
            yield
            kb.op("dve", lambda e: e.tensor_tensor(out=rr[p][:], in0=t1[p][:], in1=sw[p][:], op=ALU.add), reads=[t1[p], sw[p]], writes=[rr[p]])
            yield
        pq = banks[4 + p]
        for hh in range(2):
            kb.op("pe", lambda e: e.transpose(out=bfv(pq)[:, hh * 128:(hh + 1) * 128], in_=rr[p][:, hh * 128:(hh + 1) * 128], identity=identb[:]),
                  reads=[rr[p], identb], writes=[pq])
            yield
        kb.op("act", lambda e: e.copy(out=QT[:, qcol:qcol + 128], in_=bfv(pq)[:, 0:128]), reads=[pq], writes=[QT])
        yield
        kb.op("act", lambda e: e.copy(out=KTm[0][0:64, kcol:kcol + 128], in_=bfv(pq)[0:64, 128:256]), reads=[pq], writes=[KTm[0]])
        kb.op("act", lambda e: e.copy(out=KTm[1][64:128, kcol:kcol + 128], in_=bfv(pq)[64:128, 128:256]), reads=[pq], writes=[KTm[1]])
        yield

    i = 0
    for c in range(LC // 128):
        tile_args.append((i, ctx, c * 128, True, S + c * 128, c * 128, c))
        i += 1
    for t in range(S // 128):
        tile_args.append((i, x, t * 128, False, t * 128, LC + t * 128, LC // 128 + t))
        i += 1
    do_load(0)
    do_load(1)
    gens = [proj_tile(*a_) for a_ in tile_args]
    interleave(gens, 2)
    kb.barrier()
    p2.close()

    ST = banks[0:3]
    OT = [banks[4], banks[5]]
    PL = [banks[6], banks[7]]
    PS_ = banks[3]
    PT = dbl("pt", [128, 512], BF16, 4)
    Pacc = [kb.sb("pacc%d" % m, [128, 512], F32) for m in range(2)]
    ones_bb = kb.sb("ones_bb", [128, 128], BF16)
    kb.op("pool", lambda e: e.memset(ones_bb[:], 1.0), writes=[ones_bb])
    ones_ff = kb.sb("ones_ff", [128, 128], F32)
    kb.op("pool", lambda e: e.memset(ones_ff[:], 1.0), writes=[ones_ff])
    subg_col = kb.sb("subg_col", [128, 1], F32)
    kb.load("sp", subg_col, subg_col[:], small.h[512:640].rearrange("(p o) -> p o", o=1), small)
    kb.op("dve", lambda e: e.tensor_scalar(out=subg_col[:], in0=subg_col[:], scalar1=0.8, scalar2=None, op0=ALU.mult), reads=[subg_col], writes=[subg_col])
    rlb = dbl("rlb", [128, 512], F32)
    eo = dbl("eo", [128, 512], F32)
    esq = kb.sb("esq", [128, 512], F32)
    outT = dbl("outT", [128, 512], BF16)
    gcount = [0]
    NSPLIT = ATT_NSPLIT

    def attend(qc0, nq, kts):
        steps = [(m, idx, kt) for m in range(2) for idx, kt in enumerate(kts)]
        nk = len(kts)
        gi = gcount[0]
        gcount[0] += 1

        def score(s):
            m, idx, kt = steps[s]
            st = ST[s % 3]
            for c0 in range(0, nq, NSPLIT):
                kb.op("pe", lambda e: e.matmul(st[:, c0:min(nq, c0 + NSPLIT)], lhsT=KTm[m][:, kt * 128:(kt + 1) * 128], rhs=QT[:, qc0 + c0:qc0 + min(nq, c0 + NSPLIT)],
                                              start=True, stop=True), reads=[KTm[m], QT], writes=[st])

        used = {}

        def rest(s):
            m, idx, kt = steps[s]
            st = ST[s % 3]
            pt = PT[s % 4]
            kb.op("act", lambda e: e.activation(out=pt[:, 0:nq], in_=st[:, 0:nq], func=AF.Exp, scale=0.125), reads=[st], writes=[pt])
            for c0 in range(0, nq, NSPLIT):
                kb.op("pe", lambda e: e.matmul(OT[m][:, c0:min(nq, c0 + NSPLIT)], lhsT=Vx[:, kt, 0:128], rhs=pt[:, c0:min(nq, c0 + NSPLIT)], start=(idx == 0 and c0 == 0), stop=(idx == nk - 1),
                                              skip_group_check=True), reads=[pt, Vx], writes=[OT[m]])
            if s + 3 < len(steps):
                score(s + 3)
            if idx % 3 == 2:
                kb.op("pe", lambda e: e.matmul(PL[m][:, 0:nq], lhsT=ones_bb[:], rhs=pt[:, 0:nq], start=((m, "pe") not in used), stop=False), reads=[ones_bb, pt], writes=[PL[m]])
                used[(m, "pe")] = True
            elif (m, "dve") not in used:
                used[(m, "dve")] = True
                kb.op("dve", lambda e: e.tensor_copy(out=Pacc[m][:, 0:nq], in_=pt[:, 0:nq]), reads=[pt], writes=[Pacc[m]])
            else:
                kb.op("dve", lambda e: e.tensor_tensor(out=Pacc[m][:, 0:nq], in0=pt[:, 0:nq], in1=Pacc[m][:, 0:nq], op=ALU.add), reads=[pt, Pacc[m]], writes=[Pacc[m]])

        for s0 in range(min(3, len(steps))):
            score(s0)
        for s in range(len(steps)):
            rest(s)
        for m in range(2):
            kb.op("pe", lambda e: e.matmul(PL[m][:, 0:nq], lhsT=ones_ff[:], rhs=Pacc[m][:, 0:nq], start=((m, "pe") not in used), stop=True), reads=[ones_ff, Pacc[m]], writes=[PL[m]])
            kb.op("dve", lambda e: e.reciprocal(out=rlb[m][:, 0:nq], in_=PL[m][:, 0:nq]), reads=[PL[m]], writes=[rlb[m]])
            kb.op("dve", lambda e: e.tensor_tensor(out=eo[m][:, 0:nq], in0=OT[m][:, 0:nq], in1=rlb[m][:, 0:nq], op=ALU.mult), reads=[OT[m], rlb[m]], writes=[eo[m]])
        kb.op("dve", lambda e: e.scalar_tensor_tensor(out=eo[0][:, 0:nq], in0=eo[1][:, 0:nq], scalar=neglam[:, 0:1], in1=eo[0][:, 0:nq], op0=ALU.mult, op1=ALU.add),
              reads=[eo[1], neglam, eo[0]], writes=[eo[0]])
        kb.op("act", lambda e: e.activation(out=esq[:, 0:nq], in_=eo[0][:, 0:nq], func=AF.Square), reads=[eo[0]], writes=[esq])
        kb.op("pe", lambda e: e.matmul(PS_[:, 0:nq], lhsT=ones_ff[:], rhs=esq[:, 0:nq], start=True, stop=True), reads=[ones_ff, esq], writes=[PS_])
        kb.op("dve", lambda e: e.tensor_scalar(out=rlb[0][:, 0:nq], in0=PS_[:, 0:nq], scalar1=1.0 / 128, scalar2=EPS, op0=ALU.mult, op1=ALU.add), reads=[PS_], writes=[rlb[0]])
        kb.op("act", lambda e: e.activation(out=rlb[0][:, 0:nq], in_=rlb[0][:, 0:nq], func=AF.Sqrt), reads=[rlb[0]], writes=[rlb[0]])
        kb.op("dve", lambda e: e.reciprocal(out=rlb[1][:, 0:nq], in_=rlb[0][:, 0:nq]), reads=[rlb[0]], writes=[rlb[1]])
        ot = outT[gi % 2]
        kb.op("dve", lambda e: e.scalar_tensor_tensor(out=ot[:, 0:nq], in0=eo[0][:, 0:nq], scalar=subg_col[:, 0:1], in1=rlb[1][:, 0:nq], op0=ALU.mult, op1=ALU.mult),
              reads=[eo[0], subg_col, rlb[1]], writes=[ot])
        if qc0 >= S:
            for q in range(4):
                kb.store("sp", x1in, x1in.h[q * 128:(q + 1) * 128, 4096:4160], ot, ot[:, q * 64:(q + 1) * 64])
        else:
            q, col = qc0 // 4096, qc0 % 4096
            kb.store("sp", x1in, x1in.h[q * 128:(q + 1) * 128, col:col + nq], ot, ot[:, 0:nq])

    attend(S, LC, list(range(LC // 128)))
    for g in range(n_groups):
        attend(g * 512, 512, list(range(NKT)))
    print("l0a instructions:", kb.n_ins)
    kb.end_stage()


def rope_tables():
    half = 32
    inv = (10000.0 ** (-np.arange(0, half, 2, dtype=np.float32) / half)).astype(np.float32)
    t = np.arange(S)
    r = (t // 64).astype(np.float32)[:, None] * inv[None, :]
    c = (t % 64).astype(np.float32)[:, None] * inv[None, :]
    ang = np.concatenate([r, r, c, c], axis=-1).astype(np.float32)
    cos = np.cos(ang).astype(np.float32)
    sin = np.sin(ang).astype(np.float32)
    sgn = np.concatenate([-np.ones(16), np.ones(16), -np.ones(16), np.ones(16)]).astype(np.float32)
    sin = sin * sgn[None, :]
    return np.ascontiguousarray(np.tile(cos, (1, 4))), np.ascontiguousarray(np.tile(sin, (1, 4)))


def fop(v, n):
    return np.ascontiguousarray(np.asarray(v, np.float32).reshape(n, 128).T)


def host_l0a(inp):
    cos4, sin4 = rope_tables()
    maps = []
    wi = inp["ab_w_in"][0]
    for b in range(2):
        for h in range(4):
            sv = np.stack([inp["c"][b], inp["c_ctx"]], -1).reshape(8, 128, 2).transpose(1, 0, 2).reshape(128, 16)
            w = np.concatenate([wi[:, 1024 + h * 128:1024 + (h + 1) * 128], wi[:, 1536 + h * 128:1536 + (h + 1) * 128],
                                wi[:, 2048 + h * 128:2048 + (h + 1) * 128]], axis=1)
            qg, kg = inp["diff_qnorm_g"][0], inp["diff_knorm_g"][0]
            small = np.concatenate([qg, qg, kg, kg, inp["diff_lq1"][0], inp["diff_lk1"][0], inp["diff_lq2"][0], inp["diff_lk2"][0],
                                    inp["diff_subln_g"][0]]).astype(np.float32)
            maps.append({
                "x": np.ascontiguousarray(inp["x"][b]), "ctx": np.ascontiguousarray(inp["ctx"][b]),
                "svec": np.ascontiguousarray(sv.astype(np.float32)),
                "adaw": np.ascontiguousarray(inp["ada_w"][0][:, 0:2048]), "adab": fop(inp["ada_b"][0][0:2048], 16),
                "g1": fop(inp["norm1_g"][0], 8), "w": np.ascontiguousarray(w), "small": small, "cos4": cos4, "sin4": sin4,
            })
    return maps


BIG = 1.0e30


def build_b(layer, kb, banks, mixsrc, hin_t, hout_t, debug=False):
    L0 = (layer == 0)
    NTL = 32
    NT = NTL + (1 if L0 else 0)
    NTOK = NT * 128
    NTOKV = 4096 + (64 if L0 else 0)
    NB = (2 * NTOKV + 32 * 255 + 255) // 256
    NROWS = NB * 256
    NMIX = 4 if L0 else 8

    kb.begin_stage("b%d_" % layer)
    hin = hin_t if hin_t is not None else kb.dram("hin", [NTOK, 1024], F32, "ExternalInput")
    svec = kb.dram("svec", [128, 16], F32, "ExternalInput")
    adaw = kb.dram("adaw", [1024, 6144], F32, "ExternalInput")
    adabf = kb.dram("adabf", [128, 48], F32, "ExternalInput")
    adabr = kb.dram("adabr", [2048], F32, "ExternalInput")
    gfop = kb.dram("gfop", [128, 16], F32, "ExternalInput")
    mixidx = kb.dram("mixidx", [128, NMIX], I32, "ExternalInput")
    wout = kb.dram("wout", [1024, 1024], F32, "ExternalInput")
    rw = kb.dram("rw", [1024, 36], F32, "ExternalInput")
    rb = kb.dram("rb", [36], F32, "ExternalInput")
    w1t = kb.dram("w1t", [4096, 4096], F32, "ExternalInput")
    w3t = kb.dram("w3t", [4096, 4096], F32, "ExternalInput")
    w2t = kb.dram("w2t", [4096, 4096], F32, "ExternalInput")
    valid = kb.dram("valid", [128, 1], F32, "ExternalInput")
    if L0:
        xhalo = kb.dram("xhalo", [128, 1024], F32, "ExternalInput")
        cxh = kb.dram("cxh", [128, 1024], F32, "ExternalInput")
        edge = kb.dram("edge", [2], F32, "ExternalInput")
        win = kb.dram("win", [1024, 1024], F32, "ExternalInput")
        cw = kb.dram("cw", [128, 124], F32, "ExternalInput")
        cvec = kb.dram("cvec", [128, 12], F32, "ExternalInput")
    hout = hout_t if hout_t is not None else kb.dram("hout", [NTOK, 1024], F32, "ExternalOutput")
    hlm = kb.dram("hlm", [NTOK, 1024], F32)
    nl2d = kb.dram("nl2d", [NTOK, 1024], BF16)
    xs = kb.dram("xs", [NROWS + 128, 1024], BF16)
    ys = kb.dram("ys", [NROWS + 128, 1024], F32)

    def bfv(t):
        return t[:].bitcast(BF16)

    def dbl(name, shape, dt, n=2, es=None):
        return [kb.sb("%s%d" % (name, i), shape, dt, es) for i in range(n)]

    identb = kb.identity("identb", BF16)
    zer = kb.sb("zer", [128, 128], F32)
    kb.op("pool", lambda e: e.memset(zer[:], 0.0), writes=[zer])
    zerb = kb.sb("zerb", [128, 2048], BF16)
    kb.op("pool", lambda e: e.memset(zerb[:], 0.0), writes=[zerb])
    for a in range(0, NROWS // 128, 2):
        kb.store("pool", xs, xs.h[a * 128:(a + 2) * 128, :].rearrange("(a p) n -> p a n", p=128), zerb, zerb[:].rearrange("p (a n) -> p a n", a=2))

    OH = kb.sb("OH", [128, NT, 2, 32], F32)
    GT = kb.sb("GT", [128, NT, 2], F32)
    RK = kb.sb("RK", [128, NT, 2], F32)
    Rbc = kb.sb("Rbc", [128, 32], F32)
    DESTI = kb.sb("DESTI", [128, NT * 2], I32)
    WIDX = kb.sb("WIDX", [128, NB], I32)
    validt = kb.sb("validt", [128, 1], F32)
    gate_bc = [[kb.sb("gate_bc%d%d" % (j, w), [128, 1024], F32) for w in range(2)] for j in range(2)]
    mod = kb.sb("mod", [128, 48, 2], F32)
    gs1 = kb.sb("gs1", [128, 8, 2], F32)
    gs2 = kb.sb("gs2", [128, 8, 2], F32)
    s_sb = kb.sb("s_sb", [128, 16], F32)
    adabf_sb = kb.sb("adabf_sb", [128, 48], F32)
    gfop_sb = kb.sb("gfop_sb", [128, 16], F32)
    iop = kb.sb("iop", [128, 1], F32)
    blkst = kb.sb("blkst", [128, NB], F32)
    ltri_b = kb.sb("ltri_b", [128, 128], BF16)
    ones_b = kb.sb("ones_b", [128, 128], BF16)
    pesA = ExitStack()

    def rstd_chain(stt, c_in, c_tmp, c_out, n, inv_n):
        kb.op("dve", lambda e: e.tensor_scalar(out=stt[:, c_tmp:c_tmp + n], in0=stt[:, c_in:c_in + n], scalar1=inv_n, scalar2=EPS, op0=ALU.mult, op1=ALU.add),
              reads=[stt], writes=[stt])
        kb.op("act", lambda e: e.activation(out=stt[:, c_tmp:c_tmp + n], in_=stt[:, c_tmp:c_tmp + n], func=AF.Sqrt), reads=[stt], writes=[stt])
        kb.op("dve", lambda e: e.reciprocal(out=stt[:, c_out:c_out + n], in_=stt[:, c_tmp:c_tmp + n]), reads=[stt], writes=[stt])

    kb.load("sp", s_sb, s_sb[:], svec.h, svec)
    kb.op("act", lambda e: e.activation(out=s_sb[:], in_=s_sb[:], func=AF.Silu), reads=[s_sb], writes=[s_sb])
    kb.load("sp", adabf_sb, adabf_sb[:], adabf.h, adabf)
    kb.load("sp", gfop_sb, gfop_sb[:], gfop.h, gfop)
    kb.load("sp", validt, validt[:], valid.h, valid)
    pm = banks[0]
    with ExitStack() as pes:
        adabr_sb = kb.sb("adabr_sb", [128, 2048], F32, pes)
        kb.load("sp", adabr_sb, adabr_sb[:], adabr.h.partition_broadcast(128), adabr)
        s_bc = [kb.sb("s_bc%d" % j, [128, 8, 128], F32, pes) for j in range(2)]
        for j in range(2):
            for kc in range(8):
                kb.op("dve", lambda e: e.tensor_scalar(out=s_bc[j][:, kc, :], in0=zer[:, 0:128], scalar1=s_sb[:, kc * 2 + j:kc * 2 + j + 1], scalar2=None, op0=ALU.add),
                      reads=[zer, s_sb], writes=[s_bc[j]])
        adaw_sb = dbl("adaw_sb", [128, 8, 512], F32, 2, pes)
        for v in range(12):
            aw = adaw_sb[v % 2]
            kb.load("sp", aw, aw[:], adaw.h[:, v * 512:(v + 1) * 512].rearrange("(kc p) n -> p kc n", p=128), adaw)
            for oc in range(4):
                g = v * 4 + oc
                for kc in range(8):
                    kb.op("pe", lambda e: e.matmul(pm[:, g * 2:g * 2 + 2], lhsT=aw[:, kc, oc * 128:(oc + 1) * 128], rhs=s_sb[:, kc * 2:kc * 2 + 2],
                                                  start=(kc == 0), stop=(kc == 7)), reads=[aw, s_sb], writes=[pm])
            if v in (4, 5, 10, 11):
                which = 0 if v < 6 else 1
                half = v % 2
                for j in range(2):
                    pr = banks[1 + j]
                    for kc in range(8):
                        kb.op("pe", lambda e: e.matmul(pr[:, :], lhsT=s_bc[j][:, kc, :], rhs=aw[:, kc, :], start=(kc == 0), stop=(kc == 7)),
                              reads=[s_bc[j], aw], writes=[pr])
                    kb.op("dve", lambda e: e.tensor_tensor(out=gate_bc[j][which][:, half * 512:(half + 1) * 512], in0=pr[:, :],
                                                           in1=adabr_sb[:, which * 1024 + half * 512: which * 1024 + (half + 1) * 512], op=ALU.add),
                          reads=[pr, adabr_sb], writes=[gate_bc[j][which]])
        pm3 = pm[:, 0:96].rearrange("p (g j) -> p g j", j=2)
        for j in range(2):
            kb.op("dve", lambda e: e.tensor_tensor(out=mod[:, :, j], in0=pm3[:, :, j], in1=adabf_sb[:], op=ALU.add), reads=[pm, adabf_sb], writes=[mod])
        kb.barrier()
    for j in range(2):
        kb.op("dve", lambda e: e.scalar_tensor_tensor(out=gs1[:, :, j], in0=mod[:, 8:16, j], scalar=1.0, in1=gfop_sb[:, 0:8], op0=ALU.add, op1=ALU.mult),
              reads=[mod, gfop_sb], writes=[gs1])
        kb.op("dve", lambda e: e.scalar_tensor_tensor(out=gs2[:, :, j], in0=mod[:, 32:40, j], scalar=1.0, in1=gfop_sb[:, 8:16], op0=ALU.add, op1=ALU.mult),
              reads=[mod, gfop_sb], writes=[gs2])
    SH1, SH2 = 0, 24

    xn_b = dbl("xn_b", [128, 1024], BF16, 2, pesA)
    junk = kb.sb("junk", [128, 1024], BF16, pesA)
    stn = dbl("stn", [128, 4], F32, 2, pesA)

    def norm_T(i, xt_tile, gs, shoff, j, dstT, dcol, psT):
        p = i % 2
        kb.op("act", lambda e: e.activation(out=junk[:], in_=xt_tile[:], func=AF.Square, accum_out=stn[p][:, 0:1]), reads=[xt_tile], writes=[junk, stn[p]])
        rstd_chain(stn[p], 0, 1, 2, 1, 1.0 / 1024)
        kb.op("act", lambda e: e.activation(out=xn_b[p][:], in_=xt_tile[:], func=AF.Copy, scale=stn[p][:, 2:3]), reads=[xt_tile, stn[p]], writes=[xn_b[p]])
        for kc in range(8):
            kb.op("pe", lambda e: e.transpose(out=bfv(psT)[:, kc * 128:(kc + 1) * 128], in_=xn_b[p][:, kc * 128:(kc + 1) * 128], identity=identb[:]),
                  reads=[xn_b[p], identb], writes=[psT])
        for kc in range(8):
            kb.op("act", lambda e: e.activation(out=dstT[:, kc, dcol:dcol + 128], in_=bfv(psT)[:, kc * 128:(kc + 1) * 128], func=AF.Identity,
                                                scale=gs[:, kc, j:j + 1], bias=mod[:, shoff + kc, j:j + 1]), reads=[psT, gs, mod], writes=[dstT])

    xt = dbl("xt", [128, 1024], F32, 2, pesA)
    convT = kb.sb("convT", [128, 4, NTOK], BF16, pesA) if L0 else None

    if L0:
        with ExitStack() as pes:
            HW = 15 + 4096 + 15
            hT = kb.sb("hT", [128, 4, HW], F32, pes)
            hTc = kb.sb("hTc", [128, 4, 128], F32, pes)
            win_b = kb.sb("win_b", [128, 8, 1024], BF16, pes)
            stg = xt
            for kc in range(8):
                kb.load("sp", stg[kc % 2], stg[kc % 2][:], win.h[kc * 128:(kc + 1) * 128, :], win)
                kb.op("pool", lambda e: e.tensor_copy(out=win_b[:, kc, :], in_=stg[kc % 2][:]), reads=[stg[kc % 2]], writes=[win_b])
            cw_sb = kb.sb("cw_sb", [128, 4, 31], F32, pes)
            kb.load("sp", cw_sb, cw_sb[:], cw.h.rearrange("p (c t) -> p c t", c=4), cw)
            cvec_sb = kb.sb("cvec_sb", [128, 12], F32, pes)
            kb.load("sp", cvec_sb, cvec_sb[:], cvec.h, cvec)
            edge_sb = kb.sb("edge_sb", [128, 2], F32, pes)
            kb.load("sp", edge_sb, edge_sb[:], edge.h.partition_broadcast(128), edge)
            ones_s = kb.sb("ones_s", [128, 128], F32, pes)
            kb.op("pool", lambda e: e.memset(ones_s[:], 1.0 / 512), writes=[ones_s])
            nlT = dbl("nlT", [128, 8, 512], BF16, 1, pes) * 2
            sig = dbl("sig", [128, 512], F32, 2, pes)
            htmp = kb.sb("htmp", [128, 4, 128], F32, pes)

            def u_group(gi, nl, ncols, dst_fn):
                for cc in range(4):
                    pa, pg = banks[2], banks[3]
                    for kc in range(8):
                        kb.op("pe", lambda e: e.matmul(pa[:, 0:ncols], lhsT=win_b[:, kc, cc * 128:(cc + 1) * 128], rhs=nl[:, kc, 0:ncols], start=(kc == 0), stop=(kc == 7)),
                              reads=[win_b, nl], writes=[pa])
                    for kc in range(8):
                        kb.op("pe", lambda e: e.matmul(pg[:, 0:ncols], lhsT=win_b[:, kc, 512 + cc * 128:512 + (cc + 1) * 128], rhs=nl[:, kc, 0:ncols], start=(kc == 0), stop=(kc == 7)),
                              reads=[win_b, nl], writes=[pg])
                    sg = sig[cc % 2]
                    kb.op("act", lambda e: e.activation(out=sg[:, 0:ncols], in_=pg[:, 0:ncols], func=AF.Sigmoid), reads=[pg], writes=[sg])
                    dt_, dap = dst_fn(cc)
                    kb.op("dve", lambda e: e.tensor_tensor(out=dap, in0=pa[:, 0:ncols], in1=sg[:, 0:ncols], op=ALU.mult), reads=[pa, sg], writes=[dt_])

            ti = 0
            for g in range(8):
                nl = nlT[g % 2]
                for tt in range(4):
                    t = g * 4 + tt
                    kb.load("sp", xt[ti % 2], xt[ti % 2][:], hin.h[t * 128:(t + 1) * 128, :], hin)
                    norm_T(ti, xt[ti % 2], gs1, SH1, 0, nl, tt * 128, banks[ti % 2])
                    ti += 1
                u_group(g, nl, 512, lambda cc: (hT, hT[:, cc, 15 + g * 512:15 + (g + 1) * 512]))
            nl = nlT[0]
            kb.load("sp", xt[ti % 2], xt[ti % 2][:], xhalo.h, xhalo)
            norm_T(ti, xt[ti % 2], gs1, SH1, 0, nl, 0, banks[ti % 2])
            ti += 1
            u_group(8, nl, 128, lambda cc: (htmp, htmp[:, cc, :]))
            for cc in range(4):
                kb.op("dve", lambda e: e.tensor_scalar(out=hT[:, cc, 0:15], in0=htmp[:, cc, 0:15], scalar1=edge_sb[:, 0:1], scalar2=None, op0=ALU.mult),
                      reads=[htmp, edge_sb], writes=[hT])
                kb.op("dve", lambda e: e.tensor_scalar(out=hT[:, cc, 15 + 4096:HW], in0=htmp[:, cc, 15:30], scalar1=edge_sb[:, 1:2], scalar2=None, op0=ALU.mult),
                      reads=[htmp, edge_sb], writes=[hT])
            nl = nlT[1]
            kb.load("sp", xt[ti % 2], xt[ti % 2][:], cxh.h, cxh)
            norm_T(ti, xt[ti % 2], gs1, SH1, 1, nl, 0, banks[ti % 2])
            ti += 1
            u_group(9, nl, 128, lambda cc: (hTc, hTc[:, cc, :]))
            for cc in range(4):
                kb.op("dve", lambda e: e.tensor_scalar(out=hTc[:, cc, 0:15], in0=hTc[:, cc, 0:15], scalar1=edge_sb[:, 0:1], scalar2=None, op0=ALU.mult),
                      reads=[hTc, edge_sb], writes=[hTc])
                kb.op("dve", lambda e: e.tensor_scalar(out=hTc[:, cc, 79:94], in0=hTc[:, cc, 79:94], scalar1=edge_sb[:, 1:2], scalar2=None, op0=ALU.mult),
                      reads=[hTc, edge_sb], writes=[hTc])

            acc = [kb.sb("acc%d" % c, [128, 512], F32, pes) for c in range(4)]
            sqt = dbl("sqt", [128, 512], F32, 1, pes) * 2
            mean_sb = kb.sb("mean_sb", [128, 512], F32, pes)
            m2 = kb.sb("m2", [128, 512], F32, pes)
            rstd_bc = kb.sb("rstd_bc", [128, 512], F32, pes)
            tt_ = dbl("tt_", [128, 512], F32, 1, pes) * 2

            def conv_block(src, c0, n, out_c0):
                for tau in range(31):
                    for cc in range(4):
                        en = "dve"
                        if tau == 0:
                            kb.op(en, lambda e: e.tensor_scalar(out=acc[cc][:, 0:n], in0=src[:, cc, c0:c0 + n], scalar1=cw_sb[:, cc, 0:1], scalar2=cvec_sb[:, cc:cc + 1],
                                                                op0=ALU.mult, op1=ALU.add), reads=[src, cw_sb, cvec_sb], writes=[acc[cc]])
                        else:
                            kb.op(en, lambda e: e.scalar_tensor_tensor(out=acc[cc][:, 0:n], in0=src[:, cc, c0 + tau:c0 + tau + n], scalar=cw_sb[:, cc, tau:tau + 1],
                                                                       in1=acc[cc][:, 0:n], op0=ALU.mult, op1=ALU.add), reads=[src, cw_sb, acc[cc]], writes=[acc[cc]])
                pmean, pex2 = banks[4], banks[5]
                for cc in range(4):
                    kb.op("pe", lambda e: e.matmul(pmean[:, 0:n], lhsT=ones_s[:], rhs=acc[cc][:, 0:n], start=(cc == 0), stop=(cc == 3)), reads=[ones_s, acc[cc]], writes=[pmean])
                for cc in range(4):
                    sq_ = sqt[cc % 2]
                    kb.op("act", lambda e: e.activation(out=sq_[:, 0:n], in_=acc[cc][:, 0:n], func=AF.Square), reads=[acc[cc]], writes=[sq_])
                    kb.op("pe", lambda e: e.matmul(pex2[:, 0:n], lhsT=ones_s[:], rhs=sq_[:, 0:n], start=(cc == 0), stop=(cc == 3)), reads=[ones_s, sq_], writes=[pex2])
                kb.op("act", lambda e: e.copy(out=mean_sb[:, 0:n], in_=pmean[:, 0:n]), reads=[pmean], writes=[mean_sb])
                kb.op("pool", lambda e: e.tensor_tensor(out=m2[:, 0:n], in0=mean_sb[:, 0:n], in1=mean_sb[:, 0:n], op=ALU.mult), reads=[mean_sb], writes=[m2])
                kb.op("dve", lambda e: e.tensor_tensor(out=m2[:, 0:n], in0=pex2[:, 0:n], in1=m2[:, 0:n], op=ALU.subtract), reads=[pex2, m2], writes=[m2])
                kb.op("dve", lambda e: e.tensor_scalar(out=m2[:, 0:n], in0=m2[:, 0:n], scalar1=EPS, scalar2=None, op0=ALU.add), reads=[m2], writes=[m2])
                kb.op("act", lambda e: e.activation(out=m2[:, 0:n], in_=m2[:, 0:n], func=AF.Sqrt), reads=[m2], writes=[m2])
                kb.op("dve", lambda e: e.reciprocal(out=rstd_bc[:, 0:n], in_=m2[:, 0:n]), reads=[m2], writes=[rstd_bc])
                for cc in range(4):
                    t_ = tt_[cc % 2]
                    kb.op("dve", lambda e: e.tensor_tensor(out=t_[:, 0:n], in0=acc[cc][:, 0:n], in1=mean_sb[:, 0:n], op=ALU.subtract), reads=[acc[cc], mean_sb], writes=[t_])
                    kb.op("pool", lambda e: e.tensor_tensor(out=t_[:, 0:n], in0=t_[:, 0:n], in1=rstd_bc[:, 0:n], op=ALU.mult), reads=[t_, rstd_bc], writes=[t_])
                    kb.op("act", lambda e: e.activation(out=convT[:, cc, out_c0:out_c0 + n], in_=t_[:, 0:n], func=AF.Silu, scale=cvec_sb[:, 4 + cc:5 + cc],
                                                        bias=cvec_sb[:, 8 + cc:9 + cc]), reads=[t_, cvec_sb], writes=[convT])

            for tb in range(8):
                conv_block(hT, tb * 512, 512, tb * 512)
            conv_block(hTc, 0, 64, 4096)
            kb.op("pool", lambda e: e.memset(convT[:, :, 4096 + 64:4096 + 128], 0.0), writes=[convT])
            kb.barrier()

    mix_sb = kb.sb("mix_sb", [128, NMIX, NTOK], BF16, pesA)
    mixidx_sb = kb.sb("mixidx_sb", [128, NMIX], I32, pesA)
    kb.load("sp", mixidx_sb, mixidx_sb[:], mixidx.h, mixidx)
    for hh in range(NMIX):
        kb.dma("pool", lambda e: e.indirect_dma_start(out=mix_sb[:, hh, :], out_offset=None, in_=mixsrc.h[:, :],
                                                      in_offset=bass.IndirectOffsetOnAxis(ap=mixidx_sb[:, hh:hh + 1], axis=0)), reads=[mixidx_sb, mixsrc], writes=[mix_sb])
    wout_b = kb.sb("wout_b", [128, 8, 1024], BF16, pesA)
    rw_b = kb.sb("rw_b", [128, 8, 36], BF16, pesA)
    rb_bc = kb.sb("rb_bc", [128, 36], F32, pesA)
    kb.load("sp", rb_bc, rb_bc[:], rb.h.partition_broadcast(128), rb)
    kb.op("pool", lambda e: e.memset(Rbc[:], 0.0), writes=[Rbc])
    ltri = kb.sb("ltri", [128, 128], F32, pesA)
    kb.op("pool", lambda e: e.memset(ltri[:], 1.0), writes=[ltri])
    kb.op("pool", lambda e: e.affine_select(out=ltri[:], in_=ltri[:], pattern=[[1, 128]], compare_op=ALU.is_gt, fill=0.0, base=0, channel_multiplier=-1),
          reads=[ltri], writes=[ltri])
    kb.op("pool", lambda e: e.tensor_copy(out=ltri_b[:], in_=ltri[:]), reads=[ltri], writes=[ltri_b])
    kb.op("pool", lambda e: e.memset(ones_b[:], 1.0), writes=[ones_b])
    kb.op("pool", lambda e: e.iota(iop[:], pattern=[[0, 1]], base=0, channel_multiplier=1, allow_small_or_imprecise_dtypes=True), writes=[iop])
    kb.op("pool", lambda e: e.iota(blkst[:], pattern=[[256, NB]], base=0, channel_multiplier=0, allow_small_or_imprecise_dtypes=True), writes=[blkst])

    with ExitStack() as pes:
        stg = dbl("stg2", [128, 1024], F32, 2, pes)
        for kc in range(8):
            kb.load("sp", stg[kc % 2], stg[kc % 2][:], wout.h[kc * 128:(kc + 1) * 128, :], wout)
            kb.op("pool", lambda e: e.tensor_copy(out=wout_b[:, kc, :], in_=stg[kc % 2][:]), reads=[stg[kc % 2]], writes=[wout_b])
        rw_f = kb.sb("rw_f", [128, 8, 36], F32, pes)
        kb.load("sp", rw_f, rw_f[:], rw.h.rearrange("(kc p) n -> p kc n", p=128), rw)
        kb.op("pool", lambda e: e.tensor_copy(out=rw_b[:], in_=rw_f[:]), reads=[rw_f], writes=[rw_b])

        ytmp = dbl("ytmp", [128, 1024], F32, 2, pes)
        hl = dbl("hl", [128, 1024], F32, 2, pes)
        nl2T = dbl("nl2T", [128, 8, 128], BF16, 2, pes)
        nl2 = dbl("nl2", [128, 1024], BF16, 2, pes)
        lg = dbl("lg", [128, 36], F32, 2, pes)
        rt = dbl("rt", [128, 16], F32, 2, pes)
        lem = dbl("lem", [128, 32], F32, 2, pes)
        lem2 = dbl("lem2", [128, 32], F32, 2, pes)
        cb_ = dbl("cb_", [128, 32], BF16, 2, pes)
        rbase = dbl("rbase", [128, 32], F32, 2, pes)
        tmp32 = dbl("tmp32", [128, 2, 32], F32, 2, pes)
        ejunk = kb.sb("ejunk", [128, 4], F32, pes)

        for t in range(NT):
            p = t % 2
            j = 1 if (L0 and t == NT - 1) else 0
            kb.load("sp", xt[p], xt[p][:], hin.h[t * 128:(t + 1) * 128, :], hin)
            chunks = []
            if L0:
                for cc in range(4):
                    chunks.append((convT, convT[:, cc, t * 128:(t + 1) * 128]))
            for hh in range(NMIX):
                chunks.append((mix_sb, mix_sb[:, hh, t * 128:(t + 1) * 128]))
            for half in range(2):
                py = banks[half]
                for ci, (ct, cap) in enumerate(chunks):
                    kb.op("pe", lambda e: e.matmul(py[:, :], lhsT=cap, rhs=wout_b[:, ci, half * 512:(half + 1) * 512], start=(ci == 0), stop=(ci == 7)),
                          reads=[ct, wout_b], writes=[py])
                kb.op("dve", lambda e: e.tensor_tensor(out=ytmp[p][:, half * 512:(half + 1) * 512], in0=py[:, :], in1=gate_bc[j][0][:, half * 512:(half + 1) * 512], op=ALU.mult),
                      reads=[py, gate_bc[j][0]], writes=[ytmp[p]])
            kb.op("dve", lambda e: e.tensor_tensor(out=hl[p][:], in0=ytmp[p][:], in1=xt[p][:], op=ALU.add), reads=[ytmp[p], xt[p]], writes=[hl[p]])
            kb.store("sp", hlm, hlm.h[t * 128:(t + 1) * 128, :], hl[p], hl[p][:])
            norm_T(t, hl[p], gs2, SH2, j, nl2T[p], 0, banks[2])
            pl = banks[3]
            for kc in range(8):
                kb.op("pe", lambda e: e.matmul(pl[:, 0:36], lhsT=nl2T[p][:, kc, :], rhs=rw_b[:, kc, :], start=(kc == 0), stop=(kc == 7)), reads=[nl2T[p], rw_b], writes=[pl])
            kb.op("dve", lambda e: e.tensor_tensor(out=lg[p][:], in0=pl[:, 0:36], in1=rb_bc[:], op=ALU.add), reads=[pl, rb_bc], writes=[lg[p]])
            pbk = banks[4]
            for kc in range(8):
                kb.op("pe", lambda e: e.transpose(out=bfv(pbk)[:, kc * 128:(kc + 1) * 128], in_=nl2T[p][:, kc, :], identity=identb[:]), reads=[nl2T[p], identb], writes=[pbk])
            kb.op("act", lambda e: e.copy(out=nl2[p][:], in_=bfv(pbk)[:, 0:1024]), reads=[pbk], writes=[nl2[p]])
            kb.store("sp", nl2d, nl2d.h[t * 128:(t + 1) * 128, :], nl2[p], nl2[p][:])
            r_ = rt[p]
            kb.op("dve", lambda e: e.tensor_reduce(out=r_[:, 0:1], in_=lg[p][:, 0:4], axis=AX.X, op=ALU.max), reads=[lg[p]], writes=[r_])
            kb.op("dve", lambda e: e.tensor_scalar(out=r_[:, 1:5], in0=lg[p][:, 0:4], scalar1=r_[:, 0:1], scalar2=None, op0=ALU.is_equal), reads=[lg[p], r_], writes=[r_])
            kb.op("dve", lambda e: e.tensor_scalar(out=r_[:, 5:6], in0=r_[:, 0:1], scalar1=-1.0, scalar2=None, op0=ALU.mult), reads=[r_], writes=[r_])
            kb.op("act", lambda e: e.activation(out=ejunk[:], in_=lg[p][:, 0:4], func=AF.Exp, bias=r_[:, 5:6], accum_out=r_[:, 6:7]), reads=[lg[p], r_], writes=[ejunk, r_])
            kb.op("dve", lambda e: e.reciprocal(out=r_[:, 7:8], in_=r_[:, 6:7]), reads=[r_], writes=[r_])
            kb.op("dve", lambda e: e.tensor_scalar(out=r_[:, 8:12], in0=r_[:, 1:5], scalar1=-1.0, scalar2=BIG, op0=ALU.add, op1=ALU.mult), reads=[r_], writes=[r_])
            for g in range(4):
                kb.op("dve", lambda e: e.tensor_scalar(out=lem[p][:, g * 8:(g + 1) * 8], in0=lg[p][:, 4 + g * 8:4 + (g + 1) * 8], scalar1=r_[:, 8 + g:9 + g], scalar2=None, op0=ALU.add),
                      reads=[lg[p], r_], writes=[lem[p]])
            oh1 = OH[:, t, 0, :]
            oh2 = OH[:, t, 1, :]
            kb.op("dve", lambda e: e.tensor_reduce(out=r_[:, 12:13], in_=lem[p][:], axis=AX.X, op=ALU.max), reads=[lem[p]], writes=[r_])
            kb.op("dve", lambda e: e.tensor_scalar(out=oh1, in0=lem[p][:], scalar1=r_[:, 12:13], scalar2=None, op0=ALU.is_equal), reads=[lem[p], r_], writes=[OH])
            kb.op("dve", lambda e: e.scalar_tensor_tensor(out=lem2[p][:], in0=oh1, scalar=-BIG, in1=lem[p][:], op0=ALU.mult, op1=ALU.add), reads=[OH, lem[p]], writes=[lem2[p]])
            kb.op("dve", lambda e: e.tensor_reduce(out=r_[:, 13:14], in_=lem2[p][:], axis=AX.X, op=ALU.max), reads=[lem2[p]], writes=[r_])
            kb.op("dve", lambda e: e.tensor_scalar(out=oh2, in0=lem2[p][:], scalar1=r_[:, 13:14], scalar2=None, op0=ALU.is_equal), reads=[lem2[p], r_], writes=[OH])
            kb.op("dve", lambda e: e.tensor_tensor(out=r_[:, 14:15], in0=r_[:, 12:13], in1=r_[:, 13:14], op=ALU.subtract), reads=[r_], writes=[r_])
            kb.op("act", lambda e: e.activation(out=r_[:, 15:16], in_=r_[:, 14:15], func=AF.Sigmoid), reads=[r_], writes=[r_])
            kb.op("dve", lambda e: e.tensor_tensor(out=GT[:, t, 0:1], in0=r_[:, 15:16], in1=r_[:, 7:8], op=ALU.mult), reads=[r_], writes=[GT])
            kb.op("dve", lambda e: e.tensor_tensor(out=GT[:, t, 1:2], in0=r_[:, 7:8], in1=GT[:, t, 0:1], op=ALU.subtract), reads=[r_, GT], writes=[GT])
            if j == 1:
                kb.op("dve", lambda e: e.tensor_scalar(out=OH[:, t, :, :], in0=OH[:, t, :, :], scalar1=validt[:, 0:1], scalar2=None, op0=ALU.mult), reads=[OH, validt], writes=[OH])
            kb.op("dve", lambda e: e.tensor_tensor(out=cb_[p][:], in0=OH[:, t, 0, :], in1=OH[:, t, 1, :], op=ALU.add), reads=[OH], writes=[cb_[p]])
            pc = banks[5]
            kb.op("pe", lambda e: e.matmul(pc[:, 0:32], lhsT=ltri_b[:], rhs=cb_[p][:], start=True, stop=True), reads=[ltri_b, cb_[p]], writes=[pc])
            kb.op("dve", lambda e: e.tensor_tensor(out=rbase[p][:], in0=pc[:, 0:32], in1=Rbc[:], op=ALU.add), reads=[pc, Rbc], writes=[rbase[p]])
            pt_ = banks[6]
            kb.op("pe", lambda e: e.matmul(pt_[:, 0:32], lhsT=ones_b[:], rhs=cb_[p][:], start=True, stop=True), reads=[ones_b, cb_[p]], writes=[pt_])
            kb.op("dve", lambda e: e.tensor_tensor(out=Rbc[:], in0=pt_[:, 0:32], in1=Rbc[:], op=ALU.add), reads=[pt_, Rbc], writes=[Rbc])
            for k in range(2):
                kb.op("dve", lambda e: e.tensor_tensor(out=tmp32[p][:, k, :], in0=OH[:, t, k, :], in1=rbase[p][:], op=ALU.mult), reads=[OH, rbase[p]], writes=[tmp32[p]])
            kb.op("dve", lambda e: e.tensor_reduce(out=RK[:, t, :], in_=tmp32[p][:], axis=AX.X, op=ALU.add), reads=[tmp32[p]], writes=[RK])
        kb.barrier()
    pesA.close()
    pesD = ExitStack()

    cnt_i = kb.sb("cnt_i", [128, 32], I32, pesD)
    pcnt = kb.sb("pcnt", [128, 32], F32, pesD)
    pend = [kb.sb("pend%d" % i, [128, 32], F32, pesD) for i in range(2)]
    kb.op("dve", lambda e: e.tensor_scalar(out=pcnt[:], in0=Rbc[:], scalar1=255.0, scalar2=None, op0=ALU.add), reads=[Rbc], writes=[pcnt])
    kb.op("dve", lambda e: e.tensor_copy(out=cnt_i[:], in_=pcnt[:]), reads=[pcnt], writes=[cnt_i])
    kb.op("dve", lambda e: e.tensor_scalar(out=cnt_i[:], in0=cnt_i[:], scalar1=8, scalar2=8, op0=ALU.arith_shift_right, op1=ALU.logical_shift_left), reads=[cnt_i], writes=[cnt_i])
    kb.op("dve", lambda e: e.tensor_copy(out=pcnt[:], in_=cnt_i[:]), reads=[cnt_i], writes=[pcnt])
    kb.op("dve", lambda e: e.tensor_copy(out=pend[0][:], in_=pcnt[:]), reads=[pcnt], writes=[pend[0]])
    cur = 0
    for sft in (1, 2, 4, 8, 16):
        a, b = pend[cur], pend[1 - cur]
        kb.op("dve", lambda e: e.tensor_copy(out=b[:, 0:sft], in_=a[:, 0:sft]), reads=[a], writes=[b])
        kb.op("dve", lambda e: e.tensor_tensor(out=b[:, sft:32], in0=a[:, sft:32], in1=a[:, 0:32 - sft], op=ALU.add), reads=[a], writes=[b])
        cur = 1 - cur
    pendf = pend[cur]
    poff = kb.sb("poff", [128, 32], F32, pesD)
    kb.op("dve", lambda e: e.tensor_tensor(out=poff[:], in0=pendf[:], in1=pcnt[:], op=ALU.subtract), reads=[pendf, pcnt], writes=[poff])
    DEST = kb.sb("DEST", [128, NT, 2], F32, pesD)
    tmpd = kb.sb("tmpd", [128, NT * 2, 32], F32, pesD)
    for t in range(NT):
        for k in range(2):
            kb.op("dve", lambda e: e.tensor_tensor(out=tmpd[:, t * 2 + k, :], in0=OH[:, t, k, :], in1=poff[:], op=ALU.mult), reads=[OH, poff], writes=[tmpd])
    kb.op("dve", lambda e: e.tensor_reduce(out=DEST[:].rearrange("p t k -> p (t k)"), in_=tmpd[:], axis=AX.X, op=ALU.add), reads=[tmpd], writes=[DEST])
    kb.op("dve", lambda e: e.tensor_tensor(out=DEST[:], in0=DEST[:], in1=RK[:], op=ALU.add), reads=[DEST, RK], writes=[DEST])
    if L0:
        inval = kb.sb("inval", [128, 2], F32, pesD)
        kb.op("dve", lambda e: e.tensor_scalar(out=inval[:, 0:1], in0=validt[:], scalar1=-1.0, scalar2=-1.0, op0=ALU.add, op1=ALU.mult), reads=[validt], writes=[inval])
        kb.op("dve", lambda e: e.scalar_tensor_tensor(out=inval[:, 1:2], in0=iop[:], scalar=float(NROWS), in1=inval[:, 0:1], op0=ALU.add, op1=ALU.mult), reads=[iop, inval], writes=[inval])
        kb.op("dve", lambda e: e.tensor_scalar(out=DEST[:, NT - 1, :], in0=DEST[:, NT - 1, :], scalar1=validt[:, 0:1], scalar2=inval[:, 1:2], op0=ALU.mult, op1=ALU.add),
              reads=[DEST, validt, inval], writes=[DEST])
    kb.op("dve", lambda e: e.tensor_copy(out=DESTI[:], in_=DEST[:].rearrange("p t k -> p (t k)")), reads=[DEST], writes=[DESTI])
    eb = kb.sb("eb", [128, NB], F32, pesD)
    kb.op("pool", lambda e: e.memset(eb[:], 0.0), writes=[eb])
    for ee in range(32):
        kb.op("dve", lambda e: e.scalar_tensor_tensor(out=eb[:], in0=blkst[:], scalar=pendf[:, ee:ee + 1], in1=eb[:], op0=ALU.is_ge, op1=ALU.add), reads=[blkst, pendf, eb], writes=[eb])
    kb.op("dve", lambda e: e.tensor_scalar(out=eb[:], in0=eb[:], scalar1=31.0, scalar2=128.0, op0=ALU.min, op1=ALU.mult), reads=[eb], writes=[eb])
    flag = kb.sb("flag", [128, NB], F32, pesD)
    kb.op("pool", lambda e: e.memset(flag[:], 1.0), writes=[flag])
    kb.op("dve", lambda e: e.tensor_tensor(out=flag[:, 1:NB], in0=eb[:, 1:NB], in1=eb[:, 0:NB - 1], op=ALU.not_equal), reads=[eb], writes=[flag])
    kb.op("dve", lambda e: e.tensor_scalar(out=flag[:], in0=flag[:], scalar1=-1.0, scalar2=-1.0e9, op0=ALU.add, op1=ALU.mult), reads=[flag], writes=[flag])
    kb.op("dve", lambda e: e.tensor_scalar(out=eb[:], in0=eb[:], scalar1=iop[:, 0:1], scalar2=None, op0=ALU.add), reads=[eb, iop], writes=[eb])
    kb.op("dve", lambda e: e.tensor_tensor(out=eb[:], in0=eb[:], in1=flag[:], op=ALU.add), reads=[eb, flag], writes=[eb])
    kb.op("dve", lambda e: e.tensor_copy(out=WIDX[:], in_=eb[:]), reads=[eb], writes=[WIDX])

    srow = dbl("srow", [128, 1024], BF16, 3, pesD)
    for t in range(NT):
        sr = srow[t % 3]
        kb.load("sp", sr, sr[:], nl2d.h[t * 128:(t + 1) * 128, :], nl2d)
        for k in range(2):
            kb.dma("pool", lambda e: e.indirect_dma_start(out=xs.h[:, :], out_offset=bass.IndirectOffsetOnAxis(ap=DESTI[:, t * 2 + k:t * 2 + k + 1], axis=0),
                                                          in_=sr[:], in_offset=None), reads=[DESTI, sr], writes=[xs])
    kb.barrier()
    pesD.close()
    pesE = ExitStack()

    w1f = dbl("w1f", [128, 4096], F32, 1, pesE) * 2
    w3f = dbl("w3f", [128, 4096], F32, 1, pesE) * 2
    w2f = dbl("w2f", [128, 4096], F32, 1, pesE) * 2
    w1b = dbl("w1b", [128, 8, 512], BF16, 2, pesE)
    w3b = dbl("w3b", [128, 8, 512], BF16, 2, pesE)
    w2b = dbl("w2b", [128, 4, 1024], BF16, 2, pesE)
    xr = dbl("xr", [128, 2, 1024], BF16, 2, pesE)
    xsT = dbl("xsT", [128, 8, 256], BF16, 2, pesE)
    sl = dbl("sl", [128, 256], F32, 2, pesE)
    hhT = dbl("hhT", [128, 4, 256], BF16, 2, pesE)
    yo = dbl("yo", [128, 1024], F32, 2, pesE)
    wreg = kb.nc.gpsimd.to_reg(4095)
    def fetch_w(b):
        p = b % 2
        for (tab, wf) in ((w1t, w1f[p]), (w3t, w3f[p]), (w2t, w2f[p])):
            kb.dma("pool", lambda e: e.indirect_dma_start(out=wf[:], out_offset=None, in_=tab.h[:, :], in_offset=bass.IndirectOffsetOnAxis(ap=WIDX[:, b:b + 1], axis=0),
                                                          bounds_check=wreg, oob_is_err=False), reads=[WIDX, tab], writes=[wf])
        kb.op("act", lambda e: e.copy(out=w1b[p][:].rearrange("p a b -> p (a b)"), in_=w1f[p][:]), reads=[w1f[p]], writes=[w1b[p]])
        kb.op("dve", lambda e: e.tensor_copy(out=w3b[p][:].rearrange("p a b -> p (a b)"), in_=w3f[p][:]), reads=[w3f[p]], writes=[w3b[p]])
        kb.op("act", lambda e: e.copy(out=w2b[p][:].rearrange("p a b -> p (a b)")[:, 0:2048], in_=w2f[p][:, 0:2048]), reads=[w2f[p]], writes=[w2b[p]])
        kb.op("dve", lambda e: e.tensor_copy(out=w2b[p][:].rearrange("p a b -> p (a b)")[:, 2048:4096], in_=w2f[p][:, 2048:4096]), reads=[w2f[p]], writes=[w2b[p]])

    fetch_w(0)
    for b in range(NB):
        p = b % 2
        if b + 1 < NB:
            fetch_w(b + 1)
        kb.load("sp", xr[p], xr[p][:], xs.h[b * 256:(b + 1) * 256, :].rearrange("(a p) n -> p a n", p=128), xs)
        for sub in range(2):
            pT = banks[6]
            for kc in range(8):
                kb.op("pe", lambda e: e.transpose(out=bfv(pT)[:, kc * 128:(kc + 1) * 128], in_=xr[p][:, sub, kc * 128:(kc + 1) * 128], identity=identb[:]),
                      reads=[xr[p], identb], writes=[pT])
            kb.op("act", lambda e: e.copy(out=xsT[p][:, :, sub * 128:(sub + 1) * 128], in_=bfv(pT)[:, 0:1024].rearrange("p (a b) -> p a b", a=8)), reads=[pT], writes=[xsT[p]])
        for fc in range(4):
            ph1 = banks[0 + fc // 2]
            ph3 = banks[2 + fc // 2]
            c0 = (fc % 2) * 256
            for kc in range(8):
                kb.op("pe", lambda e: e.matmul(ph1[:, c0:c0 + 256], lhsT=w1b[p][:, kc, fc * 128:(fc + 1) * 128], rhs=xsT[p][:, kc, :], start=(kc == 0), stop=(kc == 7)),
                      reads=[w1b[p], xsT[p]], writes=[ph1])
            for kc in range(8):
                kb.op("pe", lambda e: e.matmul(ph3[:, c0:c0 + 256], lhsT=w3b[p][:, kc, fc * 128:(fc + 1) * 128], rhs=xsT[p][:, kc, :], start=(kc == 0), stop=(kc == 7)),
                      reads=[w3b[p], xsT[p]], writes=[ph3])
            s_ = sl[fc % 2]
            kb.op("act", lambda e: e.activation(out=s_[:], in_=ph1[:, c0:c0 + 256], func=AF.Silu), reads=[ph1], writes=[s_])
            kb.op("dve", lambda e: e.tensor_tensor(out=hhT[p][:, fc, :], in0=ph3[:, c0:c0 + 256], in1=s_[:], op=ALU.mult), reads=[ph3, s_], writes=[hhT[p]])
        for sub in range(2):
            y_ = yo[sub]
            for half in range(2):
                py = banks[4 + half]
                for fc in range(4):
                    kb.op("pe", lambda e: e.matmul(py[:, :], lhsT=hhT[p][:, fc, sub * 128:(sub + 1) * 128], rhs=w2b[p][:, fc, half * 512:(half + 1) * 512], start=(fc == 0), stop=(fc == 3)),
                          reads=[hhT[p], w2b[p]], writes=[py])
                if half == 0:
                    kb.op("act", lambda e: e.copy(out=y_[:, 0:512], in_=py[:, :]), reads=[py], writes=[y_])
                else:
                    kb.op("dve", lambda e: e.tensor_copy(out=y_[:, 512:1024], in_=py[:, :]), reads=[py], writes=[y_])
            kb.store("sp", ys, ys.h[b * 256 + sub * 128:b * 256 + (sub + 1) * 128, :], y_, y_[:])
    kb.barrier()
    pesE.close()
    pesF = ExitStack()

    y1 = dbl("y1", [128, 1024], F32, 2, pesF)
    y2 = dbl("y2", [128, 1024], F32, 2, pesF)
    hm = dbl("hm", [128, 1024], F32, 2, pesF)
    for t in range(NT):
        p = t % 2
        j = 1 if (L0 and t == NT - 1) else 0
        if j == 1:
            kb.op("pool", lambda e: e.memset(y1[p][:], 0.0), writes=[y1[p]])
            kb.op("pool", lambda e: e.memset(y2[p][:], 0.0), writes=[y2[p]])
        for k, yk in ((0, y1[p]), (1, y2[p])):
            kb.dma("pool", lambda e: e.indirect_dma_start(out=yk[:], out_offset=None, in_=ys.h[:, :], in_offset=bass.IndirectOffsetOnAxis(ap=DESTI[:, t * 2 + k:t * 2 + k + 1], axis=0)), reads=[DESTI, ys], writes=[yk])
        kb.load("sp", hm[p], hm[p][:], hlm.h[t * 128:(t + 1) * 128, :], hlm)
        kb.op("dve", lambda e: e.tensor_scalar(out=y1[p][:], in0=y1[p][:], scalar1=GT[:, t, 0:1], scalar2=None, op0=ALU.mult), reads=[y1[p], GT], writes=[y1[p]])
        kb.op("dve", lambda e: e.scalar_tensor_tensor(out=y1[p][:], in0=y2[p][:], scalar=GT[:, t, 1:2], in1=y1[p][:], op0=ALU.mult, op1=ALU.add), reads=[y2[p], GT, y1[p]], writes=[y1[p]])
        kb.op("dve", lambda e: e.tensor_tensor(out=y1[p][:], in0=y1[p][:], in1=gate_bc[j][1][:], op=ALU.mult), reads=[y1[p], gate_bc[j][1]], writes=[y1[p]])
        kb.op("dve", lambda e: e.tensor_tensor(out=hm[p][:], in0=hm[p][:], in1=y1[p][:], op=ALU.add), reads=[hm[p], y1[p]], writes=[hm[p]])
        kb.store("sp", hout, hout.h[t * 128:(t + 1) * 128, :], hm[p], hm[p][:])
    print("lb%d instructions:" % layer, kb.n_ins, "sems:", len(kb.sems))
    pesF.close()
    kb.end_stage()


def fop(v, n):
    return np.ascontiguousarray(np.asarray(v, np.float32).reshape(n, 128).T)


def moe_tables(inp, l):
    w1 = np.ascontiguousarray(inp["moe_w1"][l].reshape(32, 8, 128, 512).transpose(0, 2, 1, 3).reshape(4096, 4096))
    w3 = np.ascontiguousarray(inp["moe_w3"][l].reshape(32, 8, 128, 512).transpose(0, 2, 1, 3).reshape(4096, 4096))
    w2 = np.ascontiguousarray(inp["moe_w2"][l].reshape(32, 4, 128, 1024).transpose(0, 2, 1, 3).reshape(4096, 4096))
    return w1, w3, w2


def host_b(layer, inp):
    L0 = layer == 0
    l = layer
    w1, w3, w2 = moe_tables(inp, l)
    rw = np.ascontiguousarray(np.concatenate([inp["rg_w"][l], inp["re_w"][l]], axis=1).astype(np.float32))
    rb = np.concatenate([inp["rg_b"][l], inp["re_b"][l]]).astype(np.float32)
    adabr = np.concatenate([inp["ada_b"][l][2048:3072], inp["ada_b"][l][5120:6144]]).astype(np.float32)
    gfop = np.ascontiguousarray(np.concatenate([fop(inp["norm1_g"][l], 8), fop(inp["norm2_g"][l], 8)], axis=1))
    wout = np.ascontiguousarray(inp["ab_w_out"][0] if L0 else inp["gla_w_out"][0])
    hin_lat, hin_ctx = inp["x"], inp["ctx"]
    maps = []
    pp = np.arange(128, dtype=np.int32)
    for b in range(2):
        sv = np.stack([inp["c"][b], inp["c_ctx"]], -1).reshape(8, 128, 2).transpose(1, 0, 2).reshape(128, 16).astype(np.float32)
        for jq in range(4):
            r0, r1 = jq * 4096, (jq + 1) * 4096
            m = {"svec": np.ascontiguousarray(sv), "adaw": np.ascontiguousarray(inp["ada_w"][l]), "adabf": fop(inp["ada_b"][l], 48), "adabr": adabr, "gfop": gfop,
                 "wout": wout, "rw": rw, "rb": rb, "w1t": w1, "w3t": w3, "w2t": w2}
            if L0:
                cpad = np.zeros((128, 1024), np.float32)
                cpad[:64] = hin_ctx[b, 64 * jq:64 * jq + 64]
                m["hin"] = np.ascontiguousarray(np.concatenate([hin_lat[b, r0:r1], cpad], 0))
                m["mixidx"] = np.ascontiguousarray(np.stack([np.array([ag_row(jq * 128 + int(p_), h, 64, 512) for p_ in pp]) for h in range(4)], axis=1).astype(np.int32))
                xh = np.zeros((128, 1024), np.float32)
                if jq > 0:
                    xh[0:15] = hin_lat[b, r0 - 15:r0]
                if jq < 3:
                    xh[15:30] = hin_lat[b, r1:r1 + 15]
                m["xhalo"] = xh
                ch = np.zeros((128, 1024), np.float32)
                for r in range(94):
                    pos = 64 * jq - 15 + r
                    if 0 <= pos < 256:
                        ch[r] = hin_ctx[b, pos]
                m["cxh"] = ch
                m["edge"] = np.array([1.0 if jq > 0 else 0.0, 1.0 if jq < 3 else 0.0], np.float32)
                v = np.zeros((128, 1), np.float32)
                v[:64] = 1
                m["valid"] = v
                m["win"] = np.ascontiguousarray(inp["ab_w_in"][0][:, 0:1024])
                m["cw"] = np.ascontiguousarray(inp["conv_w"][0].T.reshape(4, 128, 31).transpose(1, 0, 2).reshape(128, 124))
                m["cvec"] = np.ascontiguousarray(np.concatenate([fop(inp["conv_b"][0], 4), fop(inp["conv_ln_g"][0], 4), fop(inp["conv_ln_b"][0], 4)], axis=1))
            else:
                m["mixidx"] = np.ascontiguousarray(np.stack([np.array([ag_row(jq * 256 + c2 * 128 + int(p_), h, 128, 1024) for p_ in pp]) for h in range(4) for c2 in range(2)], axis=1).astype(np.int32))
                m["valid"] = np.ones((128, 1), np.float32)
            maps.append(m)
    return maps


def gather_b(layer, results):
    L0 = layer == 0
    hl = np.zeros((2, 16384, 1024), np.float32)
    hc = np.zeros((2, 256, 1024), np.float32) if L0 else None
    for b in range(2):
        for jq in range(4):
            o = results[b * 4 + jq]["hout"]
            hl[b, jq * 4096:(jq + 1) * 4096] = o[:4096]
            if L0:
                hc[b, 64 * jq:64 * jq + 64] = o[4096:4160]
    return hl, hc


def build_l1a(kb, banks, x2out, x3in, n_lat_tiles=128, do_scan=True):
    kb.begin_stage("a1_")
    svec = kb.dram("svec", [128, 16], F32, "ExternalInput")
    adaw = kb.dram("adaw", [1024, 2048], F32, "ExternalInput")
    adab = kb.dram("adab", [128, 16], F32, "ExternalInput")
    g1 = kb.dram("g1", [128, 8], F32, "ExternalInput")
    w = kb.dram("w", [1024, 768], F32, "ExternalInput")
    waT = kb.dram("waT", [2, 16, 1024], F32, "ExternalInput")
    wa2 = kb.dram("wa2", [2, 16, 128], F32, "ExternalInput")
    small = kb.dram("small", [512], F32, "ExternalInput")
    proj = kb.dram("proj", [S + LC, 1024], F32)
    of_d = kb.dram("of_d", [S, 256], F32)

    def bfv(t):
        return t[:].bitcast(BF16)

    def dbl(name, shape, dt, n=2, es=None):
        return [kb.sb("%s%d" % (name, i), shape, dt, es) for i in range(n)]

    identb = kb.identity("identb", BF16)
    smallb = kb.sb("smallb", [128, 512], F32)
    kb.load("sp", smallb, smallb[:], small.h.partition_broadcast(128), small)
    zer = kb.sb("zer", [128, 128], F32)
    kb.op("pool", lambda e: e.memset(zer[:], 0.0), writes=[zer])

    def rstd_chain(stt, c_in, c_tmp, c_out, n, inv_n):
        kb.op("dve", lambda e: e.tensor_scalar(out=stt[:, c_tmp:c_tmp + n], in0=stt[:, c_in:c_in + n], scalar1=inv_n, scalar2=EPS, op0=ALU.mult, op1=ALU.add),
              reads=[stt], writes=[stt])
        kb.op("act", lambda e: e.activation(out=stt[:, c_tmp:c_tmp + n], in_=stt[:, c_tmp:c_tmp + n], func=AF.Sqrt), reads=[stt], writes=[stt])
        kb.op("dve", lambda e: e.reciprocal(out=stt[:, c_out:c_out + n], in_=stt[:, c_tmp:c_tmp + n]), reads=[stt], writes=[stt])

    wq = [kb.sb("wq%d" % j, [128, 8, 1024], BF16) for j in range(2)]
    bias = [kb.sb("bias%d" % j, [128, 1024], F32) for j in range(2)]
    pesA = ExitStack()
    s_sb = kb.sb("s_sb", [128, 16], F32, pesA)
    kb.load("sp", s_sb, s_sb[:], svec.h, svec)
    kb.op("act", lambda e: e.activation(out=s_sb[:], in_=s_sb[:], func=AF.Silu), reads=[s_sb], writes=[s_sb])
    adab_sb = kb.sb("adab_sb", [128, 16], F32, pesA)
    kb.load("sp", adab_sb, adab_sb[:], adab.h, adab)
    g1_sb = kb.sb("g1_sb", [128, 8], F32, pesA)
    kb.load("sp", g1_sb, g1_sb[:], g1.h, g1)
    mod = kb.sb("mod", [128, 16, 2], F32, pesA)
    gs = kb.sb("gs", [128, 8, 2], F32, pesA)
    w_sb = kb.sb("w_sb", [128, 8, 1024], F32, pesA)
    shiftbc = kb.sb("shiftbc", [128, 8, 128], F32, pesA)
    pm = banks[0]
    with ExitStack() as pes:
        adaw_sb = kb.sb("adaw_sb", [128, 8, 512], F32, pes)
        for v in range(4):
            kb.load("sp", adaw_sb, adaw_sb[:], adaw.h[:, v * 512:(v + 1) * 512].rearrange("(kc p) n -> p kc n", p=128), adaw)
            for oc in range(4):
                g = v * 4 + oc
                for kc in range(8):
                    kb.op("pe", lambda e: e.matmul(pm[:, g * 2:g * 2 + 2], lhsT=adaw_sb[:, kc, oc * 128:(oc + 1) * 128], rhs=s_sb[:, kc * 2:kc * 2 + 2],
                                                  start=(kc == 0), stop=(kc == 7)), reads=[adaw_sb, s_sb], writes=[pm])
        pm3 = pm[:, 0:32].rearrange("p (g j) -> p g j", j=2)
        for j in range(2):
            kb.op("dve", lambda e: e.tensor_tensor(out=mod[:, :, j], in0=pm3[:, :, j], in1=adab_sb[:], op=ALU.add), reads=[pm, adab_sb], writes=[mod])
            kb.op("dve", lambda e: e.scalar_tensor_tensor(out=gs[:, :, j], in0=mod[:, 8:16, j], scalar=1.0, in1=g1_sb[:], op0=ALU.add, op1=ALU.mult),
                  reads=[mod, g1_sb], writes=[gs])
        kb.load("sp", w_sb, w_sb[:, :, 0:768], w.h.rearrange("(kc p) n -> p kc n", p=128), w)
        waT_sb = [kb.sb("waT_sb%d" % d, [32, 1024], F32, pes) for d in range(2)]
        wa2_sb = [kb.sb("wa2_sb%d" % d, [32, 128], F32, pes) for d in range(2)]
        for d in range(2):
            kb.op("pool", lambda e: e.memset(waT_sb[d][:], 0.0), writes=[waT_sb[d]])
            kb.op("pool", lambda e: e.memset(wa2_sb[d][:], 0.0), writes=[wa2_sb[d]])
            kb.load("sp", waT_sb[d], waT_sb[d][0:16, :], waT.h[d], waT)
            kb.load("sp", wa2_sb[d], wa2_sb[d][0:16, :], wa2.h[d], wa2)
            for kc in range(8):
                pz = banks[1]
                kb.op("pe", lambda e: e.matmul(pz[:, 0:128], lhsT=waT_sb[d][:, kc * 128:(kc + 1) * 128], rhs=wa2_sb[d][:], start=True, stop=True),
                      reads=[waT_sb[d], wa2_sb[d]], writes=[pz])
                kb.op("dve", lambda e: e.tensor_copy(out=w_sb[:, kc, 768 + d * 128:768 + (d + 1) * 128], in_=pz[:, 0:128]), reads=[pz], writes=[w_sb])
        for j in range(2):
            for kc in range(8):
                kb.op("dve", lambda e: e.tensor_scalar(out=wq[j][:, kc, :], in0=w_sb[:, kc, :], scalar1=gs[:, kc, j:j + 1], scalar2=None, op0=ALU.mult),
                      reads=[w_sb, gs], writes=[wq[j]])
                kb.op("dve", lambda e: e.tensor_scalar(out=shiftbc[:, kc, :], in0=zer[:], scalar1=mod[:, kc, j:j + 1], scalar2=None, op0=ALU.add),
                      reads=[zer, mod], writes=[shiftbc])
            for half in range(2):
                pb = banks[2 + half]
                for kc in range(8):
                    kb.op("pe", lambda e: e.matmul(pb[:, :], lhsT=shiftbc[:, kc, :], rhs=w_sb[:, kc, half * 512:(half + 1) * 512], start=(kc == 0), stop=(kc == 7)),
                          reads=[shiftbc, w_sb], writes=[pb])
                kb.op("dve", lambda e: e.tensor_copy(out=bias[j][:, half * 512:(half + 1) * 512], in_=pb[:, :]), reads=[pb], writes=[bias[j]])
            kb.op("dve", lambda e: e.tensor_tensor(out=bias[j][:, 768:1024], in0=bias[j][:, 768:1024], in1=smallb[:, 0:256], op=ALU.add), reads=[bias[j], smallb], writes=[bias[j]])
        kb.barrier()
    kb.barrier()
    pesA.close()

    pesB = ExitStack()
    xt = dbl("xt", [128, 1024], F32, 4, pesB)
    junk = kb.sb("junk", [128, 1024], BF16, pesB)
    st1 = dbl("st1", [128, 4], F32, 2, pesB)
    xn = dbl("xn", [128, 1024], BF16, 2, pesB)
    xnT = dbl("xnT", [128, 1024], BF16, 2, pesB)
    pj = dbl("pj", [128, 1024], F32, 2, pesB)
    ez = dbl("ez", [128, 256], F32, 2, pesB)
    one_col = kb.sb("one_col", [128, 1], F32, pesB)
    kb.op("pool", lambda e: e.memset(one_col[:], 1.0), writes=[one_col])

    tile_args = []

    def do_load(i):
        _, _, row0, is_ctx, _ = tile_args[i]
        xb = xt[i % 4]
        if is_ctx:
            c = row0 // 128
            for hf in range(2):
                r = ag_row(4096, 2 * c + hf, 256, 4224)
                kb.load("sp", xb, xb[hf * 64:(hf + 1) * 64, :], x2out.h[r:r + 64, :], x2out)
        else:
            t = row0 // 128
            r = ag_row((t % 32) * 128, t // 32, 256, 4224)
            kb.load("sp", xb, xb[:], x2out.h[r:r + 128, :], x2out)

    def proj_tile(i, src, row0, is_ctx, drow):
        p = i % 2
        j = 1 if is_ctx else 0
        if i + 2 < len(tile_args):
            do_load(i + 2)
        yield
        kb.op("act", lambda e: e.activation(out=junk[:], in_=xt[i % 4][:], func=AF.Square, accum_out=st1[p][:, 0:1]), reads=[xt[i % 4]], writes=[junk, st1[p]])
        yield
        rstd_chain(st1[p], 0, 1, 2, 1, 1.0 / 1024)
        kb.op("act", lambda e: e.activation(out=xn[p][:], in_=xt[i % 4][:], func=AF.Copy, scale=st1[p][:, 2:3]), reads=[xt[i % 4], st1[p]], writes=[xn[p]])
        yield
        psT = banks[p]
        for kc in range(8):
            kb.op("pe", lambda e: e.transpose(out=bfv(psT)[:, kc * 128:(kc + 1) * 128], in_=xn[p][:, kc * 128:(kc + 1) * 128], identity=identb[:]),
                  reads=[xn[p], identb], writes=[psT])
            yield
        kb.op("dve", lambda e: e.tensor_copy(out=xnT[p][:], in_=bfv(psT)[:, 0:1024]), reads=[psT], writes=[xnT[p]])
        yield
        for half in range(2):
            pp = banks[2 + 2 * p + half]
            for kc in range(8):
                kb.op("pe", lambda e: e.matmul(pp[:, :], lhsT=xnT[p][:, kc * 128:(kc + 1) * 128], rhs=wq[j][:, kc, half * 512:(half + 1) * 512], start=(kc == 0), stop=(kc == 7)),
                      reads=[xnT[p], wq[j]], writes=[pp])
                yield
            kb.op("dve", lambda e: e.tensor_tensor(out=pj[p][:, half * 512:(half + 1) * 512], in0=pp[:, :], in1=bias[j][:, half * 512:(half + 1) * 512], op=ALU.add),
                  reads=[pp, bias[j]], writes=[pj[p]])
            yield
        kb.op("act", lambda e: e.activation(out=ez[p][:], in_=pj[p][:, 768:1024], func=AF.Exp, scale=-1.0), reads=[pj[p]], writes=[ez[p]])
        yield
        kb.op("act", lambda e: e.activation(out=pj[p][:, 768:1024], in_=ez[p][:], func=AF.Ln, bias=one_col[:, 0:1]), reads=[ez[p], one_col], writes=[pj[p]])
        yield
        kb.store("sp", proj, proj.h[drow:drow + 128, :], pj[p], pj[p][:])
        yield

    i = 0
    for c in range(2):
        tile_args.append((i, None, c * 128, True, S + c * 128))
        i += 1
    for t in range(n_lat_tiles):
        tile_args.append((i, None, t * 128, False, t * 128))
        i += 1
    do_load(0)
    do_load(1)
    gens = [proj_tile(*a_) for a_ in tile_args]
    interleave(gens, 2)
    kb.barrier()
    pesB.close()

    mask = []
    for d in range(2):
        mf = kb.sb("mask%d" % d, [128, 128], F32)
        kb.op("pool", lambda e: e.memset(mf[:], 1.0), writes=[mf])
        if d == 0:
            kb.op("pool", lambda e: e.affine_select(out=mf[:], in_=mf[:], pattern=[[1, 128]], compare_op=ALU.is_ge, fill=0.0, base=0, channel_multiplier=-1), reads=[mf], writes=[mf])
        else:
            kb.op("pool", lambda e: e.affine_select(out=mf[:], in_=mf[:], pattern=[[-1, 128]], compare_op=ALU.is_ge, fill=0.0, base=0, channel_multiplier=1), reads=[mf], writes=[mf])
        mask.append(mf)
    LS = -1.0 / 16
    maskS = []
    for d in range(2):
        ms_ = kb.sb("maskS%d" % d, [128, 128], F32)
        kb.op("dve", lambda e: e.tensor_scalar(out=ms_[:], in0=mask[d][:], scalar1=LS, scalar2=None, op0=ALU.mult), reads=[mask[d]], writes=[ms_])
        maskS.append(ms_)
    ones_f = kb.sb("ones_f", [128, 128], F32)
    kb.op("pool", lambda e: e.memset(ones_f[:], -1.0 / 16), writes=[ones_f])
    Sst = kb.sb("Sst", [128, 256], F32)
    Sb = dbl("Sb", [128, 256], BF16)
    pt = dbl("pt", [128, 1024], F32, 3)
    bc = dbl("bc", [128, 128], F32)
    eb = dbl("eb", [128, 128], F32)
    enb = dbl("enb", [128, 128], F32)
    dlt = dbl("dlt", [128, 128], F32)
    dec = dbl("dec", [128, 1], F32)
    qt = dbl("qt", [128, 128], BF16)
    ktl = dbl("ktl", [128, 128], BF16)
    kh = dbl("kh", [128, 128], BF16)
    vb = dbl("vb", [128, 256], BF16)
    qkT = dbl("qkT", [128, 256], BF16)
    attm = dbl("attm", [128, 128], BF16)
    ofs = dbl("ofs", [128, 256], F32)
    osum = dbl("osum", [128, 256], F32)
    fst = dbl("fst", [128, 4], F32)
    sg = dbl("sg", [128, 256], F32)
    ogb = dbl("ogb", [128, 256], BF16)
    ogT_sb = dbl("ogT_sb", [128, 2, 128], BF16)
    QS = 128.0 ** -0.5
    step = [0]

    def gla_prep(c, row, d):
        p = c % 2
        B = banks[4 * p:4 * p + 4]
        t_ = pt[c % 3]
        kb.load("sp", t_, t_[:], proj.h[row:row + 128, :], proj)
        yield
        la = t_[:, 768 + d * 128:768 + (d + 1) * 128]
        kb.op("pe", lambda e: e.matmul(B[0][:, 0:128], lhsT=maskS[d][:], rhs=la, start=True, stop=True), reads=[maskS[d], t_], writes=[B[0]])
        yield
        kb.op("pe", lambda e: e.matmul(B[0][:, 128:256], lhsT=ones_f[:], rhs=la, start=True, stop=True), reads=[ones_f, t_], writes=[B[0]])
        yield
        kb.op("pe", lambda e: e.matmul(B[0][:, 256:384], lhsT=la, rhs=ones_f[:], start=True, stop=True), reads=[ones_f, t_], writes=[B[0]])
        yield
        kb.op("act", lambda e: e.copy(out=bc[p][:], in_=B[0][:, 0:128]), reads=[B[0]], writes=[bc[p]])
        yield
        kb.op("act", lambda e: e.activation(out=eb[p][:], in_=B[0][:, 0:128], func=AF.Exp), reads=[B[0]], writes=[eb[p]])
        yield
        kb.op("act", lambda e: e.activation(out=enb[p][:], in_=B[0][:, 0:128], func=AF.Exp, scale=-1.0), reads=[B[0]], writes=[enb[p]])
        yield
        kb.op("dve", lambda e: e.tensor_tensor(out=dlt[p][:], in0=B[0][:, 128:256], in1=bc[p][:], op=ALU.subtract), reads=[B[0], bc[p]], writes=[dlt[p]])
        yield
        kb.op("act", lambda e: e.activation(out=dlt[p][:], in_=dlt[p][:], func=AF.Exp), reads=[dlt[p]], writes=[dlt[p]])
        yield
        kb.op("act", lambda e: e.activation(out=dec[p][:], in_=B[0][:, 256:257], func=AF.Exp), reads=[B[0]], writes=[dec[p]])
        yield
        kb.op("dve", lambda e: e.scalar_tensor_tensor(out=qt[p][:], in0=t_[:, 0:128], scalar=QS, in1=eb[p][:], op0=ALU.mult, op1=ALU.mult), reads=[t_, eb[p]], writes=[qt[p]])
        yield
        kb.op("dve", lambda e: e.tensor_tensor(out=ktl[p][:], in0=t_[:, 128:256], in1=enb[p][:], op=ALU.mult), reads=[t_, enb[p]], writes=[ktl[p]])
        yield
        kb.op("pool", lambda e: e.tensor_tensor(out=kh[p][:], in0=t_[:, 128:256], in1=dlt[p][:], op=ALU.mult), reads=[t_, dlt[p]], writes=[kh[p]])
        yield
        kb.op("pool", lambda e: e.tensor_copy(out=vb[p][:], in_=t_[:, 256:512]), reads=[t_], writes=[vb[p]])
        yield
        kb.op("pe", lambda e: e.transpose(out=bfv(B[1])[:, 0:128], in_=qt[p][:], identity=identb[:]), reads=[qt[p], identb], writes=[B[1]])
        yield
        kb.op("pe", lambda e: e.transpose(out=bfv(B[1])[:, 128:256], in_=ktl[p][:], identity=identb[:]), reads=[ktl[p], identb], writes=[B[1]])
        yield
        kb.op("act", lambda e: e.copy(out=qkT[p][:], in_=bfv(B[1])[:, 0:256]), reads=[B[1]], writes=[qkT[p]])
        yield
        kb.op("pe", lambda e: e.matmul(B[2][:, 0:128], lhsT=qkT[p][:, 128:256], rhs=qkT[p][:, 0:128], start=True, stop=True), reads=[qkT[p]], writes=[B[2]])
        yield
        kb.op("dve", lambda e: e.tensor_tensor(out=attm[p][:], in0=B[2][:, 0:128], in1=mask[d][:], op=ALU.mult), reads=[B[2], mask[d]], writes=[attm[p]])
        yield

    def gla_fin(c, d, out_mode, out_row):
        p = c % 2
        B = banks[4 * p:4 * p + 4]
        t_ = pt[c % 3]
        sb_cur = Sb[c % 2]
        sb_next = Sb[(c + 1) % 2]
        if out_mode is not None:
            kb.op("pe", lambda e: e.matmul(B[3][:, 0:256], lhsT=qkT[p][:, 0:128], rhs=sb_cur[:], start=True, stop=False), reads=[qkT[p], sb_cur], writes=[B[3]])
            yield
            kb.op("pe", lambda e: e.matmul(B[3][:, 0:256], lhsT=attm[p][:], rhs=vb[p][:], start=False, stop=True), reads=[attm[p], vb[p]], writes=[B[3]])
            yield
        kb.op("pe", lambda e: e.matmul(B[2][:, 128:384], lhsT=kh[p][:], rhs=vb[p][:], start=True, stop=True), reads=[kh[p], vb[p]], writes=[B[2]])
        yield
        kb.op("dve", lambda e: e.scalar_tensor_tensor(out=Sst[:], in0=Sst[:], scalar=dec[p][:, 0:1], in1=B[2][:, 128:384], op0=ALU.mult, op1=ALU.add),
              reads=[Sst, dec[p], B[2]], writes=[Sst])
        yield
        kb.op("act", lambda e: e.copy(out=sb_next[:], in_=Sst[:]), reads=[Sst], writes=[sb_next])
        yield
        if out_mode == "store":
            kb.op("act", lambda e: e.copy(out=ofs[p][:], in_=B[3][:, 0:256]), reads=[B[3]], writes=[ofs[p]])
            yield
            kb.store("sp", of_d, of_d.h[out_row:out_row + 128, :], ofs[p], ofs[p][:])
            yield
        elif out_mode == "final":
            kb.load("sp", ofs[p], ofs[p][:], of_d.h[out_row:out_row + 128, :], of_d)
            yield
            kb.op("dve", lambda e: e.tensor_tensor(out=osum[p][:], in0=B[3][:, 0:256], in1=ofs[p][:], op=ALU.add), reads=[B[3], ofs[p]], writes=[osum[p]])
            yield
            kb.op("act", lambda e: e.activation(out=sg[p][:], in_=osum[p][:], func=AF.Square, accum_out=fst[p][:, 0:1]), reads=[osum[p]], writes=[sg[p], fst[p]])
            yield
            rstd_chain(fst[p], 0, 1, 2, 1, 1.0 / 256)
            kb.op("dve", lambda e: e.scalar_tensor_tensor(out=osum[p][:], in0=osum[p][:], scalar=fst[p][:, 2:3], in1=smallb[:, 256:512], op0=ALU.mult, op1=ALU.mult),
                  reads=[osum[p], fst[p], smallb], writes=[osum[p]])
            yield
            kb.op("act", lambda e: e.activation(out=sg[p][:], in_=t_[:, 512:768], func=AF.Silu), reads=[t_], writes=[sg[p]])
            yield
            kb.op("dve", lambda e: e.tensor_tensor(out=ogb[p][:], in0=osum[p][:], in1=sg[p][:], op=ALU.mult), reads=[osum[p], sg[p]], writes=[ogb[p]])
            yield
            for hh in range(2):
                kb.op("pe", lambda e: e.transpose(out=bfv(B[1])[:, 256 + hh * 128:256 + (hh + 1) * 128], in_=ogb[p][:, hh * 128:(hh + 1) * 128], identity=identb[:]),
                      reads=[ogb[p], identb], writes=[B[1]])
                yield
            kb.op("act", lambda e: e.copy(out=ogT_sb[p][:].rearrange("p a b -> p (a b)"), in_=bfv(B[1])[:, 256:512]), reads=[B[1]], writes=[ogT_sb[p]])
            yield
            tq_, tc_ = (out_row // 128) // 32, ((out_row // 128) % 32) * 128
            kb.store("sp", x3in, x3in.h[tq_ * 256:(tq_ + 1) * 256, tc_:tc_ + 128].rearrange("(a p) n -> p a n", p=128), ogT_sb[p], ogT_sb[p][:])
            yield

    def reset_state(c):
        kb.op("pool", lambda e: e.memset(Sst[:], 0.0), writes=[Sst])
        kb.op("pool", lambda e: e.memset(Sb[c % 2][:], 0.0), writes=[Sb[c % 2]])

    def run_scan(chunks, c0):
        n = len(chunks)
        for _ in gla_prep(c0, chunks[0][0], chunks[0][1]):
            pass
        for k in range(n):
            row, d, om, orow = chunks[k]
            gens = [gla_fin(c0 + k, d, om, orow)]
            if k + 1 < n:
                gens.append(gla_prep(c0 + k + 1, chunks[k + 1][0], chunks[k + 1][1]))
            interleave(gens, 2)
        return c0 + n

    fwd = [(S + c * 128, 0, None, None) for c in range(2)] + [(t * 128, 0, "store", t * 128) for t in range(n_lat_tiles)]
    bwd = [(S + c * 128, 1, None, None) for c in (1, 0)] + [(t * 128, 1, "final", t * 128) for t in range(n_lat_tiles - 1, -1, -1)]
    reset_state(0)
    cn = run_scan(fwd, 0)
    kb.barrier()
    reset_state(cn)
    run_scan(bwd, cn)
    print("l1a instructions:", kb.n_ins, "sems:", len(kb.sems))
    kb.end_stage()


def fop(v, n):
    return np.ascontiguousarray(np.asarray(v, np.float32).reshape(n, 128).T)


def host_l1a(inp):
    maps = []
    wi = inp["gla_w_in"][0]
    for b in range(2):
        sv = np.stack([inp["c"][b], inp["c_ctx"]], -1).reshape(8, 128, 2).transpose(1, 0, 2).reshape(128, 16).astype(np.float32)
        for h in range(4):
            w = np.concatenate([wi[:, h * 128:(h + 1) * 128], wi[:, 512 + h * 128:512 + (h + 1) * 128], wi[:, 1024 + h * 256:1024 + (h + 1) * 256],
                                wi[:, 2048 + h * 256:2048 + (h + 1) * 256]], axis=1)
            waT = np.ascontiguousarray(wi[:, 3072:3104].T.reshape(2, 16, 1024))
            wa2 = np.ascontiguousarray(inp["gla_w_a2"][0][:, :, h * 128:(h + 1) * 128])
            small = np.concatenate([inp["gla_b_a2"][0][0, h * 128:(h + 1) * 128], inp["gla_b_a2"][0][1, h * 128:(h + 1) * 128], inp["gla_norm_g"][0]]).astype(np.float32)
            maps.append({"svec": np.ascontiguousarray(sv),
                         "adaw": np.ascontiguousarray(inp["ada_w"][1][:, 0:2048]), "adab": fop(inp["ada_b"][1][0:2048], 16), "g1": fop(inp["norm1_g"][1], 8),
                         "w": np.ascontiguousarray(w), "waT": waT, "wa2": wa2, "small": small})
    return maps


RG = [[0, 1, 2, 3], [4, 5, 6, 7]]


def build_all():
    kb = KB()
    banks = [kb.ps("bank%d" % i) for i in range(8)]
    x1in = kb.dram("x1in", [512, 4224], BF16)
    x1out = kb.dram("x1out", [2048, 4224], BF16)
    x2in = kb.dram("x2in", [4224, 1024], F32)
    x2out = kb.dram("x2out", [4 * 4224, 1024], F32)
    x3in = kb.dram("x3in", [1024, 4096], BF16)
    x3out = kb.dram("x3out", [4096, 4096], BF16)
    build_l0a(kb, banks, x1in)
    kb.all_gather(x1in, x1out, RG, 64)
    build_b(0, kb, banks, x1out, None, x2in)
    kb.all_gather(x2in, x2out, RG, 256)
    build_l1a(kb, banks, x2out, x3in)
    kb.all_gather(x3in, x3out, RG, 128)
    build_b(1, kb, banks, x3out, x2in, None)
    print("total instructions:", kb.n_ins, "sems:", len(kb.sems))
    return kb.finish()


def kernel(**inputs):
    inp = {k: np.asarray(v) for k, v in inputs.items()}
    parts = [("a0_", host_l0a(inp)), ("b0_", host_b(0, inp)), ("a1_", host_l1a(inp)), ("b1_", host_b(1, inp))]
    maps = []
    for c in range(8):
        m = {}
        for pre, ms in parts:
            for k, v in ms[c].items():
                m[pre + k] = v
        maps.append(m)
    nc = build_all()
    res = run_bass_kernel_spmd(nc, maps, core_ids=list(range(8)))
    out = np.zeros((2, 16384, 1024), np.float32)
    for b in range(2):
        for jq in range(4):
            out[b, jq * 4096:(jq + 1) * 4096] = np.asarray(res.results[b * 4 + jq]["b1_hout"])[:4096]
    return out
```

```python
import numpy as np
from contextlib import ExitStack
import concourse.bass as bass
import concourse.mybir as mybir
from concourse.bass_utils import run_bass_kernel_spmd
import ml_dtypes

F32 = mybir.dt.float32
BF16 = mybir.dt.bfloat16
I32 = mybir.dt.int32
AF = mybir.ActivationFunctionType
ALU = mybir.AluOpType
AX = mybir.AxisListType
NPBF16 = ml_dtypes.bfloat16


def interleave(gens, width):
    active = []
    it = iter(gens)
    while True:
        while len(active) < width:
            g = next(it, None)
            if g is None:
                break
            active.append(g)
        if not active:
            break
        for g in list(active):
            try:
                next(g)
            except StopIteration:
                active.remove(g)


def ag_row(i, rank, chunk_rows, total_rows, world=4):
    r0 = (i // chunk_rows) * chunk_rows
    n = min(chunk_rows, total_rows - r0)
    return world * r0 + rank * n + (i - r0)


class T:
    def __init__(self, h, name, kind):
        self.h = h
        self.name = name
        self.kind = kind
        self.w = None
        self.r = {}
        self.dkey = None

    def __getitem__(self, idx):
        return self.h[idx]


class KB:
    def __init__(self):
        self.nc = bass.Bass("TRN2", target_bir_lowering=False)
        nc = self.nc
        self.es = ExitStack()
        self.eng = {"pe": nc.tensor, "act": nc.scalar, "dve": nc.vector, "pool": nc.gpsimd, "sp": nc.sync}
        self.sems = {}
        self.cnt = {}
        self.seen = {e: {} for e in self.eng}
        for e in self.eng:
            self.sems[e] = self.es.enter_context(nc.semaphore("e_" + e))
            self.cnt[e] = 0
        self.issued = {}
        self.n_ins = 0
        self.outs = []
        self._uid = 0
        self.cur = self.es
        self.prefix = ""
        self.tiles = []
        self.free_dsems = []
        self.stage_tiles0 = 0

    def sb(self, name, shape, dt, es=None):
        h = (es or self.cur).enter_context(self.nc.sbuf_tensor(self.prefix + name, list(shape), dt))
        t = T(h, name, "sb")
        self.tiles.append(t)
        return t

    def ps(self, name, shape=(128, 512), dt=F32):
        h = self.es.enter_context(self.nc.psum_tensor(name, list(shape), dt))
        t = T(h, name, "ps")
        self.tiles.append(t)
        return t

    def dram(self, name, shape, dt, kind="Internal"):
        h = self.nc.dram_tensor(self.prefix + name, list(shape), dt, kind=kind)
        t = T(h.ap(), name, "dram")
        self.tiles.append(t)
        if kind == "ExternalOutput":
            self.outs.append(t)
        return t

    def _dsem(self, t):
        if t.dkey is None:
            self._uid += 1
            t.dkey = "d%d_%s" % (self._uid, t.name)
            if self.free_dsems:
                h, v = self.free_dsems.pop()
                self.sems[t.dkey] = h
                self.issued[t.dkey] = v
            else:
                self.sems[t.dkey] = self.es.enter_context(self.nc.semaphore(t.dkey))
                self.issued[t.dkey] = 0
        return t.dkey

    def begin_stage(self, prefix):
        self.prefix = prefix
        self.cur = ExitStack()
        self.stage_tiles0 = len(self.tiles)

    def end_stage(self):
        self.barrier()
        self.cur.close()
        self.cur = self.es
        for t in self.tiles[self.stage_tiles0:]:
            if t.kind == "sb" and t.dkey is not None:
                self.free_dsems.append((self.sems[t.dkey], self.issued[t.dkey]))
                del self.issued[t.dkey]
                del self.sems[t.dkey]
                t.dkey = None
        for t in self.tiles:
            t.w = None
            t.r = {}
        for e in self.eng:
            self._uid += 1
            self.sems[e] = self.es.enter_context(self.nc.semaphore("e%d_%s" % (self._uid, e)))
            self.cnt[e] = 0
        self.seen = {e: {} for e in self.eng}
        self.prefix = ""

    def all_gather(self, src, dst, groups, chunk_rows):
        self.barrier()
        self._uid += 1
        sem = self.es.enter_context(self.nc.semaphore("cc%d" % self._uid))
        R = src.h.shape[0]
        k = 0
        for r0 in range(0, R, chunk_rows):
            n = min(chunk_rows, R - r0)
            self.nc.gpsimd.collective_compute("AllGather", ALU.bypass, replica_groups=groups, ins=[src.h[r0:r0 + n, :]],
                                              outs=[dst.h[4 * r0:4 * r0 + 4 * n, :]]).then_inc(sem, 1)
            k += 1
        self.nc.gpsimd.wait_ge(sem, k)
        if not hasattr(self, "_fence"):
            self._fence = T(self.es.enter_context(self.nc.sbuf_tensor("cc_fence", [128, 8], F32)), "cc_fence", "sb")
            self.tiles.append(self._fence)
        f = self._fence
        self.op("pool", lambda e: e.memset(f[:], 0.0), writes=[f])
        for en in self.eng:
            if en != "pool":
                self._waits(en, {"pool": self.cnt["pool"]})
        self.n_ins += k + 1

    def _deps(self, en, reads, writes, is_dma=False):
        deps = {}

        def add(key, val, kind):
            if key == en and not is_dma:
                if en == "pe" or kind == "war":
                    return
            if is_dma and kind == "waw" and key in self.issued:
                return
            deps[key] = max(deps.get(key, 0), val)

        for t in reads:
            if t.w is not None:
                add(t.w[0], t.w[1], "raw")
            if t.kind == "ps":
                for k, v in t.r.items():
                    if k != en:
                        add(k, v, "rar")
        for t in writes:
            if t.w is not None:
                add(t.w[0], t.w[1], "waw")
            for k, v in t.r.items():
                add(k, v, "war")
        return deps

    def _waits(self, en, deps):
        e = self.eng[en]
        for key, val in deps.items():
            if key in self.issued:
                val = self.issued[key]
            if self.seen[en].get(key, 0) >= val:
                continue
            e.wait_ge(self.sems[key], val)
            self.seen[en][key] = val
            self.n_ins += 1

    def op(self, en, fn, reads=(), writes=()):
        self._waits(en, self._deps(en, reads, writes))
        ins = fn(self.eng[en])
        self.cnt[en] += 1
        self.n_ins += 1
        ins.then_inc(self.sems[en], 1)
        c = self.cnt[en]
        for t in reads:
            t.r[en] = c
        for t in writes:
            t.w = (en, c)
            t.r = {}
        return ins

    def dma(self, q, fn, reads=(), writes=()):
        self._waits(q, self._deps(q, reads, writes, is_dma=True))
        cand = [t for t in writes if t.kind != "dram"] or [t for t in reads if t.kind != "dram"] or list(writes) or list(reads)
        key = self._dsem(cand[0])
        ins = fn(self.eng[q])
        self.issued[key] += 16
        self.n_ins += 1
        ins.then_inc(self.sems[key], 16)
        v = self.issued[key]
        for t in reads:
            t.r[key] = v
        for t in writes:
            t.w = (key, v)
            t.r = {}
        return ins

    def load(self, q, dst_t, dst_ap, src_ap, src_t=None, **kw):
        return self.dma(q, lambda e: e.dma_start(out=dst_ap, in_=src_ap, **kw),
                        reads=[src_t] if src_t is not None else [], writes=[dst_t])

    def store(self, q, dst_t, dst_ap, src_t, src_ap, **kw):
        return self.dma(q, lambda e: e.dma_start(out=dst_ap, in_=src_ap, **kw), reads=[src_t], writes=[dst_t])

    def finish(self):
        deps = {}
        for t in self.outs:
            if t.w is not None:
                deps[t.w[0]] = max(deps.get(t.w[0], 0), t.w[1])
        self._waits("sp", deps)
        self.es.close()
        return self.nc

    def barrier(self):
        for en in self.eng:
            deps = {}
            for k in self.eng:
                if k != en and self.cnt[k] > 0:
                    deps[k] = self.cnt[k]
            for k, v in self.issued.items():
                if v > 0:
                    deps[k] = v
            self._waits(en, deps)

    def identity(self, name, dt):
        f = self.sb(name + "_f", [128, 128], F32)
        self.op("pool", lambda e: e.memset(f[:], 0.0), writes=[f])
        self.op("pool", lambda e: e.affine_select(out=f[:], in_=f[:], pattern=[[-1, 128]], compare_op=ALU.not_equal,
                                                  fill=1.0, base=0, channel_multiplier=1), reads=[f], writes=[f])
        if dt == F32:
            return f
        b = self.sb(name, [128, 128], dt)
        self.op("pool", lambda e: e.tensor_copy(out=b[:], in_=f[:]), reads=[f], writes=[b])
        return b

EPS = 1e-6
S = 16384
LC = 256

NKT = (S + LC) // 128
ATT_NSPLIT = 512


def build_l0a(kb, banks, x1in, n_groups=32, debug=False):
    kb.begin_stage("a0_")
    x = kb.dram("x", [S, 1024], F32, "ExternalInput")
    ctx = kb.dram("ctx", [LC, 1024], F32, "ExternalInput")
    svec = kb.dram("svec", [128, 16], F32, "ExternalInput")
    adaw = kb.dram("adaw", [1024, 2048], F32, "ExternalInput")
    adab = kb.dram("adab", [128, 16], F32, "ExternalInput")
    g1 = kb.dram("g1", [128, 8], F32, "ExternalInput")
    w = kb.dram("w", [1024, 384], F32, "ExternalInput")
    small = kb.dram("small", [640], F32, "ExternalInput")
    cos4 = kb.dram("cos4", [S, 256], F32, "ExternalInput")
    sin4 = kb.dram("sin4", [S, 256], F32, "ExternalInput")
    zpad = kb.sb("zpad", [128, 4, 64], BF16)
    kb.op("pool", lambda e: e.memset(zpad[:], 0.0), writes=[zpad])
    kb.store("sp", x1in, x1in.h[:, 4160:4224].rearrange("(q p) n -> p q n", p=128), zpad, zpad[:])

    def bfv(t):
        return t[:].bitcast(BF16)

    identb = kb.identity("identb", BF16)
    smallb = kb.sb("smallb", [128, 640], F32)
    kb.load("sp", smallb, smallb[:], small.h.partition_broadcast(128), small)

    tmp64 = kb.sb("tmp64", [128, 2, 64], F32)
    dots = kb.sb("dots", [128, 4], F32)
    kb.op("dve", lambda e: e.tensor_tensor(out=tmp64[:, 0, :], in0=smallb[:, 256:320], in1=smallb[:, 320:384], op=ALU.mult), reads=[smallb], writes=[tmp64])
    kb.op("dve", lambda e: e.tensor_tensor(out=tmp64[:, 1, :], in0=smallb[:, 384:448], in1=smallb[:, 448:512], op=ALU.mult), reads=[smallb], writes=[tmp64])
    kb.op("dve", lambda e: e.tensor_reduce(out=dots[:, 0:2], in_=tmp64[:], axis=AX.X, op=ALU.add), reads=[tmp64], writes=[dots])
    kb.op("act", lambda e: e.activation(out=dots[:, 2:4], in_=dots[:, 0:2], func=AF.Exp), reads=[dots], writes=[dots])
    neglam = kb.sb("neglam", [128, 1], F32)
    kb.op("dve", lambda e: e.scalar_tensor_tensor(out=neglam[:], in0=dots[:, 3:4], scalar=-0.2, in1=dots[:, 2:3], op0=ALU.add, op1=ALU.subtract), reads=[dots], writes=[neglam])
    subg_s = kb.sb("subg_s", [128, 128], F32)
    kb.op("dve", lambda e: e.tensor_scalar(out=subg_s[:], in0=smallb[:, 512:640], scalar1=0.8, scalar2=None, op0=ALU.mult), reads=[smallb], writes=[subg_s])

    s_sb = kb.sb("s_sb", [128, 16], F32)
    kb.load("sp", s_sb, s_sb[:], svec.h, svec)
    kb.op("act", lambda e: e.activation(out=s_sb[:], in_=s_sb[:], func=AF.Silu), reads=[s_sb], writes=[s_sb])
    adab_sb = kb.sb("adab_sb", [128, 16], F32)
    kb.load("sp", adab_sb, adab_sb[:], adab.h, adab)
    g1_sb = kb.sb("g1_sb", [128, 8], F32)
    kb.load("sp", g1_sb, g1_sb[:], g1.h, g1)
    mod = kb.sb("mod", [128, 16, 2], F32)
    gs = kb.sb("gs", [128, 8, 2], F32)
    zer = kb.sb("zer", [128, 128], F32)
    wq = [kb.sb("wq%d" % j, [128, 8, 384], BF16) for j in range(2)]
    bias = [kb.sb("bias%d" % j, [128, 384], F32) for j in range(2)]
    p0 = ExitStack()
    adaw_sb = kb.sb("adaw_sb", [128, 8, 512], F32, p0)
    pm = banks[0]
    for v in range(4):
        kb.load("sp", adaw_sb, adaw_sb[:], adaw.h[:, v * 512:(v + 1) * 512].rearrange("(kc p) n -> p kc n", p=128), adaw)
        for oc in range(4):
            g = v * 4 + oc
            for kc in range(8):
                kb.op("pe", lambda e: e.matmul(pm[:, g * 2:g * 2 + 2], lhsT=adaw_sb[:, kc, oc * 128:(oc + 1) * 128],
                                              rhs=s_sb[:, kc * 2:kc * 2 + 2], start=(kc == 0), stop=(kc == 7)),
                      reads=[adaw_sb, s_sb], writes=[pm])
    pm3 = pm[:, 0:32].rearrange("p (g j) -> p g j", j=2)
    for j in range(2):
        kb.op("dve", lambda e: e.tensor_tensor(out=mod[:, :, j], in0=pm3[:, :, j], in1=adab_sb[:], op=ALU.add), reads=[pm, adab_sb], writes=[mod])
        kb.op("dve", lambda e: e.scalar_tensor_tensor(out=gs[:, :, j], in0=mod[:, 8:16, j], scalar=1.0, in1=g1_sb[:], op0=ALU.add, op1=ALU.mult),
              reads=[mod, g1_sb], writes=[gs])

    w_sb = kb.sb("w_sb", [128, 8, 384], F32, p0)
    kb.load("sp", w_sb, w_sb[:], w.h.rearrange("(kc p) n -> p kc n", p=128), w)
    kb.op("pool", lambda e: e.memset(zer[:], 0.0), writes=[zer])
    shiftbc = kb.sb("shiftbc", [128, 8, 128], F32, p0)
    for j in range(2):
        for kc in range(8):
            kb.op("dve", lambda e: e.tensor_scalar(out=wq[j][:, kc, :], in0=w_sb[:, kc, :], scalar1=gs[:, kc, j:j + 1], scalar2=None, op0=ALU.mult),
                  reads=[w_sb, gs], writes=[wq[j]])
            kb.op("dve", lambda e: e.tensor_scalar(out=shiftbc[:, kc, :], in0=zer[:], scalar1=mod[:, kc, j:j + 1], scalar2=None, op0=ALU.add),
                  reads=[zer, mod], writes=[shiftbc])
        pb = banks[1]
        for kc in range(8):
            kb.op("pe", lambda e: e.matmul(pb[:, 0:384], lhsT=shiftbc[:, kc, :], rhs=w_sb[:, kc, :], start=(kc == 0), stop=(kc == 7)),
                  reads=[shiftbc, w_sb], writes=[pb])
        kb.op("dve", lambda e: e.tensor_copy(out=bias[j][:], in_=pb[:, 0:384]), reads=[pb], writes=[bias[j]])

    kb.barrier()
    p0.close()
    QT = kb.sb("QT", [128, S + LC], BF16)
    KTm = [kb.sb("KT%d" % m, [128, S + LC], BF16) for m in range(2)]
    kb.op("pool", lambda e: e.memset(KTm[0][64:128, :], 0.0), writes=[KTm[0]])
    kb.op("pool", lambda e: e.memset(KTm[1][0:64, :], 0.0), writes=[KTm[1]])
    Vx = kb.sb("Vx", [128, NKT, 129], BF16)
    kb.op("pool", lambda e: e.memset(Vx[:, :, 128:129], 1.0), writes=[Vx])

    def dbl(name, shape, dt, n=2, es=None):
        return [kb.sb("%s%d" % (name, i), shape, dt, es) for i in range(n)]

    p2 = ExitStack()

    xt = dbl("xt", [128, 1024], F32, 4, p2)
    junk = kb.sb("junk", [128, 1024], BF16, p2)
    st1 = dbl("st1", [128, 4], F32, 2, p2)
    xn = dbl("xn", [128, 1024], BF16, 2, p2)
    xnT = dbl("xnT", [128, 1024], BF16, 2, p2)
    qkv = dbl("qkv", [128, 384], F32, 2, p2)
    cs = dbl("cs", [128, 256], F32, 2, p2)
    sn = dbl("sn", [128, 256], F32, 2, p2)
    sq = dbl("sq", [128, 256], F32, 2, p2)
    st2 = dbl("st2", [128, 12], F32, 2, p2)
    qkn = dbl("qkn", [128, 256], F32, 2, p2)
    sw = dbl("sw", [128, 256], F32, 2, p2)
    t1 = dbl("t1", [128, 256], F32, 2, p2)
    rr = dbl("rr", [128, 256], BF16, 2, p2)

    def rstd_chain(stt, c_in, c_tmp, c_out, n, inv_n, srcs):
        kb.op("dve", lambda e: e.tensor_scalar(out=stt[:, c_tmp:c_tmp + n], in0=stt[:, c_in:c_in + n], scalar1=inv_n, scalar2=EPS, op0=ALU.mult, op1=ALU.add),
              reads=[stt], writes=[stt])
        kb.op("act", lambda e: e.activation(out=stt[:, c_tmp:c_tmp + n], in_=stt[:, c_tmp:c_tmp + n], func=AF.Sqrt), reads=[stt], writes=[stt])
        kb.op("dve", lambda e: e.reciprocal(out=stt[:, c_out:c_out + n], in_=stt[:, c_tmp:c_tmp + n]), reads=[stt], writes=[stt])

    tile_args = []

    def do_load(i):
        _, src, row0 = tile_args[i][0:3]
        kb.load("sp", xt[i % 4], xt[i % 4][:], src.h[row0:row0 + 128, :], src)

    def proj_tile(i, src, row0, is_ctx, qcol, kcol, kt):
        p = i % 2
        j = 1 if is_ctx else 0
        if i + 2 < len(tile_args):
            do_load(i + 2)
        yield
        if not is_ctx:
            kb.load("pool", cs[p], cs[p][:], cos4.h[row0:row0 + 128, :], cos4)
            yield
            kb.load("pool", sn[p], sn[p][:], sin4.h[row0:row0 + 128, :], sin4)
            yield
        kb.op("act", lambda e: e.activation(out=junk[:], in_=xt[i % 4][:], func=AF.Square, accum_out=st1[p][:, 0:1]), reads=[xt[i % 4]], writes=[junk, st1[p]])
        yield
        rstd_chain(st1[p], 0, 1, 2, 1, 1.0 / 1024, None)
        kb.op("act", lambda e: e.activation(out=xn[p][:], in_=xt[i % 4][:], func=AF.Copy, scale=st1[p][:, 2:3]), reads=[xt[i % 4], st1[p]], writes=[xn[p]])
        yield
        psT = banks[p]
        for kc in range(8):
            kb.op("pe", lambda e: e.transpose(out=bfv(psT)[:, kc * 128:(kc + 1) * 128], in_=xn[p][:, kc * 128:(kc + 1) * 128], identity=identb[:]),
                  reads=[xn[p], identb], writes=[psT])
            yield
        kb.op("dve", lambda e: e.tensor_copy(out=xnT[p][:], in_=bfv(psT)[:, 0:1024]), reads=[psT], writes=[xnT[p]])
        yield
        pp = banks[2 + p]
        for kc in range(8):
            kb.op("pe", lambda e: e.matmul(pp[:, 0:384], lhsT=xnT[p][:, kc * 128:(kc + 1) * 128], rhs=wq[j][:, kc, :], start=(kc == 0), stop=(kc == 7)),
                  reads=[xnT[p], wq[j]], writes=[pp])
            yield
        kb.op("dve", lambda e: e.tensor_tensor(out=qkv[p][:], in0=pp[:, 0:384], in1=bias[j][:], op=ALU.add), reads=[pp, bias[j]], writes=[qkv[p]])
        yield
        kb.op("pool", lambda e: e.tensor_copy(out=Vx[:, kt, 0:128], in_=qkv[p][:, 256:384]), reads=[qkv[p]], writes=[Vx])
        yield
        kb.op("act", lambda e: e.activation(out=sq[p][:], in_=qkv[p][:, 0:256], func=AF.Square), reads=[qkv[p]], writes=[sq[p]])
        yield
        kb.op("dve", lambda e: e.tensor_reduce(out=st2[p][:, 0:4], in_=sq[p][:].rearrange("p (g d) -> p g d", g=4), axis=AX.X, op=ALU.add),
              reads=[sq[p]], writes=[st2[p]])
        yield
        rstd_chain(st2[p], 0, 4, 8, 4, 1.0 / 64, None)
        for g in range(4):
            kb.op("dve", lambda e: e.scalar_tensor_tensor(out=qkn[p][:, g * 64:(g + 1) * 64], in0=qkv[p][:, g * 64:(g + 1) * 64], scalar=st2[p][:, 8 + g:9 + g],
                                                          in1=smallb[:, g * 64:(g + 1) * 64], op0=ALU.mult, op1=ALU.mult),
                  reads=[qkv[p], st2[p], smallb], writes=[qkn[p]])
            yield
        if is_ctx:
            kb.op("pool", lambda e: e.tensor_copy(out=rr[p][:], in_=qkn[p][:]), reads=[qkn[p]], writes=[rr[p]])
            yield
        else:
            q5 = qkn[p][:].rearrange("p (a h d) -> p a h d", h=2, d=16)
            s5 = sw[p][:].rearrange("p (a h d) -> p a h d", h=2, d=16)
            kb.op("pool", lambda e: e.tensor_copy(out=s5[:, :, 0, :], in_=q5[:, :, 1, :]), reads=[qkn[p]], writes=[sw[p]])
            yield
            kb.op("pool", lambda e: e.tensor_copy(out=s5[:, :, 1, :], in_=q5[:, :, 0, :]), reads=[qkn[p]], writes=[sw[p]])
            yield
            kb.op("pool", lambda e: e.tensor_tensor(out=sw[p][:], in0=sw[p][:], in1=sn[p][:], op=ALU.mult), reads=[sw[p], sn[p]], writes=[sw[p]])
            yield
            kb.op("dve", lambda e: e.tensor_tensor(out=t1[p][:], in0=qkn[p][:], in1=cs[p][:], op=ALU.mult), reads=[qkn[p], cs[p]], writes=[t1[p]])
            yield
            kb.op("dve", lambda e: e.tensor_tensor(out=rr[p][:], in0=t1[p][:], in1=sw[p][:], op=ALU.add), reads=[t1[p], sw[p]], writes=[rr[p]])
            yield
        pq = banks[4 + p]
        for hh in range(2):
            kb.op("pe", lambda e: e.transpose(out=bfv(pq)[:, hh * 128:(hh + 1) * 128], in_=rr[p][:, hh * 128:(hh + 1) * 128], identity=identb[:]),
                  reads=[rr[p], identb], writes=[pq])
            yield
        kb.op("act", lambda e: e.copy(out=QT[:, qcol:qcol + 128], in_=bfv(pq)[:, 0:128]), reads=[pq], writes=[QT])
        yield
        kb.op("act", lambda e: e.copy(out=KTm[0][0:64, kcol:kcol + 128], in_=bfv(pq)[0:64, 128:256]), reads=[pq], writes=[KTm[0]])
        kb.op("act", lambda e: e.copy(out=KTm[1][64:128, kcol:kcol + 128], in_=bfv(pq)[64:128, 128:256]), reads=[pq], writes=[KTm[1]])
        yield

    i = 0
    for c in range(LC // 128):
        tile_args.append((i, ctx, c * 128, True, S + c * 128, c * 128, c))
        i += 1
    for t in range(S // 128):
        tile_args.append((i, x, t * 128, False, t * 128, LC + t * 128, LC // 128 + t))
        i += 1
    do_load(0)
    do_load(1)
    gens = [proj_tile(*a_) for a_ in tile_args]
    interleave(gens, 2)
    kb.barrier()
    p2.close()

    ST = banks[0:3]
    OT = [banks[4], banks[5]]
    PL = [banks[6], banks[7]]
    PS_ = banks[3]
    PT = dbl("pt", [128, 512], BF16, 4)
    Pacc = [kb.sb("pacc%d" % m, [128, 512], F32) for m in range(2)]
    ones_bb = kb.sb("ones_bb", [128, 128], BF16)
    kb.op("pool", lambda e: e.memset(ones_bb[:], 1.0), writes=[ones_bb])
    ones_ff = kb.sb("ones_ff", [128, 128], F32)
    kb.op("pool", lambda e: e.memset(ones_ff[:], 1.0), writes=[ones_ff])
    subg_col = kb.sb("subg_col", [128, 1], F32)
    kb.load("sp", subg_col, subg_col[:], small.h[512:640].rearrange("(p o) -> p o", o=1), small)
    kb.op("dve", lambda e: e.tensor_scalar(out=subg_col[:], in0=subg_col[:], scalar1=0.8, scalar2=None, op0=ALU.mult), reads=[subg_col], writes=[subg_col])
    rlb = dbl("rlb", [128, 512], F32)
    eo = dbl("eo", [128, 512], F32)
    esq = kb.sb("esq", [128, 512], F32)
    outT = dbl("outT", [128, 512], BF16)
    gcount = [0]
    NSPLIT = ATT_NSPLIT

    def attend(qc0, nq, kts):
        steps = [(m, idx, kt) for m in range(2) for idx, kt in enumerate(kts)]
        nk = len(kts)
        gi = gcount[0]
        gcount[0] += 1

        def score(s):
            m, idx, kt = steps[s]
            st = ST[s % 3]
            for c0 in range(0, nq, NSPLIT):
                kb.op("pe", lambda e: e.matmul(st[:, c0:min(nq, c0 + NSPLIT)], lhsT=KTm[m][:, kt * 128:(kt + 1) * 128], rhs=QT[:, qc0 + c0:qc0 + min(nq, c0 + NSPLIT)],
                                              start=True, stop=True), reads=[KTm[m], QT], writes=[st])

        used = {}

        def rest(s):
            m, idx, kt = steps[s]
            st = ST[s % 3]
            pt = PT[s % 4]
            kb.op("act", lambda e: e.activation(out=pt[:, 0:nq], in_=st[:, 0:nq], func=AF.Exp, scale=0.125), reads=[st], writes=[pt])
            for c0 in range(0, nq, NSPLIT):
                kb.op("pe", lambda e: e.matmul(OT[m][:, c0:min(nq, c0 + NSPLIT)], lhsT=Vx[:, kt, 0:128], rhs=pt[:, c0:min(nq, c0 + NSPLIT)], start=(idx == 0 and c0 == 0), stop=(idx == nk - 1),
                                              skip_group_check=True), reads=[pt, Vx], writes=[OT[m]])
            if s + 3 < len(steps):
                score(s + 3)
            if idx % 4 == 3:
                kb.op("pe", lambda e: e.matmul(PL[m][:, 0:nq], lhsT=ones_bb[:], rhs=pt[:, 0:nq], start=((m, "pe") not in used), stop=False), reads=[ones_bb, pt], writes=[PL[m]])
                used[(m, "pe")] = True
            elif (m, "dve") not in used:
                used[(m, "dve")] = True
                kb.op("dve", lambda e: e.tensor_copy(out=Pacc[m][:, 0:nq], in_=pt[:, 0:nq]), reads=[pt], writes=[Pacc[m]])
            else:
                kb.op("dve", lambda e: e.tensor_tensor(out=Pacc[m][:, 0:nq], in0=pt[:, 0:nq], in1=Pacc[m][:, 0:nq], op=ALU.add), reads=[pt, Pacc[m]], writes=[Pacc[m]])

        for s0 in range(min(3, len(steps))):
            score(s0)
        for s in range(len(steps)):
            rest(s)
        for m in range(2):
            kb.op("pe", lambda e: e.matmul(PL[m][:, 0:nq], lhsT=ones_ff[:], rhs=Pacc[m][:, 0:nq], start=((m, "pe") not in used), stop=True), reads=[ones_ff, Pacc[m]], writes=[PL[m]])
            kb.op("dve", lambda e: e.reciprocal(out=rlb[m][:, 0:nq], in_=PL[m][:, 0:nq]), reads=[PL[m]], writes=[rlb[m]])
            kb.op("dve", lambda e: e.tensor_tensor(out=eo[m][:, 0:nq], in0=OT[m][:, 0:nq], in1=rlb[m][:, 0:nq], op=ALU.mult), reads=[OT[m], rlb[m]], writes=[eo[m]])
        kb.op("dve", lambda e: e.scalar_tensor_tensor(out=eo[0][:, 0:nq], in0=eo[1][:, 0:nq], scalar=neglam[:, 0:1], in1=eo[0][:, 0:nq], op0=ALU.mult, op1=ALU.add),
              reads=[eo[1], neglam, eo[0]], writes=[eo[0]])
        kb.op("act", lambda e: e.activation(out=esq[:, 0:nq], in_=eo[0][:, 0:nq], func=AF.Square), reads=[eo[0]], writes=[esq])
        kb.op("pe", lambda e: e.matmul(PS_[:, 0:nq], lhsT=ones_ff[:], rhs=esq[:, 0:nq], start=True, stop=True), reads=[ones_ff, esq], writes=[PS_])
        kb.op("dve", lambda e: e.tensor_scalar(out=rlb[0][:, 0:nq], in0=PS_[:, 0:nq], scalar1=1.0 / 128, scalar2=EPS, op0=ALU.mult, op1=ALU.add), reads=[PS_], writes=[rlb[0]])
        kb.op("act", lambda e: e.activation(out=rlb[0][:, 0:nq], in_=rlb[0][:, 0:nq], func=AF.Sqrt), reads=[rlb[0]], writes=[rlb[0]])
        kb.op("dve", lambda e: e.reciprocal(out=rlb[1][:, 0:nq], in_=rlb[0][:, 0:nq]), reads=[rlb[0]], writes=[rlb[1]])
        ot = outT[gi % 2]
        kb.op("dve", lambda e: e.scalar_tensor_tensor(out=ot[:, 0:nq], in0=eo[0][:, 0:nq], scalar=subg_col[:, 0:1], in1=rlb[1][:, 0:nq], op0=ALU.mult, op1=ALU.mult),
              reads=[eo[0], subg_col, rlb[1]], writes=[ot])
        if qc0 >= S:
            for q in range(4):
                kb.store("sp", x1in, x1in.h[q * 128:(q + 1) * 128, 4096:4160], ot, ot[:, q * 64:(q + 1) * 64])
        else:
            q, col = qc0 // 4096, qc0 % 4096
            kb.store("sp", x1in, x1in.h[q * 128:(q + 1) * 128, col:col + nq], ot, ot[:, 0:nq])

    attend(S, LC, list(range(LC // 128)))
    for g in range(n_groups):
        attend(g * 512, 512, list(range(NKT)))
    print("l0a instructions:", kb.n_ins)
    kb.end_stage()


def rope_tables():
    half = 32
    inv = (10000.0 ** (-np.arange(0, half, 2, dtype=np.float32) / half)).astype(np.float32)
    t = np.arange(S)
    r = (t // 64).astype(np.float32)[:, None] * inv[None, :]
    c = (t % 64).astype(np.float32)[:, None] * inv[None, :]
    ang = np.concatenate([r, r, c, c], axis=-1).astype(np.float32)
    cos = np.cos(ang).astype(np.float32)
    sin = np.sin(ang).astype(np.float32)
    sgn = np.concatenate([-np.ones(16), np.ones(16), -np.ones(16), np.ones(16)]).astype(np.float32)
    sin = sin * sgn[None, :]
    return np.ascontiguousarray(np.tile(cos, (1, 4))), np.ascontiguousarray(np.tile(sin, (1, 4)))


def fop(v, n):
    return np.ascontiguousarray(np.asarray(v, np.float32).reshape(n, 128).T)


def host_l0a(inp):
    cos4, sin4 = rope_tables()
    maps = []
    wi = inp["ab_w_in"][0]
    for b in range(2):
        for h in range(4):
            sv = np.stack([inp["c"][b], inp["c_ctx"]], -1).reshape(8, 128, 2).transpose(1, 0, 2).reshape(128, 16)
            w = np.concatenate([wi[:, 1024 + h * 128:1024 + (h + 1) * 128], wi[:, 1536 + h * 128:1536 + (h + 1) * 128],
                                wi[:, 2048 + h * 128:2048 + (h + 1) * 128]], axis=1)
            qg, kg = inp["diff_qnorm_g"][0], inp["diff_knorm_g"][0]
            small = np.concatenate([qg, qg, kg, kg, inp["diff_lq1"][0], inp["diff_lk1"][0], inp["diff_lq2"][0], inp["diff_lk2"][0],
                                    inp["diff_subln_g"][0]]).astype(np.float32)
            maps.append({
                "x": np.ascontiguousarray(inp["x"][b]), "ctx": np.ascontiguousarray(inp["ctx"][b]),
                "svec": np.ascontiguousarray(sv.astype(np.float32)),
                "adaw": np.ascontiguousarray(inp["ada_w"][0][:, 0:2048]), "adab": fop(inp["ada_b"][0][0:2048], 16),
                "g1": fop(inp["norm1_g"][0], 8), "w": np.ascontiguousarray(w), "small": small, "cos4": cos4, "sin4": sin4,
            })
    return maps


BIG = 1.0e30


def build_b(layer, kb, banks, mixsrc, hin_t, hout_t, debug=False):
    L0 = (layer == 0)
    NTL = 32
    NT = NTL + (1 if L0 else 0)
    NTOK = NT * 128
    NTOKV = 4096 + (64 if L0 else 0)
    NB = (2 * NTOKV + 32 * 255 + 255) // 256
    NROWS = NB * 256
    NMIX = 4 if L0 else 8

    kb.begin_stage("b%d_" % layer)
    hin = hin_t if hin_t is not None else kb.dram("hin", [NTOK, 1024], F32, "ExternalInput")
    svec = kb.dram("svec", [128, 16], F32, "ExternalInput")
    adaw = kb.dram("adaw", [1024, 6144], F32, "ExternalInput")
    adabf = kb.dram("adabf", [128, 48], F32, "ExternalInput")
    adabr = kb.dram("adabr", [2048], F32, "ExternalInput")
    gfop = kb.dram("gfop", [128, 16], F32, "ExternalInput")
    mixidx = kb.dram("mixidx", [128, NMIX], I32, "ExternalInput")
    wout = kb.dram("wout", [1024, 1024], F32, "ExternalInput")
    rw = kb.dram("rw", [1024, 36], F32, "ExternalInput")
    rb = kb.dram("rb", [36], F32, "ExternalInput")
    w1t = kb.dram("w1t", [4096, 4096], F32, "ExternalInput")
    w3t = kb.dram("w3t", [4096, 4096], F32, "ExternalInput")
    w2t = kb.dram("w2t", [4096, 4096], F32, "ExternalInput")
    valid = kb.dram("valid", [128, 1], F32, "ExternalInput")
    if L0:
        xhalo = kb.dram("xhalo", [128, 1024], F32, "ExternalInput")
        cxh = kb.dram("cxh", [128, 1024], F32, "ExternalInput")
        edge = kb.dram("edge", [2], F32, "ExternalInput")
        win = kb.dram("win", [1024, 1024], F32, "ExternalInput")
        cw = kb.dram("cw", [128, 124], F32, "ExternalInput")
        cvec = kb.dram("cvec", [128, 12], F32, "ExternalInput")
    hout = hout_t if hout_t is not None else kb.dram("hout", [NTOK, 1024], F32, "ExternalOutput")
    hlm = kb.dram("hlm", [NTOK, 1024], F32)
    nl2d = kb.dram("nl2d", [NTOK, 1024], BF16)
    xs = kb.dram("xs", [NROWS + 128, 1024], BF16)
    ys = kb.dram("ys", [NROWS + 128, 1024], F32)

    def bfv(t):
        return t[:].bitcast(BF16)

    def dbl(name, shape, dt, n=2, es=None):
        return [kb.sb("%s%d" % (name, i), shape, dt, es) for i in range(n)]

    identb = kb.identity("identb", BF16)
    zer = kb.sb("zer", [128, 128], F32)
    kb.op("pool", lambda e: e.memset(zer[:], 0.0), writes=[zer])
    zerb = kb.sb("zerb", [128, 2048], BF16)
    kb.op("pool", lambda e: e.memset(zerb[:], 0.0), writes=[zerb])
    for a in range(0, NROWS // 128, 2):
        kb.store("pool", xs, xs.h[a * 128:(a + 2) * 128, :].rearrange("(a p) n -> p a n", p=128), zerb, zerb[:].rearrange("p (a n) -> p a n", a=2))

    OH = kb.sb("OH", [128, NT, 2, 32], F32)
    GT = kb.sb("GT", [128, NT, 2], F32)
    RK = kb.sb("RK", [128, NT, 2], F32)
    Rbc = kb.sb("Rbc", [128, 32], F32)
    DESTI = kb.sb("DESTI", [128, NT * 2], I32)
    WIDX = kb.sb("WIDX", [128, NB], I32)
    validt = kb.sb("validt", [128, 1], F32)
    gate_bc = [[kb.sb("gate_bc%d%d" % (j, w), [128, 1024], F32) for w in range(2)] for j in range(2)]
    mod = kb.sb("mod", [128, 48, 2], F32)
    gs1 = kb.sb("gs1", [128, 8, 2], F32)
    gs2 = kb.sb("gs2", [128, 8, 2], F32)
    s_sb = kb.sb("s_sb", [128, 16], F32)
    adabf_sb = kb.sb("adabf_sb", [128, 48], F32)
    gfop_sb = kb.sb("gfop_sb", [128, 16], F32)
    iop = kb.sb("iop", [128, 1], F32)
    blkst = kb.sb("blkst", [128, NB], F32)
    ltri_b = kb.sb("ltri_b", [128, 128], BF16)
    ones_b = kb.sb("ones_b", [128, 128], BF16)
    pesA = ExitStack()

    def rstd_chain(stt, c_in, c_tmp, c_out, n, inv_n):
        kb.op("dve", lambda e: e.tensor_scalar(out=stt[:, c_tmp:c_tmp + n], in0=stt[:, c_in:c_in + n], scalar1=inv_n, scalar2=EPS, op0=ALU.mult, op1=ALU.add),
              reads=[stt], writes=[stt])
        kb.op("act", lambda e: e.activation(out=stt[:, c_tmp:c_tmp + n], in_=stt[:, c_tmp:c_tmp + n], func=AF.Sqrt), reads=[stt], writes=[stt])
        kb.op("dve", lambda e: e.reciprocal(out=stt[:, c_out:c_out + n], in_=stt[:, c_tmp:c_tmp + n]), reads=[stt], writes=[stt])

    kb.load("sp", s_sb, s_sb[:], svec.h, svec)
    kb.op("act", lambda e: e.activation(out=s_sb[:], in_=s_sb[:], func=AF.Silu), reads=[s_sb], writes=[s_sb])
    kb.load("sp", adabf_sb, adabf_sb[:], adabf.h, adabf)
    kb.load("sp", gfop_sb, gfop_sb[:], gfop.h, gfop)
    kb.load("sp", validt, validt[:], valid.h, valid)
    pm = banks[0]
    with ExitStack() as pes:
        adabr_sb = kb.sb("adabr_sb", [128, 2048], F32, pes)
        kb.load("sp", adabr_sb, adabr_sb[:], adabr.h.partition_broadcast(128), adabr)
        s_bc = [kb.sb("s_bc%d" % j, [128, 8, 128], F32, pes) for j in range(2)]
        for j in range(2):
            for kc in range(8):
                kb.op("dve", lambda e: e.tensor_scalar(out=s_bc[j][:, kc, :], in0=zer[:, 0:128], scalar1=s_sb[:, kc * 2 + j:kc * 2 + j + 1], scalar2=None, op0=ALU.add),
                      reads=[zer, s_sb], writes=[s_bc[j]])
        adaw_sb = dbl("adaw_sb", [128, 8, 512], F32, 2, pes)
        for v in range(12):
            aw = adaw_sb[v % 2]
            kb.load("sp", aw, aw[:], adaw.h[:, v * 512:(v + 1) * 512].rearrange("(kc p) n -> p kc n", p=128), adaw)
            for oc in range(4):
                g = v * 4 + oc
                for kc in range(8):
                    kb.op("pe", lambda e: e.matmul(pm[:, g * 2:g * 2 + 2], lhsT=aw[:, kc, oc * 128:(oc + 1) * 128], rhs=s_sb[:, kc * 2:kc * 2 + 2],
                                                  start=(kc == 0), stop=(kc == 7)), reads=[aw, s_sb], writes=[pm])
            if v in (4, 5, 10, 11):
                which = 0 if v < 6 else 1
                half = v % 2
                for j in range(2):
                    pr = banks[1 + j]
                    for kc in range(8):
                        kb.op("pe", lambda e: e.matmul(pr[:, :], lhsT=s_bc[j][:, kc, :], rhs=aw[:, kc, :], start=(kc == 0), stop=(kc == 7)),
                              reads=[s_bc[j], aw], writes=[pr])
                    kb.op("dve", lambda e: e.tensor_tensor(out=gate_bc[j][which][:, half * 512:(half + 1) * 512], in0=pr[:, :],
                                                           in1=adabr_sb[:, which * 1024 + half * 512: which * 1024 + (half + 1) * 512], op=ALU.add),
                          reads=[pr, adabr_sb], writes=[gate_bc[j][which]])
        pm3 = pm[:, 0:96].rearrange("p (g j) -> p g j", j=2)
        for j in range(2):
            kb.op("dve", lambda e: e.tensor_tensor(out=mod[:, :, j], in0=pm3[:, :, j], in1=adabf_sb[:], op=ALU.add), reads=[pm, adabf_sb], writes=[mod])
        kb.barrier()
    for j in range(2):
        kb.op("dve", lambda e: e.scalar_tensor_tensor(out=gs1[:, :, j], in0=mod[:, 8:16, j], scalar=1.0, in1=gfop_sb[:, 0:8], op0=ALU.add, op1=ALU.mult),
              reads=[mod, gfop_sb], writes=[gs1])
        kb.op("dve", lambda e: e.scalar_tensor_tensor(out=gs2[:, :, j], in0=mod[:, 32:40, j], scalar=1.0, in1=gfop_sb[:, 8:16], op0=ALU.add, op1=ALU.mult),
              reads=[mod, gfop_sb], writes=[gs2])
    SH1, SH2 = 0, 24

    xn_b = dbl("xn_b", [128, 1024], BF16, 2, pesA)
    junk = kb.sb("junk", [128, 1024], BF16, pesA)
    stn = dbl("stn", [128, 4], F32, 2, pesA)

    def norm_T(i, xt_tile, gs, shoff, j, dstT, dcol, psT):
        p = i % 2
        kb.op("act", lambda e: e.activation(out=junk[:], in_=xt_tile[:], func=AF.Square, accum_out=stn[p][:, 0:1]), reads=[xt_tile], writes=[junk, stn[p]])
        rstd_chain(stn[p], 0, 1, 2, 1, 1.0 / 1024)
        kb.op("act", lambda e: e.activation(out=xn_b[p][:], in_=xt_tile[:], func=AF.Copy, scale=stn[p][:, 2:3]), reads=[xt_tile, stn[p]], writes=[xn_b[p]])
        for kc in range(8):
            kb.op("pe", lambda e: e.transpose(out=bfv(psT)[:, kc * 128:(kc + 1) * 128], in_=xn_b[p][:, kc * 128:(kc + 1) * 128], identity=identb[:]),
                  reads=[xn_b[p], identb], writes=[psT])
        for kc in range(8):
            kb.op("act", lambda e: e.activation(out=dstT[:, kc, dcol:dcol + 128], in_=bfv(psT)[:, kc * 128:(kc + 1) * 128], func=AF.Identity,
                                                scale=gs[:, kc, j:j + 1], bias=mod[:, shoff + kc, j:j + 1]), reads=[psT, gs, mod], writes=[dstT])

    xt = dbl("xt", [128, 1024], F32, 2, pesA)
    convT = kb.sb("convT", [128, 4, NTOK], BF16, pesA) if L0 else None

    if L0:
        with ExitStack() as pes:
            HW = 15 + 4096 + 15
            hT = kb.sb("hT", [128, 4, HW], F32, pes)
            hTc = kb.sb("hTc", [128, 4, 128], F32, pes)
            win_b = kb.sb("win_b", [128, 8, 1024], BF16, pes)
            stg = xt
            for kc in range(8):
                kb.load("sp", stg[kc % 2], stg[kc % 2][:], win.h[kc * 128:(kc + 1) * 128, :], win)
                kb.op("pool", lambda e: e.tensor_copy(out=win_b[:, kc, :], in_=stg[kc % 2][:]), reads=[stg[kc % 2]], writes=[win_b])
            cw_sb = kb.sb("cw_sb", [128, 4, 31], F32, pes)
            kb.load("sp", cw_sb, cw_sb[:], cw.h.rearrange("p (c t) -> p c t", c=4), cw)
            cvec_sb = kb.sb("cvec_sb", [128, 12], F32, pes)
            kb.load("sp", cvec_sb, cvec_sb[:], cvec.h, cvec)
            edge_sb = kb.sb("edge_sb", [128, 2], F32, pes)
            kb.load("sp", edge_sb, edge_sb[:], edge.h.partition_broadcast(128), edge)
            ones_s = kb.sb("ones_s", [128, 128], F32, pes)
            kb.op("pool", lambda e: e.memset(ones_s[:], 1.0 / 512), writes=[ones_s])
            nlT = dbl("nlT", [128, 8, 512], BF16, 1, pes) * 2
            sig = dbl("sig", [128, 512], F32, 2, pes)
            htmp = kb.sb("htmp", [128, 4, 128], F32, pes)

            def u_group(gi, nl, ncols, dst_fn):
                for cc in range(4):
                    pa, pg = banks[2], banks[3]
                    for kc in range(8):
                        kb.op("pe", lambda e: e.matmul(pa[:, 0:ncols], lhsT=win_b[:, kc, cc * 128:(cc + 1) * 128], rhs=nl[:, kc, 0:ncols], start=(kc == 0), stop=(kc == 7)),
                              reads=[win_b, nl], writes=[pa])
                    for kc in range(8):
                        kb.op("pe", lambda e: e.matmul(pg[:, 0:ncols], lhsT=win_b[:, kc, 512 + cc * 128:512 + (cc + 1) * 128], rhs=nl[:, kc, 0:ncols], start=(kc == 0), stop=(kc == 7)),
                              reads=[win_b, nl], writes=[pg])
                    sg = sig[cc % 2]
                    kb.op("act", lambda e: e.activation(out=sg[:, 0:ncols], in_=pg[:, 0:ncols], func=AF.Sigmoid), reads=[pg], writes=[sg])
                    dt_, dap = dst_fn(cc)
                    kb.op("dve", lambda e: e.tensor_tensor(out=dap, in0=pa[:, 0:ncols], in1=sg[:, 0:ncols], op=ALU.mult), reads=[pa, sg], writes=[dt_])

            ti = 0
            for g in range(8):
                nl = nlT[g % 2]
                for tt in range(4):
                    t = g * 4 + tt
                    kb.load("sp", xt[ti % 2], xt[ti % 2][:], hin.h[t * 128:(t + 1) * 128, :], hin)
                    norm_T(ti, xt[ti % 2], gs1, SH1, 0, nl, tt * 128, banks[ti % 2])
                    ti += 1
                u_group(g, nl, 512, lambda cc: (hT, hT[:, cc, 15 + g * 512:15 + (g + 1) * 512]))
            nl = nlT[0]
            kb.load("sp", xt[ti % 2], xt[ti % 2][:], xhalo.h, xhalo)
            norm_T(ti, xt[ti % 2], gs1, SH1, 0, nl, 0, banks[ti % 2])
            ti += 1
            u_group(8, nl, 128, lambda cc: (htmp, htmp[:, cc, :]))
            for cc in range(4):
                kb.op("dve", lambda e: e.tensor_scalar(out=hT[:, cc, 0:15], in0=htmp[:, cc, 0:15], scalar1=edge_sb[:, 0:1], scalar2=None, op0=ALU.mult),
                      reads=[htmp, edge_sb], writes=[hT])
                kb.op("dve", lambda e: e.tensor_scalar(out=hT[:, cc, 15 + 4096:HW], in0=htmp[:, cc, 15:30], scalar1=edge_sb[:, 1:2], scalar2=None, op0=ALU.mult),
                      reads=[htmp, edge_sb], writes=[hT])
            nl = nlT[1]
            kb.load("sp", xt[ti % 2], xt[ti % 2][:], cxh.h, cxh)
            norm_T(ti, xt[ti % 2], gs1, SH1, 1, nl, 0, banks[ti % 2])
            ti += 1
            u_group(9, nl, 128, lambda cc: (hTc, hTc[:, cc, :]))
            for cc in range(4):
                kb.op("dve", lambda e: e.tensor_scalar(out=hTc[:, cc, 0:15], in0=hTc[:, cc, 0:15], scalar1=edge_sb[:, 0:1], scalar2=None, op0=ALU.mult),
                      reads=[hTc, edge_sb], writes=[hTc])
                kb.op("dve", lambda e: e.tensor_scalar(out=hTc[:, cc, 79:94], in0=hTc[:, cc, 79:94], scalar1=edge_sb[:, 1:2], scalar2=None, op0=ALU.mult),
                      reads=[hTc, edge_sb], writes=[hTc])

            acc = [kb.sb("acc%d" % c, [128, 512], F32, pes) for c in range(4)]
            sqt = dbl("sqt", [128, 512], F32, 1, pes) * 2
            mean_sb = kb.sb("mean_sb", [128, 512], F32, pes)
            m2 = kb.sb("m2", [128, 512], F32, pes)
            rstd_bc = kb.sb("rstd_bc", [128, 512], F32, pes)
            tt_ = dbl("tt_", [128, 512], F32, 1, pes) * 2

            def conv_block(src, c0, n, out_c0):
                for tau in range(31):
                    for cc in range(4):
                        en = "dve"
                        if tau == 0:
                            kb.op(en, lambda e: e.tensor_scalar(out=acc[cc][:, 0:n], in0=src[:, cc, c0:c0 + n], scalar1=cw_sb[:, cc, 0:1], scalar2=cvec_sb[:, cc:cc + 1],
                                                                op0=ALU.mult, op1=ALU.add), reads=[src, cw_sb, cvec_sb], writes=[acc[cc]])
                        else:
                            kb.op(en, lambda e: e.scalar_tensor_tensor(out=acc[cc][:, 0:n], in0=src[:, cc, c0 + tau:c0 + tau + n], scalar=cw_sb[:, cc, tau:tau + 1],
                                                                       in1=acc[cc][:, 0:n], op0=ALU.mult, op1=ALU.add), reads=[src, cw_sb, acc[cc]], writes=[acc[cc]])
                pmean, pex2 = banks[4], banks[5]
                for cc in range(4):
                    kb.op("pe", lambda e: e.matmul(pmean[:, 0:n], lhsT=ones_s[:], rhs=acc[cc][:, 0:n], start=(cc == 0), stop=(cc == 3)), reads=[ones_s, acc[cc]], writes=[pmean])
                for cc in range(4):
                    sq_ = sqt[cc % 2]
                    kb.op("act", lambda e: e.activation(out=sq_[:, 0:n], in_=acc[cc][:, 0:n], func=AF.Square), reads=[acc[cc]], writes=[sq_])
                    kb.op("pe", lambda e: e.matmul(pex2[:, 0:n], lhsT=ones_s[:], rhs=sq_[:, 0:n], start=(cc == 0), stop=(cc == 3)), reads=[ones_s, sq_], writes=[pex2])
                kb.op("act", lambda e: e.copy(out=mean_sb[:, 0:n], in_=pmean[:, 0:n]), reads=[pmean], writes=[mean_sb])
                kb.op("pool", lambda e: e.tensor_tensor(out=m2[:, 0:n], in0=mean_sb[:, 0:n], in1=mean_sb[:, 0:n], op=ALU.mult), reads=[mean_sb], writes=[m2])
                kb.op("dve", lambda e: e.tensor_tensor(out=m2[:, 0:n], in0=pex2[:, 0:n], in1=m2[:, 0:n], op=ALU.subtract), reads=[pex2, m2], writes=[m2])
                kb.op("dve", lambda e: e.tensor_scalar(out=m2[:, 0:n], in0=m2[:, 0:n], scalar1=EPS, scalar2=None, op0=ALU.add), reads=[m2], writes=[m2])
                kb.op("act", lambda e: e.activation(out=m2[:, 0:n], in_=m2[:, 0:n], func=AF.Sqrt), reads=[m2], writes=[m2])
                kb.op("dve", lambda e: e.reciprocal(out=rstd_bc[:, 0:n], in_=m2[:, 0:n]), reads=[m2], writes=[rstd_bc])
                for cc in range(4):
                    t_ = tt_[cc % 2]
                    kb.op("dve", lambda e: e.tensor_tensor(out=t_[:, 0:n], in0=acc[cc][:, 0:n], in1=mean_sb[:, 0:n], op=ALU.subtract), reads=[acc[cc], mean_sb], writes=[t_])
                    kb.op("pool", lambda e: e.tensor_tensor(out=t_[:, 0:n], in0=t_[:, 0:n], in1=rstd_bc[:, 0:n], op=ALU.mult), reads=[t_, rstd_bc], writes=[t_])
                    kb.op("act", lambda e: e.activation(out=convT[:, cc, out_c0:out_c0 + n], in_=t_[:, 0:n], func=AF.Silu, scale=cvec_sb[:, 4 + cc:5 + cc],
                                                        bias=cvec_sb[:, 8 + cc:9 + cc]), reads=[t_, cvec_sb], writes=[convT])

            for tb in range(8):
                conv_block(hT, tb * 512, 512, tb * 512)
            conv_block(hTc, 0, 64, 4096)
            kb.op("pool", lambda e: e.memset(convT[:, :, 4096 + 64:4096 + 128], 0.0), writes=[convT])
            kb.barrier()

    mix_sb = kb.sb("mix_sb", [128, NMIX, NTOK], BF16, pesA)
    mixidx_sb = kb.sb("mixidx_sb", [128, NMIX], I32, pesA)
    kb.load("sp", mixidx_sb, mixidx_sb[:], mixidx.h, mixidx)
    for hh in range(NMIX):
        kb.dma("pool", lambda e: e.indirect_dma_start(out=mix_sb[:, hh, :], out_offset=None, in_=mixsrc.h[:, :],
                                                      in_offset=bass.IndirectOffsetOnAxis(ap=mixidx_sb[:, hh:hh + 1], axis=0)), reads=[mixidx_sb, mixsrc], writes=[mix_sb])
    wout_b = kb.sb("wout_b", [128, 8, 1024], BF16, pesA)
    rw_b = kb.sb("rw_b", [128, 8, 36], BF16, pesA)
    rb_bc = kb.sb("rb_bc", [128, 36], F32, pesA)
    kb.load("sp", rb_bc, rb_bc[:], rb.h.partition_broadcast(128), rb)
    kb.op("pool", lambda e: e.memset(Rbc[:], 0.0), writes=[Rbc])
    ltri = kb.sb("ltri", [128, 128], F32, pesA)
    kb.op("pool", lambda e: e.memset(ltri[:], 1.0), writes=[ltri])
    kb.op("pool", lambda e: e.affine_select(out=ltri[:], in_=ltri[:], pattern=[[1, 128]], compare_op=ALU.is_gt, fill=0.0, base=0, channel_multiplier=-1),
          reads=[ltri], writes=[ltri])
    kb.op("pool", lambda e: e.tensor_copy(out=ltri_b[:], in_=ltri[:]), reads=[ltri], writes=[ltri_b])
    kb.op("pool", lambda e: e.memset(ones_b[:], 1.0), writes=[ones_b])
    kb.op("pool", lambda e: e.iota(iop[:], pattern=[[0, 1]], base=0, channel_multiplier=1, allow_small_or_imprecise_dtypes=True), writes=[iop])
    kb.op("pool", lambda e: e.iota(blkst[:], pattern=[[256, NB]], base=0, channel_multiplier=0, allow_small_or_imprecise_dtypes=True), writes=[blkst])

    with ExitStack() as pes:
        stg = dbl("stg2", [128, 1024], F32, 2, pes)
        for kc in range(8):
            kb.load("sp", stg[kc % 2], stg[kc % 2][:], wout.h[kc * 128:(kc + 1) * 128, :], wout)
            kb.op("pool", lambda e: e.tensor_copy(out=wout_b[:, kc, :], in_=stg[kc % 2][:]), reads=[stg[kc % 2]], writes=[wout_b])
        rw_f = kb.sb("rw_f", [128, 8, 36], F32, pes)
        kb.load("sp", rw_f, rw_f[:], rw.h.rearrange("(kc p) n -> p kc n", p=128), rw)
        kb.op("pool", lambda e: e.tensor_copy(out=rw_b[:], in_=rw_f[:]), reads=[rw_f], writes=[rw_b])

        ytmp = dbl("ytmp", [128, 1024], F32, 2, pes)
        hl = dbl("hl", [128, 1024], F32, 2, pes)
        nl2T = dbl("nl2T", [128, 8, 128], BF16, 2, pes)
        nl2 = dbl("nl2", [128, 1024], BF16, 2, pes)
        lg = dbl("lg", [128, 36], F32, 2, pes)
        rt = dbl("rt", [128, 16], F32, 2, pes)
        lem = dbl("lem", [128, 32], F32, 2, pes)
        lem2 = dbl("lem2", [128, 32], F32, 2, pes)
        cb_ = dbl("cb_", [128, 32], BF16, 2, pes)
        rbase = dbl("rbase", [128, 32], F32, 2, pes)
        tmp32 = dbl("tmp32", [128, 2, 32], F32, 2, pes)
        ejunk = kb.sb("ejunk", [128, 4], F32, pes)

        for t in range(NT):
            p = t % 2
            j = 1 if (L0 and t == NT - 1) else 0
            kb.load("sp", xt[p], xt[p][:], hin.h[t * 128:(t + 1) * 128, :], hin)
            chunks = []
            if L0:
                for cc in range(4):
                    chunks.append((convT, convT[:, cc, t * 128:(t + 1) * 128]))
            for hh in range(NMIX):
                chunks.append((mix_sb, mix_sb[:, hh, t * 128:(t + 1) * 128]))
            for half in range(2):
                py = banks[half]
                for ci, (ct, cap) in enumerate(chunks):
                    kb.op("pe", lambda e: e.matmul(py[:, :], lhsT=cap, rhs=wout_b[:, ci, half * 512:(half + 1) * 512], start=(ci == 0), stop=(ci == 7)),
                          reads=[ct, wout_b], writes=[py])
                kb.op("dve", lambda e: e.tensor_tensor(out=ytmp[p][:, half * 512:(half + 1) * 512], in0=py[:, :], in1=gate_bc[j][0][:, half * 512:(half + 1) * 512], op=ALU.mult),
                      reads=[py, gate_bc[j][0]], writes=[ytmp[p]])
            kb.op("dve", lambda e: e.tensor_tensor(out=hl[p][:], in0=ytmp[p][:], in1=xt[p][:], op=ALU.add), reads=[ytmp[p], xt[p]], writes=[hl[p]])
            kb.store("sp", hlm, hlm.h[t * 128:(t + 1) * 128, :], hl[p], hl[p][:])
            norm_T(t, hl[p], gs2, SH2, j, nl2T[p], 0, banks[2])
            pl = banks[3]
            for kc in range(8):
                kb.op("pe", lambda e: e.matmul(pl[:, 0:36], lhsT=nl2T[p][:, kc, :], rhs=rw_b[:, kc, :], start=(kc == 0), stop=(kc == 7)), reads=[nl2T[p], rw_b], writes=[pl])
            kb.op("dve", lambda e: e.tensor_tensor(out=lg[p][:], in0=pl[:, 0:36], in1=rb_bc[:], op=ALU.add), reads=[pl, rb_bc], writes=[lg[p]])
            pbk = banks[4]
            for kc in range(8):
                kb.op("pe", lambda e: e.transpose(out=bfv(pbk)[:, kc * 128:(kc + 1) * 128], in_=nl2T[p][:, kc, :], identity=identb[:]), reads=[nl2T[p], identb], writes=[pbk])
            kb.op("act", lambda e: e.copy(out=nl2[p][:], in_=bfv(pbk)[:, 0:1024]), reads=[pbk], writes=[nl2[p]])
            kb.store("sp", nl2d, nl2d.h[t * 128:(t + 1) * 128, :], nl2[p], nl2[p][:])
            r_ = rt[p]
            kb.op("dve", lambda e: e.tensor_reduce(out=r_[:, 0:1], in_=lg[p][:, 0:4], axis=AX.X, op=ALU.max), reads=[lg[p]], writes=[r_])
            kb.op("dve", lambda e: e.tensor_scalar(out=r_[:, 1:5], in0=lg[p][:, 0:4], scalar1=r_[:, 0:1], scalar2=None, op0=ALU.is_equal), reads=[lg[p], r_], writes=[r_])
            kb.op("dve", lambda e: e.tensor_scalar(out=r_[:, 5:6], in0=r_[:, 0:1], scalar1=-1.0, scalar2=None, op0=ALU.mult), reads=[r_], writes=[r_])
            kb.op("act", lambda e: e.activation(out=ejunk[:], in_=lg[p][:, 0:4], func=AF.Exp, bias=r_[:, 5:6], accum_out=r_[:, 6:7]), reads=[lg[p], r_], writes=[ejunk, r_])
            kb.op("dve", lambda e: e.reciprocal(out=r_[:, 7:8], in_=r_[:, 6:7]), reads=[r_], writes=[r_])
            kb.op("dve", lambda e: e.tensor_scalar(out=r_[:, 8:12], in0=r_[:, 1:5], scalar1=-1.0, scalar2=BIG, op0=ALU.add, op1=ALU.mult), reads=[r_], writes=[r_])
            for g in range(4):
                kb.op("dve", lambda e: e.tensor_scalar(out=lem[p][:, g * 8:(g + 1) * 8], in0=lg[p][:, 4 + g * 8:4 + (g + 1) * 8], scalar1=r_[:, 8 + g:9 + g], scalar2=None, op0=ALU.add),
                      reads=[lg[p], r_], writes=[lem[p]])
            oh1 = OH[:, t, 0, :]
            oh2 = OH[:, t, 1, :]
            kb.op("dve", lambda e: e.tensor_reduce(out=r_[:, 12:13], in_=lem[p][:], axis=AX.X, op=ALU.max), reads=[lem[p]], writes=[r_])
            kb.op("dve", lambda e: e.tensor_scalar(out=oh1, in0=lem[p][:], scalar1=r_[:, 12:13], scalar2=None, op0=ALU.is_equal), reads=[lem[p], r_], writes=[OH])
            kb.op("dve", lambda e: e.scalar_tensor_tensor(out=lem2[p][:], in0=oh1, scalar=-BIG, in1=lem[p][:], op0=ALU.mult, op1=ALU.add), reads=[OH, lem[p]], writes=[lem2[p]])
            kb.op("dve", lambda e: e.tensor_reduce(out=r_[:, 13:14], in_=lem2[p][:], axis=AX.X, op=ALU.max), reads=[lem2[p]], writes=[r_])
            kb.op("dve", lambda e: e.tensor_scalar(out=oh2, in0=lem2[p][:], scalar1=r_[:, 13:14], scalar2=None, op0=ALU.is_equal), reads=[lem2[p], r_], writes=[OH])
            kb.op("dve", lambda e: e.tensor_tensor(out=r_[:, 14:15], in0=r_[:, 12:13], in1=r_[:, 13:14], op=ALU.subtract), reads=[r_], writes=[r_])
            kb.op("act", lambda e: e.activation(out=r_[:, 15:16], in_=r_[:, 14:15], func=AF.Sigmoid), reads=[r_], writes=[r_])
            kb.op("dve", lambda e: e.tensor_tensor(out=GT[:, t, 0:1], in0=r_[:, 15:16], in1=r_[:, 7:8], op=ALU.mult), reads=[r_], writes=[GT])
            kb.op("dve", lambda e: e.tensor_tensor(out=GT[:, t, 1:2], in0=r_[:, 7:8], in1=GT[:, t, 0:1], op=ALU.subtract), reads=[r_, GT], writes=[GT])
            if j == 1:
                kb.op("dve", lambda e: e.tensor_scalar(out=OH[:, t, :, :], in0=OH[:, t, :, :], scalar1=validt[:, 0:1], scalar2=None, op0=ALU.mult), reads=[OH, validt], writes=[OH])
            kb.op("dve", lambda e: e.tensor_tensor(out=cb_[p][:], in0=OH[:, t, 0, :], in1=OH[:, t, 1, :], op=ALU.add), reads=[OH], writes=[cb_[p]])
            pc = banks[5]
            kb.op("pe", lambda e: e.matmul(pc[:, 0:32], lhsT=ltri_b[:], rhs=cb_[p][:], start=True, stop=True), reads=[ltri_b, cb_[p]], writes=[pc])
            kb.op("dve", lambda e: e.tensor_tensor(out=rbase[p][:], in0=pc[:, 0:32], in1=Rbc[:], op=ALU.add), reads=[pc, Rbc], writes=[rbase[p]])
            pt_ = banks[6]
            kb.op("pe", lambda e: e.matmul(pt_[:, 0:32], lhsT=ones_b[:], rhs=cb_[p][:], start=True, stop=True), reads=[ones_b, cb_[p]], writes=[pt_])
            kb.op("dve", lambda e: e.tensor_tensor(out=Rbc[:], in0=pt_[:, 0:32], in1=Rbc[:], op=ALU.add), reads=[pt_, Rbc], writes=[Rbc])
            for k in range(2):
                kb.op("dve", lambda e: e.tensor_tensor(out=tmp32[p][:, k, :], in0=OH[:, t, k, :], in1=rbase[p][:], op=ALU.mult), reads=[OH, rbase[p]], writes=[tmp32[p]])
            kb.op("dve", lambda e: e.tensor_reduce(out=RK[:, t, :], in_=tmp32[p][:], axis=AX.X, op=ALU.add), reads=[tmp32[p]], writes=[RK])
        kb.barrier()
    pesA.close()
    pesD = ExitStack()

    cnt_i = kb.sb("cnt_i", [128, 32], I32, pesD)
    pcnt = kb.sb("pcnt", [128, 32], F32, pesD)
    pend = [kb.sb("pend%d" % i, [128, 32], F32, pesD) for i in range(2)]
    kb.op("dve", lambda e: e.tensor_scalar(out=pcnt[:], in0=Rbc[:], scalar1=255.0, scalar2=None, op0=ALU.add), reads=[Rbc], writes=[pcnt])
    kb.op("dve", lambda e: e.tensor_copy(out=cnt_i[:], in_=pcnt[:]), reads=[pcnt], writes=[cnt_i])
    kb.op("dve", lambda e: e.tensor_scalar(out=cnt_i[:], in0=cnt_i[:], scalar1=8, scalar2=8, op0=ALU.arith_shift_right, op1=ALU.logical_shift_left), reads=[cnt_i], writes=[cnt_i])
    kb.op("dve", lambda e: e.tensor_copy(out=pcnt[:], in_=cnt_i[:]), reads=[cnt_i], writes=[pcnt])
    kb.op("dve", lambda e: e.tensor_copy(out=pend[0][:], in_=pcnt[:]), reads=[pcnt], writes=[pend[0]])
    cur = 0
    for sft in (1, 2, 4, 8, 16):
        a, b = pend[cur], pend[1 - cur]
        kb.op("dve", lambda e: e.tensor_copy(out=b[:, 0:sft], in_=a[:, 0:sft]), reads=[a], writes=[b])
        kb.op("dve", lambda e: e.tensor_tensor(out=b[:, sft:32], in0=a[:, sft:32], in1=a[:, 0:32 - sft], op=ALU.add), reads=[a], writes=[b])
        cur = 1 - cur
    pendf = pend[cur]
    poff = kb.sb("poff", [128, 32], F32, pesD)
    kb.op("dve", lambda e: e.tensor_tensor(out=poff[:], in0=pendf[:], in1=pcnt[:], op=ALU.subtract), reads=[pendf, pcnt], writes=[poff])
    DEST = kb.sb("DEST", [128, NT, 2], F32, pesD)
    tmpd = kb.sb("tmpd", [128, NT * 2, 32], F32, pesD)
    for t in range(NT):
        for k in range(2):
            kb.op("dve", lambda e: e.tensor_tensor(out=tmpd[:, t * 2 + k, :], in0=OH[:, t, k, :], in1=poff[:], op=ALU.mult), reads=[OH, poff], writes=[tmpd])
    kb.op("dve", lambda e: e.tensor_reduce(out=DEST[:].rearrange("p t k -> p (t k)"), in_=tmpd[:], axis=AX.X, op=ALU.add), reads=[tmpd], writes=[DEST])
    kb.op("dve", lambda e: e.tensor_tensor(out=DEST[:], in0=DEST[:], in1=RK[:], op=ALU.add), reads=[DEST, RK], writes=[DEST])
    if L0:
        inval = kb.sb("inval", [128, 2], F32, pesD)
        kb.op("dve", lambda e: e.tensor_scalar(out=inval[:, 0:1], in0=validt[:], scalar1=-1.0, scalar2=-1.0, op0=ALU.add, op1=ALU.mult), reads=[validt], writes=[inval])
        kb.op("dve", lambda e: e.scalar_tensor_tensor(out=inval[:, 1:2], in0=iop[:], scalar=float(NROWS), in1=inval[:, 0:1], op0=ALU.add, op1=ALU.mult), reads=[iop, inval], writes=[inval])
        kb.op("dve", lambda e: e.tensor_scalar(out=DEST[:, NT - 1, :], in0=DEST[:, NT - 1, :], scalar1=validt[:, 0:1], scalar2=inval[:, 1:2], op0=ALU.mult, op1=ALU.add),
              reads=[DEST, validt, inval], writes=[DEST])
    kb.op("dve", lambda e: e.tensor_copy(out=DESTI[:], in_=DEST[:].rearrange("p t k -> p (t k)")), reads=[DEST], writes=[DESTI])
    eb = kb.sb("eb", [128, NB], F32, pesD)
    kb.op("pool", lambda e: e.memset(eb[:], 0.0), writes=[eb])
    for ee in range(32):
        kb.op("dve", lambda e: e.scalar_tensor_tensor(out=eb[:], in0=blkst[:], scalar=pendf[:, ee:ee + 1], in1=eb[:], op0=ALU.is_ge, op1=ALU.add), reads=[blkst, pendf, eb], writes=[eb])
    kb.op("dve", lambda e: e.tensor_scalar(out=eb[:], in0=eb[:], scalar1=31.0, scalar2=128.0, op0=ALU.min, op1=ALU.mult), reads=[eb], writes=[eb])
    flag = kb.sb("flag", [128, NB], F32, pesD)
    kb.op("pool", lambda e: e.memset(flag[:], 1.0), writes=[flag])
    kb.op("dve", lambda e: e.tensor_tensor(out=flag[:, 1:NB], in0=eb[:, 1:NB], in1=eb[:, 0:NB - 1], op=ALU.not_equal), reads=[eb], writes=[flag])
    kb.op("dve", lambda e: e.tensor_scalar(out=flag[:], in0=flag[:], scalar1=-1.0, scalar2=-1.0e9, op0=ALU.add, op1=ALU.mult), reads=[flag], writes=[flag])
    kb.op("dve", lambda e: e.tensor_scalar(out=eb[:], in0=eb[:], scalar1=iop[:, 0:1], scalar2=None, op0=ALU.add), reads=[eb, iop], writes=[eb])
    kb.op("dve", lambda e: e.tensor_tensor(out=eb[:], in0=eb[:], in1=flag[:], op=ALU.add), reads=[eb, flag], writes=[eb])
    kb.op("dve", lambda e: e.tensor_copy(out=WIDX[:], in_=eb[:]), reads=[eb], writes=[WIDX])

    srow = dbl("srow", [128, 1024], BF16, 3, pesD)
    for t in range(NT):
        sr = srow[t % 3]
        kb.load("sp", sr, sr[:], nl2d.h[t * 128:(t + 1) * 128, :], nl2d)
        for k in range(2):
            kb.dma("pool", lambda e: e.indirect_dma_start(out=xs.h[:, :], out_offset=bass.IndirectOffsetOnAxis(ap=DESTI[:, t * 2 + k:t * 2 + k + 1], axis=0),
                                                          in_=sr[:], in_offset=None), reads=[DESTI, sr], writes=[xs])
    kb.barrier()
    pesD.close()
    pesE = ExitStack()

    w1f = dbl("w1f", [128, 4096], F32, 1, pesE) * 2
    w3f = dbl("w3f", [128, 4096], F32, 1, pesE) * 2
    w2f = dbl("w2f", [128, 4096], F32, 1, pesE) * 2
    w1b = dbl("w1b", [128, 8, 512], BF16, 2, pesE)
    w3b = dbl("w3b", [128, 8, 512], BF16, 2, pesE)
    w2b = dbl("w2b", [128, 4, 1024], BF16, 2, pesE)
    xr = dbl("xr", [128, 2, 1024], BF16, 2, pesE)
    xsT = dbl("xsT", [128, 8, 256], BF16, 2, pesE)
    sl = dbl("sl", [128, 256], F32, 2, pesE)
    hhT = dbl("hhT", [128, 4, 256], BF16, 2, pesE)
    yo = dbl("yo", [128, 1024], F32, 2, pesE)
    wreg = kb.nc.gpsimd.to_reg(4095)
    def fetch_w(b):
        p = b % 2
        for (tab, wf) in ((w1t, w1f[p]), (w3t, w3f[p]), (w2t, w2f[p])):
            kb.dma("pool", lambda e: e.indirect_dma_start(out=wf[:], out_offset=None, in_=tab.h[:, :], in_offset=bass.IndirectOffsetOnAxis(ap=WIDX[:, b:b + 1], axis=0),
                                                          bounds_check=wreg, oob_is_err=False), reads=[WIDX, tab], writes=[wf])
        kb.op("act", lambda e: e.copy(out=w1b[p][:].rearrange("p a b -> p (a b)"), in_=w1f[p][:]), reads=[w1f[p]], writes=[w1b[p]])
        kb.op("dve", lambda e: e.tensor_copy(out=w3b[p][:].rearrange("p a b -> p (a b)"), in_=w3f[p][:]), reads=[w3f[p]], writes=[w3b[p]])
        kb.op("act", lambda e: e.copy(out=w2b[p][:].rearrange("p a b -> p (a b)")[:, 0:2048], in_=w2f[p][:, 0:2048]), reads=[w2f[p]], writes=[w2b[p]])
        kb.op("dve", lambda e: e.tensor_copy(out=w2b[p][:].rearrange("p a b -> p (a b)")[:, 2048:4096], in_=w2f[p][:, 2048:4096]), reads=[w2f[p]], writes=[w2b[p]])

    fetch_w(0)
    for b in range(NB):
        p = b % 2
        if b + 1 < NB:
            fetch_w(b + 1)
        kb.load("sp", xr[p], xr[p][:], xs.h[b * 256:(b + 1) * 256, :].rearrange("(a p) n -> p a n", p=128), xs)
        for sub in range(2):
            pT = banks[6 + sub]
            for kc in range(8):
                kb.op("pe", lambda e: e.transpose(out=bfv(pT)[:, kc * 128:(kc + 1) * 128], in_=xr[p][:, sub, kc * 128:(kc + 1) * 128], identity=identb[:]),
                      reads=[xr[p], identb], writes=[pT])
            kb.op("act", lambda e: e.copy(out=xsT[p][:, :, sub * 128:(sub + 1) * 128], in_=bfv(pT)[:, 0:1024].rearrange("p (a b) -> p a b", a=8)), reads=[pT], writes=[xsT[p]])
        for fc in range(4):
            ph1 = banks[0 + fc % 2]
            ph3 = banks[2 + fc % 2]
            c0 = 0
            for kc in range(8):
                kb.op("pe", lambda e: e.matmul(ph1[:, c0:c0 + 256], lhsT=w1b[p][:, kc, fc * 128:(fc + 1) * 128], rhs=xsT[p][:, kc, :], start=(kc == 0), stop=(kc == 7)),
                      reads=[w1b[p], xsT[p]], writes=[ph1])
            for kc in range(8):
                kb.op("pe", lambda e: e.matmul(ph3[:, c0:c0 + 256], lhsT=w3b[p][:, kc, fc * 128:(fc + 1) * 128], rhs=xsT[p][:, kc, :], start=(kc == 0), stop=(kc == 7)),
                      reads=[w3b[p], xsT[p]], writes=[ph3])
            s_ = sl[fc % 2]
            kb.op("act", lambda e: e.activation(out=s_[:], in_=ph1[:, c0:c0 + 256], func=AF.Silu), reads=[ph1], writes=[s_])
            kb.op("dve", lambda e: e.tensor_tensor(out=hhT[p][:, fc, :], in0=ph3[:, c0:c0 + 256], in1=s_[:], op=ALU.mult), reads=[ph3, s_], writes=[hhT[p]])
        for sub in range(2):
            y_ = yo[sub]
            for half in range(2):
                py = banks[4 + half]
                for fc in range(4):
                    kb.op("pe", lambda e: e.matmul(py[:, :], lhsT=hhT[p][:, fc, sub * 128:(sub + 1) * 128], rhs=w2b[p][:, fc, half * 512:(half + 1) * 512], start=(fc == 0), stop=(fc == 3)),
                          reads=[hhT[p], w2b[p]], writes=[py])
                if half == 0:
                    kb.op("act", lambda e: e.copy(out=y_[:, 0:512], in_=py[:, :]), reads=[py], writes=[y_])
                else:
                    kb.op("dve", lambda e: e.tensor_copy(out=y_[:, 512:1024], in_=py[:, :]), reads=[py], writes=[y_])
            kb.store("sp", ys, ys.h[b * 256 + sub * 128:b * 256 + (sub + 1) * 128, :], y_, y_[:])
    kb.barrier()
    pesE.close()
    pesF = ExitStack()

    y1 = dbl("y1", [128, 1024], F32, 2, pesF)
    y2 = dbl("y2", [128, 1024], F32, 2, pesF)
    hm = dbl("hm", [128, 1024], F32, 2, pesF)
    for t in range(NT):
        p = t % 2
        j = 1 if (L0 and t == NT - 1) else 0
        if j == 1:
            kb.op("pool", lambda e: e.memset(y1[p][:], 0.0), writes=[y1[p]])
            kb.op("pool", lambda e: e.memset(y2[p][:], 0.0), writes=[y2[p]])
        for k, yk in ((0, y1[p]), (1, y2[p])):
            kb.dma("pool", lambda e: e.indirect_dma_start(out=yk[:], out_offset=None, in_=ys.h[:, :], in_offset=bass.IndirectOffsetOnAxis(ap=DESTI[:, t * 2 + k:t * 2 + k + 1], axis=0)), reads=[DESTI, ys], writes=[yk])
        kb.load("sp", hm[p], hm[p][:], hlm.h[t * 128:(t + 1) * 128, :], hlm)
        kb.op("dve", lambda e: e.tensor_scalar(out=y1[p][:], in0=y1[p][:], scalar1=GT[:, t, 0:1], scalar2=None, op0=ALU.mult), reads=[y1[p], GT], writes=[y1[p]])
        kb.op("dve", lambda e: e.scalar_tensor_tensor(out=y1[p][:], in0=y2[p][:], scalar=GT[:, t, 1:2], in1=y1[p][:], op0=ALU.mult, op1=ALU.add), reads=[y2[p], GT, y1[p]], writes=[y1[p]])
        kb.op("dve", lambda e: e.tensor_tensor(out=y1[p][:], in0=y1[p][:], in1=gate_bc[j][1][:], op=ALU.mult), reads=[y1[p], gate_bc[j][1]], writes=[y1[p]])
        kb.op("dve", lambda e: e.tensor_tensor(out=hm[p][:], in0=hm[p][:], in1=y1[p][:], op=ALU.add), reads=[hm[p], y1[p]], writes=[hm[p]])
        kb.store("sp", hout, hout.h[t * 128:(t + 1) * 128, :], hm[p], hm[p][:])
    print("lb%d instructions:" % layer, kb.n_ins, "sems:", len(kb.sems))
    pesF.close()
    kb.end_stage()


def fop(v, n):
    return np.ascontiguousarray(np.asarray(v, np.float32).reshape(n, 128).T)


def moe_tables(inp, l):
    w1 = np.ascontiguousarray(inp["moe_w1"][l].reshape(32, 8, 128, 512).transpose(0, 2, 1, 3).reshape(4096, 4096))
    w3 = np.ascontiguousarray(inp["moe_w3"][l].reshape(32, 8, 128, 512).transpose(0, 2, 1, 3).reshape(4096, 4096))
    w2 = np.ascontiguousarray(inp["moe_w2"][l].reshape(32, 4, 128, 1024).transpose(0, 2, 1, 3).reshape(4096, 4096))
    return w1, w3, w2


def host_b(layer, inp):
    L0 = layer == 0
    l = layer
    w1, w3, w2 = moe_tables(inp, l)
    rw = np.ascontiguousarray(np.concatenate([inp["rg_w"][l], inp["re_w"][l]], axis=1).astype(np.float32))
    rb = np.concatenate([inp["rg_b"][l], inp["re_b"][l]]).astype(np.float32)
    adabr = np.concatenate([inp["ada_b"][l][2048:3072], inp["ada_b"][l][5120:6144]]).astype(np.float32)
    gfop = np.ascontiguousarray(np.concatenate([fop(inp["norm1_g"][l], 8), fop(inp["norm2_g"][l], 8)], axis=1))
    wout = np.ascontiguousarray(inp["ab_w_out"][0] if L0 else inp["gla_w_out"][0])
    hin_lat, hin_ctx = inp["x"], inp["ctx"]
    maps = []
    pp = np.arange(128, dtype=np.int32)
    for b in range(2):
        sv = np.stack([inp["c"][b], inp["c_ctx"]], -1).reshape(8, 128, 2).transpose(1, 0, 2).reshape(128, 16).astype(np.float32)
        for jq in range(4):
            r0, r1 = jq * 4096, (jq + 1) * 4096
            m = {"svec": np.ascontiguousarray(sv), "adaw": np.ascontiguousarray(inp["ada_w"][l]), "adabf": fop(inp["ada_b"][l], 48), "adabr": adabr, "gfop": gfop,
                 "wout": wout, "rw": rw, "rb": rb, "w1t": w1, "w3t": w3, "w2t": w2}
            if L0:
                cpad = np.zeros((128, 1024), np.float32)
                cpad[:64] = hin_ctx[b, 64 * jq:64 * jq + 64]
                m["hin"] = np.ascontiguousarray(np.concatenate([hin_lat[b, r0:r1], cpad], 0))
                m["mixidx"] = np.ascontiguousarray(np.stack([np.array([ag_row(jq * 128 + int(p_), h, 64, 512) for p_ in pp]) for h in range(4)], axis=1).astype(np.int32))
                xh = np.zeros((128, 1024), np.float32)
                if jq > 0:
                    xh[0:15] = hin_lat[b, r0 - 15:r0]
                if jq < 3:
                    xh[15:30] = hin_lat[b, r1:r1 + 15]
                m["xhalo"] = xh
                ch = np.zeros((128, 1024), np.float32)
                for r in range(94):
                    pos = 64 * jq - 15 + r
                    if 0 <= pos < 256:
                        ch[r] = hin_ctx[b, pos]
                m["cxh"] = ch
                m["edge"] = np.array([1.0 if jq > 0 else 0.0, 1.0 if jq < 3 else 0.0], np.float32)
                v = np.zeros((128, 1), np.float32)
                v[:64] = 1
                m["valid"] = v
                m["win"] = np.ascontiguousarray(inp["ab_w_in"][0][:, 0:1024])
                m["cw"] = np.ascontiguousarray(inp["conv_w"][0].T.reshape(4, 128, 31).transpose(1, 0, 2).reshape(128, 124))
                m["cvec"] = np.ascontiguousarray(np.concatenate([fop(inp["conv_b"][0], 4), fop(inp["conv_ln_g"][0], 4), fop(inp["conv_ln_b"][0], 4)], axis=1))
            else:
                m["mixidx"] = np.ascontiguousarray(np.stack([np.array([ag_row(jq * 256 + c2 * 128 + int(p_), h, 128, 1024) for p_ in pp]) for h in range(4) for c2 in range(2)], axis=1).astype(np.int32))
                m["valid"] = np.ones((128, 1), np.float32)
            maps.append(m)
    return maps


def gather_b(layer, results):
    L0 = layer == 0
    hl = np.zeros((2, 16384, 1024), np.float32)
    hc = np.zeros((2, 256, 1024), np.float32) if L0 else None
    for b in range(2):
        for jq in range(4):
            o = results[b * 4 + jq]["hout"]
            hl[b, jq * 4096:(jq + 1) * 4096] = o[:4096]
            if L0:
                hc[b, 64 * jq:64 * jq + 64] = o[4096:4160]
    return hl, hc


def build_l1a(kb, banks, x2out, x3in, n_lat_tiles=128, do_scan=True):
    kb.begin_stage("a1_")
    svec = kb.dram("svec", [128, 16], F32, "ExternalInput")
    adaw = kb.dram("adaw", [1024, 2048], F32, "ExternalInput")
    adab = kb.dram("adab", [128, 16], F32, "ExternalInput")
    g1 = kb.dram("g1", [128, 8], F32, "ExternalInput")
    w = kb.dram("w", [1024, 768], F32, "ExternalInput")
    waT = kb.dram("waT", [2, 16, 1024], F32, "ExternalInput")
    wa2 = kb.dram("wa2", [2, 16, 128], F32, "ExternalInput")
    small = kb.dram("small", [512], F32, "ExternalInput")
    proj = kb.dram("proj", [S + LC, 1024], F32)
    of_d = kb.dram("of_d", [S, 256], F32)

    def bfv(t):
        return t[:].bitcast(BF16)

    def dbl(name, shape, dt, n=2, es=None):
        return [kb.sb("%s%d" % (name, i), shape, dt, es) for i in range(n)]

    identb = kb.identity("identb", BF16)
    smallb = kb.sb("smallb", [128, 512], F32)
    kb.load("sp", smallb, smallb[:], small.h.partition_broadcast(128), small)
    zer = kb.sb("zer", [128, 128], F32)
    kb.op("pool", lambda e: e.memset(zer[:], 0.0), writes=[zer])

    def rstd_chain(stt, c_in, c_tmp, c_out, n, inv_n):
        kb.op("dve", lambda e: e.tensor_scalar(out=stt[:, c_tmp:c_tmp + n], in0=stt[:, c_in:c_in + n], scalar1=inv_n, scalar2=EPS, op0=ALU.mult, op1=ALU.add),
              reads=[stt], writes=[stt])
        kb.op("act", lambda e: e.activation(out=stt[:, c_tmp:c_tmp + n], in_=stt[:, c_tmp:c_tmp + n], func=AF.Sqrt), reads=[stt], writes=[stt])
        kb.op("dve", lambda e: e.reciprocal(out=stt[:, c_out:c_out + n], in_=stt[:, c_tmp:c_tmp + n]), reads=[stt], writes=[stt])

    wq = [kb.sb("wq%d" % j, [128, 8, 1024], BF16) for j in range(2)]
    bias = [kb.sb("bias%d" % j, [128, 1024], F32) for j in range(2)]
    pesA = ExitStack()
    s_sb = kb.sb("s_sb", [128, 16], F32, pesA)
    kb.load("sp", s_sb, s_sb[:], svec.h, svec)
    kb.op("act", lambda e: e.activation(out=s_sb[:], in_=s_sb[:], func=AF.Silu), reads=[s_sb], writes=[s_sb])
    adab_sb = kb.sb("adab_sb", [128, 16], F32, pesA)
    kb.load("sp", adab_sb, adab_sb[:], adab.h, adab)
    g1_sb = kb.sb("g1_sb", [128, 8], F32, pesA)
    kb.load("sp", g1_sb, g1_sb[:], g1.h, g1)
    mod = kb.sb("mod", [128, 16, 2], F32, pesA)
    gs = kb.sb("gs", [128, 8, 2], F32, pesA)
    w_sb = kb.sb("w_sb", [128, 8, 1024], F32, pesA)
    shiftbc = kb.sb("shiftbc", [128, 8, 128], F32, pesA)
    pm = banks[0]
    with ExitStack() as pes:
        adaw_sb = kb.sb("adaw_sb", [128, 8, 512], F32, pes)
        for v in range(4):
            kb.load("sp", adaw_sb, adaw_sb[:], adaw.h[:, v * 512:(v + 1) * 512].rearrange("(kc p) n -> p kc n", p=128), adaw)
            for oc in range(4):
                g = v * 4 + oc
                for kc in range(8):
                    kb.op("pe", lambda e: e.matmul(pm[:, g * 2:g * 2 + 2], lhsT=adaw_sb[:, kc, oc * 128:(oc + 1) * 128], rhs=s_sb[:, kc * 2:kc * 2 + 2],
                                                  start=(kc == 0), stop=(kc == 7)), reads=[adaw_sb, s_sb], writes=[pm])
        pm3 = pm[:, 0:32].rearrange("p (g j) -> p g j", j=2)
        for j in range(2):
            kb.op("dve", lambda e: e.tensor_tensor(out=mod[:, :, j], in0=pm3[:, :, j], in1=adab_sb[:], op=ALU.add), reads=[pm, adab_sb], writes=[mod])
            kb.op("dve", lambda e: e.scalar_tensor_tensor(out=gs[:, :, j], in0=mod[:, 8:16, j], scalar=1.0, in1=g1_sb[:], op0=ALU.add, op1=ALU.mult),
                  reads=[mod, g1_sb], writes=[gs])
        kb.load("sp", w_sb, w_sb[:, :, 0:768], w.h.rearrange("(kc p) n -> p kc n", p=128), w)
        waT_sb = [kb.sb("waT_sb%d" % d, [32, 1024], F32, pes) for d in range(2)]
        wa2_sb = [kb.sb("wa2_sb%d" % d, [32, 128], F32, pes) for d in range(2)]
        for d in range(2):
            kb.op("pool", lambda e: e.memset(waT_sb[d][:], 0.0), writes=[waT_sb[d]])
            kb.op("pool", lambda e: e.memset(wa2_sb[d][:], 0.0), writes=[wa2_sb[d]])
            kb.load("sp", waT_sb[d], waT_sb[d][0:16, :], waT.h[d], waT)
            kb.load("sp", wa2_sb[d], wa2_sb[d][0:16, :], wa2.h[d], wa2)
            for kc in range(8):
                pz = banks[1]
                kb.op("pe", lambda e: e.matmul(pz[:, 0:128], lhsT=waT_sb[d][:, kc * 128:(kc + 1) * 128], rhs=wa2_sb[d][:], start=True, stop=True),
                      reads=[waT_sb[d], wa2_sb[d]], writes=[pz])
                kb.op("dve", lambda e: e.tensor_copy(out=w_sb[:, kc, 768 + d * 128:768 + (d + 1) * 128], in_=pz[:, 0:128]), reads=[pz], writes=[w_sb])
        for j in range(2):
            for kc in range(8):
                kb.op("dve", lambda e: e.tensor_scalar(out=wq[j][:, kc, :], in0=w_sb[:, kc, :], scalar1=gs[:, kc, j:j + 1], scalar2=None, op0=ALU.mult),
                      reads=[w_sb, gs], writes=[wq[j]])
                kb.op("dve", lambda e: e.tensor_scalar(out=shiftbc[:, kc, :], in0=zer[:], scalar1=mod[:, kc, j:j + 1], scalar2=None, op0=ALU.add),
                      reads=[zer, mod], writes=[shiftbc])
            for half in range(2):
                pb = banks[2 + half]
                for kc in range(8):
                    kb.op("pe", lambda e: e.matmul(pb[:, :], lhsT=shiftbc[:, kc, :], rhs=w_sb[:, kc, half * 512:(half + 1) * 512], start=(kc == 0), stop=(kc == 7)),
                          reads=[shiftbc, w_sb], writes=[pb])
                kb.op("dve", lambda e: e.tensor_copy(out=bias[j][:, half * 512:(half + 1) * 512], in_=pb[:, :]), reads=[pb], writes=[bias[j]])
            kb.op("dve", lambda e: e.tensor_tensor(out=bias[j][:, 768:1024], in0=bias[j][:, 768:1024], in1=smallb[:, 0:256], op=ALU.add), reads=[bias[j], smallb], writes=[bias[j]])
        kb.barrier()
    kb.barrier()
    pesA.close()

    pesB = ExitStack()
    xt = dbl("xt", [128, 1024], F32, 4, pesB)
    junk = kb.sb("junk", [128, 1024], BF16, pesB)
    st1 = dbl("st1", [128, 4], F32, 2, pesB)
    xn = dbl("xn", [128, 1024], BF16, 2, pesB)
    xnT = dbl("xnT", [128, 1024], BF16, 2, pesB)
    pj = dbl("pj", [128, 1024], F32, 2, pesB)
    ez = dbl("ez", [128, 256], F32, 2, pesB)
    one_col = kb.sb("one_col", [128, 1], F32, pesB)
    kb.op("pool", lambda e: e.memset(one_col[:], 1.0), writes=[one_col])

    tile_args = []

    def do_load(i):
        _, _, row0, is_ctx, _ = tile_args[i]
        xb = xt[i % 4]
        if is_ctx:
            c = row0 // 128
            for hf in range(2):
                r = ag_row(4096, 2 * c + hf, 256, 4224)
                kb.load("sp", xb, xb[hf * 64:(hf + 1) * 64, :], x2out.h[r:r + 64, :], x2out)
        else:
            t = row0 // 128
            r = ag_row((t % 32) * 128, t // 32, 256, 4224)
            kb.load("sp", xb, xb[:], x2out.h[r:r + 128, :], x2out)

    def proj_tile(i, src, row0, is_ctx, drow):
        p = i % 2
        j = 1 if is_ctx else 0
        if i + 2 < len(tile_args):
            do_load(i + 2)
        yield
        kb.op("act", lambda e: e.activation(out=junk[:], in_=xt[i % 4][:], func=AF.Square, accum_out=st1[p][:, 0:1]), reads=[xt[i % 4]], writes=[junk, st1[p]])
        yield
        rstd_chain(st1[p], 0, 1, 2, 1, 1.0 / 1024)
        kb.op("act", lambda e: e.activation(out=xn[p][:], in_=xt[i % 4][:], func=AF.Copy, scale=st1[p][:, 2:3]), reads=[xt[i % 4], st1[p]], writes=[xn[p]])
        yield
        psT = banks[p]
        for kc in range(8):
            kb.op("pe", lambda e: e.transpose(out=bfv(psT)[:, kc * 128:(kc + 1) * 128], in_=xn[p][:, kc * 128:(kc + 1) * 128], identity=identb[:]),
                  reads=[xn[p], identb], writes=[psT])
            yield
        kb.op("dve", lambda e: e.tensor_copy(out=xnT[p][:], in_=bfv(psT)[:, 0:1024]), reads=[psT], writes=[xnT[p]])
        yield
        for half in range(2):
            pp = banks[2 + 2 * p + half]
            for kc in range(8):
                kb.op("pe", lambda e: e.matmul(pp[:, :], lhsT=xnT[p][:, kc * 128:(kc + 1) * 128], rhs=wq[j][:, kc, half * 512:(half + 1) * 512], start=(kc == 0), stop=(kc == 7)),
                      reads=[xnT[p], wq[j]], writes=[pp])
                yield
            kb.op("dve", lambda e: e.tensor_tensor(out=pj[p][:, half * 512:(half + 1) * 512], in0=pp[:, :], in1=bias[j][:, half * 512:(half + 1) * 512], op=ALU.add),
                  reads=[pp, bias[j]], writes=[pj[p]])
            yield
        kb.op("act", lambda e: e.activation(out=ez[p][:], in_=pj[p][:, 768:1024], func=AF.Exp, scale=-1.0), reads=[pj[p]], writes=[ez[p]])
        yield
        kb.op("act", lambda e: e.activation(out=pj[p][:, 768:1024], in_=ez[p][:], func=AF.Ln, bias=one_col[:, 0:1]), reads=[ez[p], one_col], writes=[pj[p]])
        yield
        kb.store("sp", proj, proj.h[drow:drow + 128, :], pj[p], pj[p][:])
        yield

    i = 0
    for c in range(2):
        tile_args.append((i, None, c * 128, True, S + c * 128))
        i += 1
    for t in range(n_lat_tiles):
        tile_args.append((i, None, t * 128, False, t * 128))
        i += 1
    do_load(0)
    do_load(1)
    gens = [proj_tile(*a_) for a_ in tile_args]
    interleave(gens, 2)
    kb.barrier()
    pesB.close()

    mask = []
    for d in range(2):
        mf = kb.sb("mask%d" % d, [128, 128], F32)
        kb.op("pool", lambda e: e.memset(mf[:], 1.0), writes=[mf])
        if d == 0:
            kb.op("pool", lambda e: e.affine_select(out=mf[:], in_=mf[:], pattern=[[1, 128]], compare_op=ALU.is_ge, fill=0.0, base=0, channel_multiplier=-1), reads=[mf], writes=[mf])
        else:
            kb.op("pool", lambda e: e.affine_select(out=mf[:], in_=mf[:], pattern=[[-1, 128]], compare_op=ALU.is_ge, fill=0.0, base=0, channel_multiplier=1), reads=[mf], writes=[mf])
        mask.append(mf)
    LS = -1.0 / 16
    maskS = []
    for d in range(2):
        ms_ = kb.sb("maskS%d" % d, [128, 128], F32)
        kb.op("dve", lambda e: e.tensor_scalar(out=ms_[:], in0=mask[d][:], scalar1=LS, scalar2=None, op0=ALU.mult), reads=[mask[d]], writes=[ms_])
        maskS.append(ms_)
    ones_f = kb.sb("ones_f", [128, 128], F32)
    kb.op("pool", lambda e: e.memset(ones_f[:], -1.0 / 16), writes=[ones_f])
    Sst = kb.sb("Sst", [128, 256], F32)
    Sb = dbl("Sb", [128, 256], BF16)
    pt = dbl("pt", [128, 1024], F32, 3)
    bc = dbl("bc", [128, 128], F32)
    eb = dbl("eb", [128, 128], F32)
    enb = dbl("enb", [128, 128], F32)
    dlt = dbl("dlt", [128, 128], F32)
    dec = dbl("dec", [128, 1], F32)
    qt = dbl("qt", [128, 128], BF16)
    ktl = dbl("ktl", [128, 128], BF16)
    kh = dbl("kh", [128, 128], BF16)
    vb = dbl("vb", [128, 256], BF16)
    qkT = dbl("qkT", [128, 256], BF16)
    attm = dbl("attm", [128, 128], BF16)
    ofs = dbl("ofs", [128, 256], F32)
    osum = dbl("osum", [128, 256], F32)
    fst = dbl("fst", [128, 4], F32)
    sg = dbl("sg", [128, 256], F32)
    ogb = dbl("ogb", [128, 256], BF16)
    ogT_sb = dbl("ogT_sb", [128, 2, 128], BF16)
    QS = 128.0 ** -0.5
    step = [0]

    def gla_prep(c, row, d):
        p = c % 2
        B = banks[4 * p:4 * p + 4]
        t_ = pt[c % 3]
        kb.load("sp", t_, t_[:], proj.h[row:row + 128, :], proj)
        yield
        la = t_[:, 768 + d * 128:768 + (d + 1) * 128]
        kb.op("pe", lambda e: e.matmul(B[0][:, 0:128], lhsT=maskS[d][:], rhs=la, start=True, stop=True), reads=[maskS[d], t_], writes=[B[0]])
        yield
        kb.op("pe", lambda e: e.matmul(B[0][:, 128:256], lhsT=ones_f[:], rhs=la, start=True, stop=True), reads=[ones_f, t_], writes=[B[0]])
        yield
        kb.op("pe", lambda e: e.matmul(B[0][:, 256:384], lhsT=la, rhs=ones_f[:], start=True, stop=True), reads=[ones_f, t_], writes=[B[0]])
        yield
        kb.op("act", lambda e: e.copy(out=bc[p][:], in_=B[0][:, 0:128]), reads=[B[0]], writes=[bc[p]])
        yield
        kb.op("act", lambda e: e.activation(out=eb[p][:], in_=B[0][:, 0:128], func=AF.Exp), reads=[B[0]], writes=[eb[p]])
        yield
        kb.op("act", lambda e: e.activation(out=enb[p][:], in_=B[0][:, 0:128], func=AF.Exp, scale=-1.0), reads=[B[0]], writes=[enb[p]])
        yield
        kb.op("dve", lambda e: e.tensor_tensor(out=dlt[p][:], in0=B[0][:, 128:256], in1=bc[p][:], op=ALU.subtract), reads=[B[0], bc[p]], writes=[dlt[p]])
        yield
        kb.op("act", lambda e: e.activation(out=dlt[p][:], in_=dlt[p][:], func=AF.Exp), reads=[dlt[p]], writes=[dlt[p]])
        yield
        kb.op("act", lambda e: e.activation(out=dec[p][:], in_=B[0][:, 256:257], func=AF.Exp), reads=[B[0]], writes=[dec[p]])
        yield
        kb.op("dve", lambda e: e.scalar_tensor_tensor(out=qt[p][:], in0=t_[:, 0:128], scalar=QS, in1=eb[p][:], op0=ALU.mult, op1=ALU.mult), reads=[t_, eb[p]], writes=[qt[p]])
        yield
        kb.op("dve", lambda e: e.tensor_tensor(out=ktl[p][:], in0=t_[:, 128:256], in1=enb[p][:], op=ALU.mult), reads=[t_, enb[p]], writes=[ktl[p]])
        yield
        kb.op("pool", lambda e: e.tensor_tensor(out=kh[p][:], in0=t_[:, 128:256], in1=dlt[p][:], op=ALU.mult), reads=[t_, dlt[p]], writes=[kh[p]])
        yield
        kb.op("pool", lambda e: e.tensor_copy(out=vb[p][:], in_=t_[:, 256:512]), reads=[t_], writes=[vb[p]])
        yield
        kb.op("pe", lambda e: e.transpose(out=bfv(B[1])[:, 0:128], in_=qt[p][:], identity=identb[:]), reads=[qt[p], identb], writes=[B[1]])
        yield
        kb.op("pe", lambda e: e.transpose(out=bfv(B[1])[:, 128:256], in_=ktl[p][:], identity=identb[:]), reads=[ktl[p], identb], writes=[B[1]])
        yield
        kb.op("act", lambda e: e.copy(out=qkT[p][:], in_=bfv(B[1])[:, 0:256]), reads=[B[1]], writes=[qkT[p]])
        yield
        kb.op("pe", lambda e: e.matmul(B[2][:, 0:128], lhsT=qkT[p][:, 128:256], rhs=qkT[p][:, 0:128], start=True, stop=True), reads=[qkT[p]], writes=[B[2]])
        yield
        kb.op("dve", lambda e: e.tensor_tensor(out=attm[p][:], in0=B[2][:, 0:128], in1=mask[d][:], op=ALU.mult), reads=[B[2], mask[d]], writes=[attm[p]])
        yield

    def gla_fin(c, d, out_mode, out_row):
        p = c % 2
        B = banks[4 * p:4 * p + 4]
        t_ = pt[c % 3]
        sb_cur = Sb[c % 2]
        sb_next = Sb[(c + 1) % 2]
        if out_mode is not None:
            kb.op("pe", lambda e: e.matmul(B[3][:, 0:256], lhsT=qkT[p][:, 0:128], rhs=sb_cur[:], start=True, stop=False), reads=[qkT[p], sb_cur], writes=[B[3]])
            yield
            kb.op("pe", lambda e: e.matmul(B[3][:, 0:256], lhsT=attm[p][:], rhs=vb[p][:], start=False, stop=True), reads=[attm[p], vb[p]], writes=[B[3]])
            yield
        kb.op("pe", lambda e: e.matmul(B[2][:, 128:384], lhsT=kh[p][:], rhs=vb[p][:], start=True, stop=True), reads=[kh[p], vb[p]], writes=[B[2]])
        yield
        kb.op("dve", lambda e: e.scalar_tensor_tensor(out=Sst[:], in0=Sst[:], scalar=dec[p][:, 0:1], in1=B[2][:, 128:384], op0=ALU.mult, op1=ALU.add),
              reads=[Sst, dec[p], B[2]], writes=[Sst])
        yield
        kb.op("act", lambda e: e.copy(out=sb_next[:], in_=Sst[:]), reads=[Sst], writes=[sb_next])
        yield
        if out_mode == "store":
            kb.op("act", lambda e: e.copy(out=ofs[p][:], in_=B[3][:, 0:256]), reads=[B[3]], writes=[ofs[p]])
            yield
            kb.store("sp", of_d, of_d.h[out_row:out_row + 128, :], ofs[p], ofs[p][:])
            yield
        elif out_mode == "final":
            kb.load("sp", ofs[p], ofs[p][:], of_d.h[out_row:out_row + 128, :], of_d)
            yield
            kb.op("dve", lambda e: e.tensor_tensor(out=osum[p][:], in0=B[3][:, 0:256], in1=ofs[p][:], op=ALU.add), reads=[B[3], ofs[p]], writes=[osum[p]])
            yield
            kb.op("act", lambda e: e.activation(out=sg[p][:], in_=osum[p][:], func=AF.Square, accum_out=fst[p][:, 0:1]), reads=[osum[p]], writes=[sg[p], fst[p]])
            yield
            rstd_chain(fst[p], 0, 1, 2, 1, 1.0 / 256)
            kb.op("dve", lambda e: e.scalar_tensor_tensor(out=osum[p][:], in0=osum[p][:], scalar=fst[p][:, 2:3], in1=smallb[:, 256:512], op0=ALU.mult, op1=ALU.mult),
                  reads=[osum[p], fst[p], smallb], writes=[osum[p]])
            yield
            kb.op("act", lambda e: e.activation(out=sg[p][:], in_=t_[:, 512:768], func=AF.Silu), reads=[t_], writes=[sg[p]])
            yield
            kb.op("dve", lambda e: e.tensor_tensor(out=ogb[p][:], in0=osum[p][:], in1=sg[p][:], op=ALU.mult), reads=[osum[p], sg[p]], writes=[ogb[p]])
            yield
            for hh in range(2):
                kb.op("pe", lambda e: e.transpose(out=bfv(B[1])[:, 256 + hh * 128:256 + (hh + 1) * 128], in_=ogb[p][:, hh * 128:(hh + 1) * 128], identity=identb[:]),
                      reads=[ogb[p], identb], writes=[B[1]])
                yield
            kb.op("act", lambda e: e.copy(out=ogT_sb[p][:].rearrange("p a b -> p (a b)"), in_=bfv(B[1])[:, 256:512]), reads=[B[1]], writes=[ogT_sb[p]])
            yield
            tq_, tc_ = (out_row // 128) // 32, ((out_row // 128) % 32) * 128
            kb.store("sp", x3in, x3in.h[tq_ * 256:(tq_ + 1) * 256, tc_:tc_ + 128].rearrange("(a p) n -> p a n", p=128), ogT_sb[p], ogT_sb[p][:])
            yield

    def reset_state(c):
        kb.op("pool", lambda e: e.memset(Sst[:], 0.0), writes=[Sst])
        kb.op("pool", lambda e: e.memset(Sb[c % 2][:], 0.0), writes=[Sb[c % 2]])

    def run_scan(chunks, c0):
        n = len(chunks)
        for _ in gla_prep(c0, chunks[0][0], chunks[0][1]):
            pass
        for k in range(n):
            row, d, om, orow = chunks[k]
            gens = [gla_fin(c0 + k, d, om, orow)]
            if k + 1 < n:
                gens.append(gla_prep(c0 + k + 1, chunks[k + 1][0], chunks[k + 1][1]))
            interleave(gens, 2)
        return c0 + n

    fwd = [(S + c * 128, 0, None, None) for c in range(2)] + [(t * 128, 0, "store", t * 128) for t in range(n_lat_tiles)]
    bwd = [(S + c * 128, 1, None, None) for c in (1, 0)] + [(t * 128, 1, "final", t * 128) for t in range(n_lat_tiles - 1, -1, -1)]
    reset_state(0)
    cn = run_scan(fwd, 0)
    kb.barrier()
    reset_state(cn)
    run_scan(bwd, cn)
    print("l1a instructions:", kb.n_ins, "sems:", len(kb.sems))
    kb.end_stage()


def fop(v, n):
    return np.ascontiguousarray(np.asarray(v, np.float32).reshape(n, 128).T)


def host_l1a(inp):
    maps = []
    wi = inp["gla_w_in"][0]
    for b in range(2):
        sv = np.stack([inp["c"][b], inp["c_ctx"]], -1).reshape(8, 128, 2).transpose(1, 0, 2).reshape(128, 16).astype(np.float32)
        for h in range(4):
            w = np.concatenate([wi[:, h * 128:(h + 1) * 128], wi[:, 512 + h * 128:512 + (h + 1) * 128], wi[:, 1024 + h * 256:1024 + (h + 1) * 256],
                                wi[:, 2048 + h * 256:2048 + (h + 1) * 256]], axis=1)
            waT = np.ascontiguousarray(wi[:, 3072:3104].T.reshape(2, 16, 1024))
            wa2 = np.ascontiguousarray(inp["gla_w_a2"][0][:, :, h * 128:(h + 1) * 128])
            small = np.concatenate([inp["gla_b_a2"][0][0, h * 128:(h + 1) * 128], inp["gla_b_a2"][0][1, h * 128:(h + 1) * 128], inp["gla_norm_g"][0]]).astype(np.float32)
            maps.append({"svec": np.ascontiguousarray(sv),
                         "adaw": np.ascontiguousarray(inp["ada_w"][1][:, 0:2048]), "adab": fop(inp["ada_b"][1][0:2048], 16), "g1": fop(inp["norm1_g"][1], 8),
                         "w": np.ascontiguousarray(w), "waT": waT, "wa2": wa2, "small": small})
    return maps


RG = [[0, 1, 2, 3], [4, 5, 6, 7]]


def build_all():
    kb = KB()
    banks = [kb.ps("bank%d" % i) for i in range(8)]
    x1in = kb.dram("x1in", [512, 4224], BF16)
    x1out = kb.dram("x1out", [2048, 4224], BF16)
    x2in = kb.dram("x2in", [4224, 1024], F32)
    x2out = kb.dram("x2out", [4 * 4224, 1024], F32)
    x3in = kb.dram("x3in", [1024, 4096], BF16)
    x3out = kb.dram("x3out", [4096, 4096], BF16)
    build_l0a(kb, banks, x1in)
    kb.all_gather(x1in, x1out, RG, 64)
    build_b(0, kb, banks, x1out, None, x2in)
    kb.all_gather(x2in, x2out, RG, 256)
    build_l1a(kb, banks, x2out, x3in)
    kb.all_gather(x3in, x3out, RG, 128)
    build_b(1, kb, banks, x3out, x2in, None)
    print("total instructions:", kb.n_ins, "sems:", len(kb.sems))
    return kb.finish()


def kernel(**inputs):
    inp = {k: np.asarray(v) for k, v in inputs.items()}
    parts = [("a0_", host_l0a(inp)), ("b0_", host_b(0, inp)), ("a1_", host_l1a(inp)), ("b1_", host_b(1, inp))]
    maps = []
    for c in range(8):
        m = {}
        for pre, ms in parts:
            for k, v in ms[c].items():
                m[pre + k] = v
        maps.append(m)
    nc = build_all()
    res = run_bass_kernel_spmd(nc, maps, core_ids=list(range(8)))
    out = np.zeros((2, 16384, 1024), np.float32)
    for b in range(2):
        for jq in range(4):
            out[b, jq * 4096:(jq + 1) * 4096] = np.asarray(res.results[b * 4 + jq]["b1_hout"])[:4096]
    return out
```
